# Optimizing a Trainium2 kernel written in Bass

```python
import math
import jax, jax.numpy as jnp
from jax import lax
import numpy as np

D_MODEL = 1024
BATCH = 8
SEQ = 2048
DEPTH = 4
DEC_BATCH = 128
DEC_SEQ = 8
PAST_LEN = 16384
PAGE_SIZE = 128

EPS = 1e-6
FFN_DIM = 2816
RET_HEADS = 4
RET_DK = 128
RET_DV = 256
RET_QK_W = RET_HEADS * RET_DK
RET_V_W = RET_HEADS * RET_DV
RET_CHUNK = 128
ROPE_BASE = 10000.0
S5_GROUP = 16
S5_WIDTH = D_MODEL
S5_GROUPS = S5_WIDTH // S5_GROUP
S5_STATE = 64
SSD_INNER = 2 * D_MODEL
SSD_HEADDIM = 64
SSD_HEADS = SSD_INNER // SSD_HEADDIM
SSD_GROUPS = 4
SSD_HPG = SSD_HEADS // SSD_GROUPS
SSD_STATE = 128
SSD_CONV = 4
SSD_CHUNK = 128
SSD_CONV_DIM = SSD_INNER + 2 * SSD_GROUPS * SSD_STATE
N_BRANCH = 3
IN_SPLITS = (RET_QK_W, RET_QK_W, RET_V_W, RET_V_W, S5_WIDTH, SSD_INNER, SSD_CONV_DIM, SSD_HEADS, N_BRANCH * D_MODEL)
IN_DIM = sum(IN_SPLITS)
IN_OFFSETS = tuple(int(o) for o in np.cumsum(IN_SPLITS)[:-1])

kernel_name = "hybrid_retention_s5_ssd_gated_step"


def rmsnorm(x, g):
    xf = x.astype(jnp.float32)
    y = xf * lax.rsqrt(jnp.mean(xf * xf, axis=-1, keepdims=True) + EPS)
    return (y * g.astype(jnp.float32)).astype(x.dtype)


def swiglu_ffn(x, w_gu, w_down):
    g, u = jnp.split(x @ w_gu, 2, axis=-1)
    return (jax.nn.silu(g) * u) @ w_down


def rotary(x, pos):
    half = x.shape[-1] // 2
    inv = ROPE_BASE ** (-jnp.arange(half, dtype=jnp.float32) / half)
    ang = pos.astype(jnp.float32)[:, None] * inv[None, :]
    cos = jnp.cos(ang)[None, :, None, :]
    sin = jnp.sin(ang)[None, :, None, :]
    xf = x.astype(jnp.float32)
    x1, x2 = xf[..., :half], xf[..., half:]
    return jnp.concatenate([x1 * cos - x2 * sin, x2 * cos + x1 * sin], axis=-1)


def retention_chunkwise(q, k, v, s0):
    bsz, L = q.shape[:2]
    C = RET_CHUNK if L % RET_CHUNK == 0 else L
    nc = L // C
    lg = jnp.log1p(-jnp.exp2(-5.0 - jnp.arange(RET_HEADS, dtype=jnp.float32)))
    idx = jnp.arange(C, dtype=jnp.float32)
    diff = idx[:, None] - idx[None, :]
    causal = diff >= 0
    decay = jnp.where(causal[None], jnp.exp(jnp.where(causal, diff, 0.0)[None] * lg[:, None, None]), 0.0)
    q_dec = jnp.exp((idx + 1.0)[:, None] * lg[None, :])[None, :, :, None]
    k_dec = jnp.exp((C - 1.0 - idx)[:, None] * lg[None, :])[None, :, :, None]
    c_dec = jnp.exp(C * lg)[None, :, None, None]

    def chunks(t):
        return jnp.swapaxes(t.reshape((bsz, nc, C) + t.shape[2:]), 0, 1)

    def step(s, inp):
        qc, kc, vc = inp
        vc = vc.astype(jnp.float32)
        scores = jnp.einsum("bihd,bjhd->bhij", qc, kc) * decay[None]
        inner = jnp.einsum("bhij,bjhe->bihe", scores, vc)
        cross = jnp.einsum("bihd,bhde->bihe", qc, s) * q_dec
        s_new = s * c_dec + jnp.einsum("bjhd,bjhe->bhde", kc * k_dec, vc)
        return s_new, inner + cross

    s_fin, o = lax.scan(step, s0.astype(jnp.float32), (chunks(q), chunks(k), chunks(v)))
    return jnp.swapaxes(o, 0, 1).reshape(bsz, L, RET_HEADS, RET_DV), s_fin


def retention_branch(q, k, v, g, pos, s0, ln_g, w_o):
    bsz, L = q.shape[:2]
    q = rotary(q.reshape(bsz, L, RET_HEADS, RET_DK), pos)
    k = rotary(k.reshape(bsz, L, RET_HEADS, RET_DK), pos) * (RET_DK ** -0.5)
    v = v.reshape(bsz, L, RET_HEADS, RET_DV)
    o, s_new = retention_chunkwise(q, k, v, s0)
    mu = jnp.mean(o, axis=-1, keepdims=True)
    var = jnp.mean(jnp.square(o - mu), axis=-1, keepdims=True)
    o = ((o - mu) * lax.rsqrt(var + EPS)).reshape(bsz, L, RET_V_W) * ln_g.astype(jnp.float32)
    o = jax.nn.silu(g.astype(jnp.float32)) * o
    return o.astype(g.dtype) @ w_o, s_new


def s5_branch(u, s0, a_re, a_im, log_dt, b_re, b_im, c_re, c_im, d, w_glu):
    bsz, L = u.shape[:2]
    uf = u.astype(jnp.float32).reshape(bsz, L, S5_GROUPS, S5_GROUP)
    dt = jnp.exp(log_dt.astype(jnp.float32))[:, None]
    ar, ai = a_re.astype(jnp.float32), a_im.astype(jnp.float32)
    mag = jnp.exp(dt * ar)
    abar_re, abar_im = mag * jnp.cos(dt * ai), mag * jnp.sin(dt * ai)
    nr, ni = abar_re - 1.0, abar_im
    den = ar * ar + ai * ai
    f_re, f_im = (nr * ar + ni * ai) / den, (ni * ar - nr * ai) / den
    bb_re = f_re[..., None] * b_re - f_im[..., None] * b_im
    bb_im = f_re[..., None] * b_im + f_im[..., None] * b_re
    bu_re = jnp.einsum("gnc,blgc->blgn", bb_re, uf)
    bu_im = jnp.einsum("gnc,blgc->blgn", bb_im, uf)
    sr, si = s0[..., 0].astype(jnp.float32), s0[..., 1].astype(jnp.float32)
    bu_re = bu_re.at[:, 0].add(abar_re * sr - abar_im * si)
    bu_im = bu_im.at[:, 0].add(abar_re * si + abar_im * sr)
    a_re_t = jnp.broadcast_to(abar_re, bu_re.shape)
    a_im_t = jnp.broadcast_to(abar_im, bu_im.shape)

    def combine(e1, e2):
        a1r, a1i, b1r, b1i = e1
        a2r, a2i, b2r, b2i = e2
        return (a1r * a2r - a1i * a2i, a1r * a2i + a1i * a2r,
                a2r * b1r - a2i * b1i + b2r, a2r * b1i + a2i * b1r + b2i)

    _, _, x_re, x_im = lax.associative_scan(combine, (a_re_t, a_im_t, bu_re, bu_im), axis=1)
    y = jnp.einsum("gcn,blgn->blgc", c_re, x_re) - jnp.einsum("gcn,blgn->blgc", c_im, x_im)
    y = y.reshape(bsz, L, S5_WIDTH) + d.astype(jnp.float32) * uf.reshape(bsz, L, S5_WIDTH)
    y = jax.nn.gelu(y).astype(u.dtype)
    ya, yg = jnp.split(y @ w_glu, 2, axis=-1)
    s_new = jnp.stack([x_re[:, -1], x_im[:, -1]], axis=-1)
    return ya * jax.nn.sigmoid(yg), s_new


def causal_dwconv(xbc, conv_prev, w, b):
    xp = jnp.concatenate([conv_prev.astype(xbc.dtype), xbc], axis=1)
    y = lax.conv_general_dilated(xp, w.astype(xbc.dtype)[:, None, :], window_strides=(1,), padding="VALID",
                                 dimension_numbers=("NWC", "WIO", "NWC"), feature_group_count=SSD_CONV_DIM)
    return y + b.astype(xbc.dtype), xp[:, -(SSD_CONV - 1):]


def ssd_chunked(x, dt, a, bm, cm, h0):
    bsz, L = x.shape[:2]
    C = SSD_CHUNK if L % SSD_CHUNK == 0 else L
    nc = L // C
    causal = jnp.tril(jnp.ones((C, C), dtype=bool))[None, :, :, None, None]

    def chunks(t):
        return jnp.swapaxes(t.reshape((bsz, nc, C) + t.shape[2:]), 0, 1)

    def step(h, inp):
        xc, dtc, bc, cc = inp
        cum = jnp.cumsum(dtc * a, axis=1)
        seg = cum[:, :, None] - cum[:, None, :]
        lmat = jnp.where(causal, jnp.exp(jnp.where(causal, seg, 0.0)), 0.0)
        xdt = xc * dtc[..., None]
        cb = jnp.einsum("bign,bjgn->bijg", cc, bc)
        y_diag = jnp.einsum("bijgh,bjghp->bighp", cb[..., None] * lmat, xdt)
        y_off = jnp.einsum("bign,bghpn->bighp", cc, h) * jnp.exp(cum)[..., None]
        w_end = jnp.exp(cum[:, -1:] - cum)
        h_new = h * jnp.exp(cum[:, -1])[..., None, None] + jnp.einsum("bjgn,bjghp->bghpn", bc, xdt * w_end[..., None])
        return h_new, y_diag + y_off

    h, y = lax.scan(step, h0, (chunks(x), chunks(dt), chunks(bm), chunks(cm)))
    return jnp.swapaxes(y, 0, 1).reshape(x.shape), h


def ssd_branch(z, xbc, dt_raw, h0, conv0, conv_w, conv_b, dt_bias, a_log, d_skip, norm_g, w_o):
    bsz, L = z.shape[:2]
    xbc, conv_new = causal_dwconv(xbc, conv0, conv_w, conv_b)
    xbc = jax.nn.silu(xbc.astype(jnp.float32))
    xs, bm, cm = jnp.split(xbc, [SSD_INNER, SSD_INNER + SSD_GROUPS * SSD_STATE], axis=-1)
    xs = xs.reshape(bsz, L, SSD_GROUPS, SSD_HPG, SSD_HEADDIM)
    bm = bm.reshape(bsz, L, SSD_GROUPS, SSD_STATE)
    cm = cm.reshape(bsz, L, SSD_GROUPS, SSD_STATE)
    dt = jax.nn.softplus(dt_raw.astype(jnp.float32) + dt_bias.astype(jnp.float32)).reshape(bsz, L, SSD_GROUPS, SSD_HPG)
    a = -jnp.exp(a_log.astype(jnp.float32)).reshape(SSD_GROUPS, SSD_HPG)
    h0 = h0.astype(jnp.float32).reshape(bsz, SSD_GROUPS, SSD_HPG, SSD_HEADDIM, SSD_STATE)
    y, h = ssd_chunked(xs, dt, a, bm, cm, h0)
    y = y + d_skip.astype(jnp.float32).reshape(SSD_GROUPS, SSD_HPG)[:, :, None] * xs
    y = (y.reshape(bsz, L, SSD_INNER) * jax.nn.silu(z.astype(jnp.float32))).reshape(bsz, L, SSD_GROUPS, -1)
    y = y * lax.rsqrt(jnp.mean(y * y, axis=-1, keepdims=True) + EPS)
    y = y.reshape(bsz, L, SSD_INNER) * norm_g.astype(jnp.float32)
    return y.astype(z.dtype) @ w_o, h.reshape(bsz, SSD_HEADS, SSD_HEADDIM, SSD_STATE), conv_new


def trunk_layer(x, pos, s_ret, s_s5, s_ssm, s_conv, p):
    bsz, L, _ = x.shape
    x = x + 0.5 * swiglu_ffn(rmsnorm(x, p["ffn1_norm"]), p["ffn1_w_gu"], p["ffn1_w_down"])
    h = rmsnorm(x, p["mix_norm"])
    q, k, v, g_ret, u, z, xbc, dt_raw, gate_logits = jnp.split(h @ p["w_in"], IN_OFFSETS, axis=-1)
    y_ret, s_ret = retention_branch(q, k, v, g_ret, pos, s_ret, p["ret_ln_g"], p["ret_w_o"])
    y_s5, s_s5 = s5_branch(u, s_s5, p["s5_a_re"], p["s5_a_im"], p["s5_log_dt"], p["s5_b_re"], p["s5_b_im"],
                           p["s5_c_re"], p["s5_c_im"], p["s5_d"], p["s5_w_glu"])
    y_ssd, s_ssm, s_conv = ssd_branch(z, xbc, dt_raw, s_ssm, s_conv, p["ssd_conv_w"], p["ssd_conv_b"],
                                      p["ssd_dt_bias"], p["ssd_a_log"], p["ssd_d"], p["ssd_norm"], p["ssd_w_o"])
    gates = jax.nn.sigmoid(gate_logits.astype(jnp.float32)).reshape(bsz, L, N_BRANCH, D_MODEL)
    merged = gates[:, :, 0] * y_ret + gates[:, :, 1] * y_s5 + gates[:, :, 2] * y_ssd
    x = x + merged.astype(x.dtype) @ p["w_out"]
    x = x + 0.5 * swiglu_ffn(rmsnorm(x, p["ffn2_norm"]), p["ffn2_w_gu"], p["ffn2_w_down"])
    return x, s_ret, s_s5, s_ssm, s_conv


def setup_inputs(seed: int = 0) -> dict:
    key = jax.random.key(seed)
    ks = iter(jax.random.split(key, 48))
    f32 = jnp.float32

    def nrm(shape, scale):
        return jax.random.normal(next(ks), shape, f32) * scale

    def gain(shape):
        return 1.0 + 0.01 * jax.random.normal(next(ks), shape, f32)

    n_idx = jnp.arange(S5_STATE, dtype=f32)
    dt0 = jnp.exp(jax.random.uniform(next(ks), (DEPTH, SSD_HEADS), f32, math.log(1e-3), math.log(1e-1)))
    inp = {}
    inp["x_prompt"] = nrm((BATCH, SEQ, D_MODEL), 1.0)
    inp["x_sample"] = nrm((DEC_BATCH, DEC_SEQ, D_MODEL), 1.0)
    inp["state_ret"] = nrm((DEPTH, DEC_BATCH, RET_HEADS, RET_DK, RET_DV), 0.5)
    inp["state_s5"] = nrm((DEPTH, DEC_BATCH, S5_GROUPS, S5_STATE, 2), 0.5)
    inp["state_ssm"] = nrm((DEPTH, DEC_BATCH, SSD_HEADS, SSD_HEADDIM, SSD_STATE), 0.2)
    inp["state_conv"] = nrm((DEPTH, DEC_BATCH, SSD_CONV - 1, SSD_CONV_DIM), 1.0)
    inp["ffn1_norm"] = gain((DEPTH, D_MODEL))
    inp["ffn1_w_gu"] = nrm((DEPTH, D_MODEL, 2 * FFN_DIM), D_MODEL ** -0.5)
    inp["ffn1_w_down"] = nrm((DEPTH, FFN_DIM, D_MODEL), FFN_DIM ** -0.5)
    inp["mix_norm"] = gain((DEPTH, D_MODEL))
    inp["w_in"] = nrm((DEPTH, D_MODEL, IN_DIM), D_MODEL ** -0.5)
    inp["ret_ln_g"] = gain((DEPTH, RET_V_W))
    inp["ret_w_o"] = nrm((DEPTH, RET_V_W, D_MODEL), RET_V_W ** -0.5)
    inp["s5_a_re"] = -0.5 + nrm((DEPTH, S5_GROUPS, S5_STATE), 0.01)
    inp["s5_a_im"] = jnp.pi * n_idx + nrm((DEPTH, S5_GROUPS, S5_STATE), 0.01)
    inp["s5_log_dt"] = jax.random.uniform(next(ks), (DEPTH, S5_GROUPS), f32, math.log(1e-3), math.log(1e-1))
    inp["s5_b_re"] = nrm((DEPTH, S5_GROUPS, S5_STATE, S5_GROUP), (2 * S5_GROUP) ** -0.5)
    inp["s5_b_im"] = nrm((DEPTH, S5_GROUPS, S5_STATE, S5_GROUP), (2 * S5_GROUP) ** -0.5)
    inp["s5_c_re"] = nrm((DEPTH, S5_GROUPS, S5_GROUP, S5_STATE), (2 * S5_STATE) ** -0.5)
    inp["s5_c_im"] = nrm((DEPTH, S5_GROUPS, S5_GROUP, S5_STATE), (2 * S5_STATE) ** -0.5)
    inp["s5_d"] = nrm((DEPTH, S5_WIDTH), 1.0)
    inp["s5_w_glu"] = nrm((DEPTH, S5_WIDTH, 2 * D_MODEL), S5_WIDTH ** -0.5)
    inp["ssd_conv_w"] = nrm((DEPTH, SSD_CONV, SSD_CONV_DIM), SSD_CONV ** -0.5)
    inp["ssd_conv_b"] = nrm((DEPTH, SSD_CONV_DIM), 0.02)
    inp["ssd_dt_bias"] = dt0 + jnp.log(-jnp.expm1(-dt0))
    inp["ssd_a_log"] = jnp.log(jax.random.uniform(next(ks), (DEPTH, SSD_HEADS), f32, 1.0, 16.0))
    inp["ssd_d"] = 1.0 + nrm((DEPTH, SSD_HEADS), 0.1)
    inp["ssd_norm"] = gain((DEPTH, SSD_INNER))
    inp["ssd_w_o"] = nrm((DEPTH, SSD_INNER, D_MODEL), SSD_INNER ** -0.5)
    inp["w_out"] = nrm((DEPTH, D_MODEL, D_MODEL), D_MODEL ** -0.5)
    inp["ffn2_norm"] = gain((DEPTH, D_MODEL))
    inp["ffn2_w_gu"] = nrm((DEPTH, D_MODEL, 2 * FFN_DIM), D_MODEL ** -0.5)
    inp["ffn2_w_down"] = nrm((DEPTH, FFN_DIM, D_MODEL), FFN_DIM ** -0.5)
    inp["final_norm"] = gain((D_MODEL,))
    return inp


def reference(x_prompt, x_sample, state_ret, state_s5, state_ssm, state_conv,
              ffn1_norm, ffn1_w_gu, ffn1_w_down, mix_norm, w_in, ret_ln_g, ret_w_o,
              s5_a_re, s5_a_im, s5_log_dt, s5_b_re, s5_b_im, s5_c_re, s5_c_im, s5_d, s5_w_glu,
              ssd_conv_w, ssd_conv_b, ssd_dt_bias, ssd_a_log, ssd_d, ssd_norm, ssd_w_o,
              w_out, ffn2_norm, ffn2_w_gu, ffn2_w_down, final_norm):
    f32 = jnp.float32
    bp = x_prompt.shape[0]
    pos_p = jnp.arange(x_prompt.shape[1], dtype=jnp.int32)
    pos_s = PAST_LEN + jnp.arange(x_sample.shape[1], dtype=jnp.int32)
    zr = jnp.zeros((bp, RET_HEADS, RET_DK, RET_DV), f32)
    zs5 = jnp.zeros((bp, S5_GROUPS, S5_STATE, 2), f32)
    zssm = jnp.zeros((bp, SSD_HEADS, SSD_HEADDIM, SSD_STATE), f32)
    zconv = jnp.zeros((bp, SSD_CONV - 1, SSD_CONV_DIM), x_prompt.dtype)
    xp, xs = x_prompt, x_sample
    ret_p, ret_s, s5_p, s5_s, ssm_p, ssm_s, conv_p, conv_s = [], [], [], [], [], [], [], []
    for l in range(DEPTH):
        p = dict(ffn1_norm=ffn1_norm[l], ffn1_w_gu=ffn1_w_gu[l], ffn1_w_down=ffn1_w_down[l],
                 mix_norm=mix_norm[l], w_in=w_in[l], ret_ln_g=ret_ln_g[l], ret_w_o=ret_w_o[l],
                 s5_a_re=s5_a_re[l], s5_a_im=s5_a_im[l], s5_log_dt=s5_log_dt[l], s5_b_re=s5_b_re[l],
                 s5_b_im=s5_b_im[l], s5_c_re=s5_c_re[l], s5_c_im=s5_c_im[l], s5_d=s5_d[l], s5_w_glu=s5_w_glu[l],
                 ssd_conv_w=ssd_conv_w[l], ssd_conv_b=ssd_conv_b[l], ssd_dt_bias=ssd_dt_bias[l],
                 ssd_a_log=ssd_a_log[l], ssd_d=ssd_d[l], ssd_norm=ssd_norm[l], ssd_w_o=ssd_w_o[l],
                 w_out=w_out[l], ffn2_norm=ffn2_norm[l], ffn2_w_gu=ffn2_w_gu[l], ffn2_w_down=ffn2_w_down[l])
        xp, r1, s1, m1, c1 = trunk_layer(xp, pos_p, zr, zs5, zssm, zconv, p)
        xs, r2, s2, m2, c2 = trunk_layer(xs, pos_s, state_ret[l], state_s5[l], state_ssm[l], state_conv[l], p)
        ret_p.append(r1); ret_s.append(r2)
        s5_p.append(s1); s5_s.append(s2)
        ssm_p.append(m1); ssm_s.append(m2)
        conv_p.append(c1); conv_s.append(c2)
    y_prompt = rmsnorm(xp, final_norm)
    y_sample = rmsnorm(xs, final_norm)
    return (y_prompt, y_sample, jnp.stack(ret_p), jnp.stack(ret_s), jnp.stack(s5_p), jnp.stack(s5_s),
            jnp.stack(ssm_p), jnp.stack(ssm_s), jnp.stack(conv_p), jnp.stack(conv_s))
```

```python
import contextlib
import numpy as np
import concourse.bass as bass
import concourse.mybir as mybir
from concourse.bass_utils import run_bass_kernel_spmd

F32 = mybir.dt.float32
BF16 = mybir.dt.bfloat16
ALU = mybir.AluOpType
AF = mybir.ActivationFunctionType

NCORES = 8
D = 1024
KC = 8
DEPTH = 4
SEQ = 2048
NSS = 16
DSEQ = 8
T = SEQ + NSS * DSEQ
NCH = T // 128
FFN = 2816
EPS = 1e-6
IN_DIM = 12320
TILES = [(0, 512), (512, 512), (1024, 512), (1536, 512), (2048, 128)]
NDS = 40


class KB:
    def __init__(self, nc, es):
        self.nc = nc
        self.E = {"pe": nc.tensor, "act": nc.scalar, "dve": nc.vector, "pool": nc.gpsimd, "sp": nc.sync}
        self.csem = {e: es.enter_context(nc.semaphore("s_" + e)) for e in ["pe", "act", "dve", "pool"]}
        self.ccnt = {e: 0 for e in self.csem}
        self.dsem = [es.enter_context(nc.semaphore("d%d" % i)) for i in range(NDS)]
        self.dcnt = [0] * NDS
        self.dnext = 0
        self.waited = {e: {} for e in self.E}
        self.writer = {}
        self.readers = {}
        self.ninst = 0

    def _wait(self, eng, tok):
        semid, sem, val, src = tok
        if self.waited[eng].get(semid, 0) >= val:
            return
        self.E[eng].wait_ge(sem, val)
        self.waited[eng][semid] = val

    def _sync(self, eng, reads, writes, is_dma=False):
        for k in reads:
            w = self.writer.get(k)
            if w is not None:
                if (not is_dma) and w[3] == eng and eng == "pe":
                    continue
                self._wait(eng, w)
        for k in writes:
            w = self.writer.get(k)
            if w is not None:
                if is_dma or w[3] != eng:
                    self._wait(eng, w)
            for r in self.readers.get(k, {}).values():
                if is_dma or r[3] != eng:
                    self._wait(eng, r)

    def _record(self, tok, reads, writes):
        for k in reads:
            self.readers.setdefault(k, {})[tok[0]] = tok
        for k in writes:
            self.writer[k] = tok
            self.readers[k] = {}

    def op(self, eng, fn, reads=(), writes=(), inc=True):
        self._sync(eng, reads, writes)
        ins = fn(self.E[eng])
        self.ninst += 1
        if inc:
            self.ccnt[eng] += 1
            ins.then_inc(self.csem[eng], 1)
            tok = (eng, self.csem[eng], self.ccnt[eng], eng)
        else:
            tok = (eng, self.csem[eng], self.ccnt[eng] + 1, eng)
        self._record(tok, reads, writes)
        return tok

    def dma(self, out, in_, reads=(), writes=(), q="sp"):
        self._sync(q, reads, writes, is_dma=True)
        i = self.dnext
        self.dnext = (i + 1) % NDS
        sid = "d%d" % i
        if self.dcnt[i] > 0:
            self._wait(q, (sid, self.dsem[i], self.dcnt[i], "dma"))
        self.dcnt[i] += 16
        self.E[q].dma_start(out=out, in_=in_).then_inc(self.dsem[i], 16)
        self.ninst += 1
        tok = (sid, self.dsem[i], self.dcnt[i], "dma")
        self._record(tok, reads, writes)
        return tok

    def finish(self):
        for i in range(NDS):
            if self.dcnt[i] > 0:
                self._wait("sp", ("d%d" % i, self.dsem[i], self.dcnt[i], "dma"))
        for e in ["pe", "act", "dve", "pool"]:
            if self.ccnt[e] > 0:
                self._wait("sp", (e, self.csem[e], self.ccnt[e], e))

    def barrier(self):
        toks = []
        for e in ["pe", "act", "dve", "pool"]:
            if self.ccnt[e] > 0:
                toks.append((e, self.csem[e], self.ccnt[e], e))
        for i in range(NDS):
            if self.dcnt[i] > 0:
                toks.append(("d%d" % i, self.dsem[i], self.dcnt[i], "dma"))
        for eng in ["pe", "act", "dve", "pool", "sp"]:
            for t in toks:
                if t[3] == eng:
                    continue
                self._wait(eng, t)
        self.writer.clear()
        self.readers.clear()


RET_HEADS = 4
GAM = [1.0 - 2.0 ** (-5.0 - h) for h in range(RET_HEADS)]


def host_consts():
    c = {}
    c["ident_f"] = np.eye(128, dtype=np.float32)
    pos = np.concatenate([np.arange(SEQ), np.tile(16384 + np.arange(DSEQ), NSS)]).astype(np.float64)
    inv = 10000.0 ** (-np.arange(64, dtype=np.float64) / 64.0)
    ang = (pos.astype(np.float32)[None, :] * inv.astype(np.float32)[:, None]).astype(np.float32).astype(np.float64)
    cos = np.concatenate([np.cos(ang), np.cos(ang)], axis=0)
    sinS = np.concatenate([-np.sin(ang), np.sin(ang)], axis=0)
    sc = 128.0 ** -0.5
    c["tabqk"] = np.stack([cos, sinS, cos * sc, sinS * sc], axis=1).astype(np.float32)
    idx = np.arange(128)
    qdec = np.zeros((128, 4, 640), np.float32)
    maskT = np.zeros((128, 2, 4, 128), np.float32)
    kdec = np.zeros((128, 2, 4), np.float32)
    sj = idx % 8
    seqj = idx // 8
    for h in range(4):
        g = GAM[h]
        qdec[:, h, 0:512] = np.tile(g ** (idx + 1.0), 4)[None, :]
        qdec[:, h, 512:640] = (g ** (sj + 1.0))[None, :]
        maskT[:, 0, h, :] = (g ** (-(idx[:, None] + 1.0))) * (idx[None, :] >= idx[:, None])
        maskT[:, 1, h, :] = (g ** (-(sj[:, None] + 1.0))) * ((sj[None, :] >= sj[:, None]) & (seqj[None, :] == seqj[:, None]))
        kdec[:, 0, h] = g ** (127.0 - idx)
        kdec[:, 1, h] = g ** (7.0 - sj)
    c["qdec"] = qdec
    c["maskT"] = maskT
    c["kdec"] = kdec
    c["rowmask"] = (seqj[:, None] == np.arange(16)[None, :]).astype(np.float32)
    same = (seqj[:, None] == seqj[None, :])
    le = (idx[:, None] <= idx[None, :])
    c["triu"] = np.stack([le, le & same], axis=1).astype(np.float32)
    c["negm"] = np.stack([np.where(le, 0.0, -30000.0), np.where(le & same, 0.0, -30000.0)], axis=1).astype(np.float32)
    c["ss"] = np.stack([np.ones((128, 128)), same], axis=1).astype(np.float32)
    tp = np.stack([idx + 1.0, sj + 1.0], axis=0)
    c["tpos"] = np.repeat(tp[None, :, :], 128, axis=0).astype(np.float32)
    c["smask"] = np.repeat((sj != 0)[None, :], 128, axis=0).astype(np.float32)
    pm = np.zeros((128, 128), np.float32)
    for m_ in range(64):
        pm[m_ + 64, m_] = -1.0
        pm[m_, m_ + 64] = 1.0
    c["Pm"] = pm
    c["gmask"] = ((idx[:, None] // 16) == np.arange(8)[None, :]).astype(np.float32)
    c["cmask"] = np.repeat(((idx[None, :] // 16) == np.arange(8)[:, None])[None, :, :], 128, axis=0).astype(np.float32)
    c["rm"] = np.repeat((seqj[:, None] == np.arange(16)[None, :])[:, :, None], 128, axis=2).astype(np.float32)
    return c


def build(stages):
    nc = bass.Bass("TRN2", target_bir_lowering=False)
    es = contextlib.ExitStack()
    es.enter_context(nc.allow_non_contiguous_dma(reason="small param / layout loads"))
    try:
        es.enter_context(nc.allow_low_precision(reason="bf16 matmul operands by design"))
    except Exception:
        pass
    NL = stages.get("layers", DEPTH)

    def din(name, shape, dt=F32):
        return nc.dram_tensor(name, list(shape), dt, kind="ExternalInput").ap()

    def dout(name, shape, dt=F32):
        return nc.dram_tensor(name, list(shape), dt, kind="ExternalOutput").ap()

    def dscr(name, shape, dt=F32):
        return nc.dram_tensor(name, list(shape), dt, kind="Internal").ap()

    x_in = din("x_in", [T, D])
    CST = {"ident_f": din("ident_f", [128, 128]), "tabqk": din("tabqk", [128, 4, T]), "qdec": din("qdec", [128, 4, 640]),
           "maskT": din("maskT", [128, 2, 4, 128]), "kdec": din("kdec", [128, 2, 4]), "rowmask": din("rowmask", [128, 16]),
           "triu": din("triu", [128, 2, 128]), "negm": din("negm", [128, 2, 128]), "ss": din("ss", [128, 2, 128]), "rm": din("rm", [128, 16, 128]),
           "tpos": din("tpos", [128, 2, 128]), "smask": din("smask", [128, 128]), "Pm": din("Pm", [128, 128]),
           "gmask": din("gmask", [128, 8]), "cmask": din("cmask", [128, 8, 128])}
    W = {}
    for nm, shp in [("ffn1_norm", [DEPTH, D]), ("ffn1_w_gu", [DEPTH, D, 2 * FFN]), ("ffn1_w_down", [DEPTH, FFN, D]),
                    ("ffn2_norm", [DEPTH, D]), ("ffn2_w_gu", [DEPTH, D, 2 * FFN]), ("ffn2_w_down", [DEPTH, FFN, D]),
                    ("final_norm", [1, D]), ("mix_norm", [DEPTH, D]), ("w_in", [DEPTH, D, IN_DIM]),
                    ("ret_ln_g", [DEPTH, D]), ("ret_w_o", [DEPTH, D, D]), ("w_out", [DEPTH, D, D]),
                    ("state_ret", [DEPTH, NSS, 4, 128, 256]),
                    ("s5_a_re", [DEPTH, 64, 64]), ("s5_a_im", [DEPTH, 64, 64]), ("s5_log_dt", [DEPTH, 64]),
                    ("s5_b_re", [DEPTH, 64, 64, 16]), ("s5_b_im", [DEPTH, 64, 64, 16]), ("s5_c_re", [DEPTH, 64, 16, 64]),
                    ("s5_c_im", [DEPTH, 64, 16, 64]), ("s5_d", [DEPTH, D]), ("s5_w_glu", [DEPTH, D, 2 * D]),
                    ("state_s5", [DEPTH, NSS, 64, 64, 2]),
                    ("ssd_conv_w", [DEPTH, 4, 3072]), ("ssd_conv_b", [DEPTH, 3072]), ("ssd_dt_bias", [DEPTH, 32]),
                    ("ssd_a_log", [DEPTH, 32]), ("ssd_d", [DEPTH, 32]), ("ssd_norm", [DEPTH, 2048]), ("ssd_w_o", [DEPTH, 2048, D]),
                    ("state_ssm", [DEPTH, NSS, 32, 64, 128]), ("state_conv", [DEPTH, NSS, 3, 3072])]:
        W[nm] = din(nm, shp)
    y_out = dout("y_out", [T, D])
    ret_p = dout("ret_p", [DEPTH, 4, 128, 256])
    ret_s = dout("ret_s", [DEPTH, NSS, 4, 128, 256])
    s5_p = dout("s5_p", [DEPTH, 64, 64, 2])
    s5_s = dout("s5_s", [DEPTH, NSS, 64, 64, 2])
    ssm_p = dout("ssm_p", [DEPTH, 32, 64, 128])
    ssm_s = dout("ssm_s", [DEPTH, NSS, 32, 64, 128])
    conv_p = dout("conv_p", [DEPTH, 3, 3072])
    conv_s = dout("conv_s", [DEPTH, NSS, 3, 3072])
    xsb_scr = dscr("xsb_scr", [T, 2560], BF16)
    bc_scr = dscr("bc_scr", [128, 8, T], BF16)
    xscr = dscr("xscr", [128, KC, T])
    brscr = dscr("brscr", [128, 16, T], BF16)

    kb = KB(nc, es)

    uniq = [0]

    def sbp(stack, name, shape, dt):
        uniq[0] += 1
        return stack.enter_context(nc.sbuf_tensor("sb%d_%s" % (uniq[0], name), list(shape), dt))

    hT = sbp(es, "hT", [128, KC * T], BF16)
    hTv = hT[:, :].rearrange("p (k t) -> p k t", k=KC)
    identf = sbp(es, "identf", [128, 128], F32)
    identb = sbp(es, "identb", [128, 128], BF16)
    ones_bf = sbp(es, "ones_bf", [128, 128], BF16)
    gcols = sbp(es, "gcols", [128, 24 * 16], F32)
    gcv = gcols[:, :].rearrange("p (s k) -> p s k", k=16)
    sqb = sbp(es, "sqb", [128, KC * 512], BF16)
    sqv = sqb[:, :].rearrange("p (k t) -> p k t", k=KC)
    sdb = sbp(es, "sdb", [128, 512], F32)
    rstd = sbp(es, "rstd", [128, 512], F32)
    wst = [sbp(es, "wst%d" % i, [128, 2048], F32) for i in range(2)]
    wbf = [sbp(es, "wbf%d" % i, [128, 2048], BF16) for i in range(2)]
    PS = [es.enter_context(nc.psum_tensor("ps%d" % i, [128, 512], F32)) for i in range(8)]

    kb.dma(identf[:, :], CST["ident_f"][:, :], writes=["identf"])
    kb.op("dve", lambda e: e.tensor_copy(identb[:, :], identf[:, :]), reads=["identf"], writes=["identb"])
    kb.op("dve", lambda e: e.memset(ones_bf[:, :], 1.0), writes=["ones_bf"])

    gslot = {}

    def load_gain(name, ap_row, nk=KC):
        s = len(gslot) % 24
        gslot[name] = s
        kb.dma(gcv[:, s, 0:nk], ap_row.rearrange("(k p) -> p k", p=128), writes=[("g", s)])
        return s

    def ck(name, t0, n):
        return [(name, c) for c in range(t0 // 128, (t0 + n) // 128)]

    wslot = [0]

    def load_w(pieces, kcn, ncols, dst=None, dstkey=None, rowscale=None):
        s = wslot[0]
        wslot[0] ^= 1
        st = wst[s][:, 0:kcn * ncols].rearrange("p (k n) -> p k n", k=kcn)
        for (src, off) in pieces:
            wdt = src.shape[1]
            kb.dma(st[:, :, off:off + wdt], src.rearrange("(k p) n -> p k n", p=128), writes=[("wst", s)])
        if dst is None:
            bf = wbf[s][:, 0:kcn * ncols].rearrange("p (k n) -> p k n", k=kcn)
            key = ("wbf", s)
        else:
            bf = dst
            key = dstkey
        if rowscale is None:
            kb.op("pool", lambda e: e.tensor_copy(bf, st), reads=[("wst", s)], writes=[key])
        else:
            for k in range(kcn):
                kb.op("pool", lambda e, k=k: e.tensor_scalar(out=bf[:, k, :], in0=st[:, k, :], scalar1=gcv[:, rowscale, k:k + 1],
                                                           scalar2=None, op0=ALU.mult),
                      reads=[("wst", s), ("g", rowscale)], writes=[key])
        return bf, key

    def rmsnorm(xTv, gs, dst_fn, dst_keys_fn):
        for (t0, n) in TILES:
            for kc in range(KC):
                kb.op("act", lambda e, kc=kc, t0=t0, n=n: e.activation(
                    out=sqv[:, kc, 0:n], in_=xTv[:, kc, t0:t0 + n], func=AF.Square),
                    reads=ck("x", t0, n), writes=[("sq", kc)])
            for kc in range(KC):
                kb.op("pe", lambda e, kc=kc, n=n: e.matmul(
                    PS[6][:, 0:n], lhsT=ones_bf[:, :], rhs=sqv[:, kc, 0:n], start=(kc == 0), stop=(kc == KC - 1)),
                    reads=["ones_bf", ("sq", kc)], writes=["ps6"], inc=(kc == KC - 1))
            kb.op("act", lambda e, n=n: e.activation(
                out=sdb[:, 0:n], in_=PS[6][:, 0:n], func=AF.Sqrt, scale=1.0 / D, bias=EPS),
                reads=["ps6"], writes=["sdb"])
            kb.op("dve", lambda e, n=n: e.reciprocal(rstd[:, 0:n], sdb[:, 0:n]), reads=["sdb"], writes=["rstd"])
            for kc in range(KC):
                kb.op("dve", lambda e, kc=kc, t0=t0, n=n: e.scalar_tensor_tensor(
                    out=dst_fn(kc, t0, n), in0=xTv[:, kc, t0:t0 + n], scalar=gcv[:, gs, kc:kc + 1],
                    in1=rstd[:, 0:n], op0=ALU.mult, op1=ALU.mult),
                    reads=ck("x", t0, n) + ["rstd", ("g", gs)], writes=dst_keys_fn(kc, t0, n))

    def norm_to_h(xTv, gs):
        rmsnorm(xTv, gs, lambda kc, t0, n: hTv[:, kc, t0:t0 + n], lambda kc, t0, n: ck("h", t0, n))

    def ffn(P, xTv, l, pre):
        actb = P["actb"]
        sgb = P["sgb"]
        gs = load_gain("%s_norm%d" % (pre, l), W[pre + "_norm"][l])
        norm_to_h(xTv, gs)
        wgu = W[pre + "_w_gu"][l]
        wdn = W[pre + "_w_down"][l]
        NHG = FFN // 256
        gu_bank = 0
        dn_bank = 0
        for hg in range(NHG):
            ab = actb[hg % 2]
            abv = ab[:, :].rearrange("p (b t) -> p b t", b=2)
            kab = ("actb", hg % 2)
            wg, kwg = load_w([(wgu[:, hg * 256:(hg + 1) * 256], 0)], KC, 256)
            wu, kwu = load_w([(wgu[:, FFN + hg * 256:FFN + (hg + 1) * 256], 0)], KC, 256)
            for (t0, n) in TILES:
                for blk in range(2):
                    bg = gu_bank % 4
                    bu = (gu_bank + 1) % 4
                    gu_bank += 2
                    for kc in range(KC):
                        kb.op("pe", lambda e, kc=kc, bg=bg, blk=blk, t0=t0, n=n, wg=wg: e.matmul(
                            PS[bg][:, 0:n], lhsT=wg[:, kc, blk * 128:(blk + 1) * 128], rhs=hTv[:, kc, t0:t0 + n],
                            start=(kc == 0), stop=(kc == KC - 1)),
                            reads=[kwg] + ck("h", t0, n), writes=["ps%d" % bg], inc=(kc == KC - 1))
                    for kc in range(KC):
                        kb.op("pe", lambda e, kc=kc, bu=bu, blk=blk, t0=t0, n=n, wu=wu: e.matmul(
                            PS[bu][:, 0:n], lhsT=wu[:, kc, blk * 128:(blk + 1) * 128], rhs=hTv[:, kc, t0:t0 + n],
                            start=(kc == 0), stop=(kc == KC - 1)),
                            reads=[kwu] + ck("h", t0, n), writes=["ps%d" % bu], inc=(kc == KC - 1))
                    sg = sgb[(gu_bank // 2) % 2]
                    ksg = ("sgb", (gu_bank // 2) % 2)
                    kb.op("act", lambda e, bg=bg, n=n, sg=sg: e.activation(
                        out=sg[:, 0:n], in_=PS[bg][:, 0:n], func=AF.Silu),
                        reads=["ps%d" % bg], writes=[ksg])
                    kb.op("dve", lambda e, bu=bu, n=n, sg=sg, blk=blk, t0=t0, abv=abv: e.tensor_tensor(
                        out=abv[:, blk, t0:t0 + n], in0=PS[bu][:, 0:n], in1=sg[:, 0:n], op=ALU.mult),
                        reads=["ps%d" % bu, ksg], writes=[kab])
            wd, kwd = load_w([(wdn[hg * 256:(hg + 1) * 256, :], 0)], 2, D)
            for (t0, n) in TILES:
                for oc in range(KC):
                    bo = 4 + (dn_bank % 2)
                    dn_bank += 1
                    for blk in range(2):
                        kb.op("pe", lambda e, blk=blk, bo=bo, oc=oc, t0=t0, n=n, wd=wd, abv=abv: e.matmul(
                            PS[bo][:, 0:n], lhsT=wd[:, blk, oc * 128:(oc + 1) * 128], rhs=abv[:, blk, t0:t0 + n],
                            start=(blk == 0), stop=(blk == 1)),
                            reads=[kwd, kab], writes=["ps%d" % bo], inc=(blk == 1))
                    kb.op("dve", lambda e, bo=bo, oc=oc, t0=t0, n=n: e.scalar_tensor_tensor(
                        out=xTv[:, oc, t0:t0 + n], in0=PS[bo][:, 0:n], scalar=0.5, in1=xTv[:, oc, t0:t0 + n],
                        op0=ALU.mult, op1=ALU.add),
                        reads=["ps%d" % bo] + ck("x", t0, n), writes=ck("x", t0, n))

    def phase_x(l):
        with contextlib.ExitStack() as pes:
            xT = sbp(pes, "xT", [128, KC * T], F32)
            xTv = xT[:, :].rearrange("p (k t) -> p k t", k=KC)
            P = {"actb": [sbp(pes, "actb%d" % i, [128, 2 * T], BF16) for i in range(2)],
                 "sgb": [sbp(pes, "sgb%d" % i, [128, 512], BF16) for i in range(2)]}
            iobuf = [sbp(pes, "iobuf%d" % i, [128, D], F32) for i in range(2)]
            if l == 0:
                for c in range(NCH):
                    io = iobuf[c % 2]
                    kio = "io%d" % (c % 2)
                    kb.dma(io[:, :], x_in[c * 128:(c + 1) * 128, :], writes=[kio])
                    for half in range(2):
                        bank = PS[6 + half]
                        kbk = "ps%d" % (6 + half)
                        for j in range(4):
                            kc = half * 4 + j
                            kb.op("pe", lambda e, j=j, kc=kc, bank=bank, io=io: e.transpose(
                                bank[:, j * 128:(j + 1) * 128], io[:, kc * 128:(kc + 1) * 128], identf[:, :]),
                                reads=[kio, "identf"], writes=[kbk], inc=(j == 3))
                        if half == 0:
                            kb.op("act", lambda e, bank=bank, c=c: e.copy(
                                xTv[:, 0:4, c * 128:(c + 1) * 128], bank[:, :].rearrange("p (k t) -> p k t", k=4)),
                                reads=[kbk], writes=[("x", c)])
                        else:
                            kb.op("dve", lambda e, bank=bank, c=c: e.tensor_copy(
                                xTv[:, 4:8, c * 128:(c + 1) * 128], bank[:, :].rearrange("p (k t) -> p k t", k=4)),
                                reads=[kbk], writes=[("x", c)])
            else:
                for (t0, n) in TILES:
                    kb.dma(xTv[:, :, t0:t0 + n], xscr[:, :, t0:t0 + n], reads=[("xscr", t0)], writes=ck("x", t0, n))
                if stages.get("ffn2", True):
                    ffn(P, xTv, l - 1, "ffn2")
            if l < NL:
                if stages.get("ffn1", True):
                    ffn(P, xTv, l, "ffn1")
                gs = load_gain("mix%d" % l, W["mix_norm"][l])
                norm_to_h(xTv, gs)
                for (t0, n) in TILES:
                    kb.dma(xscr[:, :, t0:t0 + n], xTv[:, :, t0:t0 + n], reads=ck("x", t0, n), writes=[("xscr", t0)])
            else:
                gs = load_gain("final", W["final_norm"][0])
                finb = sbp(pes, "finb", [128, 4096], F32)
                fin = finb[:, :].rearrange("p (k t) -> p k t", k=KC)
                for (t0, n) in TILES:
                    rm_tiles = [(t0, n)]
                    for kc in range(KC):
                        kb.op("act", lambda e, kc=kc, t0=t0, n=n: e.activation(
                            out=sqv[:, kc, 0:n], in_=xTv[:, kc, t0:t0 + n], func=AF.Square),
                            reads=ck("x", t0, n), writes=[("sq", kc)])
                    for kc in range(KC):
                        kb.op("pe", lambda e, kc=kc, n=n: e.matmul(
                            PS[6][:, 0:n], lhsT=ones_bf[:, :], rhs=sqv[:, kc, 0:n], start=(kc == 0), stop=(kc == KC - 1)),
                            reads=["ones_bf", ("sq", kc)], writes=["ps6"], inc=(kc == KC - 1))
                    kb.op("act", lambda e, n=n: e.activation(
                        out=sdb[:, 0:n], in_=PS[6][:, 0:n], func=AF.Sqrt, scale=1.0 / D, bias=EPS),
                        reads=["ps6"], writes=["sdb"])
                    kb.op("dve", lambda e, n=n: e.reciprocal(rstd[:, 0:n], sdb[:, 0:n]), reads=["sdb"], writes=["rstd"])
                    for kc in range(KC):
                        kb.op("dve", lambda e, kc=kc, t0=t0, n=n: e.scalar_tensor_tensor(
                            out=fin[:, kc, 0:n], in0=xTv[:, kc, t0:t0 + n], scalar=gcv[:, gs, kc:kc + 1],
                            in1=rstd[:, 0:n], op0=ALU.mult, op1=ALU.mult),
                            reads=ck("x", t0, n) + ["rstd", ("g", gs)], writes=["fin"])
                    for c in range(n // 128):
                        io = iobuf[c % 2]
                        kio = "io%d" % (c % 2)
                        for half in range(2):
                            bank = PS[half]
                            kbk = "ps%d" % half
                            for j in range(4):
                                kc = half * 4 + j
                                kb.op("pe", lambda e, j=j, kc=kc, bank=bank, c=c: e.transpose(
                                    bank[:, j * 128:(j + 1) * 128], fin[:, kc, c * 128:(c + 1) * 128], identf[:, :]),
                                    reads=["fin", "identf"], writes=[kbk], inc=(j == 3))
                            if half == 0:
                                kb.op("act", lambda e, bank=bank, io=io: e.copy(io[:, 0:512], bank[:, :]),
                                      reads=[kbk], writes=[(kio, half)])
                            else:
                                kb.op("dve", lambda e, bank=bank, io=io: e.tensor_copy(io[:, 512:1024], bank[:, :]),
                                      reads=[kbk], writes=[(kio, half)])
                        kb.dma(y_out[t0 + c * 128:t0 + (c + 1) * 128, :], io[:, :], reads=[(kio, 0), (kio, 1)],
                               writes=[("yout", t0, c)])
            kb.barrier()

    def retention(l):
        win = W["w_in"][l]
        with contextlib.ExitStack() as pes:
            Wv = sbp(pes, "Wv", [128, KC * 1024], BF16)
            Wg = sbp(pes, "Wg", [128, KC * 1024], BF16)
            Wvv = Wv[:, :].rearrange("p (k n) -> p k n", k=KC)
            Wgv = Wg[:, :].rearrange("p (k n) -> p k n", k=KC)
            qd = sbp(pes, "qd", [128, 4 * 512], BF16)
            kT = sbp(pes, "kT", [128, 4 * 512], BF16)
            qdv = qd[:, :].rearrange("p (h t) -> p h t", h=4)
            kTv = kT[:, :].rearrange("p (h t) -> p h t", h=4)
            tab = [sbp(pes, "tab%d" % i, [128, 4 * 512], F32) for i in range(1)]
            qdec = sbp(pes, "qdec", [128, 4 * 640], F32)
            qdecv = qdec[:, :].rearrange("p (h t) -> p h t", h=4)
            maskT = sbp(pes, "maskT", [128, 2 * 512], F32)
            maskTv = maskT[:, :].rearrange("p (s n) -> p s n", s=2)
            kdec = sbp(pes, "kdec", [128, 8], F32)
            rowmask = sbp(pes, "rowmask", [128, 16], F32)
            tmpA = sbp(pes, "tmpA", [128, 512], F32)
            tmpB = sbp(pes, "tmpB", [128, 512], F32)
            tmpC = sbp(pes, "tmpC", [128, 512], F32)
            tmpD = sbp(pes, "tmpD", [128, 512], F32)
            v_tm = sbp(pes, "v_tm", [128, 1024], BF16)
            sg = sbp(pes, "sg", [128, 1024], BF16)
            kd_tm = sbp(pes, "kd_tm", [128, 512], BF16)
            kdm = sbp(pes, "kdm", [128, 512], BF16)
            sc = sbp(pes, "sc", [128, 512], BF16)
            S_f = sbp(pes, "S_f", [128, 1024], F32)
            S_b = sbp(pes, "S_b", [128, 1024], BF16)
            S0f = [sbp(pes, "S0f%d" % i, [128, 1024], F32) for i in range(2)]
            S0b = [sbp(pes, "S0b%d" % i, [128, 1024], BF16) for i in range(2)]
            Snew = [sbp(pes, "Snew%d" % i, [128, 1024], F32) for i in range(2)]
            qblk = sbp(pes, "qblk", [128, 4 * 2048], BF16)
            qblkv = qblk[:, :].rearrange("p (h s t) -> p h s t", h=4, s=16)
            bst = sbp(pes, "bst", [128, 4 * 6], F32)
            mv = sbp(pes, "mv", [128, 4 * 2], F32)
            mvv = mv[:, :].rearrange("p (h t) -> p h t", h=4)
            sdv = sbp(pes, "sdv", [128, 4], F32)
            rsv = sbp(pes, "rsv", [128, 4], F32)
            on = sbp(pes, "on", [128, 1024], BF16)
            og = sbp(pes, "og", [128, 1024], BF16)
            ogT = [sbp(pes, "ogT%d" % i, [128, 1024], BF16) for i in range(2)]

            kb.dma(qdecv, CST["qdec"], writes=["qdec"])
            kb.dma(maskT[:, :].rearrange("p (s h n) -> p s h n", s=2, h=4), CST["maskT"], writes=["maskT"])
            kb.dma(kdec[:, :].rearrange("p (s h) -> p s h", s=2), CST["kdec"], writes=["kdec"])
            kb.dma(rowmask[:, :], CST["rowmask"], writes=["rowmask"])
            kb.op("pool", lambda e: e.memset(qblk[:, :], 0.0), writes=["qblk"])
            kb.op("pool", lambda e: e.memset(S_f[:, :], 0.0), writes=["S_f"])
            kb.op("pool", lambda e: e.memset(S_b[:, :], 0.0), writes=["S_b"])
            for j in range(4):
                load_w([(win[:, 1024 + j * 256:1024 + (j + 1) * 256], 0)], KC, 256, dst=Wvv[:, :, j * 256:(j + 1) * 256], dstkey="Wv")
            for j in range(4):
                load_w([(win[:, 2048 + j * 256:2048 + (j + 1) * 256], 0)], KC, 256, dst=Wgv[:, :, j * 256:(j + 1) * 256], dstkey="Wg")

            for ti, (t0, n) in enumerate(TILES):
                tb = tab[0]
                tbv = tb[:, :].rearrange("p (s t) -> p s t", s=4)
                ktb = ("tab", 0)
                kb.dma(tbv[:, :, 0:n], CST["tabqk"][:, :, t0:t0 + n], writes=[ktb])
                samp = (t0 >= SEQ)
                sel = 1 if samp else 0
                qoff = 512 if samp else 0
                for h in range(4):
                    c0 = h * 128
                    k0 = 512 + h * 128
                    wq, kwq = load_w([(win[:, c0:c0 + 128], 0), (win[:, c0 + 64:c0 + 128], 128), (win[:, c0:c0 + 64], 192)], KC, 256)
                    wk, kwk = load_w([(win[:, k0:k0 + 128], 0), (win[:, k0 + 64:k0 + 128], 128), (win[:, k0:k0 + 64], 192)], KC, 256)
                    for b in range(4):
                        ww, kww = (wq, kwq) if b < 2 else (wk, kwk)
                        for kc in range(KC):
                            kb.op("pe", lambda e, kc=kc, b=b, ww=ww, t0=t0, n=n: e.matmul(
                                PS[b][:, 0:n], lhsT=ww[:, kc, (b % 2) * 128:(b % 2 + 1) * 128], rhs=hTv[:, kc, t0:t0 + n],
                                start=(kc == 0), stop=(kc == KC - 1)),
                                reads=[kww] + ck("h", t0, n), writes=["ps%d" % b], inc=(kc == KC - 1))
                    kb.op("dve", lambda e, n=n, tbv=tbv: e.tensor_tensor(out=tmpA[:, 0:n], in0=PS[0][:, 0:n], in1=tbv[:, 0, 0:n], op=ALU.mult),
                          reads=["ps0", ktb], writes=["tmpA"])
                    kb.op("dve", lambda e, n=n, tbv=tbv: e.tensor_tensor(out=tmpB[:, 0:n], in0=PS[1][:, 0:n], in1=tbv[:, 1, 0:n], op=ALU.mult),
                          reads=["ps1", ktb], writes=["tmpB"])
                    kb.op("pool", lambda e, n=n: e.tensor_tensor(out=tmpA[:, 0:n], in0=tmpA[:, 0:n], in1=tmpB[:, 0:n], op=ALU.add),
                          reads=["tmpA", "tmpB"], writes=["tmpA"])
                    kb.op("pool", lambda e, n=n, h=h, qoff=qoff: e.tensor_tensor(
                        out=qdv[:, h, 0:n], in0=tmpA[:, 0:n], in1=qdecv[:, h, qoff:qoff + n], op=ALU.mult),
                        reads=["tmpA", "qdec"], writes=[("qd", h)])
                    kb.op("dve", lambda e, n=n, tbv=tbv: e.tensor_tensor(out=tmpC[:, 0:n], in0=PS[2][:, 0:n], in1=tbv[:, 2, 0:n], op=ALU.mult),
                          reads=["ps2", ktb], writes=["tmpC"])
                    kb.op("dve", lambda e, n=n, tbv=tbv: e.tensor_tensor(out=tmpD[:, 0:n], in0=PS[3][:, 0:n], in1=tbv[:, 3, 0:n], op=ALU.mult),
                          reads=["ps3", ktb], writes=["tmpD"])
                    kb.op("pool", lambda e, n=n, h=h: e.tensor_tensor(out=kTv[:, h, 0:n], in0=tmpC[:, 0:n], in1=tmpD[:, 0:n], op=ALU.add),
                          reads=["tmpC", "tmpD"], writes=[("kT", h)])
                QD = [("qd", h) for h in range(4)]
                KT = [("kT", h) for h in range(4)]
                for ci in range(n // 128):
                    c = t0 // 128 + ci
                    cs = slice(ci * 128, (ci + 1) * 128)
                    ts = slice(t0 + ci * 128, t0 + (ci + 1) * 128)
                    for half in range(2):
                        for kc in range(KC):
                            kb.op("pe", lambda e, kc=kc, half=half, ts=ts: e.matmul(
                                PS[half][:, :], lhsT=hTv[:, kc, ts], rhs=Wvv[:, kc, half * 512:(half + 1) * 512],
                                start=(kc == 0), stop=(kc == KC - 1)),
                                reads=["Wv", ("h", c)], writes=["ps%d" % half], inc=(kc == KC - 1))
                        kb.op("act", lambda e, half=half: e.copy(v_tm[:, half * 512:(half + 1) * 512], PS[half][:, :]),
                              reads=["ps%d" % half], writes=[("v_tm", half)])
                    for half in range(2):
                        for kc in range(KC):
                            kb.op("pe", lambda e, kc=kc, half=half, ts=ts: e.matmul(
                                PS[2 + half][:, :], lhsT=hTv[:, kc, ts], rhs=Wgv[:, kc, half * 512:(half + 1) * 512],
                                start=(kc == 0), stop=(kc == KC - 1)),
                                reads=["Wg", ("h", c)], writes=["ps%d" % (2 + half)], inc=(kc == KC - 1))
                        kb.op("act", lambda e, half=half: e.activation(out=sg[:, half * 512:(half + 1) * 512], in_=PS[2 + half][:, :], func=AF.Silu),
                              reads=["ps%d" % (2 + half)], writes=[("sg", half)])
                    VT = [("v_tm", 0), ("v_tm", 1)]
                    p4 = PS[4][:, :].bitcast(BF16)
                    for h in range(4):
                        kb.op("pe", lambda e, h=h, cs=cs: e.transpose(p4[:, h * 128:(h + 1) * 128], kTv[:, h, cs], identb[:, :]),
                              reads=[("kT", h), "identb"], writes=["ps4"], inc=(h == 3))
                    for h in range(4):
                        kb.op("dve", lambda e, h=h, sel=sel: e.tensor_scalar(
                            out=kd_tm[:, h * 128:(h + 1) * 128], in0=p4[:, h * 128:(h + 1) * 128],
                            scalar1=kdec[:, sel * 4 + h:sel * 4 + h + 1], scalar2=None, op0=ALU.mult),
                            reads=["ps4", "kdec"], writes=["kd_tm"])
                    for h in range(4):
                        kb.op("pe", lambda e, h=h, cs=cs: e.matmul(PS[5][:, h * 128:(h + 1) * 128], lhsT=kTv[:, h, cs], rhs=qdv[:, h, cs],
                                                                 start=True, stop=True),
                              reads=[("kT", h), ("qd", h)], writes=["ps5"], inc=(h == 3))
                    kb.op("dve", lambda e, sel=sel: e.tensor_tensor(out=sc[:, :], in0=PS[5][:, :], in1=maskTv[:, sel, :], op=ALU.mult),
                          reads=["ps5", "maskT"], writes=["sc"])
                    if not samp:
                        for h in range(4):
                            ob = PS[6 + h // 2][:, (h % 2) * 256:(h % 2) * 256 + 256]
                            kb.op("pe", lambda e, h=h, ob=ob: e.matmul(ob, lhsT=sc[:, h * 128:(h + 1) * 128], rhs=v_tm[:, h * 256:(h + 1) * 256],
                                                                     start=True, stop=False),
                                  reads=["sc"] + VT, writes=["ps%d" % (6 + h // 2)], inc=False)
                            kb.op("pe", lambda e, h=h, ob=ob, cs=cs: e.matmul(ob, lhsT=qdv[:, h, cs], rhs=S_b[:, h * 256:(h + 1) * 256],
                                                                            start=False, stop=True),
                                  reads=[("qd", h), "S_b"], writes=["ps%d" % (6 + h // 2)], inc=True)
                        for h in range(4):
                            sbk = PS[h // 2][:, (h % 2) * 256:(h % 2) * 256 + 256]
                            kb.op("pe", lambda e, h=h, sbk=sbk: e.matmul(sbk, lhsT=kd_tm[:, h * 128:(h + 1) * 128], rhs=v_tm[:, h * 256:(h + 1) * 256],
                                                                       start=True, stop=True),
                                  reads=["kd_tm"] + VT, writes=["ps%d" % (h // 2)], inc=True)
                            kb.op("dve", lambda e, h=h, sbk=sbk: e.scalar_tensor_tensor(
                                out=S_f[:, h * 256:(h + 1) * 256], in0=S_f[:, h * 256:(h + 1) * 256], scalar=float(GAM[h] ** 128),
                                in1=sbk, op0=ALU.mult, op1=ALU.add),
                                reads=["ps%d" % (h // 2), "S_f"], writes=["S_f"])
                        kb.op("act", lambda e: e.copy(S_b[:, :], S_f[:, :]), reads=["S_f"], writes=["S_b"])
                        if c == SEQ // 128 - 1:
                            kb.dma(ret_p[l].rearrange("h d e -> d h e"), S_f[:, :].rearrange("p (h e) -> p h e", h=4),
                                   reads=["S_f"], writes=[("ret_p", l)])
                    else:
                        for h in range(4):
                            ob = PS[6 + h // 2][:, (h % 2) * 256:(h % 2) * 256 + 256]
                            for s in range(NSS):
                                kb.op("pool", lambda e, h=h, s=s: e.tensor_copy(qblkv[:, h, s, s * 8:(s + 1) * 8], qdv[:, h, s * 8:(s + 1) * 8]),
                                      reads=[("qd", h)], writes=["qblk"])
                            kb.op("pe", lambda e, h=h, ob=ob: e.matmul(ob, lhsT=sc[:, h * 128:(h + 1) * 128], rhs=v_tm[:, h * 256:(h + 1) * 256],
                                                                     start=True, stop=False),
                                  reads=["sc"] + VT, writes=["ps%d" % (6 + h // 2)], inc=False)
                            for s in range(NSS):
                                i2 = (h * NSS + s) % 2
                                s0f = S0f[i2]
                                s0b = S0b[i2]
                                sn = Snew[i2]
                                kb.dma(s0f[:, 0:256], W["state_ret"][l, s, h], writes=[("S0f", i2)])
                                kb.op("act", lambda e, s0f=s0f, s0b=s0b: e.copy(s0b[:, 0:256], s0f[:, 0:256]), reads=[("S0f", i2)], writes=[("S0b", i2)])
                                kb.op("pe", lambda e, h=h, ob=ob, s=s, s0b=s0b: e.matmul(
                                    ob, lhsT=qblkv[:, h, s, :], rhs=s0b[:, 0:256], start=False, stop=(s == NSS - 1)),
                                    reads=["qblk", ("S0b", i2)], writes=["ps%d" % (6 + h // 2)], inc=True)
                                kb.op("dve", lambda e, s=s, h=h: e.tensor_scalar(out=kdm[:, 0:128], in0=kd_tm[:, h * 128:(h + 1) * 128],
                                                                               scalar1=rowmask[:, s:s + 1], scalar2=None, op0=ALU.mult),
                                      reads=["kd_tm", "rowmask"], writes=["kdm"])
                                sbk = PS[i2][:, 0:256]
                                kb.op("pe", lambda e, h=h, sbk=sbk: e.matmul(sbk, lhsT=kdm[:, 0:128], rhs=v_tm[:, h * 256:(h + 1) * 256],
                                                                           start=True, stop=True),
                                      reads=["kdm"] + VT, writes=["ps%d" % i2], inc=True)
                                kb.op("dve", lambda e, h=h, sbk=sbk, s0f=s0f, sn=sn: e.scalar_tensor_tensor(
                                    out=sn[:, 0:256], in0=s0f[:, 0:256], scalar=float(GAM[h] ** 8),
                                    in1=sbk, op0=ALU.mult, op1=ALU.add),
                                    reads=["ps%d" % i2, ("S0f", i2)], writes=[("Snew", i2)])
                                kb.dma(ret_s[l, s, h], sn[:, 0:256], reads=[("Snew", i2)], writes=[("ret_s", l, s, h)])
                    for h in range(4):
                        ob = PS[6 + h // 2][:, (h % 2) * 256:(h % 2) * 256 + 256]
                        kb.op("dve", lambda e, h=h, ob=ob: e.bn_stats(bst[:, h * 6:(h + 1) * 6], ob), reads=["ps%d" % (6 + h // 2)], writes=[("bst", h)])
                        kb.op("dve", lambda e, h=h: e.bn_aggr(mv[:, h * 2:(h + 1) * 2], bst[:, h * 6:(h + 1) * 6]), reads=[("bst", h)], writes=[("mv", h)])
                    MV = [("mv", h) for h in range(4)]
                    kb.op("act", lambda e: e.activation(out=sdv[:, :], in_=mvv[:, :, 1], func=AF.Sqrt, bias=EPS, scale=1.0), reads=MV, writes=["sdv"])
                    kb.op("dve", lambda e: e.reciprocal(rsv[:, :], sdv[:, :]), reads=["sdv"], writes=["rsv"])
                    for h in range(4):
                        ob = PS[6 + h // 2][:, (h % 2) * 256:(h % 2) * 256 + 256]
                        kb.op("dve", lambda e, h=h, ob=ob: e.tensor_scalar(
                            out=on[:, h * 256:(h + 1) * 256], in0=ob, scalar1=mvv[:, h, 0:1], scalar2=rsv[:, h:h + 1],
                            op0=ALU.subtract, op1=ALU.mult),
                            reads=["ps%d" % (6 + h // 2), "rsv"] + MV, writes=["on"])
                    kb.op("pool", lambda e: e.tensor_tensor(out=og[:, :], in0=on[:, :], in1=sg[:, :], op=ALU.mult),
                          reads=["on", ("sg", 0), ("sg", 1)], writes=["og"])
                    ogt = ogT[c % 2]
                    for kc in range(KC):
                        kb.op("pe", lambda e, kc=kc: e.transpose(p4[:, kc * 128:(kc + 1) * 128], og[:, kc * 128:(kc + 1) * 128], identb[:, :]),
                              reads=["og", "identb"], writes=["ps4"], inc=(kc == KC - 1))
                    kb.op("act", lambda e, ogt=ogt: e.copy(ogt[:, :], p4[:, :]), reads=["ps4"], writes=[("ogT", c % 2)])
                    kb.dma(brscr[:, 0:KC, ts], ogt[:, :].rearrange("p (k t) -> p k t", k=KC), reads=[("ogT", c % 2)], writes=[("brscr", c)])
            kb.barrier()

    def stage_c(l, branch, wo_ap, kcb, gate_col0, rowscale_name=None, rowscale_ap=None, glu=False):
        win = W["w_in"][l]
        with contextlib.ExitStack() as pes:
            nco = 2048 if glu else 1024
            Wo = sbp(pes, "Wo", [128, kcb * nco], BF16)
            Wov = Wo[:, :].rearrange("p (k n) -> p k n", k=kcb)
            tglu = [sbp(pes, "tglu%d" % i, [128, 512], F32) for i in range(2)] if glu else None
            Wgt = sbp(pes, "Wgt", [128, KC * 1024], BF16)
            Wgtv = Wgt[:, :].rearrange("p (k n) -> p k n", k=KC)
            Wout = sbp(pes, "Wout", [128, KC * 1024], BF16)
            Woutv = Wout[:, :].rearrange("p (k n) -> p k n", k=KC)
            nbr = 2 if kcb == 8 else 1
            brt = [sbp(pes, "brt%d" % i, [128, kcb * 512], BF16) for i in range(nbr)]
            xt = [sbp(pes, "xt%d" % i, [128, KC * 512], F32) for i in range(2)]
            mt = sbp(pes, "mt", [128, KC * 512], BF16)
            mtv = mt[:, :].rearrange("p (k t) -> p k t", k=KC)
            sig = [sbp(pes, "sig%d" % i, [128, 512], F32) for i in range(2)]
            rs = None
            if rowscale_ap is not None:
                for k0 in range(0, kcb, KC):
                    pass
                rs = load_gain(rowscale_name, rowscale_ap, kcb)
            for k0 in range(0, kcb, 2):
                for j in range(nco // 256):
                    if rs is None:
                        load_w([(wo_ap[k0 * 128:(k0 + 2) * 128, j * 256:(j + 1) * 256], 0)], 2, 256,
                               dst=Wov[:, k0:k0 + 2, j * 256:(j + 1) * 256], dstkey="Wo")
                    else:
                        s = wslot[0]
                        wslot[0] ^= 1
                        st = wst[s][:, 0:512].rearrange("p (k n) -> p k n", k=2)
                        kb.dma(st, wo_ap[k0 * 128:(k0 + 2) * 128, j * 256:(j + 1) * 256].rearrange("(k p) n -> p k n", p=128), writes=[("wst", s)])
                        for kk in range(2):
                            kb.op("pool", lambda e, kk=kk, st=st, k0=k0, j=j: e.tensor_scalar(
                                out=Wov[:, k0 + kk, j * 256:(j + 1) * 256], in0=st[:, kk, :], scalar1=gcv[:, rs, k0 + kk:k0 + kk + 1],
                                scalar2=None, op0=ALU.mult),
                                reads=[("wst", s), ("g", rs)], writes=["Wo"])
            for j in range(4):
                load_w([(win[:, gate_col0 + j * 256:gate_col0 + (j + 1) * 256], 0)], KC, 256, dst=Wgtv[:, :, j * 256:(j + 1) * 256], dstkey="Wgt")
            for j in range(4):
                load_w([(W["w_out"][l][:, j * 256:(j + 1) * 256], 0)], KC, 256, dst=Woutv[:, :, j * 256:(j + 1) * 256], dstkey="Wout")
            bank = 0
            for ti, (t0, n) in enumerate(TILES):
                b_t = brt[ti % nbr]
                b_v = b_t[:, :].rearrange("p (k t) -> p k t", k=kcb)
                x_t = xt[ti % 2]
                x_v = x_t[:, :].rearrange("p (k t) -> p k t", k=KC)
                kb.dma(b_v[:, :, 0:n], brscr[:, 0:kcb, t0:t0 + n], reads=ck("brscr", t0, n), writes=[("brt", ti % nbr)])
                kb.dma(x_v[:, :, 0:n], xscr[:, :, t0:t0 + n], reads=[("xscr", t0)], writes=[("xt", ti % 2)])
                for oc in range(KC):
                    by = bank % 4
                    bg = (bank + 1) % 4
                    bank += 2
                    for kc in range(kcb):
                        kb.op("pe", lambda e, kc=kc, oc=oc, by=by, b_v=b_v, n=n: e.matmul(
                            PS[by][:, 0:n], lhsT=Wov[:, kc, oc * 128:(oc + 1) * 128], rhs=b_v[:, kc, 0:n], start=(kc == 0), stop=(kc == kcb - 1)),
                            reads=["Wo", ("brt", ti % nbr)], writes=["ps%d" % by], inc=(kc == kcb - 1))
                    for kc in range(KC):
                        kb.op("pe", lambda e, kc=kc, oc=oc, bg=bg, t0=t0, n=n: e.matmul(
                            PS[bg][:, 0:n], lhsT=Wgtv[:, kc, oc * 128:(oc + 1) * 128], rhs=hTv[:, kc, t0:t0 + n], start=(kc == 0), stop=(kc == KC - 1)),
                            reads=["Wgt"] + ck("h", t0, n), writes=["ps%d" % bg], inc=(kc == KC - 1))
                    sgm = sig[oc % 2]
                    if glu:
                        b2 = 6 + (oc % 2)
                        for kc in range(kcb):
                            kb.op("pe", lambda e, kc=kc, oc=oc, b2=b2, b_v=b_v, n=n: e.matmul(
                                PS[b2][:, 0:n], lhsT=Wov[:, kc, 1024 + oc * 128:1024 + (oc + 1) * 128], rhs=b_v[:, kc, 0:n], start=(kc == 0), stop=(kc == kcb - 1)),
                                reads=["Wo", ("brt", ti % nbr)], writes=["ps%d" % b2], inc=(kc == kcb - 1))
                        tg = tglu[oc % 2]
                        kb.op("act", lambda e, b2=b2, n=n, tg=tg: e.activation(out=tg[:, 0:n], in_=PS[b2][:, 0:n], func=AF.Sigmoid),
                              reads=["ps%d" % b2], writes=[("tglu", oc % 2)])
                        kb.op("dve", lambda e, by=by, n=n, tg=tg: e.tensor_tensor(out=tg[:, 0:n], in0=PS[by][:, 0:n], in1=tg[:, 0:n], op=ALU.mult),
                              reads=["ps%d" % by, ("tglu", oc % 2)], writes=[("tglu", oc % 2)])
                        kb.op("act", lambda e, bg=bg, n=n, sgm=sgm: e.activation(out=sgm[:, 0:n], in_=PS[bg][:, 0:n], func=AF.Sigmoid),
                              reads=["ps%d" % bg], writes=[("sig", oc % 2)])
                        kb.op("dve", lambda e, n=n, sgm=sgm, oc=oc, tg=tg: e.tensor_tensor(out=mtv[:, oc, 0:n], in0=tg[:, 0:n], in1=sgm[:, 0:n], op=ALU.mult),
                              reads=[("tglu", oc % 2), ("sig", oc % 2)], writes=[("mt", oc)])
                    else:
                        kb.op("act", lambda e, bg=bg, n=n, sgm=sgm: e.activation(out=sgm[:, 0:n], in_=PS[bg][:, 0:n], func=AF.Sigmoid),
                              reads=["ps%d" % bg], writes=[("sig", oc % 2)])
                        kb.op("dve", lambda e, by=by, n=n, sgm=sgm, oc=oc: e.tensor_tensor(out=mtv[:, oc, 0:n], in0=PS[by][:, 0:n], in1=sgm[:, 0:n], op=ALU.mult),
                              reads=["ps%d" % by, ("sig", oc % 2)], writes=[("mt", oc)])
                MT = [("mt", oc) for oc in range(KC)]
                for oc in range(KC):
                    bo = 4 + (oc % 2)
                    for kc in range(KC):
                        kb.op("pe", lambda e, kc=kc, oc=oc, bo=bo, n=n: e.matmul(
                            PS[bo][:, 0:n], lhsT=Woutv[:, kc, oc * 128:(oc + 1) * 128], rhs=mtv[:, kc, 0:n], start=(kc == 0), stop=(kc == KC - 1)),
                            reads=["Wout"] + MT, writes=["ps%d" % bo], inc=(kc == KC - 1))
                    kb.op("dve", lambda e, oc=oc, bo=bo, n=n, x_v=x_v: e.tensor_tensor(out=x_v[:, oc, 0:n], in0=PS[bo][:, 0:n], in1=x_v[:, oc, 0:n], op=ALU.add),
                          reads=["ps%d" % bo, ("xt", ti % 2)], writes=[("xt", ti % 2)])
                kb.dma(xscr[:, :, t0:t0 + n], x_v[:, :, 0:n], reads=[("xt", ti % 2)], writes=[("xscr", t0)])
            kb.barrier()

    def ssd(l):
        win = W["w_in"][l]
        XB0, Z0, DT0 = 6144, 4096, 9216
        Ident = AF.Identity
        with contextlib.ExitStack() as pes:
            cw = sbp(pes, "cw", [128, 24 * 5], F32)
            cwv = cw[:, :].rearrange("p (f k) -> p f k", k=5)
            cb = None
            with contextlib.ExitStack() as p0:
                cwt = sbp(p0, "cwt", [5, 3072], F32)
                kb.dma(cwt[0:4, :], W["ssd_conv_w"][l], writes=["cwt"])
                kb.dma(cwt[4:5, :], W["ssd_conv_b"][l:l + 1, :], writes=["cwt"])
                for fc in range(24):
                    kb.op("pe", lambda e, fc=fc: e.transpose(PS[7][:, fc * 5:(fc + 1) * 5], cwt[0:5, fc * 128:(fc + 1) * 128], identf[0:5, 0:5]),
                          reads=["cwt", "identf"], writes=["ps7"], inc=(fc == 23))
                kb.op("act", lambda e: e.copy(cw[:, :], PS[7][:, 0:120]), reads=["ps7"], writes=["cw"])
                kb.barrier()
            with contextlib.ExitStack() as p1:
                cin = sbp(p1, "cin", [48, 3072], F32)
                histT = sbp(p1, "histT", [128, 24 * 48], F32)
                hv = histT[:, :].rearrange("p (f s r) -> p f s r", f=24, s=16)
                tailT = sbp(p1, "tailT", [128, 24 * 48], F32)
                tv = tailT[:, :].rearrange("p (f s r) -> p f s r", f=24, s=16)
                tailP = sbp(p1, "tailP", [128, 72], F32)
                cout = sbp(p1, "cout", [48, 3072], F32)
                raw = [sbp(p1, "raw%d" % i, [128, 515], F32) for i in range(2)]
                acc = sbp(p1, "acc", [128, 512], F32)
                xc = [sbp(p1, "xc%d" % i, [128, 512], BF16) for i in range(2)]
                xst = [sbp(p1, "xst%d" % i, [128, 512], BF16) for i in range(2)]
                kb.dma(cin[:, :], W["state_conv"][l].rearrange("s r f -> (s r) f"), writes=["cin"])
                for fc in range(24):
                    bi = 6 + (fc // 4) % 2
                    j = fc % 4
                    kb.op("pe", lambda e, bi=bi, j=j, fc=fc: e.transpose(PS[bi][:, j * 48:(j + 1) * 48], cin[0:48, fc * 128:(fc + 1) * 128], identf[0:48, 0:48]),
                          reads=["cin", "identf"], writes=["ps%d" % bi], inc=(j == 3))
                    if j == 3:
                        kb.op("act", lambda e, bi=bi, fc=fc: e.copy(histT[:, (fc - 3) * 48:(fc + 1) * 48], PS[bi][:, 0:192]),
                              reads=["ps%d" % bi], writes=["histT"])
                rix = 0
                p5 = PS[5][:, :].bitcast(BF16)
                for fcp in range(12):
                    wx, kwx = load_w([(win[:, XB0 + fcp * 256:XB0 + (fcp + 1) * 256], 0)], KC, 256)
                    for sub in range(2):
                        fc = fcp * 2 + sub
                        prevR, kprev = None, None
                        for ti, (t0, n) in enumerate(TILES):
                            pb = rix % 4
                            bank = PS[pb]
                            kbank = "ps%d" % pb
                            for kc in range(KC):
                                kb.op("pe", lambda e, kc=kc, bank=bank, sub=sub, t0=t0, n=n, wx=wx: e.matmul(
                                    bank[:, 0:n], lhsT=wx[:, kc, sub * 128:(sub + 1) * 128], rhs=hTv[:, kc, t0:t0 + n],
                                    start=(kc == 0), stop=(kc == KC - 1)),
                                    reads=[kwx] + ck("h", t0, n), writes=[kbank], inc=(kc == KC - 1))
                            R = raw[rix % 2]
                            kR = ("raw", rix % 2)
                            xcb = xc[rix % 2]
                            kxc = ("xc", rix % 2)
                            xsb_ = xst[rix % 2]
                            kxs = ("xst", rix % 2)
                            rix += 1
                            samp = t0 >= SEQ
                            if not samp:
                                if t0 == 0:
                                    kb.op("pool", lambda e, R=R: e.memset(R[:, 0:3], 0.0), writes=[kR])
                                else:
                                    kb.op("dve", lambda e, R=R, prevR=prevR: e.tensor_copy(R[:, 0:3], prevR[:, 512:515]), reads=[kprev], writes=[kR])
                                kb.op("act", lambda e, R=R, bank=bank, n=n: e.copy(R[:, 3:3 + n], bank[:, 0:n]), reads=[kbank], writes=[kR])
                                X = [R[:, k:k + n] for k in range(4)]
                                A = acc[:, 0:n]
                            else:
                                R3 = R[:, 0:176].rearrange("p (s c) -> p s c", c=11)
                                kb.op("dve", lambda e, R3=R3, fc=fc: e.tensor_copy(R3[:, :, 0:3], hv[:, fc]), reads=["histT"], writes=[kR])
                                kb.op("act", lambda e, R3=R3, bank=bank: e.copy(R3[:, :, 3:11], bank[:, 0:128].rearrange("p (s c) -> p s c", c=8)),
                                      reads=[kbank], writes=[kR])
                                X = [R3[:, :, k:k + 8] for k in range(4)]
                                A = acc[:, 0:128].rearrange("p (s c) -> p s c", c=8)
                            kb.op("act", lambda e, A=A, X=X, fc=fc: e.activation(out=A, in_=X[3], func=Ident, scale=cwv[:, fc, 3:4], bias=cwv[:, fc, 4:5]),
                                  reads=[kR, "cw"], writes=["acc"])
                            for k in (2, 1, 0):
                                kb.op("dve", lambda e, A=A, X=X, fc=fc, k=k: e.scalar_tensor_tensor(
                                    out=A, in0=X[k], scalar=cwv[:, fc, k:k + 1], in1=A, op0=ALU.mult, op1=ALU.add),
                                    reads=[kR, "cw", "acc"], writes=["acc"])
                            kb.op("act", lambda e, xcb=xcb, n=n: e.activation(out=xcb[:, 0:n], in_=acc[:, 0:n], func=AF.Silu),
                                  reads=["acc"], writes=[kxc])
                            if t0 == 1536:
                                kb.op("pool", lambda e, R=R, fc=fc: e.tensor_copy(tailP[:, fc * 3:(fc + 1) * 3], R[:, 512:515]), reads=[kR], writes=["tailP"])
                            if samp:
                                kb.op("pool", lambda e, R3=R3, fc=fc: e.tensor_copy(tv[:, fc], R3[:, :, 8:11]), reads=[kR], writes=["tailT"])
                            if fc >= 16:
                                kb.dma(bc_scr[:, fc - 16, t0:t0 + n], xcb[:, 0:n], reads=[kxc], writes=[("bc_scr", fc, t0)])
                            if fc < 20:
                                nci = n // 128
                                for ci in range(nci):
                                    kb.op("pe", lambda e, ci=ci, xcb=xcb: e.transpose(p5[:, ci * 128:(ci + 1) * 128], xcb[:, ci * 128:(ci + 1) * 128], identb[:, :]),
                                          reads=[kxc, "identb"], writes=["ps5"], inc=(ci == nci - 1))
                                kb.op("act", lambda e, xsb_=xsb_, n=n: e.copy(xsb_[:, 0:n], p5[:, 0:n]), reads=["ps5"], writes=[kxs])
                                kb.dma(xsb_scr[t0:t0 + n, fc * 128:(fc + 1) * 128].rearrange("(c p) f -> p c f", p=128),
                                       xsb_[:, 0:n].rearrange("p (c f) -> p c f", f=128), reads=[kxs], writes=[("xsb_scr", fc, t0)])
                            prevR, kprev = R, kR
                for fc in range(24):
                    bi = 6 + (fc // 4) % 2
                    j = fc % 4
                    kb.op("pe", lambda e, bi=bi, j=j, fc=fc: e.transpose(PS[bi][0:3, j * 128:(j + 1) * 128], tailP[:, fc * 3:(fc + 1) * 3], identf[:, :]),
                          reads=["tailP", "identf"], writes=["ps%d" % bi], inc=(j == 3))
                    if j == 3:
                        kb.op("act", lambda e, bi=bi, fc=fc: e.copy(cin[0:3, (fc - 3) * 128:(fc + 1) * 128], PS[bi][0:3, 0:512]),
                              reads=["ps%d" % bi, "histT"], writes=["cin"])
                kb.dma(conv_p[l], cin[0:3, :], reads=["cin"], writes=[("conv_p", l)])
                for fc in range(24):
                    bi = 6 + (fc // 4) % 2
                    j = fc % 4
                    kb.op("pe", lambda e, bi=bi, j=j, fc=fc: e.transpose(PS[bi][0:48, j * 128:(j + 1) * 128], tailT[:, fc * 48:(fc + 1) * 48], identf[:, :]),
                          reads=["tailT", "identf"], writes=["ps%d" % bi], inc=(j == 3))
                    if j == 3:
                        kb.op("act", lambda e, bi=bi, fc=fc: e.copy(cout[0:48, (fc - 3) * 128:(fc + 1) * 128], PS[bi][0:48, 0:512]),
                              reads=["ps%d" % bi], writes=["cout"])
                kb.dma(conv_s[l].rearrange("s r f -> (s r) f"), cout[:, :], reads=["cout"], writes=[("conv_s", l)])
                kb.barrier()

            if not stages.get("ssd_s2", True):
                return
            with contextlib.ExitStack() as p2:
                Wz = sbp(p2, "Wz", [128, KC * 2048], BF16)
                Wzv = Wz[:, :].rearrange("p (k n) -> p k n", k=KC)
                Wdt = sbp(p2, "Wdt", [128, KC * 32], BF16)
                Wdtv = Wdt[:, :].rearrange("p (k n) -> p k n", k=KC)
                triu = sbp(p2, "triu", [128, 256], F32)
                triuv = triu[:, :].rearrange("p (s n) -> p s n", s=2)
                negm = sbp(p2, "negm", [128, 256], F32)
                negmv = negm[:, :].rearrange("p (s n) -> p s n", s=2)
                ssm = sbp(p2, "ssm", [128, 256], F32)
                ssv = ssm[:, :].rearrange("p (s n) -> p s n", s=2)
                rm = sbp(p2, "rm", [128, 2048], F32)
                rmv = rm[:, :].rearrange("p (s n) -> p s n", s=16)
                rowmask = sbp(p2, "rowmask2", [128, 16], F32)
                dtb = sbp(p2, "dtb", [128, 32], F32)
                a_t = sbp(p2, "a_t", [128, 32], F32)
                dsk = sbp(p2, "dsk", [128, 32], F32)
                sT = sbp(p2, "sT", [128, 2048], F32)
                sTb = sbp(p2, "sTb", [128, 2048], BF16)
                xsb = sbp(p2, "xsb", [128, 2560], BF16)
                bct = sbp(p2, "bct", [128, 1024], BF16)
                bcv = bct[:, :].rearrange("p (g t) -> p g t", g=8)
                dtt = sbp(p2, "dtt", [128, 32], F32)
                ex1 = sbp(p2, "ex1", [128, 32], F32)
                dt_ = sbp(p2, "dt_", [128, 32], F32)
                dA = sbp(p2, "dA", [128, 32], F32)
                cum = sbp(p2, "cum", [128, 32], F32)
                wtmp = sbp(p2, "wtmp", [128, 32], F32)
                wend = sbp(p2, "wend", [128, 32], F32)
                dec = sbp(p2, "dec", [128, 32], F32)
                decs = sbp(p2, "decs", [128, 512], F32)
                decsv = decs[:, :].rearrange("p (s h) -> p s h", s=16)
                ecum = sbp(p2, "ecum", [128, 32], F32)
                xdt = sbp(p2, "xdt", [128, 2048], BF16)
                xsD = sbp(p2, "xsD", [128, 2048], BF16)
                xdtw = sbp(p2, "xdtw", [128, 2048], BF16)
                scT = sbp(p2, "scT", [128, 512], BF16)
                yoff = sbp(p2, "yoff", [128, 2048], F32)
                yy = sbp(p2, "yy", [128, 2048], F32)
                zs = sbp(p2, "zs", [128, 512], F32)
                seg = [sbp(p2, "seg%d" % i, [128, 128], F32) for i in range(2)]
                Lm = [sbp(p2, "Lm%d" % i, [128, 128], BF16) for i in range(2)]
                Mm = [sbp(p2, "Mm%d" % i, [128, 128], BF16) for i in range(2)]
                bst = sbp(p2, "bst2", [128, 24], F32)
                mv = sbp(p2, "mv2", [128, 8], F32)
                mvv = mv[:, :].rearrange("p (g t) -> p g t", g=4)
                ms = sbp(p2, "ms", [128, 4], F32)
                sdv = sbp(p2, "sdv2", [128, 4], F32)
                rsv = sbp(p2, "rsv2", [128, 4], F32)
                yn = sbp(p2, "yn", [128, 2048], BF16)
                ynT = sbp(p2, "ynT", [128, 2048], BF16)
                Cblk = sbp(p2, "Cblk", [128, 2048], BF16)
                Cbv = Cblk[:, :].rearrange("p (s t) -> p s t", s=16)
                s0 = [sbp(p2, "s0_%d" % i, [128, 512], F32) for i in range(2)]
                s0T = [sbp(p2, "s0T%d" % i, [128, 512], F32) for i in range(2)]
                s0Tb = [sbp(p2, "s0Tb%d" % i, [128, 512], BF16) for i in range(2)]
                Bm = [sbp(p2, "Bm%d" % i, [128, 128], BF16) for i in range(2)]
                snat = [sbp(p2, "snat%d" % i, [128, 512], F32) for i in range(2)]
                print("S2 sbuf remaining", nc.sbuf_bytes_remaining)

                kb.dma(triuv, CST["triu"], writes=["triu"])
                kb.dma(negmv, CST["negm"], writes=["negm"])
                kb.dma(ssv, CST["ss"], writes=["ss"])
                kb.dma(rmv, CST["rm"], writes=["rm"])
                kb.dma(rowmask[:, :], CST["rowmask"], writes=["rowmask"])
                kb.dma(dtb[:, :], W["ssd_dt_bias"][l:l + 1, :].to_broadcast([128, 32]), writes=["dtb"])
                kb.dma(a_t[:, :], W["ssd_a_log"][l:l + 1, :].to_broadcast([128, 32]), writes=["a_t"])
                kb.dma(dsk[:, :], W["ssd_d"][l:l + 1, :].to_broadcast([128, 32]), writes=["dsk"])
                kb.op("act", lambda e: e.activation(out=a_t[:, :], in_=a_t[:, :], func=AF.Exp), reads=["a_t"], writes=["a_t"])
                kb.op("dve", lambda e: e.tensor_scalar(out=a_t[:, :], in0=a_t[:, :], scalar1=-1.0, scalar2=None, op0=ALU.mult), reads=["a_t"], writes=["a_t"])
                kb.op("pool", lambda e: e.memset(sT[:, :], 0.0), writes=["sT"])
                kb.op("pool", lambda e: e.memset(sTb[:, :], 0.0), writes=["sTb"])
                kb.op("pool", lambda e: e.memset(Cblk[:, :], 0.0), writes=["Cblk"])
                for j in range(8):
                    load_w([(win[:, Z0 + j * 256:Z0 + (j + 1) * 256], 0)], KC, 256, dst=Wzv[:, :, j * 256:(j + 1) * 256], dstkey="Wz")
                load_w([(win[:, DT0:DT0 + 32], 0)], KC, 32, dst=Wdtv, dstkey="Wdt")
                p5 = PS[5][:, :].bitcast(BF16)
                hcount = 0
                for c in stages.get("ssd_chunks", list(range(NCH))):
                    ts = slice(c * 128, (c + 1) * 128)
                    samp = c * 128 >= SEQ
                    sel = 1 if samp else 0
                    kb.dma(xsb[:, :], xsb_scr[ts, :], reads=[("xsb_scr", fc, (c * 128 // 512) * 512) for fc in range(20)], writes=["xsb"])
                    kb.dma(bcv, bc_scr[:, :, ts], reads=[("bc_scr", fc, (c * 128 // 512) * 512) for fc in range(16, 24)], writes=["bct"])
                    for kc in range(KC):
                        kb.op("pe", lambda e, kc=kc, ts=ts: e.matmul(PS[0][:, 0:32], lhsT=hTv[:, kc, ts], rhs=Wdtv[:, kc, :], start=(kc == 0), stop=(kc == KC - 1)),
                              reads=["Wdt", ("h", c)], writes=["ps0"], inc=(kc == KC - 1))
                    kb.op("dve", lambda e: e.tensor_tensor(out=dtt[:, :], in0=PS[0][:, 0:32], in1=dtb[:, :], op=ALU.add), reads=["ps0", "dtb"], writes=["dtt"])
                    kb.op("act", lambda e: e.activation(out=ex1[:, :], in_=dtt[:, :], func=AF.Exp), reads=["dtt"], writes=["ex1"])
                    kb.op("act", lambda e: e.activation(out=dt_[:, :], in_=ex1[:, :], func=AF.Ln, bias=1.0, scale=1.0), reads=["ex1"], writes=["dt_"])
                    kb.op("dve", lambda e: e.tensor_tensor(out=dA[:, :], in0=dt_[:, :], in1=a_t[:, :], op=ALU.mult), reads=["dt_", "a_t"], writes=["dA"])
                    kb.op("pe", lambda e, sel=sel: e.matmul(PS[0][:, 32:64], lhsT=triuv[:, sel, :], rhs=dA[:, :], start=True, stop=True),
                          reads=["triu", "dA"], writes=["ps0"], inc=True)
                    kb.op("act", lambda e: e.copy(cum[:, :], PS[0][:, 32:64]), reads=["ps0"], writes=["cum"])
                    kb.op("pe", lambda e, sel=sel: e.matmul(PS[0][:, 64:96], lhsT=ssv[:, sel, :], rhs=dA[:, :], start=True, stop=True),
                          reads=["ss", "dA"], writes=["ps0"], inc=True)
                    kb.op("dve", lambda e: e.tensor_tensor(out=wtmp[:, :], in0=PS[0][:, 64:96], in1=cum[:, :], op=ALU.subtract), reads=["ps0", "cum"], writes=["wtmp"])
                    kb.op("act", lambda e: e.activation(out=wend[:, :], in_=wtmp[:, :], func=AF.Exp), reads=["wtmp"], writes=["wend"])
                    kb.op("act", lambda e: e.activation(out=dec[:, :], in_=PS[0][:, 64:96], func=AF.Exp), reads=["ps0"], writes=["dec"])
                    kb.op("act", lambda e: e.activation(out=ecum[:, :], in_=cum[:, :], func=AF.Exp), reads=["cum"], writes=["ecum"])
                    xs3 = xsb[:, 0:2048].rearrange("p (h q) -> p h q", h=32)
                    kb.op("dve", lambda e, xs3=xs3: e.tensor_tensor(out=xdt[:, :].rearrange("p (h q) -> p h q", h=32), in0=xs3,
                                                                  in1=dt_[:, :].unsqueeze(2).to_broadcast([128, 32, 64]), op=ALU.mult),
                          reads=["xsb", "dt_"], writes=["xdt"])
                    kb.op("pool", lambda e, xs3=xs3: e.tensor_tensor(out=xsD[:, :].rearrange("p (h q) -> p h q", h=32), in0=xs3,
                                                                   in1=dsk[:, :].unsqueeze(2).to_broadcast([128, 32, 64]), op=ALU.mult),
                          reads=["xsb", "dsk"], writes=["xsD"])
                    kb.op("pool", lambda e: e.tensor_tensor(out=xdtw[:, :].rearrange("p (h q) -> p h q", h=32), in0=xdt[:, :].rearrange("p (h q) -> p h q", h=32),
                                                          in1=wend[:, :].unsqueeze(2).to_broadcast([128, 32, 64]), op=ALU.mult),
                          reads=["xdt", "wend"], writes=["xdtw"])
                    for g in range(4):
                        kb.op("pe", lambda e, g=g: e.matmul(PS[1][:, g * 128:(g + 1) * 128], lhsT=bcv[:, g, :], rhs=bcv[:, 4 + g, :], start=True, stop=True),
                              reads=["bct"], writes=["ps1"], inc=(g == 3))
                    kb.op("act", lambda e: e.copy(scT[:, :], PS[1][:, :]), reads=["ps1"], writes=["scT"])
                    if samp:
                        for s in range(NSS):
                            kb.op("pe", lambda e, s=s: e.matmul(PS[1][:, s * 32:(s + 1) * 32], lhsT=rmv[:, s, :], rhs=dA[:, :], start=True, stop=True),
                                  reads=["rm", "dA", "scT"], writes=["ps1"], inc=(s == NSS - 1))
                        kb.op("act", lambda e: e.activation(out=decs[:, :], in_=PS[1][:, :], func=AF.Exp), reads=["ps1"], writes=["decs"])
                    for g in range(4):
                        gs_ = slice(g * 512, (g + 1) * 512)
                        if not samp:
                            kb.op("pe", lambda e, g=g, gs_=gs_: e.matmul(PS[2][:, :], lhsT=bcv[:, 4 + g, :], rhs=sTb[:, gs_], start=True, stop=True),
                                  reads=["bct", "sTb"], writes=["ps2"], inc=True)
                        else:
                            for s in range(NSS):
                                kb.op("pool", lambda e, s=s, g=g: e.tensor_copy(Cbv[:, s, s * 8:(s + 1) * 8], bcv[:, 4 + g, s * 8:(s + 1) * 8]),
                                      reads=["bct"], writes=["Cblk"])
                            dbg = stages.get("dbg", 9)
                            for s in (range(stages.get("smp_ns", NSS)) if dbg >= 2 else []):
                                i2 = s % 2
                                for b4 in range(4):
                                    kb.dma(s0[i2][:, b4 * 128:(b4 + 1) * 128],
                                           W["state_ssm"][l, s, g * 8 + 2 * b4:g * 8 + 2 * b4 + 2].rearrange("h q n -> (h q) n"), writes=[("s0", i2)])
                                if dbg < 2.2:
                                    continue
                                for b4 in range(4):
                                    kb.op("pe", lambda e, b4=b4, i2=i2: e.transpose(PS[3][:, b4 * 128:(b4 + 1) * 128], s0[i2][:, b4 * 128:(b4 + 1) * 128], identf[:, :]),
                                          reads=[("s0", i2), "identf"], writes=["ps3"], inc=(b4 == 3))
                                if dbg < 2.4:
                                    continue
                                kb.op("dve", lambda e, i2=i2: e.tensor_copy(s0T[i2][:, :], PS[3][:, :]), reads=["ps3"], writes=[("s0T", i2)])
                                if dbg < 2.6:
                                    continue
                                kb.op("act", lambda e, i2=i2: e.copy(s0Tb[i2][:, :], s0T[i2][:, :]), reads=[("s0T", i2)], writes=[("s0Tb", i2)])
                                if dbg < 3:
                                    continue
                                kb.op("pe", lambda e, s=s, i2=i2: e.matmul(PS[2][:, :], lhsT=Cbv[:, s, :], rhs=s0Tb[i2][:, :], start=(s == 0), stop=(s == NSS - 1)),
                                      reads=["Cblk", ("s0Tb", i2)], writes=["ps2"], inc=True)
                                if dbg < 4:
                                    continue
                                kb.op("dve", lambda e, s=s, g=g, i2=i2: e.tensor_scalar(out=Bm[i2][:, :], in0=xsb[:, 2048 + g * 128:2048 + (g + 1) * 128],
                                                                                   scalar1=rowmask[:, s:s + 1], scalar2=None, op0=ALU.mult),
                                      reads=["xsb", "rowmask"], writes=[("Bm", i2)])
                                kb.op("pe", lambda e, i2=i2, gs_=gs_: e.matmul(PS[4][:, :], lhsT=Bm[i2][:, :], rhs=xdtw[:, gs_], start=True, stop=True),
                                      reads=[("Bm", i2), "xdtw"], writes=["ps4"], inc=True)
                                kb.op("dve", lambda e, i2=i2, s=s, g=g: e.tensor_tensor(
                                    out=s0T[i2][:, :].rearrange("p (h q) -> p h q", h=8), in0=s0T[i2][:, :].rearrange("p (h q) -> p h q", h=8),
                                    in1=decsv[:, s, g * 8:(g + 1) * 8].unsqueeze(2).to_broadcast([128, 8, 64]), op=ALU.mult),
                                    reads=[("s0T", i2), "decs"], writes=[("s0T", i2)])
                                kb.op("dve", lambda e, i2=i2: e.tensor_tensor(out=s0T[i2][:, :], in0=PS[4][:, :], in1=s0T[i2][:, :], op=ALU.add),
                                      reads=["ps4", ("s0T", i2)], writes=[("s0T", i2)])
                                if dbg < 5:
                                    continue
                                for b4 in range(4):
                                    kb.op("pe", lambda e, b4=b4, i2=i2: e.transpose(PS[3][:, b4 * 128:(b4 + 1) * 128], s0T[i2][:, b4 * 128:(b4 + 1) * 128], identf[:, :]),
                                          reads=[("s0T", i2), "identf"], writes=["ps3"], inc=(b4 == 3))
                                kb.op("act", lambda e, i2=i2: e.copy(snat[i2][:, :], PS[3][:, :]), reads=["ps3"], writes=[("snat", i2)])
                                for b4 in range(4):
                                    kb.dma(ssm_s[l, s, g * 8 + 2 * b4:g * 8 + 2 * b4 + 2].rearrange("h q n -> (h q) n"),
                                           snat[i2][:, b4 * 128:(b4 + 1) * 128], reads=[("snat", i2)], writes=[("ssm_s", l, s, g, b4)])
                        if samp and stages.get("dbg", 9) < 3:
                            kb.op("pe", lambda e, g=g, gs_=gs_: e.matmul(PS[2][:, :], lhsT=bcv[:, 4 + g, :], rhs=sTb[:, gs_], start=True, stop=True),
                                  reads=["bct", "sTb"], writes=["ps2"], inc=True)
                        kb.op("dve", lambda e, g=g, gs_=gs_: e.tensor_tensor(
                            out=yoff[:, gs_].rearrange("p (h q) -> p h q", h=8), in0=PS[2][:, :].rearrange("p (h q) -> p h q", h=8),
                            in1=ecum[:, g * 8:(g + 1) * 8].unsqueeze(2).to_broadcast([128, 8, 64]), op=ALU.mult),
                            reads=["ps2", "ecum"], writes=[("yoff", g)])
                    for h in range(32):
                        g = h // 8
                        i2 = hcount % 2
                        hcount += 1
                        cbk = 6 + i2
                        kb.op("pe", lambda e, h=h, cbk=cbk, sel=sel: e.matmul(PS[cbk][:, 0:128], lhsT=dA[:, h:h + 1].to_broadcast([128, 128]), rhs=triuv[:, sel, :],
                                                                           start=True, stop=True),
                              reads=["dA", "triu"], writes=["ps%d" % cbk], inc=True)
                        kb.op("dve", lambda e, h=h, cbk=cbk, i2=i2, sel=sel: e.scalar_tensor_tensor(
                            out=seg[i2][:, :], in0=PS[cbk][:, 0:128], scalar=cum[:, h:h + 1], in1=negmv[:, sel, :], op0=ALU.subtract, op1=ALU.add),
                            reads=["ps%d" % cbk, "cum", "negm"], writes=[("seg", i2)])
                        kb.op("act", lambda e, i2=i2: e.activation(out=Lm[i2][:, :], in_=seg[i2][:, :], func=AF.Exp), reads=[("seg", i2)], writes=[("Lm", i2)])
                        kb.op("pool", lambda e, i2=i2, g=g: e.tensor_tensor(out=Mm[i2][:, :], in0=Lm[i2][:, :], in1=scT[:, g * 128:(g + 1) * 128], op=ALU.mult),
                              reads=[("Lm", i2), "scT"], writes=[("Mm", i2)])
                        ybk = 2 + (g % 2)
                        yo = PS[ybk][:, (h % 8) * 64:(h % 8 + 1) * 64]
                        kb.op("pe", lambda e, yo=yo, i2=i2, h=h: e.matmul(yo, lhsT=Mm[i2][:, :], rhs=xdt[:, h * 64:(h + 1) * 64], start=True, stop=False),
                              reads=[("Mm", i2), "xdt"] + [("yoff", gg) for gg in range(4)], writes=["ps%d" % ybk], inc=False)
                        kb.op("pe", lambda e, yo=yo, h=h: e.matmul(yo, lhsT=identb[:, :], rhs=xsD[:, h * 64:(h + 1) * 64], start=False, stop=True),
                              reads=["identb", "xsD"], writes=["ps%d" % ybk], inc=True)
                        if h % 8 == 7:
                            kb.op("dve", lambda e, g=g, ybk=ybk: e.tensor_tensor(out=yy[:, g * 512:(g + 1) * 512], in0=PS[ybk][:, :], in1=yoff[:, g * 512:(g + 1) * 512], op=ALU.add),
                                  reads=["ps%d" % ybk, ("yoff", g)], writes=[("yy", g)])
                    for g in range(4):
                        zb = 0 + (g % 2)
                        for kc in range(KC):
                            kb.op("pe", lambda e, kc=kc, g=g, zb=zb, ts=ts: e.matmul(PS[zb][:, :], lhsT=hTv[:, kc, ts], rhs=Wzv[:, kc, g * 512:(g + 1) * 512],
                                                                                  start=(kc == 0), stop=(kc == KC - 1)),
                                  reads=["Wz", ("h", c), "cum", "wtmp", "dec"], writes=["ps%d" % zb], inc=(kc == KC - 1))
                        kb.op("act", lambda e, zb=zb: e.activation(out=zs[:, :], in_=PS[zb][:, :], func=AF.Silu), reads=["ps%d" % zb], writes=["zs"])
                        kb.op("dve", lambda e, g=g: e.tensor_tensor(out=yy[:, g * 512:(g + 1) * 512], in0=yy[:, g * 512:(g + 1) * 512], in1=zs[:, :], op=ALU.mult),
                              reads=[("yy", g), "zs"], writes=[("yy", g)])
                        kb.op("dve", lambda e, g=g: e.bn_stats(bst[:, g * 6:(g + 1) * 6], yy[:, g * 512:(g + 1) * 512]), reads=[("yy", g)], writes=[("bst", g)])
                        kb.op("dve", lambda e, g=g: e.bn_aggr(mv[:, g * 2:(g + 1) * 2], bst[:, g * 6:(g + 1) * 6]), reads=[("bst", g)], writes=[("mv", g)])
                    MV = [("mv", g) for g in range(4)]
                    kb.op("dve", lambda e: e.scalar_tensor_tensor(out=ms[:, :], in0=mvv[:, :, 0], scalar=1.0, in1=mvv[:, :, 0], op0=ALU.mult, op1=ALU.mult),
                          reads=MV, writes=["ms"])
                    kb.op("dve", lambda e: e.tensor_tensor(out=ms[:, :], in0=ms[:, :], in1=mvv[:, :, 1], op=ALU.add), reads=MV + ["ms"], writes=["ms"])
                    kb.op("act", lambda e: e.activation(out=sdv[:, :], in_=ms[:, :], func=AF.Sqrt, bias=EPS, scale=1.0), reads=["ms"], writes=["sdv"])
                    kb.op("dve", lambda e: e.reciprocal(rsv[:, :], sdv[:, :]), reads=["sdv"], writes=["rsv"])
                    for g in range(4):
                        kb.op("dve", lambda e, g=g: e.tensor_scalar(out=yn[:, g * 512:(g + 1) * 512], in0=yy[:, g * 512:(g + 1) * 512], scalar1=rsv[:, g:g + 1],
                                                                  scalar2=None, op0=ALU.mult),
                              reads=[("yy", g), "rsv"], writes=["yn"])
                    for half in range(2):
                        for j in range(8):
                            kc = half * 8 + j
                            kb.op("pe", lambda e, kc=kc, j=j: e.transpose(p5[:, j * 128:(j + 1) * 128], yn[:, kc * 128:(kc + 1) * 128], identb[:, :]),
                                  reads=["yn", "identb"], writes=["ps5"], inc=(j == 7))
                        kb.op("act", lambda e, half=half: e.copy(ynT[:, half * 1024:(half + 1) * 1024], p5[:, :]), reads=["ps5"], writes=[("ynT", half)])
                    kb.dma(brscr[:, 0:16, ts], ynT[:, :].rearrange("p (k t) -> p k t", k=16), reads=[("ynT", 0), ("ynT", 1)], writes=[("brscr", c)])
                    if not samp:
                        for g in range(4):
                            gs_ = slice(g * 512, (g + 1) * 512)
                            kb.op("pe", lambda e, g=g, gs_=gs_: e.matmul(PS[4][:, :], lhsT=xsb[:, 2048 + g * 128:2048 + (g + 1) * 128], rhs=xdtw[:, gs_], start=True, stop=True),
                                  reads=["xsb", "xdtw"], writes=["ps4"], inc=True)
                            kb.op("dve", lambda e, g=g, gs_=gs_: e.tensor_tensor(
                                out=sT[:, gs_].rearrange("p (h q) -> p h q", h=8), in0=sT[:, gs_].rearrange("p (h q) -> p h q", h=8),
                                in1=dec[:, g * 8:(g + 1) * 8].unsqueeze(2).to_broadcast([128, 8, 64]), op=ALU.mult),
                                reads=["sT", "dec"], writes=["sT"])
                            kb.op("dve", lambda e, gs_=gs_: e.tensor_tensor(out=sT[:, gs_], in0=PS[4][:, :], in1=sT[:, gs_], op=ALU.add),
                                  reads=["ps4", "sT"], writes=["sT"])
                        kb.op("act", lambda e: e.copy(sTb[:, :], sT[:, :]), reads=["sT"], writes=["sTb"])
                        if c == SEQ // 128 - 1:
                            for q4 in range(4):
                                for b4 in range(4):
                                    blk = q4 * 4 + b4
                                    kb.op("pe", lambda e, b4=b4, blk=blk: e.transpose(PS[3][:, b4 * 128:(b4 + 1) * 128], sT[:, blk * 128:(blk + 1) * 128], identf[:, :]),
                                          reads=["sT", "identf"], writes=["ps3"], inc=(b4 == 3))
                                kb.op("act", lambda e, q4=q4: e.copy(snat[q4 % 2][:, :], PS[3][:, :]), reads=["ps3"], writes=[("snat", q4 % 2)])
                                for b4 in range(4):
                                    kb.dma(ssm_p[l, q4 * 8 + 2 * b4:q4 * 8 + 2 * b4 + 2].rearrange("h q n -> (h q) n"),
                                           snat[q4 % 2][:, b4 * 128:(b4 + 1) * 128], reads=[("snat", q4 % 2)], writes=[("ssm_p", l, q4, b4)])
                kb.barrier()

    def s5(l):
        win = W["w_in"][l]
        U0 = 3072
        TWO_PI = 2.0 * np.pi
        MAGIC = 12582912.0
        with contextlib.ExitStack() as pes:
            uT = sbp(pes, "uT", [128, KC * T], BF16)
            uTv = uT[:, :].rearrange("p (k t) -> p k t", k=KC)
            Bpad = sbp(pes, "Bpad", [128, 64 * 128], BF16)
            Bspad = sbp(pes, "Bspad", [128, 64 * 128], BF16)
            Cpad = sbp(pes, "Cpad", [128, 64 * 128], BF16)
            Bpv = Bpad[:, :].rearrange("p (g n) -> p g n", g=64)
            Bsv = Bspad[:, :].rearrange("p (g n) -> p g n", g=64)
            Cpv = Cpad[:, :].rearrange("p (g n) -> p g n", g=64)
            mag = sbp(pes, "mag", [128, 64], F32)
            frc = sbp(pes, "frc", [128, 64], F32)
            dcol = sbp(pes, "dcol", [128, 8], F32)
            tpos = sbp(pes, "tpos", [128, 256], F32)
            tposv = tpos[:, :].rearrange("p (s t) -> p s t", s=2)
            smask = sbp(pes, "smask", [128, 128], F32)
            Pm = sbp(pes, "Pm", [128, 128], F32)
            xst = sbp(pes, "xstate", [128, 64], F32)
            xfs = sbp(pes, "xfs", [128, 64 * 16], F32)
            xfsv = xfs[:, :].rearrange("p (g s) -> p g s", g=64)
            x0T = sbp(pes, "x0T", [128, 16 * 64], F32)
            x0Tv = x0T[:, :].rearrange("p (s g) -> p s g", s=16)
            kb.dma(tposv, CST["tpos"], writes=["tpos"])
            kb.dma(smask[:, :], CST["smask"], writes=["smask"])
            kb.dma(Pm[:, :], CST["Pm"], writes=["Pm"])
            kb.dma(dcol[:, :], W["s5_d"][l].rearrange("(k p) -> p k", p=128), writes=["dcol"])
            kb.op("pool", lambda e: e.memset(xst[:, :], 0.0), writes=["xst"])
            for j in range(4):
                wu, kwu = load_w([(win[:, U0 + j * 256:U0 + (j + 1) * 256], 0)], KC, 256)
                for (t0, n) in TILES:
                    for sub in range(2):
                        oc = j * 2 + sub
                        bk = (oc % 2)
                        for kc in range(KC):
                            kb.op("pe", lambda e, kc=kc, bk=bk, sub=sub, t0=t0, n=n, wu=wu: e.matmul(
                                PS[bk][:, 0:n], lhsT=wu[:, kc, sub * 128:(sub + 1) * 128], rhs=hTv[:, kc, t0:t0 + n], start=(kc == 0), stop=(kc == KC - 1)),
                                reads=[kwu] + ck("h", t0, n), writes=["ps%d" % bk], inc=(kc == KC - 1))
                        kb.op("act", lambda e, bk=bk, oc=oc, t0=t0, n=n: e.copy(uTv[:, oc, t0:t0 + n], PS[bk][:, 0:n]), reads=["ps%d" % bk], writes=[("uT", oc)])
            with contextlib.ExitStack() as pp:
                def t64(name):
                    return sbp(pp, name, [128, 64], F32)
                aa = sbp(pp, "aa", [64, 256], F32)
                dt = t64("dt"); ar = t64("ar"); ai = t64("ai"); th = t64("th"); t1 = t64("t1"); t2 = t64("t2")
                sn = t64("sn"); cs = t64("cs"); abr = t64("abr"); abi = t64("abi"); den = t64("den")
                fre = t64("fre"); fim = t64("fim"); FP = t64("FP"); FQ = t64("FQ")
                bre2 = sbp(pp, "bre2", [128, 1024], F32)
                bim2 = sbp(pp, "bim2", [128, 1024], F32)
                Bn2 = sbp(pp, "Bn2", [128, 1024], F32)
                Bsn2 = sbp(pp, "Bsn2", [128, 1024], F32)
                tq = sbp(pp, "tq", [128, 1024], F32)
                Ball = sbp(pp, "Ball", [128, 256], F32)
                cT = sbp(pp, "cT", [128, 8 * 128], F32)
                cTv = cT[:, :].rearrange("p (k n) -> p k n", k=8)
                gmask = sbp(pp, "gmask", [128, 8], F32)
                cmask = sbp(pp, "cmask", [128, 1024], F32)
                cmv = cmask[:, :].rearrange("p (g n) -> p g n", g=8)
                kb.dma(gmask[:, :], CST["gmask"], writes=["gmask"])
                kb.dma(cmv, CST["cmask"], writes=["cmask"])
                kb.dma(dt[:, :], W["s5_log_dt"][l:l + 1, :].to_broadcast([128, 64]), writes=["dt"])
                kb.op("act", lambda e: e.activation(out=dt[:, :], in_=dt[:, :], func=AF.Exp), reads=["dt"], writes=["dt"])
                kb.dma(aa[:, 0:64], W["s5_a_re"][l], writes=["aa"])
                kb.dma(aa[:, 64:128], W["s5_a_re"][l], writes=["aa"])
                kb.dma(aa[:, 128:192], W["s5_a_im"][l], writes=["aa"])
                kb.dma(aa[:, 192:256], W["s5_a_im"][l], writes=["aa"])
                for i2 in range(2):
                    kb.op("pe", lambda e, i2=i2: e.transpose(PS[7][:, i2 * 64:(i2 + 1) * 64], aa[0:64, i2 * 128:(i2 + 1) * 128], identf[0:64, 0:64]),
                          reads=["aa", "identf"], writes=["ps7"], inc=(i2 == 1))
                kb.op("act", lambda e: e.copy(ar[:, :], PS[7][:, 0:64]), reads=["ps7"], writes=["ar"])
                kb.op("act", lambda e: e.copy(ai[:, :], PS[7][:, 64:128]), reads=["ps7"], writes=["ai"])
                TT = lambda o, a, b, op, eng="dve": kb.op(eng, lambda e: e.tensor_tensor(out=o[:, :], in0=a[:, :], in1=b[:, :], op=op),
                                                          reads=[id(a), id(b)], writes=[id(o)])
                TS = lambda o, a, s1, op0, s2=None, op1=None: kb.op("dve", (lambda e: e.tensor_scalar(out=o[:, :], in0=a[:, :], scalar1=s1, scalar2=s2, op0=op0, op1=op1)) if op1 is not None
                                                                      else (lambda e: e.tensor_scalar(out=o[:, :], in0=a[:, :], scalar1=s1, scalar2=None, op0=op0)),
                                                                      reads=[id(a)], writes=[id(o)])
                for tns, key in [(dt, "dt"), (ar, "ar"), (ai, "ai")]:
                    kb.writer[id(tns)] = kb.writer.get(key)
                TT(t1, dt, ar, ALU.mult)
                kb.op("act", lambda e: e.activation(out=mag[:, :], in_=t1[:, :], func=AF.Exp), reads=[id(t1)], writes=["mag"])
                TT(th, dt, ai, ALU.mult)
                TS(th, th, 1.0 / TWO_PI, ALU.mult)
                TS(t1, th, MAGIC, ALU.add)
                TS(t1, t1, MAGIC, ALU.subtract)
                kb.op("dve", lambda e: e.tensor_tensor(out=frc[:, :], in0=th[:, :], in1=t1[:, :], op=ALU.subtract), reads=[id(th), id(t1)], writes=["frc"])
                kb.op("act", lambda e: e.activation(out=sn[:, :], in_=frc[:, :], func=AF.Sin, scale=TWO_PI), reads=["frc"], writes=[id(sn)])
                kb.op("dve", lambda e: e.tensor_scalar(out=t2[:, :], in0=frc[:, :], scalar1=0.25, scalar2=None, op0=ALU.add), reads=["frc"], writes=[id(t2)])
                TS(t1, t2, 0.5, ALU.is_gt)
                TT(t2, t2, t1, ALU.subtract)
                kb.op("act", lambda e: e.activation(out=cs[:, :], in_=t2[:, :], func=AF.Sin, scale=TWO_PI), reads=[id(t2)], writes=[id(cs)])
                kb.op("dve", lambda e: e.tensor_tensor(out=abr[:, :], in0=mag[:, :], in1=cs[:, :], op=ALU.mult), reads=["mag", id(cs)], writes=[id(abr)])
                kb.op("dve", lambda e: e.tensor_tensor(out=abi[:, :], in0=mag[:, :], in1=sn[:, :], op=ALU.mult), reads=["mag", id(sn)], writes=[id(abi)])
                TS(abr, abr, -1.0, ALU.add)
                TT(t1, ar, ar, ALU.mult)
                TT(t2, ai, ai, ALU.mult)
                TT(den, t1, t2, ALU.add)
                kb.op("dve", lambda e: e.reciprocal(den[:, :], den[:, :]), reads=[id(den)], writes=[id(den)])
                TT(t1, abr, ar, ALU.mult)
                TT(t2, abi, ai, ALU.mult)
                TT(fre, t1, t2, ALU.add)
                TT(fre, fre, den, ALU.mult)
                TT(t1, abi, ar, ALU.mult)
                TT(t2, abr, ai, ALU.mult)
                TT(fim, t1, t2, ALU.subtract)
                TT(fim, fim, den, ALU.mult)
                kb.op("dve", lambda e: e.tensor_copy(FP[0:64, :], fre[0:64, :]), reads=[id(fre)], writes=[id(FP)])
                kb.op("dve", lambda e: e.tensor_copy(FP[64:128, :], fim[64:128, :]), reads=[id(fim)], writes=[id(FP)])
                kb.op("dve", lambda e: e.tensor_scalar(out=FQ[0:64, :], in0=fim[0:64, :], scalar1=-1.0, scalar2=None, op0=ALU.mult), reads=[id(fim)], writes=[id(FQ)])
                kb.op("dve", lambda e: e.tensor_copy(FQ[64:128, :], fre[64:128, :]), reads=[id(fre)], writes=[id(FQ)])
                for half in range(2):
                    kb.dma(bre2[half * 64:(half + 1) * 64, :].rearrange("p (g c) -> p g c", g=64), W["s5_b_re"][l].rearrange("g n c -> n g c"), writes=["bre2"])
                    kb.dma(bim2[half * 64:(half + 1) * 64, :].rearrange("p (g c) -> p g c", g=64), W["s5_b_im"][l].rearrange("g n c -> n g c"), writes=["bim2"])
                v3 = lambda t: t[:, :].rearrange("p (g c) -> p g c", g=64)
                bc = lambda t: t[:, :].unsqueeze(2).to_broadcast([128, 64, 16])
                kb.op("dve", lambda e: e.tensor_tensor(out=v3(Bn2), in0=v3(bre2), in1=bc(FP), op=ALU.mult), reads=["bre2", id(FP)], writes=["Bn2"])
                kb.op("dve", lambda e: e.tensor_tensor(out=v3(tq), in0=v3(bim2), in1=bc(FQ), op=ALU.mult), reads=["bim2", id(FQ)], writes=["tq"])
                kb.op("dve", lambda e: e.tensor_tensor(out=Bn2[:, :], in0=Bn2[:, :], in1=tq[:, :], op=ALU.add), reads=["Bn2", "tq"], writes=["Bn2"])
                kb.op("dve", lambda e: e.tensor_tensor(out=v3(Bsn2), in0=v3(bim2), in1=bc(FP), op=ALU.mult), reads=["bim2", id(FP)], writes=["Bsn2"])
                kb.op("dve", lambda e: e.tensor_tensor(out=v3(tq), in0=v3(bre2), in1=bc(FQ), op=ALU.mult), reads=["bre2", id(FQ), "Bn2"], writes=["tq"])
                kb.op("dve", lambda e: e.tensor_tensor(out=Bsn2[:, :], in0=Bsn2[:, :], in1=tq[:, :], op=ALU.subtract), reads=["Bsn2", "tq"], writes=["Bsn2"])
                kb.dma(cTv[:, :, 0:64], W["s5_c_re"][l].rearrange("(k g) c n -> (g c) k n", k=8), writes=["cT"])
                kb.dma(cTv[:, :, 64:128], W["s5_c_im"][l].rearrange("(k g) c n -> (g c) k n", k=8), writes=["cT"])
                kb.op("dve", lambda e: e.tensor_scalar(out=cTv[:, :, 64:128], in0=cTv[:, :, 64:128], scalar1=-1.0, scalar2=None, op0=ALU.mult), reads=["cT"], writes=["cT"])
                for gc in range(8):
                    kb.op("pe", lambda e, gc=gc: e.transpose(PS[6][:, 0:128], Bn2[:, gc * 128:(gc + 1) * 128], identf[:, :]), reads=["Bn2", "identf"], writes=["ps6"], inc=False)
                    kb.op("pe", lambda e, gc=gc: e.transpose(PS[6][:, 128:256], Bsn2[:, gc * 128:(gc + 1) * 128], identf[:, :]), reads=["Bsn2", "identf"], writes=["ps6"], inc=True)
                    kb.op("act", lambda e: e.copy(Ball[:, :], PS[6][:, 0:256]), reads=["ps6"], writes=["Ball"])
                    kb.op("pe", lambda e, gc=gc: e.transpose(PS[7][:, 0:128], cTv[:, gc, :], identf[:, :]), reads=["cT", "identf"], writes=["ps7"], inc=True)
                    for g8 in range(8):
                        g = gc * 8 + g8
                        kb.op("dve", lambda e, g=g, g8=g8: e.tensor_scalar(out=Bpv[:, g, :], in0=Ball[:, 0:128], scalar1=gmask[:, g8:g8 + 1], scalar2=None, op0=ALU.mult),
                              reads=["Ball", "gmask"], writes=["Bpad"])
                        kb.op("pool", lambda e, g=g, g8=g8: e.tensor_scalar(out=Bsv[:, g, :], in0=Ball[:, 128:256], scalar1=gmask[:, g8:g8 + 1], scalar2=None, op0=ALU.mult),
                              reads=["Ball", "gmask"], writes=["Bspad"])
                        kb.op("dve", lambda e, g=g, g8=g8: e.tensor_tensor(out=Cpv[:, g, :], in0=PS[7][:, 0:128], in1=cmv[:, g8, :], op=ALU.mult),
                              reads=["ps7", "cmask"], writes=["Cpad"])
                kb.barrier()
            with contextlib.ExitStack() as px:
                s0in = sbp(px, "s0in", [64, 16 * 128], F32)
                s0r = sbp(px, "s0r", [64, 16 * 128], F32)
                kb.dma(s0in[:, :].rearrange("p (s x) -> p s x", s=16), W["state_s5"][l].rearrange("s g n r -> g s (n r)"), writes=["s0in"])
                kb.op("dve", lambda e: e.tensor_copy(s0r[:, :].rearrange("p (s r n) -> p s r n", s=16, r=2), s0in[:, :].rearrange("p (s n r) -> p s r n", s=16, r=2)),
                      reads=["s0in"], writes=["s0r"])
                for s in range(NSS):
                    bk = 6 + (s // 8) % 2
                    kb.op("pe", lambda e, s=s, bk=bk: e.transpose(PS[bk][:, (s % 8) * 64:(s % 8 + 1) * 64], s0r[0:64, s * 128:(s + 1) * 128], identf[0:64, 0:64]),
                          reads=["s0r", "identf"], writes=["ps%d" % bk], inc=(s % 8 == 7))
                    if s % 8 == 7:
                        kb.op("act", lambda e, s=s, bk=bk: e.copy(x0T[:, (s - 7) * 64:(s + 1) * 64], PS[bk][:, :]), reads=["ps%d" % bk], writes=["x0T"])
                kb.barrier()
            with contextlib.ExitStack() as pm:
                cosT = sbp(pm, "cosT", [128, 2 * 1024], F32)
                sinT = sbp(pm, "sinT", [128, 2 * 1024], F32)
                cosv = cosT[:, :].rearrange("p (s g t) -> p s g t", s=2, g=8)
                sinv = sinT[:, :].rearrange("p (s g t) -> p s g t", s=2, g=8)
                ta = sbp(pm, "ta", [128, 1024], F32)
                tb = sbp(pm, "tb", [128, 1024], F32)
                rms_ = sbp(pm, "rms_", [128, 128], F32)
                w1 = [sbp(pm, "w1_%d" % i, [128, 128], F32) for i in range(2)]
                w2 = [sbp(pm, "w2_%d" % i, [128, 128], F32) for i in range(2)]
                zz = [sbp(pm, "zz%d" % i, [128, 128], F32) for i in range(2)]
                x1 = [sbp(pm, "x1_%d" % i, [128, 128], F32) for i in range(2)]
                x2 = [sbp(pm, "x2_%d" % i, [128, 128], F32) for i in range(2)]
                xb = [sbp(pm, "xb%d" % i, [128, 128], BF16) for i in range(2)]
                yv = sbp(pm, "yv", [128, 128], F32)
                yt = sbp(pm, "yt", [128, 128], F32)
                ysg = sbp(pm, "ysg", [128, 128], F32)
                yo = [sbp(pm, "yo%d" % i, [128, 128], BF16) for i in range(2)]
                fin = sbp(pm, "s5fin", [64, 128], F32)
                fin2 = [sbp(pm, "s5fin2_%d" % i, [64, 128], F32) for i in range(2)]
                it = 0
                for gc in range(8):
                    for sel in range(2):
                        kb.op("dve", lambda e, sel=sel, gc=gc: e.tensor_tensor(
                            out=ta[:, :].rearrange("p (g t) -> p g t", g=8), in0=tposv[:, sel:sel + 1, :].to_broadcast([128, 8, 128]),
                            in1=frc[:, gc * 8:(gc + 1) * 8].unsqueeze(2).to_broadcast([128, 8, 128]), op=ALU.mult),
                            reads=["tpos", "frc"], writes=["ta"])
                        kb.op("dve", lambda e: e.tensor_scalar(out=tb[:, :], in0=ta[:, :], scalar1=MAGIC, scalar2=None, op0=ALU.add), reads=["ta"], writes=["tb"])
                        kb.op("dve", lambda e: e.tensor_scalar(out=tb[:, :], in0=tb[:, :], scalar1=MAGIC, scalar2=None, op0=ALU.subtract), reads=["tb"], writes=["tb"])
                        kb.op("dve", lambda e: e.tensor_tensor(out=ta[:, :], in0=ta[:, :], in1=tb[:, :], op=ALU.subtract), reads=["ta", "tb"], writes=["ta"])
                        kb.op("act", lambda e, sel=sel: e.activation(out=sinT[:, sel * 1024:(sel + 1) * 1024], in_=ta[:, :], func=AF.Sin, scale=TWO_PI),
                              reads=["ta"], writes=[("sinT", sel)])
                        kb.op("dve", lambda e: e.tensor_scalar(out=ta[:, :], in0=ta[:, :], scalar1=0.25, scalar2=None, op0=ALU.add), reads=["ta"], writes=["ta"])
                        kb.op("dve", lambda e: e.tensor_scalar(out=tb[:, :], in0=ta[:, :], scalar1=0.5, scalar2=None, op0=ALU.is_gt), reads=["ta"], writes=["tb"])
                        kb.op("dve", lambda e: e.tensor_tensor(out=ta[:, :], in0=ta[:, :], in1=tb[:, :], op=ALU.subtract), reads=["ta", "tb"], writes=["ta"])
                        kb.op("act", lambda e, sel=sel: e.activation(out=cosT[:, sel * 1024:(sel + 1) * 1024], in_=ta[:, :], func=AF.Sin, scale=TWO_PI),
                              reads=["ta"], writes=[("cosT", sel)])
                    for c in range(NCH):
                        ts = slice(c * 128, (c + 1) * 128)
                        samp = c * 128 >= SEQ
                        sel = 1 if samp else 0
                        for g8 in range(8):
                            g = gc * 8 + g8
                            i2 = it % 2
                            it += 1
                            ba, bb_, bcn = (0, 1, 2) if i2 == 0 else (3, 4, 5)
                            kb.op("pe", lambda e, g=g, ba=ba, gc=gc, ts=ts: e.matmul(PS[ba][:, 0:128], lhsT=Bpv[:, g, :], rhs=uTv[:, gc, ts], start=True, stop=True),
                                  reads=["Bpad", ("uT", gc)], writes=["ps%d" % ba], inc=True)
                            kb.op("pe", lambda e, g=g, bb_=bb_, gc=gc, ts=ts: e.matmul(PS[bb_][:, 0:128], lhsT=Bsv[:, g, :], rhs=uTv[:, gc, ts], start=True, stop=True),
                                  reads=["Bspad", ("uT", gc)], writes=["ps%d" % bb_], inc=True)
                            kb.op("dve", lambda e, ba=ba, i2=i2, sel=sel, g8=g8: e.tensor_tensor(out=w1[i2][:, :], in0=PS[ba][:, 0:128], in1=cosv[:, sel, g8, :], op=ALU.mult),
                                  reads=["ps%d" % ba, ("cosT", sel)], writes=[("w1", i2)])
                            kb.op("dve", lambda e, bb_=bb_, i2=i2, sel=sel, g8=g8: e.tensor_tensor(out=w2[i2][:, :], in0=PS[bb_][:, 0:128], in1=sinv[:, sel, g8, :], op=ALU.mult),
                                  reads=["ps%d" % bb_, ("sinT", sel)], writes=[("w2", i2)])
                            kb.op("pool", lambda e, i2=i2: e.tensor_tensor(out=w1[i2][:, :], in0=w1[i2][:, :], in1=w2[i2][:, :], op=ALU.add),
                                  reads=[("w1", i2), ("w2", i2)], writes=[("w1", i2)])
                            if not samp:
                                kb.op("dve", lambda e, i2=i2, g=g: e.tensor_tensor_scan(
                                    out=zz[i2][:, :], data0=mag[:, g:g + 1].to_broadcast([128, 128]), data1=w1[i2][:, :], initial=xst[:, g:g + 1],
                                    op0=ALU.mult, op1=ALU.add),
                                    reads=[("w1", i2), "mag", "xst"], writes=[("zz", i2)])
                            else:
                                kb.op("dve", lambda e, i2=i2, g=g: e.scalar_tensor_tensor(
                                    out=w1[i2][:, 0:128:8], in0=x0Tv[:, :, g], scalar=mag[:, g:g + 1], in1=w1[i2][:, 0:128:8], op0=ALU.mult, op1=ALU.add),
                                    reads=[("w1", i2), "mag", "x0T"], writes=[("w1", i2)])
                                kb.op("dve", lambda e, g=g: e.tensor_scalar(out=rms_[:, :], in0=smask[:, :], scalar1=mag[:, g:g + 1], scalar2=None, op0=ALU.mult),
                                      reads=["smask", "mag"], writes=["rms_"])
                                kb.op("dve", lambda e, i2=i2: e.tensor_tensor_scan(
                                    out=zz[i2][:, :], data0=rms_[:, :], data1=w1[i2][:, :], initial=0.0, op0=ALU.mult, op1=ALU.add),
                                    reads=[("w1", i2), "rms_"], writes=[("zz", i2)])
                            kb.op("pe", lambda e, bcn=bcn, i2=i2: e.matmul(PS[bcn][:, 0:128], lhsT=Pm[:, :], rhs=zz[i2][:, :], start=True, stop=True),
                                  reads=["Pm", ("zz", i2)], writes=["ps%d" % bcn], inc=True)
                            kb.op("pool", lambda e, i2=i2, sel=sel, g8=g8: e.tensor_tensor(out=x1[i2][:, :], in0=zz[i2][:, :], in1=cosv[:, sel, g8, :], op=ALU.mult),
                                  reads=[("zz", i2), ("cosT", sel)], writes=[("x1", i2)])
                            kb.op("dve", lambda e, bcn=bcn, i2=i2, sel=sel, g8=g8: e.tensor_tensor(out=x2[i2][:, :], in0=PS[bcn][:, 0:128], in1=sinv[:, sel, g8, :], op=ALU.mult),
                                  reads=["ps%d" % bcn, ("sinT", sel)], writes=[("x2", i2)])
                            kb.op("pool", lambda e, i2=i2: e.tensor_tensor(out=x1[i2][:, :], in0=x1[i2][:, :], in1=x2[i2][:, :], op=ALU.add),
                                  reads=[("x1", i2), ("x2", i2)], writes=[("x1", i2)])
                            kb.op("act", lambda e, i2=i2: e.copy(xb[i2][:, :], x1[i2][:, :]), reads=[("x1", i2)], writes=[("xb", i2)])
                            if not samp:
                                kb.op("act", lambda e, i2=i2, g=g: e.copy(xst[:, g:g + 1], x1[i2][:, 127:128]), reads=[("x1", i2)], writes=["xst"])
                            else:
                                kb.op("act", lambda e, i2=i2, g=g: e.copy(xfsv[:, g, :], x1[i2][:, 7:128:8]), reads=[("x1", i2)], writes=["xfs"])
                            kb.op("pe", lambda e, g=g, i2=i2, g8=g8: e.matmul(PS[6][:, 0:128], lhsT=Cpv[:, g, :], rhs=xb[i2][:, :], start=(g8 == 0), stop=(g8 == 7)),
                                  reads=["Cpad", ("xb", i2)], writes=["ps6"], inc=True)
                        kb.op("dve", lambda e, gc=gc, ts=ts: e.scalar_tensor_tensor(out=yv[:, :], in0=uTv[:, gc, ts], scalar=dcol[:, gc:gc + 1], in1=PS[6][:, 0:128],
                                                                                 op0=ALU.mult, op1=ALU.add),
                              reads=["ps6", ("uT", gc), "dcol"], writes=["yv"])
                        kb.op("act", lambda e: e.activation(out=yt[:, :], in_=yv[:, :], func=AF.Square), reads=["yv"], writes=["yt"])
                        kb.op("pool", lambda e: e.tensor_scalar(out=yt[:, :], in0=yt[:, :], scalar1=0.044715, scalar2=1.0, op0=ALU.mult, op1=ALU.add), reads=["yt"], writes=["yt"])
                        kb.op("pool", lambda e: e.tensor_tensor(out=yt[:, :], in0=yt[:, :], in1=yv[:, :], op=ALU.mult), reads=["yt", "yv"], writes=["yt"])
                        kb.op("act", lambda e: e.activation(out=ysg[:, :], in_=yt[:, :], func=AF.Sigmoid, scale=float(2.0 * np.sqrt(2.0 / np.pi))), reads=["yt"], writes=["ysg"])
                        yob = yo[c % 2]
                        kb.op("pool", lambda e, yob=yob: e.tensor_tensor(out=yob[:, :], in0=ysg[:, :], in1=yv[:, :], op=ALU.mult), reads=["ysg", "yv"], writes=[("yo", c % 2)])
                        kb.dma(brscr[:, gc, ts], yob[:, :], reads=[("yo", c % 2)], writes=[("brscr", c, gc)])
                kb.op("pe", lambda e: e.transpose(PS[7][0:64, 0:128], xst[:, :], identf[:, :]), reads=["xst", "identf"], writes=["ps7"], inc=True)
                kb.op("dve", lambda e: e.tensor_copy(fin[:, :].rearrange("p (n r) -> p r n", r=2), PS[7][0:64, 0:128].rearrange("p (r n) -> p r n", r=2)),
                      reads=["ps7"], writes=["s5fin"])
                kb.dma(s5_p[l].rearrange("g n r -> g (n r)"), fin[:, :], reads=["s5fin"], writes=[("s5_p", l)])
                for s in range(NSS):
                    kb.op("pe", lambda e, s=s: e.transpose(PS[7][0:64, 0:128], xfsv[:, :, s], identf[:, :]), reads=["xfs", "identf"], writes=["ps7"], inc=True)
                    f2 = fin2[s % 2]
                    kb.op("dve", lambda e, f2=f2: e.tensor_copy(f2[:, :].rearrange("p (n r) -> p r n", r=2), PS[7][0:64, 0:128].rearrange("p (r n) -> p r n", r=2)),
                          reads=["ps7"], writes=[("fin2", s % 2)])
                    kb.dma(s5_s[l, s].rearrange("g n r -> g (n r)"), f2[:, :], reads=[("fin2", s % 2)], writes=[("s5_s", l, s)])
                kb.barrier()

    for l in range(NL + 1):
        phase_x(l)
        if l < NL:
            if stages.get("ret", True):
                retention(l)
                stage_c(l, "ret", W["ret_w_o"][l], 8, 9248, "retln%d" % l, W["ret_ln_g"][l])
            if stages.get("s5", True):
                s5(l)
                stage_c(l, "s5", W["s5_w_glu"][l], 8, 9248 + 1024, glu=True)
            if stages.get("ssd", True):
                ssd(l)
            if stages.get("ssd", True) and stages.get("ssd_s2", True) and stages.get("ssd_c", True):
                stage_c(l, "ssd", W["ssd_w_o"][l], 16, 9248 + 2048, "ssdn%d" % l, W["ssd_norm"][l])

    kb.finish()
    print("instructions:", kb.ninst, {e: kb.ccnt[e] for e in kb.ccnt})
    return nc, es


_CONSTS = None
STAGES = {"layers": DEPTH, "ffn1": True, "ffn2": True, "ret": True, "ssd": True, "s5": True}
WNAMES = ["ffn1_norm", "ffn1_w_gu", "ffn1_w_down", "ffn2_norm", "ffn2_w_gu", "ffn2_w_down", "mix_norm", "w_in", "ret_ln_g", "ret_w_o", "w_out",
          "ssd_conv_w", "ssd_conv_b", "ssd_dt_bias", "ssd_a_log", "ssd_d", "ssd_norm", "ssd_w_o",
          "s5_a_re", "s5_a_im", "s5_log_dt", "s5_b_re", "s5_b_im", "s5_c_re", "s5_c_im", "s5_d", "s5_w_glu"]


def make_in_map(inp, c, consts):
    xp = np.asarray(inp["x_prompt"])
    xs = np.asarray(inp["x_sample"])
    x_core = np.concatenate([xp[c], xs[c * NSS:(c + 1) * NSS].reshape(NSS * DSEQ, D)], axis=0)
    m = {"x_in": np.ascontiguousarray(x_core)}
    m.update(consts)
    for k in WNAMES:
        m[k] = np.asarray(inp[k])
    m["final_norm"] = np.asarray(inp["final_norm"]).reshape(1, D)
    for k in ["state_ret", "state_ssm", "state_conv", "state_s5"]:
        m[k] = np.ascontiguousarray(np.asarray(inp[k])[:, c * NSS:(c + 1) * NSS])
    return m


def kernel(**inp):
    nc, es = build(STAGES)
    consts = host_consts()
    in_maps = [make_in_map(inp, c, consts) for c in range(NCORES)]
    res = run_bass_kernel_spmd(nc, in_maps, core_ids=list(range(NCORES)))
    es.close()
    R = res.results
    ys = [r["y_out"] for r in R]
    y_prompt = np.stack([y[:SEQ] for y in ys], axis=0)
    y_sample = np.concatenate([y[SEQ:].reshape(NSS, DSEQ, D) for y in ys], axis=0)
    def pstack(k):
        return np.stack([r[k] for r in R], axis=1)

    def scat(k):
        return np.concatenate([r[k] for r in R], axis=1)

    return (y_prompt, y_sample, pstack("ret_p"), scat("ret_s"), pstack("s5_p"), scat("s5_s"),
            pstack("ssm_p"), scat("ssm_s"), pstack("conv_p"), scat("conv_s"))
```

```python
import contextlib
import numpy as np
import concourse.bass as bass
import concourse.mybir as mybir
from concourse.bass_utils import run_bass_kernel_spmd

F32 = mybir.dt.float32
BF16 = mybir.dt.bfloat16
ALU = mybir.AluOpType
AF = mybir.ActivationFunctionType

NCORES = 8
D = 1024
KC = 8
DEPTH = 4
SEQ = 2048
NSS = 16
DSEQ = 8
T = SEQ + NSS * DSEQ
NCH = T // 128
FFN = 2816
EPS = 1e-6
IN_DIM = 12320
TILES = [(0, 512), (512, 512), (1024, 512), (1536, 512), (2048, 128)]
NDS = 40


class KB:
    def __init__(self, nc, es):
        self.nc = nc
        self.E = {"pe": nc.tensor, "act": nc.scalar, "dve": nc.vector, "pool": nc.gpsimd, "sp": nc.sync}
        self.csem = {e: es.enter_context(nc.semaphore("s_" + e)) for e in ["pe", "act", "dve", "pool"]}
        self.ccnt = {e: 0 for e in self.csem}
        self.dsem = [es.enter_context(nc.semaphore("d%d" % i)) for i in range(NDS)]
        self.dcnt = [0] * NDS
        self.dnext = 0
        self.waited = {e: {} for e in self.E}
        self.writer = {}
        self.readers = {}
        self.ninst = 0

    def _wait(self, eng, tok):
        semid, sem, val, src = tok
        if self.waited[eng].get(semid, 0) >= val:
            return
        self.E[eng].wait_ge(sem, val)
        self.waited[eng][semid] = val

    def _sync(self, eng, reads, writes, is_dma=False):
        for k in reads:
            w = self.writer.get(k)
            if w is not None:
                if (not is_dma) and w[3] == eng and eng == "pe":
                    continue
                self._wait(eng, w)
        for k in writes:
            w = self.writer.get(k)
            if w is not None:
                if is_dma or w[3] != eng:
                    self._wait(eng, w)
            for r in self.readers.get(k, {}).values():
                if is_dma or r[3] != eng:
                    self._wait(eng, r)

    def _record(self, tok, reads, writes):
        for k in reads:
            self.readers.setdefault(k, {})[tok[0]] = tok
        for k in writes:
            self.writer[k] = tok
            self.readers[k] = {}

    def op(self, eng, fn, reads=(), writes=(), inc=True):
        self._sync(eng, reads, writes)
        ins = fn(self.E[eng])
        self.ninst += 1
        if inc:
            self.ccnt[eng] += 1
            ins.then_inc(self.csem[eng], 1)
            tok = (eng, self.csem[eng], self.ccnt[eng], eng)
        else:
            tok = (eng, self.csem[eng], self.ccnt[eng] + 1, eng)
        self._record(tok, reads, writes)
        return tok

    def dma(self, out, in_, reads=(), writes=(), q="sp"):
        self._sync(q, reads, writes, is_dma=True)
        i = self.dnext
        self.dnext = (i + 1) % NDS
        sid = "d%d" % i
        if self.dcnt[i] > 0:
            self._wait(q, (sid, self.dsem[i], self.dcnt[i], "dma"))
        self.dcnt[i] += 16
        self.E[q].dma_start(out=out, in_=in_).then_inc(self.dsem[i], 16)
        self.ninst += 1
        tok = (sid, self.dsem[i], self.dcnt[i], "dma")
        self._record(tok, reads, writes)
        return tok

    def finish(self):
        for i in range(NDS):
            if self.dcnt[i] > 0:
                self._wait("sp", ("d%d" % i, self.dsem[i], self.dcnt[i], "dma"))
        for e in ["pe", "act", "dve", "pool"]:
            if self.ccnt[e] > 0:
                self._wait("sp", (e, self.csem[e], self.ccnt[e], e))

    def barrier(self):
        toks = []
        for e in ["pe", "act", "dve", "pool"]:
            if self.ccnt[e] > 0:
                toks.append((e, self.csem[e], self.ccnt[e], e))
        for i in range(NDS):
            if self.dcnt[i] > 0:
                toks.append(("d%d" % i, self.dsem[i], self.dcnt[i], "dma"))
        for eng in ["pe", "act", "dve", "pool", "sp"]:
            for t in toks:
                if t[3] == eng:
                    continue
                self._wait(eng, t)
        self.writer.clear()
        self.readers.clear()


RET_HEADS = 4
GAM = [1.0 - 2.0 ** (-5.0 - h) for h in range(RET_HEADS)]


def host_consts():
    c = {}
    c["ident_f"] = np.eye(128, dtype=np.float32)
    pos = np.concatenate([np.arange(SEQ), np.tile(16384 + np.arange(DSEQ), NSS)]).astype(np.float64)
    inv = 10000.0 ** (-np.arange(64, dtype=np.float64) / 64.0)
    ang = (pos.astype(np.float32)[None, :] * inv.astype(np.float32)[:, None]).astype(np.float32).astype(np.float64)
    cos = np.concatenate([np.cos(ang), np.cos(ang)], axis=0)
    sinS = np.concatenate([-np.sin(ang), np.sin(ang)], axis=0)
    sc = 128.0 ** -0.5
    c["tabqk"] = np.stack([cos, sinS, cos * sc, sinS * sc], axis=1).astype(np.float32)
    idx = np.arange(128)
    qdec = np.zeros((128, 4, 640), np.float32)
    maskT = np.zeros((128, 2, 4, 128), np.float32)
    kdec = np.zeros((128, 2, 4), np.float32)
    sj = idx % 8
    seqj = idx // 8
    for h in range(4):
        g = GAM[h]
        qdec[:, h, 0:512] = np.tile(g ** (idx + 1.0), 4)[None, :]
        qdec[:, h, 512:640] = (g ** (sj + 1.0))[None, :]
        maskT[:, 0, h, :] = (g ** (-(idx[:, None] + 1.0))) * (idx[None, :] >= idx[:, None])
        maskT[:, 1, h, :] = (g ** (-(sj[:, None] + 1.0))) * ((sj[None, :] >= sj[:, None]) & (seqj[None, :] == seqj[:, None]))
        kdec[:, 0, h] = g ** (127.0 - idx)
        kdec[:, 1, h] = g ** (7.0 - sj)
    c["qdec"] = qdec
    c["maskT"] = maskT
    c["kdec"] = kdec
    c["rowmask"] = (seqj[:, None] == np.arange(16)[None, :]).astype(np.float32)
    same = (seqj[:, None] == seqj[None, :])
    le = (idx[:, None] <= idx[None, :])
    c["triu"] = np.stack([le, le & same], axis=1).astype(np.float32)
    c["negm"] = np.stack([np.where(le, 0.0, -30000.0), np.where(le & same, 0.0, -30000.0)], axis=1).astype(np.float32)
    c["ss"] = np.stack([np.ones((128, 128)), same], axis=1).astype(np.float32)
    tp = np.stack([idx + 1.0, sj + 1.0], axis=0)
    c["tpos"] = np.repeat(tp[None, :, :], 128, axis=0).astype(np.float32)
    c["smask"] = np.repeat((sj != 0)[None, :], 128, axis=0).astype(np.float32)
    pm = np.zeros((128, 128), np.float32)
    for m_ in range(64):
        pm[m_ + 64, m_] = -1.0
        pm[m_, m_ + 64] = 1.0
    c["Pm"] = pm
    c["gmask"] = ((idx[:, None] // 16) == np.arange(8)[None, :]).astype(np.float32)
    c["cmask"] = np.repeat(((idx[None, :] // 16) == np.arange(8)[:, None])[None, :, :], 128, axis=0).astype(np.float32)
    c["rm"] = np.repeat((seqj[:, None] == np.arange(16)[None, :])[:, :, None], 128, axis=2).astype(np.float32)
    return c


def build(stages):
    nc = bass.Bass("TRN2", target_bir_lowering=False)
    es = contextlib.ExitStack()
    es.enter_context(nc.allow_non_contiguous_dma(reason="small param / layout loads"))
    try:
        es.enter_context(nc.allow_low_precision(reason="bf16 matmul operands by design"))
    except Exception:
        pass
    NL = stages.get("layers", DEPTH)

    def din(name, shape, dt=F32):
        return nc.dram_tensor(name, list(shape), dt, kind="ExternalInput").ap()

    def dout(name, shape, dt=F32):
        return nc.dram_tensor(name, list(shape), dt, kind="ExternalOutput").ap()

    def dscr(name, shape, dt=F32):
        return nc.dram_tensor(name, list(shape), dt, kind="Internal").ap()

    x_in = din("x_in", [T, D])
    CST = {"ident_f": din("ident_f", [128, 128]), "tabqk": din("tabqk", [128, 4, T]), "qdec": din("qdec", [128, 4, 640]),
           "maskT": din("maskT", [128, 2, 4, 128]), "kdec": din("kdec", [128, 2, 4]), "rowmask": din("rowmask", [128, 16]),
           "triu": din("triu", [128, 2, 128]), "negm": din("negm", [128, 2, 128]), "ss": din("ss", [128, 2, 128]), "rm": din("rm", [128, 16, 128]),
           "tpos": din("tpos", [128, 2, 128]), "smask": din("smask", [128, 128]), "Pm": din("Pm", [128, 128]),
           "gmask": din("gmask", [128, 8]), "cmask": din("cmask", [128, 8, 128])}
    W = {}
    for nm, shp in [("ffn1_norm", [DEPTH, D]), ("ffn1_w_gu", [DEPTH, D, 2 * FFN]), ("ffn1_w_down", [DEPTH, FFN, D]),
                    ("ffn2_norm", [DEPTH, D]), ("ffn2_w_gu", [DEPTH, D, 2 * FFN]), ("ffn2_w_down", [DEPTH, FFN, D]),
                    ("final_norm", [1, D]), ("mix_norm", [DEPTH, D]), ("w_in", [DEPTH, D, IN_DIM]),
                    ("ret_ln_g", [DEPTH, D]), ("ret_w_o", [DEPTH, D, D]), ("w_out", [DEPTH, D, D]),
                    ("state_ret", [DEPTH, NSS, 4, 128, 256]),
                    ("s5_a_re", [DEPTH, 64, 64]), ("s5_a_im", [DEPTH, 64, 64]), ("s5_log_dt", [DEPTH, 64]),
                    ("s5_b_re", [DEPTH, 64, 64, 16]), ("s5_b_im", [DEPTH, 64, 64, 16]), ("s5_c_re", [DEPTH, 64, 16, 64]),
                    ("s5_c_im", [DEPTH, 64, 16, 64]), ("s5_d", [DEPTH, D]), ("s5_w_glu", [DEPTH, D, 2 * D]),
                    ("state_s5", [DEPTH, NSS, 64, 64, 2]),
                    ("ssd_conv_w", [DEPTH, 4, 3072]), ("ssd_conv_b", [DEPTH, 3072]), ("ssd_dt_bias", [DEPTH, 32]),
                    ("ssd_a_log", [DEPTH, 32]), ("ssd_d", [DEPTH, 32]), ("ssd_norm", [DEPTH, 2048]), ("ssd_w_o", [DEPTH, 2048, D]),
                    ("state_ssm", [DEPTH, NSS, 32, 64, 128]), ("state_conv", [DEPTH, NSS, 3, 3072])]:
        W[nm] = din(nm, shp)
    y_out = dout("y_out", [T, D])
    ret_p = dout("ret_p", [DEPTH, 4, 128, 256])
    ret_s = dout("ret_s", [DEPTH, NSS, 4, 128, 256])
    s5_p = dout("s5_p", [DEPTH, 64, 64, 2])
    s5_s = dout("s5_s", [DEPTH, NSS, 64, 64, 2])
    ssm_p = dout("ssm_p", [DEPTH, 32, 64, 128])
    ssm_s = dout("ssm_s", [DEPTH, NSS, 32, 64, 128])
    conv_p = dout("conv_p", [DEPTH, 3, 3072])
    conv_s = dout("conv_s", [DEPTH, NSS, 3, 3072])
    xsb_scr = dscr("xsb_scr", [T, 2560], BF16)
    bc_scr = dscr("bc_scr", [128, 8, T], BF16)
    xscr = dscr("xscr", [128, KC, T])
    brscr = dscr("brscr", [128, 16, T], BF16)

    kb = KB(nc, es)

    uniq = [0]

    def sbp(stack, name, shape, dt):
        uniq[0] += 1
        return stack.enter_context(nc.sbuf_tensor("sb%d_%s" % (uniq[0], name), list(shape), dt))

    hT = sbp(es, "hT", [128, KC * T], BF16)
    hTv = hT[:, :].rearrange("p (k t) -> p k t", k=KC)
    identf = sbp(es, "identf", [128, 128], F32)
    identb = sbp(es, "identb", [128, 128], BF16)
    ones_bf = sbp(es, "ones_bf", [128, 128], BF16)
    gcols = sbp(es, "gcols", [128, 24 * 16], F32)
    gcv = gcols[:, :].rearrange("p (s k) -> p s k", k=16)
    sqb = sbp(es, "sqb", [128, KC * 512], BF16)
    sqv = sqb[:, :].rearrange("p (k t) -> p k t", k=KC)
    sdb = sbp(es, "sdb", [128, 512], F32)
    rstd = sbp(es, "rstd", [128, 512], F32)
    wst = [sbp(es, "wst%d" % i, [128, 2048], F32) for i in range(2)]
    wbf = [sbp(es, "wbf%d" % i, [128, 2048], BF16) for i in range(2)]
    PS = [es.enter_context(nc.psum_tensor("ps%d" % i, [128, 512], F32)) for i in range(8)]

    kb.dma(identf[:, :], CST["ident_f"][:, :], writes=["identf"])
    kb.op("dve", lambda e: e.tensor_copy(identb[:, :], identf[:, :]), reads=["identf"], writes=["identb"])
    kb.op("dve", lambda e: e.memset(ones_bf[:, :], 1.0), writes=["ones_bf"])

    gslot = {}

    def load_gain(name, ap_row, nk=KC):
        s = len(gslot) % 24
        gslot[name] = s
        kb.dma(gcv[:, s, 0:nk], ap_row.rearrange("(k p) -> p k", p=128), writes=[("g", s)])
        return s

    def ck(name, t0, n):
        return [(name, c) for c in range(t0 // 128, (t0 + n) // 128)]

    wslot = [0]

    def load_w(pieces, kcn, ncols, dst=None, dstkey=None, rowscale=None):
        s = wslot[0]
        wslot[0] ^= 1
        st = wst[s][:, 0:kcn * ncols].rearrange("p (k n) -> p k n", k=kcn)
        for (src, off) in pieces:
            wdt = src.shape[1]
            kb.dma(st[:, :, off:off + wdt], src.rearrange("(k p) n -> p k n", p=128), writes=[("wst", s)])
        if dst is None:
            bf = wbf[s][:, 0:kcn * ncols].rearrange("p (k n) -> p k n", k=kcn)
            key = ("wbf", s)
        else:
            bf = dst
            key = dstkey
        if rowscale is None:
            kb.op("pool", lambda e: e.tensor_copy(bf, st), reads=[("wst", s)], writes=[key])
        else:
            for k in range(kcn):
                kb.op("pool", lambda e, k=k: e.tensor_scalar(out=bf[:, k, :], in0=st[:, k, :], scalar1=gcv[:, rowscale, k:k + 1],
                                                           scalar2=None, op0=ALU.mult),
                      reads=[("wst", s), ("g", rowscale)], writes=[key])
        return bf, key

    def rmsnorm(xTv, gs, dst_fn, dst_keys_fn):
        for (t0, n) in TILES:
            for kc in range(KC):
                kb.op("act", lambda e, kc=kc, t0=t0, n=n: e.activation(
                    out=sqv[:, kc, 0:n], in_=xTv[:, kc, t0:t0 + n], func=AF.Square),
                    reads=ck("x", t0, n), writes=[("sq", kc)])
            for kc in range(KC):
                kb.op("pe", lambda e, kc=kc, n=n: e.matmul(
                    PS[6][:, 0:n], lhsT=ones_bf[:, :], rhs=sqv[:, kc, 0:n], start=(kc == 0), stop=(kc == KC - 1)),
                    reads=["ones_bf", ("sq", kc)], writes=["ps6"], inc=(kc == KC - 1))
            kb.op("act", lambda e, n=n: e.activation(
                out=sdb[:, 0:n], in_=PS[6][:, 0:n], func=AF.Sqrt, scale=1.0 / D, bias=EPS),
                reads=["ps6"], writes=["sdb"])
            kb.op("dve", lambda e, n=n: e.reciprocal(rstd[:, 0:n], sdb[:, 0:n]), reads=["sdb"], writes=["rstd"])
            for kc in range(KC):
                kb.op("dve", lambda e, kc=kc, t0=t0, n=n: e.scalar_tensor_tensor(
                    out=dst_fn(kc, t0, n), in0=xTv[:, kc, t0:t0 + n], scalar=gcv[:, gs, kc:kc + 1],
                    in1=rstd[:, 0:n], op0=ALU.mult, op1=ALU.mult),
                    reads=ck("x", t0, n) + ["rstd", ("g", gs)], writes=dst_keys_fn(kc, t0, n))

    def norm_to_h(xTv, gs):
        rmsnorm(xTv, gs, lambda kc, t0, n: hTv[:, kc, t0:t0 + n], lambda kc, t0, n: ck("h", t0, n))

    def ffn(P, xTv, l, pre):
        actb = P["actb"]
        sgb = P["sgb"]
        gs = load_gain("%s_norm%d" % (pre, l), W[pre + "_norm"][l])
        norm_to_h(xTv, gs)
        wgu = W[pre + "_w_gu"][l]
        wdn = W[pre + "_w_down"][l]
        NHG = FFN // 256
        gu_bank = 0
        dn_bank = 0
        for hg in range(NHG):
            ab = actb[hg % 2]
            abv = ab[:, :].rearrange("p (b t) -> p b t", b=2)
            kab = ("actb", hg % 2)
            wg, kwg = load_w([(wgu[:, hg * 256:(hg + 1) * 256], 0)], KC, 256)
            wu, kwu = load_w([(wgu[:, FFN + hg * 256:FFN + (hg + 1) * 256], 0)], KC, 256)
            for (t0, n) in TILES:
                for blk in range(2):
                    bg = gu_bank % 4
                    bu = (gu_bank + 1) % 4
                    gu_bank += 2
                    for kc in range(KC):
                        kb.op("pe", lambda e, kc=kc, bg=bg, blk=blk, t0=t0, n=n, wg=wg: e.matmul(
                            PS[bg][:, 0:n], lhsT=wg[:, kc, blk * 128:(blk + 1) * 128], rhs=hTv[:, kc, t0:t0 + n],
                            start=(kc == 0), stop=(kc == KC - 1)),
                            reads=[kwg] + ck("h", t0, n), writes=["ps%d" % bg], inc=(kc == KC - 1))
                    for kc in range(KC):
                        kb.op("pe", lambda e, kc=kc, bu=bu, blk=blk, t0=t0, n=n, wu=wu: e.matmul(
                            PS[bu][:, 0:n], lhsT=wu[:, kc, blk * 128:(blk + 1) * 128], rhs=hTv[:, kc, t0:t0 + n],
                            start=(kc == 0), stop=(kc == KC - 1)),
                            reads=[kwu] + ck("h", t0, n), writes=["ps%d" % bu], inc=(kc == KC - 1))
                    sg = sgb[(gu_bank // 2) % 2]
                    ksg = ("sgb", (gu_bank // 2) % 2)
                    kb.op("act", lambda e, bg=bg, n=n, sg=sg: e.activation(
                        out=sg[:, 0:n], in_=PS[bg][:, 0:n], func=AF.Silu),
                        reads=["ps%d" % bg], writes=[ksg])
                    kb.op("dve", lambda e, bu=bu, n=n, sg=sg, blk=blk, t0=t0, abv=abv: e.tensor_tensor(
                        out=abv[:, blk, t0:t0 + n], in0=PS[bu][:, 0:n], in1=sg[:, 0:n], op=ALU.mult),
                        reads=["ps%d" % bu, ksg], writes=[kab])
            wd, kwd = load_w([(wdn[hg * 256:(hg + 1) * 256, :], 0)], 2, D)
            for (t0, n) in TILES:
                for oc in range(KC):
                    bo = 4 + (dn_bank % 2)
                    dn_bank += 1
                    for blk in range(2):
                        kb.op("pe", lambda e, blk=blk, bo=bo, oc=oc, t0=t0, n=n, wd=wd, abv=abv: e.matmul(
                            PS[bo][:, 0:n], lhsT=wd[:, blk, oc * 128:(oc + 1) * 128], rhs=abv[:, blk, t0:t0 + n],
                            start=(blk == 0), stop=(blk == 1)),
                            reads=[kwd, kab], writes=["ps%d" % bo], inc=(blk == 1))
                    kb.op("dve", lambda e, bo=bo, oc=oc, t0=t0, n=n: e.scalar_tensor_tensor(
                        out=xTv[:, oc, t0:t0 + n], in0=PS[bo][:, 0:n], scalar=0.5, in1=xTv[:, oc, t0:t0 + n],
                        op0=ALU.mult, op1=ALU.add),
                        reads=["ps%d" % bo] + ck("x", t0, n), writes=ck("x", t0, n))

    def run_pipeline(items, stage_fns):
        n = len(items)
        S = len(stage_fns)
        for step in range(n + S - 1):
            for si, fn in enumerate(stage_fns):
                k = step - si
                if 0 <= k < n:
                    fn(k, items[k])

    def phase_x(l):
        with contextlib.ExitStack() as pes:
            xT = sbp(pes, "xT", [128, KC * T], F32)
            xTv = xT[:, :].rearrange("p (k t) -> p k t", k=KC)
            P = {"actb": [sbp(pes, "actb%d" % i, [128, 2 * T], BF16) for i in range(2)],
                 "sgb": [sbp(pes, "sgb%d" % i, [128, 512], BF16) for i in range(2)]}
            iobuf = [sbp(pes, "iobuf%d" % i, [128, D], F32) for i in range(2)]
            if l == 0:
                for c in range(NCH):
                    io = iobuf[c % 2]
                    kio = "io%d" % (c % 2)
                    kb.dma(io[:, :], x_in[c * 128:(c + 1) * 128, :], writes=[kio])
                    for half in range(2):
                        bank = PS[6 + half]
                        kbk = "ps%d" % (6 + half)
                        for j in range(4):
                            kc = half * 4 + j
                            kb.op("pe", lambda e, j=j, kc=kc, bank=bank, io=io: e.transpose(
                                bank[:, j * 128:(j + 1) * 128], io[:, kc * 128:(kc + 1) * 128], identf[:, :]),
                                reads=[kio, "identf"], writes=[kbk], inc=(j == 3))
                        if half == 0:
                            kb.op("act", lambda e, bank=bank, c=c: e.copy(
                                xTv[:, 0:4, c * 128:(c + 1) * 128], bank[:, :].rearrange("p (k t) -> p k t", k=4)),
                                reads=[kbk], writes=[("x", c)])
                        else:
                            kb.op("dve", lambda e, bank=bank, c=c: e.tensor_copy(
                                xTv[:, 4:8, c * 128:(c + 1) * 128], bank[:, :].rearrange("p (k t) -> p k t", k=4)),
                                reads=[kbk], writes=[("x", c)])
            else:
                for (t0, n) in TILES:
                    kb.dma(xTv[:, :, t0:t0 + n], xscr[:, :, t0:t0 + n], reads=[("xscr", t0)], writes=ck("x", t0, n))
                if stages.get("ffn2", True):
                    ffn(P, xTv, l - 1, "ffn2")
            if l < NL:
                if stages.get("ffn1", True):
                    ffn(P, xTv, l, "ffn1")
                gs = load_gain("mix%d" % l, W["mix_norm"][l])
                norm_to_h(xTv, gs)
                for (t0, n) in TILES:
                    kb.dma(xscr[:, :, t0:t0 + n], xTv[:, :, t0:t0 + n], reads=ck("x", t0, n), writes=[("xscr", t0)])
            else:
                gs = load_gain("final", W["final_norm"][0])
                finb = sbp(pes, "finb", [128, 4096], F32)
                fin = finb[:, :].rearrange("p (k t) -> p k t", k=KC)
                for (t0, n) in TILES:
                    rm_tiles = [(t0, n)]
                    for kc in range(KC):
                        kb.op("act", lambda e, kc=kc, t0=t0, n=n: e.activation(
                            out=sqv[:, kc, 0:n], in_=xTv[:, kc, t0:t0 + n], func=AF.Square),
                            reads=ck("x", t0, n), writes=[("sq", kc)])
                    for kc in range(KC):
                        kb.op("pe", lambda e, kc=kc, n=n: e.matmul(
                            PS[6][:, 0:n], lhsT=ones_bf[:, :], rhs=sqv[:, kc, 0:n], start=(kc == 0), stop=(kc == KC - 1)),
                            reads=["ones_bf", ("sq", kc)], writes=["ps6"], inc=(kc == KC - 1))
                    kb.op("act", lambda e, n=n: e.activation(
                        out=sdb[:, 0:n], in_=PS[6][:, 0:n], func=AF.Sqrt, scale=1.0 / D, bias=EPS),
                        reads=["ps6"], writes=["sdb"])
                    kb.op("dve", lambda e, n=n: e.reciprocal(rstd[:, 0:n], sdb[:, 0:n]), reads=["sdb"], writes=["rstd"])
                    for kc in range(KC):
                        kb.op("dve", lambda e, kc=kc, t0=t0, n=n: e.scalar_tensor_tensor(
                            out=fin[:, kc, 0:n], in0=xTv[:, kc, t0:t0 + n], scalar=gcv[:, gs, kc:kc + 1],
                            in1=rstd[:, 0:n], op0=ALU.mult, op1=ALU.mult),
                            reads=ck("x", t0, n) + ["rstd", ("g", gs)], writes=["fin"])
                    for c in range(n // 128):
                        io = iobuf[c % 2]
                        kio = "io%d" % (c % 2)
                        for half in range(2):
                            bank = PS[half]
                            kbk = "ps%d" % half
                            for j in range(4):
                                kc = half * 4 + j
                                kb.op("pe", lambda e, j=j, kc=kc, bank=bank, c=c: e.transpose(
                                    bank[:, j * 128:(j + 1) * 128], fin[:, kc, c * 128:(c + 1) * 128], identf[:, :]),
                                    reads=["fin", "identf"], writes=[kbk], inc=(j == 3))
                            if half == 0:
                                kb.op("act", lambda e, bank=bank, io=io: e.copy(io[:, 0:512], bank[:, :]),
                                      reads=[kbk], writes=[(kio, half)])
                            else:
                                kb.op("dve", lambda e, bank=bank, io=io: e.tensor_copy(io[:, 512:1024], bank[:, :]),
                                      reads=[kbk], writes=[(kio, half)])
                        kb.dma(y_out[t0 + c * 128:t0 + (c + 1) * 128, :], io[:, :], reads=[(kio, 0), (kio, 1)],
                               writes=[("yout", t0, c)])
            kb.barrier()

    def retention(l):
        win = W["w_in"][l]
        with contextlib.ExitStack() as pes:
            Wv = sbp(pes, "Wv", [128, KC * 1024], BF16)
            Wg = sbp(pes, "Wg", [128, KC * 1024], BF16)
            Wvv = Wv[:, :].rearrange("p (k n) -> p k n", k=KC)
            Wgv = Wg[:, :].rearrange("p (k n) -> p k n", k=KC)
            qd = sbp(pes, "qd", [128, 4 * 512], BF16)
            kT = sbp(pes, "kT", [128, 4 * 512], BF16)
            qdv = qd[:, :].rearrange("p (h t) -> p h t", h=4)
            kTv = kT[:, :].rearrange("p (h t) -> p h t", h=4)
            tab = [sbp(pes, "tab%d" % i, [128, 4 * 512], F32) for i in range(1)]
            qdec = sbp(pes, "qdec", [128, 4 * 640], F32)
            qdecv = qdec[:, :].rearrange("p (h t) -> p h t", h=4)
            maskT = sbp(pes, "maskT", [128, 2 * 512], F32)
            maskTv = maskT[:, :].rearrange("p (s n) -> p s n", s=2)
            kdec = sbp(pes, "kdec", [128, 8], F32)
            rowmask = sbp(pes, "rowmask", [128, 16], F32)
            tmpA = sbp(pes, "tmpA", [128, 512], F32)
            tmpB = sbp(pes, "tmpB", [128, 512], F32)
            tmpC = sbp(pes, "tmpC", [128, 512], F32)
            tmpD = sbp(pes, "tmpD", [128, 512], F32)
            v_tm = sbp(pes, "v_tm", [128, 1024], BF16)
            sg = sbp(pes, "sg", [128, 1024], BF16)
            kd_tm = sbp(pes, "kd_tm", [128, 512], BF16)
            kdm = sbp(pes, "kdm", [128, 512], BF16)
            sc = sbp(pes, "sc", [128, 512], BF16)
            S_f = sbp(pes, "S_f", [128, 1024], F32)
            S_b = sbp(pes, "S_b", [128, 1024], BF16)
            S0f = [sbp(pes, "S0f%d" % i, [128, 1024], F32) for i in range(2)]
            S0b = [sbp(pes, "S0b%d" % i, [128, 1024], BF16) for i in range(2)]
            Snew = [sbp(pes, "Snew%d" % i, [128, 1024], F32) for i in range(2)]
            qblk = sbp(pes, "qblk", [128, 4 * 2048], BF16)
            qblkv = qblk[:, :].rearrange("p (h s t) -> p h s t", h=4, s=16)
            bst = sbp(pes, "bst", [128, 4 * 6], F32)
            mv = sbp(pes, "mv", [128, 4 * 2], F32)
            mvv = mv[:, :].rearrange("p (h t) -> p h t", h=4)
            sdv = sbp(pes, "sdv", [128, 4], F32)
            rsv = sbp(pes, "rsv", [128, 4], F32)
            on = sbp(pes, "on", [128, 1024], BF16)
            og = sbp(pes, "og", [128, 1024], BF16)
            ogT = [sbp(pes, "ogT%d" % i, [128, 1024], BF16) for i in range(2)]

            kb.dma(qdecv, CST["qdec"], writes=["qdec"])
            kb.dma(maskT[:, :].rearrange("p (s h n) -> p s h n", s=2, h=4), CST["maskT"], writes=["maskT"])
            kb.dma(kdec[:, :].rearrange("p (s h) -> p s h", s=2), CST["kdec"], writes=["kdec"])
            kb.dma(rowmask[:, :], CST["rowmask"], writes=["rowmask"])
            kb.op("pool", lambda e: e.memset(qblk[:, :], 0.0), writes=["qblk"])
            kb.op("pool", lambda e: e.memset(S_f[:, :], 0.0), writes=["S_f"])
            kb.op("pool", lambda e: e.memset(S_b[:, :], 0.0), writes=["S_b"])
            for j in range(4):
                load_w([(win[:, 1024 + j * 256:1024 + (j + 1) * 256], 0)], KC, 256, dst=Wvv[:, :, j * 256:(j + 1) * 256], dstkey="Wv")
            for j in range(4):
                load_w([(win[:, 2048 + j * 256:2048 + (j + 1) * 256], 0)], KC, 256, dst=Wgv[:, :, j * 256:(j + 1) * 256], dstkey="Wg")

            for ti, (t0, n) in enumerate(TILES):
                tb = tab[0]
                tbv = tb[:, :].rearrange("p (s t) -> p s t", s=4)
                ktb = ("tab", 0)
                kb.dma(tbv[:, :, 0:n], CST["tabqk"][:, :, t0:t0 + n], writes=[ktb])
                samp = (t0 >= SEQ)
                sel = 1 if samp else 0
                qoff = 512 if samp else 0
                for h in range(4):
                    c0 = h * 128
                    k0 = 512 + h * 128
                    wq, kwq = load_w([(win[:, c0:c0 + 128], 0), (win[:, c0 + 64:c0 + 128], 128), (win[:, c0:c0 + 64], 192)], KC, 256)
                    wk, kwk = load_w([(win[:, k0:k0 + 128], 0), (win[:, k0 + 64:k0 + 128], 128), (win[:, k0:k0 + 64], 192)], KC, 256)
                    for b in range(4):
                        ww, kww = (wq, kwq) if b < 2 else (wk, kwk)
                        for kc in range(KC):
                            kb.op("pe", lambda e, kc=kc, b=b, ww=ww, t0=t0, n=n: e.matmul(
                                PS[b][:, 0:n], lhsT=ww[:, kc, (b % 2) * 128:(b % 2 + 1) * 128], rhs=hTv[:, kc, t0:t0 + n],
                                start=(kc == 0), stop=(kc == KC - 1)),
                                reads=[kww] + ck("h", t0, n), writes=["ps%d" % b], inc=(kc == KC - 1))
                    kb.op("dve", lambda e, n=n, tbv=tbv: e.tensor_tensor(out=tmpA[:, 0:n], in0=PS[0][:, 0:n], in1=tbv[:, 0, 0:n], op=ALU.mult),
                          reads=["ps0", ktb], writes=["tmpA"])
                    kb.op("dve", lambda e, n=n, tbv=tbv: e.tensor_tensor(out=tmpB[:, 0:n], in0=PS[1][:, 0:n], in1=tbv[:, 1, 0:n], op=ALU.mult),
                          reads=["ps1", ktb], writes=["tmpB"])
                    kb.op("pool", lambda e, n=n: e.tensor_tensor(out=tmpA[:, 0:n], in0=tmpA[:, 0:n], in1=tmpB[:, 0:n], op=ALU.add),
                          reads=["tmpA", "tmpB"], writes=["tmpA"])
                    kb.op("pool", lambda e, n=n, h=h, qoff=qoff: e.tensor_tensor(
                        out=qdv[:, h, 0:n], in0=tmpA[:, 0:n], in1=qdecv[:, h, qoff:qoff + n], op=ALU.mult),
                        reads=["tmpA", "qdec"], writes=[("qd", h)])
                    kb.op("dve", lambda e, n=n, tbv=tbv: e.tensor_tensor(out=tmpC[:, 0:n], in0=PS[2][:, 0:n], in1=tbv[:, 2, 0:n], op=ALU.mult),
                          reads=["ps2", ktb], writes=["tmpC"])
                    kb.op("dve", lambda e, n=n, tbv=tbv: e.tensor_tensor(out=tmpD[:, 0:n], in0=PS[3][:, 0:n], in1=tbv[:, 3, 0:n], op=ALU.mult),
                          reads=["ps3", ktb], writes=["tmpD"])
                    kb.op("pool", lambda e, n=n, h=h: e.tensor_tensor(out=kTv[:, h, 0:n], in0=tmpC[:, 0:n], in1=tmpD[:, 0:n], op=ALU.add),
                          reads=["tmpC", "tmpD"], writes=[("kT", h)])
                QD = [("qd", h) for h in range(4)]
                KT = [("kT", h) for h in range(4)]
                for ci in range(n // 128):
                    c = t0 // 128 + ci
                    cs = slice(ci * 128, (ci + 1) * 128)
                    ts = slice(t0 + ci * 128, t0 + (ci + 1) * 128)
                    for half in range(2):
                        for kc in range(KC):
                            kb.op("pe", lambda e, kc=kc, half=half, ts=ts: e.matmul(
                                PS[half][:, :], lhsT=hTv[:, kc, ts], rhs=Wvv[:, kc, half * 512:(half + 1) * 512],
                                start=(kc == 0), stop=(kc == KC - 1)),
                                reads=["Wv", ("h", c)], writes=["ps%d" % half], inc=(kc == KC - 1))
                        kb.op("act", lambda e, half=half: e.copy(v_tm[:, half * 512:(half + 1) * 512], PS[half][:, :]),
                              reads=["ps%d" % half], writes=[("v_tm", half)])
                    for half in range(2):
                        for kc in range(KC):
                            kb.op("pe", lambda e, kc=kc, half=half, ts=ts: e.matmul(
                                PS[2 + half][:, :], lhsT=hTv[:, kc, ts], rhs=Wgv[:, kc, half * 512:(half + 1) * 512],
                                start=(kc == 0), stop=(kc == KC - 1)),
                                reads=["Wg", ("h", c)], writes=["ps%d" % (2 + half)], inc=(kc == KC - 1))
                        kb.op("act", lambda e, half=half: e.activation(out=sg[:, half * 512:(half + 1) * 512], in_=PS[2 + half][:, :], func=AF.Silu),
                              reads=["ps%d" % (2 + half)], writes=[("sg", half)])
                    VT = [("v_tm", 0), ("v_tm", 1)]
                    p4 = PS[4][:, :].bitcast(BF16)
                    for h in range(4):
                        kb.op("pe", lambda e, h=h, cs=cs: e.transpose(p4[:, h * 128:(h + 1) * 128], kTv[:, h, cs], identb[:, :]),
                              reads=[("kT", h), "identb"], writes=["ps4"], inc=(h == 3))
                    for h in range(4):
                        kb.op("dve", lambda e, h=h, sel=sel: e.tensor_scalar(
                            out=kd_tm[:, h * 128:(h + 1) * 128], in0=p4[:, h * 128:(h + 1) * 128],
                            scalar1=kdec[:, sel * 4 + h:sel * 4 + h + 1], scalar2=None, op0=ALU.mult),
                            reads=["ps4", "kdec"], writes=["kd_tm"])
                    for h in range(4):
                        kb.op("pe", lambda e, h=h, cs=cs: e.matmul(PS[5][:, h * 128:(h + 1) * 128], lhsT=kTv[:, h, cs], rhs=qdv[:, h, cs],
                                                                 start=True, stop=True),
                              reads=[("kT", h), ("qd", h)], writes=["ps5"], inc=(h == 3))
                    kb.op("dve", lambda e, sel=sel: e.tensor_tensor(out=sc[:, :], in0=PS[5][:, :], in1=maskTv[:, sel, :], op=ALU.mult),
                          reads=["ps5", "maskT"], writes=["sc"])
                    if not samp:
                        for h in range(4):
                            ob = PS[6 + h // 2][:, (h % 2) * 256:(h % 2) * 256 + 256]
                            kb.op("pe", lambda e, h=h, ob=ob: e.matmul(ob, lhsT=sc[:, h * 128:(h + 1) * 128], rhs=v_tm[:, h * 256:(h + 1) * 256],
                                                                     start=True, stop=False),
                                  reads=["sc"] + VT, writes=["ps%d" % (6 + h // 2)], inc=False)
                            kb.op("pe", lambda e, h=h, ob=ob, cs=cs: e.matmul(ob, lhsT=qdv[:, h, cs], rhs=S_b[:, h * 256:(h + 1) * 256],
                                                                            start=False, stop=True),
                                  reads=[("qd", h), "S_b"], writes=["ps%d" % (6 + h // 2)], inc=True)
                        for h in range(4):
                            sbk = PS[h // 2][:, (h % 2) * 256:(h % 2) * 256 + 256]
                            kb.op("pe", lambda e, h=h, sbk=sbk: e.matmul(sbk, lhsT=kd_tm[:, h * 128:(h + 1) * 128], rhs=v_tm[:, h * 256:(h + 1) * 256],
                                                                       start=True, stop=True),
                                  reads=["kd_tm"] + VT, writes=["ps%d" % (h // 2)], inc=True)
                            kb.op("dve", lambda e, h=h, sbk=sbk: e.scalar_tensor_tensor(
                                out=S_f[:, h * 256:(h + 1) * 256], in0=S_f[:, h * 256:(h + 1) * 256], scalar=float(GAM[h] ** 128),
                                in1=sbk, op0=ALU.mult, op1=ALU.add),
                                reads=["ps%d" % (h // 2), "S_f"], writes=["S_f"])
                        kb.op("act", lambda e: e.copy(S_b[:, :], S_f[:, :]), reads=["S_f"], writes=["S_b"])
                        if c == SEQ // 128 - 1:
                            kb.dma(ret_p[l].rearrange("h d e -> d h e"), S_f[:, :].rearrange("p (h e) -> p h e", h=4),
                                   reads=["S_f"], writes=[("ret_p", l)])
                    else:
                        for h in range(4):
                            ob = PS[6 + h // 2][:, (h % 2) * 256:(h % 2) * 256 + 256]
                            for s in range(NSS):
                                kb.op("pool", lambda e, h=h, s=s: e.tensor_copy(qblkv[:, h, s, s * 8:(s + 1) * 8], qdv[:, h, s * 8:(s + 1) * 8]),
                                      reads=[("qd", h)], writes=["qblk"])
                            kb.op("pe", lambda e, h=h, ob=ob: e.matmul(ob, lhsT=sc[:, h * 128:(h + 1) * 128], rhs=v_tm[:, h * 256:(h + 1) * 256],
                                                                     start=True, stop=False),
                                  reads=["sc"] + VT, writes=["ps%d" % (6 + h // 2)], inc=False)
                            for s in range(NSS):
                                i2 = (h * NSS + s) % 2
                                s0f = S0f[i2]
                                s0b = S0b[i2]
                                sn = Snew[i2]
                                kb.dma(s0f[:, 0:256], W["state_ret"][l, s, h], writes=[("S0f", i2)])
                                kb.op("act", lambda e, s0f=s0f, s0b=s0b: e.copy(s0b[:, 0:256], s0f[:, 0:256]), reads=[("S0f", i2)], writes=[("S0b", i2)])
                                kb.op("pe", lambda e, h=h, ob=ob, s=s, s0b=s0b: e.matmul(
                                    ob, lhsT=qblkv[:, h, s, :], rhs=s0b[:, 0:256], start=False, stop=(s == NSS - 1)),
                                    reads=["qblk", ("S0b", i2)], writes=["ps%d" % (6 + h // 2)], inc=True)
                                kb.op("dve", lambda e, s=s, h=h: e.tensor_scalar(out=kdm[:, 0:128], in0=kd_tm[:, h * 128:(h + 1) * 128],
                                                                               scalar1=rowmask[:, s:s + 1], scalar2=None, op0=ALU.mult),
                                      reads=["kd_tm", "rowmask"], writes=["kdm"])
                                sbk = PS[i2][:, 0:256]
                                kb.op("pe", lambda e, h=h, sbk=sbk: e.matmul(sbk, lhsT=kdm[:, 0:128], rhs=v_tm[:, h * 256:(h + 1) * 256],
                                                                           start=True, stop=True),
                                      reads=["kdm"] + VT, writes=["ps%d" % i2], inc=True)
                                kb.op("dve", lambda e, h=h, sbk=sbk, s0f=s0f, sn=sn: e.scalar_tensor_tensor(
                                    out=sn[:, 0:256], in0=s0f[:, 0:256], scalar=float(GAM[h] ** 8),
                                    in1=sbk, op0=ALU.mult, op1=ALU.add),
                                    reads=["ps%d" % i2, ("S0f", i2)], writes=[("Snew", i2)])
                                kb.dma(ret_s[l, s, h], sn[:, 0:256], reads=[("Snew", i2)], writes=[("ret_s", l, s, h)])
                    for h in range(4):
                        ob = PS[6 + h // 2][:, (h % 2) * 256:(h % 2) * 256 + 256]
                        kb.op("dve", lambda e, h=h, ob=ob: e.bn_stats(bst[:, h * 6:(h + 1) * 6], ob), reads=["ps%d" % (6 + h // 2)], writes=[("bst", h)])
                        kb.op("dve", lambda e, h=h: e.bn_aggr(mv[:, h * 2:(h + 1) * 2], bst[:, h * 6:(h + 1) * 6]), reads=[("bst", h)], writes=[("mv", h)])
                    MV = [("mv", h) for h in range(4)]
                    kb.op("act", lambda e: e.activation(out=sdv[:, :], in_=mvv[:, :, 1], func=AF.Sqrt, bias=EPS, scale=1.0), reads=MV, writes=["sdv"])
                    kb.op("dve", lambda e: e.reciprocal(rsv[:, :], sdv[:, :]), reads=["sdv"], writes=["rsv"])
                    for h in range(4):
                        ob = PS[6 + h // 2][:, (h % 2) * 256:(h % 2) * 256 + 256]
                        kb.op("dve", lambda e, h=h, ob=ob: e.tensor_scalar(
                            out=on[:, h * 256:(h + 1) * 256], in0=ob, scalar1=mvv[:, h, 0:1], scalar2=rsv[:, h:h + 1],
                            op0=ALU.subtract, op1=ALU.mult),
                            reads=["ps%d" % (6 + h // 2), "rsv"] + MV, writes=["on"])
                    kb.op("pool", lambda e: e.tensor_tensor(out=og[:, :], in0=on[:, :], in1=sg[:, :], op=ALU.mult),
                          reads=["on", ("sg", 0), ("sg", 1)], writes=["og"])
                    ogt = ogT[c % 2]
                    for kc in range(KC):
                        kb.op("pe", lambda e, kc=kc: e.transpose(p4[:, kc * 128:(kc + 1) * 128], og[:, kc * 128:(kc + 1) * 128], identb[:, :]),
                              reads=["og", "identb"], writes=["ps4"], inc=(kc == KC - 1))
                    kb.op("act", lambda e, ogt=ogt: e.copy(ogt[:, :], p4[:, :]), reads=["ps4"], writes=[("ogT", c % 2)])
                    kb.dma(brscr[:, 0:KC, ts], ogt[:, :].rearrange("p (k t) -> p k t", k=KC), reads=[("ogT", c % 2)], writes=[("brscr", c)])
            kb.barrier()

    def stage_c(l, branch, wo_ap, kcb, gate_col0, rowscale_name=None, rowscale_ap=None, glu=False):
        win = W["w_in"][l]
        with contextlib.ExitStack() as pes:
            nco = 2048 if glu else 1024
            Wo = sbp(pes, "Wo", [128, kcb * nco], BF16)
            Wov = Wo[:, :].rearrange("p (k n) -> p k n", k=kcb)
            tglu = [sbp(pes, "tglu%d" % i, [128, 512], F32) for i in range(2)] if glu else None
            Wgt = sbp(pes, "Wgt", [128, KC * 1024], BF16)
            Wgtv = Wgt[:, :].rearrange("p (k n) -> p k n", k=KC)
            Wout = sbp(pes, "Wout", [128, KC * 1024], BF16)
            Woutv = Wout[:, :].rearrange("p (k n) -> p k n", k=KC)
            nbr = 2 if kcb == 8 else 1
            brt = [sbp(pes, "brt%d" % i, [128, kcb * 512], BF16) for i in range(nbr)]
            xt = [sbp(pes, "xt%d" % i, [128, KC * 512], F32) for i in range(2)]
            mt = sbp(pes, "mt", [128, KC * 512], BF16)
            mtv = mt[:, :].rearrange("p (k t) -> p k t", k=KC)
            sig = [sbp(pes, "sig%d" % i, [128, 512], F32) for i in range(2)]
            rs = None
            if rowscale_ap is not None:
                for k0 in range(0, kcb, KC):
                    pass
                rs = load_gain(rowscale_name, rowscale_ap, kcb)
            for k0 in range(0, kcb, 2):
                for j in range(nco // 256):
                    if rs is None:
                        load_w([(wo_ap[k0 * 128:(k0 + 2) * 128, j * 256:(j + 1) * 256], 0)], 2, 256,
                               dst=Wov[:, k0:k0 + 2, j * 256:(j + 1) * 256], dstkey="Wo")
                    else:
                        s = wslot[0]
                        wslot[0] ^= 1
                        st = wst[s][:, 0:512].rearrange("p (k n) -> p k n", k=2)
                        kb.dma(st, wo_ap[k0 * 128:(k0 + 2) * 128, j * 256:(j + 1) * 256].rearrange("(k p) n -> p k n", p=128), writes=[("wst", s)])
                        for kk in range(2):
                            kb.op("pool", lambda e, kk=kk, st=st, k0=k0, j=j: e.tensor_scalar(
                                out=Wov[:, k0 + kk, j * 256:(j + 1) * 256], in0=st[:, kk, :], scalar1=gcv[:, rs, k0 + kk:k0 + kk + 1],
                                scalar2=None, op0=ALU.mult),
                                reads=[("wst", s), ("g", rs)], writes=["Wo"])
            for j in range(4):
                load_w([(win[:, gate_col0 + j * 256:gate_col0 + (j + 1) * 256], 0)], KC, 256, dst=Wgtv[:, :, j * 256:(j + 1) * 256], dstkey="Wgt")
            for j in range(4):
                load_w([(W["w_out"][l][:, j * 256:(j + 1) * 256], 0)], KC, 256, dst=Woutv[:, :, j * 256:(j + 1) * 256], dstkey="Wout")
            bank = 0
            for ti, (t0, n) in enumerate(TILES):
                b_t = brt[ti % nbr]
                b_v = b_t[:, :].rearrange("p (k t) -> p k t", k=kcb)
                x_t = xt[ti % 2]
                x_v = x_t[:, :].rearrange("p (k t) -> p k t", k=KC)
                kb.dma(b_v[:, :, 0:n], brscr[:, 0:kcb, t0:t0 + n], reads=ck("brscr", t0, n), writes=[("brt", ti % nbr)])
                kb.dma(x_v[:, :, 0:n], xscr[:, :, t0:t0 + n], reads=[("xscr", t0)], writes=[("xt", ti % 2)])
                for oc in range(KC):
                    by = bank % 4
                    bg = (bank + 1) % 4
                    bank += 2
                    for kc in range(kcb):
                        kb.op("pe", lambda e, kc=kc, oc=oc, by=by, b_v=b_v, n=n: e.matmul(
                            PS[by][:, 0:n], lhsT=Wov[:, kc, oc * 128:(oc + 1) * 128], rhs=b_v[:, kc, 0:n], start=(kc == 0), stop=(kc == kcb - 1)),
                            reads=["Wo", ("brt", ti % nbr)], writes=["ps%d" % by], inc=(kc == kcb - 1))
                    for kc in range(KC):
                        kb.op("pe", lambda e, kc=kc, oc=oc, bg=bg, t0=t0, n=n: e.matmul(
                            PS[bg][:, 0:n], lhsT=Wgtv[:, kc, oc * 128:(oc + 1) * 128], rhs=hTv[:, kc, t0:t0 + n], start=(kc == 0), stop=(kc == KC - 1)),
                            reads=["Wgt"] + ck("h", t0, n), writes=["ps%d" % bg], inc=(kc == KC - 1))
                    sgm = sig[oc % 2]
                    if glu:
                        b2 = 6 + (oc % 2)
                        for kc in range(kcb):
                            kb.op("pe", lambda e, kc=kc, oc=oc, b2=b2, b_v=b_v, n=n: e.matmul(
                                PS[b2][:, 0:n], lhsT=Wov[:, kc, 1024 + oc * 128:1024 + (oc + 1) * 128], rhs=b_v[:, kc, 0:n], start=(kc == 0), stop=(kc == kcb - 1)),
                                reads=["Wo", ("brt", ti % nbr)], writes=["ps%d" % b2], inc=(kc == kcb - 1))
                        tg = tglu[oc % 2]
                        kb.op("act", lambda e, b2=b2, n=n, tg=tg: e.activation(out=tg[:, 0:n], in_=PS[b2][:, 0:n], func=AF.Sigmoid),
                              reads=["ps%d" % b2], writes=[("tglu", oc % 2)])
                        kb.op("dve", lambda e, by=by, n=n, tg=tg: e.tensor_tensor(out=tg[:, 0:n], in0=PS[by][:, 0:n], in1=tg[:, 0:n], op=ALU.mult),
                              reads=["ps%d" % by, ("tglu", oc % 2)], writes=[("tglu", oc % 2)])
                        kb.op("act", lambda e, bg=bg, n=n, sgm=sgm: e.activation(out=sgm[:, 0:n], in_=PS[bg][:, 0:n], func=AF.Sigmoid),
                              reads=["ps%d" % bg], writes=[("sig", oc % 2)])
                        kb.op("dve", lambda e, n=n, sgm=sgm, oc=oc, tg=tg: e.tensor_tensor(out=mtv[:, oc, 0:n], in0=tg[:, 0:n], in1=sgm[:, 0:n], op=ALU.mult),
                              reads=[("tglu", oc % 2), ("sig", oc % 2)], writes=[("mt", oc)])
                    else:
                        kb.op("act", lambda e, bg=bg, n=n, sgm=sgm: e.activation(out=sgm[:, 0:n], in_=PS[bg][:, 0:n], func=AF.Sigmoid),
                              reads=["ps%d" % bg], writes=[("sig", oc % 2)])
                        kb.op("dve", lambda e, by=by, n=n, sgm=sgm, oc=oc: e.tensor_tensor(out=mtv[:, oc, 0:n], in0=PS[by][:, 0:n], in1=sgm[:, 0:n], op=ALU.mult),
                              reads=["ps%d" % by, ("sig", oc % 2)], writes=[("mt", oc)])
                MT = [("mt", oc) for oc in range(KC)]
                for oc in range(KC):
                    bo = 4 + (oc % 2)
                    for kc in range(KC):
                        kb.op("pe", lambda e, kc=kc, oc=oc, bo=bo, n=n: e.matmul(
                            PS[bo][:, 0:n], lhsT=Woutv[:, kc, oc * 128:(oc + 1) * 128], rhs=mtv[:, kc, 0:n], start=(kc == 0), stop=(kc == KC - 1)),
                            reads=["Wout"] + MT, writes=["ps%d" % bo], inc=(kc == KC - 1))
                    kb.op("dve", lambda e, oc=oc, bo=bo, n=n, x_v=x_v: e.tensor_tensor(out=x_v[:, oc, 0:n], in0=PS[bo][:, 0:n], in1=x_v[:, oc, 0:n], op=ALU.add),
                          reads=["ps%d" % bo, ("xt", ti % 2)], writes=[("xt", ti % 2)])
                kb.dma(xscr[:, :, t0:t0 + n], x_v[:, :, 0:n], reads=[("xt", ti % 2)], writes=[("xscr", t0)])
            kb.barrier()

    def ssd(l):
        win = W["w_in"][l]
        XB0, Z0, DT0 = 6144, 4096, 9216
        Ident = AF.Identity
        with contextlib.ExitStack() as pes:
            cw = sbp(pes, "cw", [128, 24 * 5], F32)
            cwv = cw[:, :].rearrange("p (f k) -> p f k", k=5)
            cb = None
            with contextlib.ExitStack() as p0:
                cwt = sbp(p0, "cwt", [5, 3072], F32)
                kb.dma(cwt[0:4, :], W["ssd_conv_w"][l], writes=["cwt"])
                kb.dma(cwt[4:5, :], W["ssd_conv_b"][l:l + 1, :], writes=["cwt"])
                for fc in range(24):
                    kb.op("pe", lambda e, fc=fc: e.transpose(PS[7][:, fc * 5:(fc + 1) * 5], cwt[0:5, fc * 128:(fc + 1) * 128], identf[0:5, 0:5]),
                          reads=["cwt", "identf"], writes=["ps7"], inc=(fc == 23))
                kb.op("act", lambda e: e.copy(cw[:, :], PS[7][:, 0:120]), reads=["ps7"], writes=["cw"])
                kb.barrier()
            with contextlib.ExitStack() as p1:
                cin = sbp(p1, "cin", [48, 3072], F32)
                histT = sbp(p1, "histT", [128, 24 * 48], F32)
                hv = histT[:, :].rearrange("p (f s r) -> p f s r", f=24, s=16)
                tailT = sbp(p1, "tailT", [128, 24 * 48], F32)
                tv = tailT[:, :].rearrange("p (f s r) -> p f s r", f=24, s=16)
                tailP = sbp(p1, "tailP", [128, 72], F32)
                cout = sbp(p1, "cout", [48, 3072], F32)
                raw = [sbp(p1, "raw%d" % i, [128, 515], F32) for i in range(2)]
                acc = sbp(p1, "acc", [128, 512], F32)
                xc = [sbp(p1, "xc%d" % i, [128, 512], BF16) for i in range(2)]
                xst = [sbp(p1, "xst%d" % i, [128, 512], BF16) for i in range(2)]
                kb.dma(cin[:, :], W["state_conv"][l].rearrange("s r f -> (s r) f"), writes=["cin"])
                for fc in range(24):
                    bi = 6 + (fc // 4) % 2
                    j = fc % 4
                    kb.op("pe", lambda e, bi=bi, j=j, fc=fc: e.transpose(PS[bi][:, j * 48:(j + 1) * 48], cin[0:48, fc * 128:(fc + 1) * 128], identf[0:48, 0:48]),
                          reads=["cin", "identf"], writes=["ps%d" % bi], inc=(j == 3))
                    if j == 3:
                        kb.op("act", lambda e, bi=bi, fc=fc: e.copy(histT[:, (fc - 3) * 48:(fc + 1) * 48], PS[bi][:, 0:192]),
                              reads=["ps%d" % bi], writes=["histT"])
                rix = 0
                p5 = PS[5][:, :].bitcast(BF16)
                for fcp in range(12):
                    wx, kwx = load_w([(win[:, XB0 + fcp * 256:XB0 + (fcp + 1) * 256], 0)], KC, 256)
                    for sub in range(2):
                        fc = fcp * 2 + sub
                        prevR, kprev = None, None
                        for ti, (t0, n) in enumerate(TILES):
                            pb = rix % 4
                            bank = PS[pb]
                            kbank = "ps%d" % pb
                            for kc in range(KC):
                                kb.op("pe", lambda e, kc=kc, bank=bank, sub=sub, t0=t0, n=n, wx=wx: e.matmul(
                                    bank[:, 0:n], lhsT=wx[:, kc, sub * 128:(sub + 1) * 128], rhs=hTv[:, kc, t0:t0 + n],
                                    start=(kc == 0), stop=(kc == KC - 1)),
                                    reads=[kwx] + ck("h", t0, n), writes=[kbank], inc=(kc == KC - 1))
                            R = raw[rix % 2]
                            kR = ("raw", rix % 2)
                            xcb = xc[rix % 2]
                            kxc = ("xc", rix % 2)
                            xsb_ = xst[rix % 2]
                            kxs = ("xst", rix % 2)
                            rix += 1
                            samp = t0 >= SEQ
                            if not samp:
                                if t0 == 0:
                                    kb.op("pool", lambda e, R=R: e.memset(R[:, 0:3], 0.0), writes=[kR])
                                else:
                                    kb.op("dve", lambda e, R=R, prevR=prevR: e.tensor_copy(R[:, 0:3], prevR[:, 512:515]), reads=[kprev], writes=[kR])
                                kb.op("act", lambda e, R=R, bank=bank, n=n: e.copy(R[:, 3:3 + n], bank[:, 0:n]), reads=[kbank], writes=[kR])
                                X = [R[:, k:k + n] for k in range(4)]
                                A = acc[:, 0:n]
                            else:
                                R3 = R[:, 0:176].rearrange("p (s c) -> p s c", c=11)
                                kb.op("dve", lambda e, R3=R3, fc=fc: e.tensor_copy(R3[:, :, 0:3], hv[:, fc]), reads=["histT"], writes=[kR])
                                kb.op("act", lambda e, R3=R3, bank=bank: e.copy(R3[:, :, 3:11], bank[:, 0:128].rearrange("p (s c) -> p s c", c=8)),
                                      reads=[kbank], writes=[kR])
                                X = [R3[:, :, k:k + 8] for k in range(4)]
                                A = acc[:, 0:128].rearrange("p (s c) -> p s c", c=8)
                            kb.op("act", lambda e, A=A, X=X, fc=fc: e.activation(out=A, in_=X[3], func=Ident, scale=cwv[:, fc, 3:4], bias=cwv[:, fc, 4:5]),
                                  reads=[kR, "cw"], writes=["acc"])
                            for k in (2, 1, 0):
                                kb.op("dve", lambda e, A=A, X=X, fc=fc, k=k: e.scalar_tensor_tensor(
                                    out=A, in0=X[k], scalar=cwv[:, fc, k:k + 1], in1=A, op0=ALU.mult, op1=ALU.add),
                                    reads=[kR, "cw", "acc"], writes=["acc"])
                            kb.op("act", lambda e, xcb=xcb, n=n: e.activation(out=xcb[:, 0:n], in_=acc[:, 0:n], func=AF.Silu),
                                  reads=["acc"], writes=[kxc])
                            if t0 == 1536:
                                kb.op("pool", lambda e, R=R, fc=fc: e.tensor_copy(tailP[:, fc * 3:(fc + 1) * 3], R[:, 512:515]), reads=[kR], writes=["tailP"])
                            if samp:
                                kb.op("pool", lambda e, R3=R3, fc=fc: e.tensor_copy(tv[:, fc], R3[:, :, 8:11]), reads=[kR], writes=["tailT"])
                            if fc >= 16:
                                kb.dma(bc_scr[:, fc - 16, t0:t0 + n], xcb[:, 0:n], reads=[kxc], writes=[("bc_scr", fc, t0)])
                            if fc < 20:
                                nci = n // 128
                                for ci in range(nci):
                                    kb.op("pe", lambda e, ci=ci, xcb=xcb: e.transpose(p5[:, ci * 128:(ci + 1) * 128], xcb[:, ci * 128:(ci + 1) * 128], identb[:, :]),
                                          reads=[kxc, "identb"], writes=["ps5"], inc=(ci == nci - 1))
                                kb.op("act", lambda e, xsb_=xsb_, n=n: e.copy(xsb_[:, 0:n], p5[:, 0:n]), reads=["ps5"], writes=[kxs])
                                kb.dma(xsb_scr[t0:t0 + n, fc * 128:(fc + 1) * 128].rearrange("(c p) f -> p c f", p=128),
                                       xsb_[:, 0:n].rearrange("p (c f) -> p c f", f=128), reads=[kxs], writes=[("xsb_scr", fc, t0)])
                            prevR, kprev = R, kR
                for fc in range(24):
                    bi = 6 + (fc // 4) % 2
                    j = fc % 4
                    kb.op("pe", lambda e, bi=bi, j=j, fc=fc: e.transpose(PS[bi][0:3, j * 128:(j + 1) * 128], tailP[:, fc * 3:(fc + 1) * 3], identf[:, :]),
                          reads=["tailP", "identf"], writes=["ps%d" % bi], inc=(j == 3))
                    if j == 3:
                        kb.op("act", lambda e, bi=bi, fc=fc: e.copy(cin[0:3, (fc - 3) * 128:(fc + 1) * 128], PS[bi][0:3, 0:512]),
                              reads=["ps%d" % bi, "histT"], writes=["cin"])
                kb.dma(conv_p[l], cin[0:3, :], reads=["cin"], writes=[("conv_p", l)])
                for fc in range(24):
                    bi = 6 + (fc // 4) % 2
                    j = fc % 4
                    kb.op("pe", lambda e, bi=bi, j=j, fc=fc: e.transpose(PS[bi][0:48, j * 128:(j + 1) * 128], tailT[:, fc * 48:(fc + 1) * 48], identf[:, :]),
                          reads=["tailT", "identf"], writes=["ps%d" % bi], inc=(j == 3))
                    if j == 3:
                        kb.op("act", lambda e, bi=bi, fc=fc: e.copy(cout[0:48, (fc - 3) * 128:(fc + 1) * 128], PS[bi][0:48, 0:512]),
                              reads=["ps%d" % bi], writes=["cout"])
                kb.dma(conv_s[l].rearrange("s r f -> (s r) f"), cout[:, :], reads=["cout"], writes=[("conv_s", l)])
                kb.barrier()

            if not stages.get("ssd_s2", True):
                return
            with contextlib.ExitStack() as p2:
                Wz = sbp(p2, "Wz", [128, KC * 2048], BF16)
                Wzv = Wz[:, :].rearrange("p (k n) -> p k n", k=KC)
                Wdt = sbp(p2, "Wdt", [128, KC * 32], BF16)
                Wdtv = Wdt[:, :].rearrange("p (k n) -> p k n", k=KC)
                triu = sbp(p2, "triu", [128, 256], F32)
                triuv = triu[:, :].rearrange("p (s n) -> p s n", s=2)
                negm = sbp(p2, "negm", [128, 256], F32)
                negmv = negm[:, :].rearrange("p (s n) -> p s n", s=2)
                ssm = sbp(p2, "ssm", [128, 256], F32)
                ssv = ssm[:, :].rearrange("p (s n) -> p s n", s=2)
                rm = sbp(p2, "rm", [128, 2048], F32)
                rmv = rm[:, :].rearrange("p (s n) -> p s n", s=16)
                rowmask = sbp(p2, "rowmask2", [128, 16], F32)
                dtb = sbp(p2, "dtb", [128, 32], F32)
                a_t = sbp(p2, "a_t", [128, 32], F32)
                dsk = sbp(p2, "dsk", [128, 32], F32)
                sT = sbp(p2, "sT", [128, 2048], F32)
                sTb = sbp(p2, "sTb", [128, 2048], BF16)
                xsb = sbp(p2, "xsb", [128, 2560], BF16)
                bct = sbp(p2, "bct", [128, 1024], BF16)
                bcv = bct[:, :].rearrange("p (g t) -> p g t", g=8)
                dtt = sbp(p2, "dtt", [128, 32], F32)
                ex1 = sbp(p2, "ex1", [128, 32], F32)
                dt_ = sbp(p2, "dt_", [128, 32], F32)
                dA = sbp(p2, "dA", [128, 32], F32)
                cum = sbp(p2, "cum", [128, 32], F32)
                wtmp = sbp(p2, "wtmp", [128, 32], F32)
                wend = sbp(p2, "wend", [128, 32], F32)
                dec = sbp(p2, "dec", [128, 32], F32)
                decs = sbp(p2, "decs", [128, 512], F32)
                decsv = decs[:, :].rearrange("p (s h) -> p s h", s=16)
                ecum = sbp(p2, "ecum", [128, 32], F32)
                xdt = sbp(p2, "xdt", [128, 2048], BF16)
                xsD = sbp(p2, "xsD", [128, 2048], BF16)
                xdtw = sbp(p2, "xdtw", [128, 2048], BF16)
                scT = sbp(p2, "scT", [128, 512], BF16)
                yoff = sbp(p2, "yoff", [128, 2048], F32)
                yy = sbp(p2, "yy", [128, 2048], F32)
                zs = sbp(p2, "zs", [128, 512], F32)
                seg = [sbp(p2, "seg%d" % i, [128, 128], F32) for i in range(4)]
                Lm = [sbp(p2, "Lm%d" % i, [128, 128], BF16) for i in range(4)]
                Mm = [sbp(p2, "Mm%d" % i, [128, 128], BF16) for i in range(4)]
                bst = sbp(p2, "bst2", [128, 24], F32)
                mv = sbp(p2, "mv2", [128, 8], F32)
                mvv = mv[:, :].rearrange("p (g t) -> p g t", g=4)
                ms = sbp(p2, "ms", [128, 4], F32)
                sdv = sbp(p2, "sdv2", [128, 4], F32)
                rsv = sbp(p2, "rsv2", [128, 4], F32)
                yn = sbp(p2, "yn", [128, 2048], BF16)
                ynT = sbp(p2, "ynT", [128, 2048], BF16)
                Cblk = sbp(p2, "Cblk", [128, 2048], BF16)
                Cbv = Cblk[:, :].rearrange("p (s t) -> p s t", s=16)
                s0 = [sbp(p2, "s0_%d" % i, [128, 512], F32) for i in range(2)]
                s0T = [sbp(p2, "s0T%d" % i, [128, 512], F32) for i in range(2)]
                s0Tb = [sbp(p2, "s0Tb%d" % i, [128, 512], BF16) for i in range(2)]
                Bm = [sbp(p2, "Bm%d" % i, [128, 128], BF16) for i in range(2)]
                snat = [sbp(p2, "snat%d" % i, [128, 512], F32) for i in range(2)]
                print("S2 sbuf remaining", nc.sbuf_bytes_remaining)

                kb.dma(triuv, CST["triu"], writes=["triu"])
                kb.dma(negmv, CST["negm"], writes=["negm"])
                kb.dma(ssv, CST["ss"], writes=["ss"])
                kb.dma(rmv, CST["rm"], writes=["rm"])
                kb.dma(rowmask[:, :], CST["rowmask"], writes=["rowmask"])
                kb.dma(dtb[:, :], W["ssd_dt_bias"][l:l + 1, :].to_broadcast([128, 32]), writes=["dtb"])
                kb.dma(a_t[:, :], W["ssd_a_log"][l:l + 1, :].to_broadcast([128, 32]), writes=["a_t"])
                kb.dma(dsk[:, :], W["ssd_d"][l:l + 1, :].to_broadcast([128, 32]), writes=["dsk"])
                kb.op("act", lambda e: e.activation(out=a_t[:, :], in_=a_t[:, :], func=AF.Exp), reads=["a_t"], writes=["a_t"])
                kb.op("dve", lambda e: e.tensor_scalar(out=a_t[:, :], in0=a_t[:, :], scalar1=-1.0, scalar2=None, op0=ALU.mult), reads=["a_t"], writes=["a_t"])
                kb.op("pool", lambda e: e.memset(sT[:, :], 0.0), writes=["sT"])
                kb.op("pool", lambda e: e.memset(sTb[:, :], 0.0), writes=["sTb"])
                kb.op("pool", lambda e: e.memset(Cblk[:, :], 0.0), writes=["Cblk"])
                for j in range(8):
                    load_w([(win[:, Z0 + j * 256:Z0 + (j + 1) * 256], 0)], KC, 256, dst=Wzv[:, :, j * 256:(j + 1) * 256], dstkey="Wz")
                load_w([(win[:, DT0:DT0 + 32], 0)], KC, 32, dst=Wdtv, dstkey="Wdt")
                p5 = PS[5][:, :].bitcast(BF16)
                hcount = 0
                for c in stages.get("ssd_chunks", list(range(NCH))):
                    ts = slice(c * 128, (c + 1) * 128)
                    samp = c * 128 >= SEQ
                    sel = 1 if samp else 0
                    kb.dma(xsb[:, :], xsb_scr[ts, :], reads=[("xsb_scr", fc, (c * 128 // 512) * 512) for fc in range(20)], writes=["xsb"])
                    kb.dma(bcv, bc_scr[:, :, ts], reads=[("bc_scr", fc, (c * 128 // 512) * 512) for fc in range(16, 24)], writes=["bct"])
                    for kc in range(KC):
                        kb.op("pe", lambda e, kc=kc, ts=ts: e.matmul(PS[0][:, 0:32], lhsT=hTv[:, kc, ts], rhs=Wdtv[:, kc, :], start=(kc == 0), stop=(kc == KC - 1)),
                              reads=["Wdt", ("h", c)], writes=["ps0"], inc=(kc == KC - 1))
                    kb.op("dve", lambda e: e.tensor_tensor(out=dtt[:, :], in0=PS[0][:, 0:32], in1=dtb[:, :], op=ALU.add), reads=["ps0", "dtb"], writes=["dtt"])
                    kb.op("act", lambda e: e.activation(out=ex1[:, :], in_=dtt[:, :], func=AF.Exp), reads=["dtt"], writes=["ex1"])
                    kb.op("act", lambda e: e.activation(out=dt_[:, :], in_=ex1[:, :], func=AF.Ln, bias=1.0, scale=1.0), reads=["ex1"], writes=["dt_"])
                    kb.op("dve", lambda e: e.tensor_tensor(out=dA[:, :], in0=dt_[:, :], in1=a_t[:, :], op=ALU.mult), reads=["dt_", "a_t"], writes=["dA"])
                    kb.op("pe", lambda e, sel=sel: e.matmul(PS[0][:, 32:64], lhsT=triuv[:, sel, :], rhs=dA[:, :], start=True, stop=True),
                          reads=["triu", "dA"], writes=["ps0"], inc=True)
                    kb.op("act", lambda e: e.copy(cum[:, :], PS[0][:, 32:64]), reads=["ps0"], writes=["cum"])
                    kb.op("pe", lambda e, sel=sel: e.matmul(PS[0][:, 64:96], lhsT=ssv[:, sel, :], rhs=dA[:, :], start=True, stop=True),
                          reads=["ss", "dA"], writes=["ps0"], inc=True)
                    kb.op("dve", lambda e: e.tensor_tensor(out=wtmp[:, :], in0=PS[0][:, 64:96], in1=cum[:, :], op=ALU.subtract), reads=["ps0", "cum"], writes=["wtmp"])
                    kb.op("act", lambda e: e.activation(out=wend[:, :], in_=wtmp[:, :], func=AF.Exp), reads=["wtmp"], writes=["wend"])
                    kb.op("act", lambda e: e.activation(out=dec[:, :], in_=PS[0][:, 64:96], func=AF.Exp), reads=["ps0"], writes=["dec"])
                    kb.op("act", lambda e: e.activation(out=ecum[:, :], in_=cum[:, :], func=AF.Exp), reads=["cum"], writes=["ecum"])
                    xs3 = xsb[:, 0:2048].rearrange("p (h q) -> p h q", h=32)
                    kb.op("dve", lambda e, xs3=xs3: e.tensor_tensor(out=xdt[:, :].rearrange("p (h q) -> p h q", h=32), in0=xs3,
                                                                  in1=dt_[:, :].unsqueeze(2).to_broadcast([128, 32, 64]), op=ALU.mult),
                          reads=["xsb", "dt_"], writes=["xdt"])
                    kb.op("pool", lambda e, xs3=xs3: e.tensor_tensor(out=xsD[:, :].rearrange("p (h q) -> p h q", h=32), in0=xs3,
                                                                   in1=dsk[:, :].unsqueeze(2).to_broadcast([128, 32, 64]), op=ALU.mult),
                          reads=["xsb", "dsk"], writes=["xsD"])
                    kb.op("pool", lambda e: e.tensor_tensor(out=xdtw[:, :].rearrange("p (h q) -> p h q", h=32), in0=xdt[:, :].rearrange("p (h q) -> p h q", h=32),
                                                          in1=wend[:, :].unsqueeze(2).to_broadcast([128, 32, 64]), op=ALU.mult),
                          reads=["xdt", "wend"], writes=["xdtw"])
                    for g in range(4):
                        kb.op("pe", lambda e, g=g: e.matmul(PS[1][:, g * 128:(g + 1) * 128], lhsT=bcv[:, g, :], rhs=bcv[:, 4 + g, :], start=True, stop=True),
                              reads=["bct"], writes=["ps1"], inc=(g == 3))
                    kb.op("act", lambda e: e.copy(scT[:, :], PS[1][:, :]), reads=["ps1"], writes=["scT"])
                    if samp:
                        for s in range(NSS):
                            kb.op("pe", lambda e, s=s: e.matmul(PS[1][:, s * 32:(s + 1) * 32], lhsT=rmv[:, s, :], rhs=dA[:, :], start=True, stop=True),
                                  reads=["rm", "dA", "scT"], writes=["ps1"], inc=(s == NSS - 1))
                        kb.op("act", lambda e: e.activation(out=decs[:, :], in_=PS[1][:, :], func=AF.Exp), reads=["ps1"], writes=["decs"])
                    for g in range(4):
                        gs_ = slice(g * 512, (g + 1) * 512)
                        if not samp:
                            kb.op("pe", lambda e, g=g, gs_=gs_: e.matmul(PS[2][:, :], lhsT=bcv[:, 4 + g, :], rhs=sTb[:, gs_], start=True, stop=True),
                                  reads=["bct", "sTb"], writes=["ps2"], inc=True)
                        else:
                            for s in range(NSS):
                                kb.op("pool", lambda e, s=s, g=g: e.tensor_copy(Cbv[:, s, s * 8:(s + 1) * 8], bcv[:, 4 + g, s * 8:(s + 1) * 8]),
                                      reads=["bct"], writes=["Cblk"])
                            dbg = stages.get("dbg", 9)
                            for s in (range(stages.get("smp_ns", NSS)) if dbg >= 2 else []):
                                i2 = s % 2
                                for b4 in range(4):
                                    kb.dma(s0[i2][:, b4 * 128:(b4 + 1) * 128],
                                           W["state_ssm"][l, s, g * 8 + 2 * b4:g * 8 + 2 * b4 + 2].rearrange("h q n -> (h q) n"), writes=[("s0", i2)])
                                if dbg < 2.2:
                                    continue
                                for b4 in range(4):
                                    kb.op("pe", lambda e, b4=b4, i2=i2: e.transpose(PS[3][:, b4 * 128:(b4 + 1) * 128], s0[i2][:, b4 * 128:(b4 + 1) * 128], identf[:, :]),
                                          reads=[("s0", i2), "identf"], writes=["ps3"], inc=(b4 == 3))
                                if dbg < 2.4:
                                    continue
                                kb.op("dve", lambda e, i2=i2: e.tensor_copy(s0T[i2][:, :], PS[3][:, :]), reads=["ps3"], writes=[("s0T", i2)])
                                if dbg < 2.6:
                                    continue
                                kb.op("act", lambda e, i2=i2: e.copy(s0Tb[i2][:, :], s0T[i2][:, :]), reads=[("s0T", i2)], writes=[("s0Tb", i2)])
                                if dbg < 3:
                                    continue
                                kb.op("pe", lambda e, s=s, i2=i2: e.matmul(PS[2][:, :], lhsT=Cbv[:, s, :], rhs=s0Tb[i2][:, :], start=(s == 0), stop=(s == NSS - 1)),
                                      reads=["Cblk", ("s0Tb", i2)], writes=["ps2"], inc=True)
                                if dbg < 4:
                                    continue
                                kb.op("dve", lambda e, s=s, g=g, i2=i2: e.tensor_scalar(out=Bm[i2][:, :], in0=xsb[:, 2048 + g * 128:2048 + (g + 1) * 128],
                                                                                   scalar1=rowmask[:, s:s + 1], scalar2=None, op0=ALU.mult),
                                      reads=["xsb", "rowmask"], writes=[("Bm", i2)])
                                kb.op("pe", lambda e, i2=i2, gs_=gs_: e.matmul(PS[4][:, :], lhsT=Bm[i2][:, :], rhs=xdtw[:, gs_], start=True, stop=True),
                                      reads=[("Bm", i2), "xdtw"], writes=["ps4"], inc=True)
                                kb.op("dve", lambda e, i2=i2, s=s, g=g: e.tensor_tensor(
                                    out=s0T[i2][:, :].rearrange("p (h q) -> p h q", h=8), in0=s0T[i2][:, :].rearrange("p (h q) -> p h q", h=8),
                                    in1=decsv[:, s, g * 8:(g + 1) * 8].unsqueeze(2).to_broadcast([128, 8, 64]), op=ALU.mult),
                                    reads=[("s0T", i2), "decs"], writes=[("s0T", i2)])
                                kb.op("dve", lambda e, i2=i2: e.tensor_tensor(out=s0T[i2][:, :], in0=PS[4][:, :], in1=s0T[i2][:, :], op=ALU.add),
                                      reads=["ps4", ("s0T", i2)], writes=[("s0T", i2)])
                                if dbg < 5:
                                    continue
                                for b4 in range(4):
                                    kb.op("pe", lambda e, b4=b4, i2=i2: e.transpose(PS[3][:, b4 * 128:(b4 + 1) * 128], s0T[i2][:, b4 * 128:(b4 + 1) * 128], identf[:, :]),
                                          reads=[("s0T", i2), "identf"], writes=["ps3"], inc=(b4 == 3))
                                kb.op("act", lambda e, i2=i2: e.copy(snat[i2][:, :], PS[3][:, :]), reads=["ps3"], writes=[("snat", i2)])
                                for b4 in range(4):
                                    kb.dma(ssm_s[l, s, g * 8 + 2 * b4:g * 8 + 2 * b4 + 2].rearrange("h q n -> (h q) n"),
                                           snat[i2][:, b4 * 128:(b4 + 1) * 128], reads=[("snat", i2)], writes=[("ssm_s", l, s, g, b4)])
                        if samp and stages.get("dbg", 9) < 3:
                            kb.op("pe", lambda e, g=g, gs_=gs_: e.matmul(PS[2][:, :], lhsT=bcv[:, 4 + g, :], rhs=sTb[:, gs_], start=True, stop=True),
                                  reads=["bct", "sTb"], writes=["ps2"], inc=True)
                        kb.op("dve", lambda e, g=g, gs_=gs_: e.tensor_tensor(
                            out=yoff[:, gs_].rearrange("p (h q) -> p h q", h=8), in0=PS[2][:, :].rearrange("p (h q) -> p h q", h=8),
                            in1=ecum[:, g * 8:(g + 1) * 8].unsqueeze(2).to_broadcast([128, 8, 64]), op=ALU.mult),
                            reads=["ps2", "ecum"], writes=[("yoff", g)])
                    for h in range(32):
                        g = h // 8
                        i2 = hcount % 2
                        hcount += 1
                        cbk = 6 + i2
                        kb.op("pe", lambda e, h=h, cbk=cbk, sel=sel: e.matmul(PS[cbk][:, 0:128], lhsT=dA[:, h:h + 1].to_broadcast([128, 128]), rhs=triuv[:, sel, :],
                                                                           start=True, stop=True),
                              reads=["dA", "triu"], writes=["ps%d" % cbk], inc=True)
                        kb.op("dve", lambda e, h=h, cbk=cbk, i2=i2, sel=sel: e.scalar_tensor_tensor(
                            out=seg[i2][:, :], in0=PS[cbk][:, 0:128], scalar=cum[:, h:h + 1], in1=negmv[:, sel, :], op0=ALU.subtract, op1=ALU.add),
                            reads=["ps%d" % cbk, "cum", "negm"], writes=[("seg", i2)])
                        kb.op("act", lambda e, i2=i2: e.activation(out=Lm[i2][:, :], in_=seg[i2][:, :], func=AF.Exp), reads=[("seg", i2)], writes=[("Lm", i2)])
                        kb.op("pool", lambda e, i2=i2, g=g: e.tensor_tensor(out=Mm[i2][:, :], in0=Lm[i2][:, :], in1=scT[:, g * 128:(g + 1) * 128], op=ALU.mult),
                              reads=[("Lm", i2), "scT"], writes=[("Mm", i2)])
                        ybk = 2 + (g % 2)
                        yo = PS[ybk][:, (h % 8) * 64:(h % 8 + 1) * 64]
                        kb.op("pe", lambda e, yo=yo, i2=i2, h=h: e.matmul(yo, lhsT=Mm[i2][:, :], rhs=xdt[:, h * 64:(h + 1) * 64], start=True, stop=False),
                              reads=[("Mm", i2), "xdt"] + [("yoff", gg) for gg in range(4)], writes=["ps%d" % ybk], inc=False)
                        kb.op("pe", lambda e, yo=yo, h=h: e.matmul(yo, lhsT=identb[:, :], rhs=xsD[:, h * 64:(h + 1) * 64], start=False, stop=True),
                              reads=["identb", "xsD"], writes=["ps%d" % ybk], inc=True)
                        if h % 8 == 7:
                            kb.op("dve", lambda e, g=g, ybk=ybk: e.tensor_tensor(out=yy[:, g * 512:(g + 1) * 512], in0=PS[ybk][:, :], in1=yoff[:, g * 512:(g + 1) * 512], op=ALU.add),
                                  reads=["ps%d" % ybk, ("yoff", g)], writes=[("yy", g)])
                    for g in range(4):
                        zb = 0 + (g % 2)
                        for kc in range(KC):
                            kb.op("pe", lambda e, kc=kc, g=g, zb=zb, ts=ts: e.matmul(PS[zb][:, :], lhsT=hTv[:, kc, ts], rhs=Wzv[:, kc, g * 512:(g + 1) * 512],
                                                                                  start=(kc == 0), stop=(kc == KC - 1)),
                                  reads=["Wz", ("h", c), "cum", "wtmp", "dec"], writes=["ps%d" % zb], inc=(kc == KC - 1))
                        kb.op("act", lambda e, zb=zb: e.activation(out=zs[:, :], in_=PS[zb][:, :], func=AF.Silu), reads=["ps%d" % zb], writes=["zs"])
                        kb.op("dve", lambda e, g=g: e.tensor_tensor(out=yy[:, g * 512:(g + 1) * 512], in0=yy[:, g * 512:(g + 1) * 512], in1=zs[:, :], op=ALU.mult),
                              reads=[("yy", g), "zs"], writes=[("yy", g)])
                        kb.op("dve", lambda e, g=g: e.bn_stats(bst[:, g * 6:(g + 1) * 6], yy[:, g * 512:(g + 1) * 512]), reads=[("yy", g)], writes=[("bst", g)])
                        kb.op("dve", lambda e, g=g: e.bn_aggr(mv[:, g * 2:(g + 1) * 2], bst[:, g * 6:(g + 1) * 6]), reads=[("bst", g)], writes=[("mv", g)])
                    MV = [("mv", g) for g in range(4)]
                    kb.op("dve", lambda e: e.scalar_tensor_tensor(out=ms[:, :], in0=mvv[:, :, 0], scalar=1.0, in1=mvv[:, :, 0], op0=ALU.mult, op1=ALU.mult),
                          reads=MV, writes=["ms"])
                    kb.op("dve", lambda e: e.tensor_tensor(out=ms[:, :], in0=ms[:, :], in1=mvv[:, :, 1], op=ALU.add), reads=MV + ["ms"], writes=["ms"])
                    kb.op("act", lambda e: e.activation(out=sdv[:, :], in_=ms[:, :], func=AF.Sqrt, bias=EPS, scale=1.0), reads=["ms"], writes=["sdv"])
                    kb.op("dve", lambda e: e.reciprocal(rsv[:, :], sdv[:, :]), reads=["sdv"], writes=["rsv"])
                    for g in range(4):
                        kb.op("dve", lambda e, g=g: e.tensor_scalar(out=yn[:, g * 512:(g + 1) * 512], in0=yy[:, g * 512:(g + 1) * 512], scalar1=rsv[:, g:g + 1],
                                                                  scalar2=None, op0=ALU.mult),
                              reads=[("yy", g), "rsv"], writes=["yn"])
                    for half in range(2):
                        for j in range(8):
                            kc = half * 8 + j
                            kb.op("pe", lambda e, kc=kc, j=j: e.transpose(p5[:, j * 128:(j + 1) * 128], yn[:, kc * 128:(kc + 1) * 128], identb[:, :]),
                                  reads=["yn", "identb"], writes=["ps5"], inc=(j == 7))
                        kb.op("act", lambda e, half=half: e.copy(ynT[:, half * 1024:(half + 1) * 1024], p5[:, :]), reads=["ps5"], writes=[("ynT", half)])
                    kb.dma(brscr[:, 0:16, ts], ynT[:, :].rearrange("p (k t) -> p k t", k=16), reads=[("ynT", 0), ("ynT", 1)], writes=[("brscr", c)])
                    if not samp:
                        for g in range(4):
                            gs_ = slice(g * 512, (g + 1) * 512)
                            kb.op("pe", lambda e, g=g, gs_=gs_: e.matmul(PS[4][:, :], lhsT=xsb[:, 2048 + g * 128:2048 + (g + 1) * 128], rhs=xdtw[:, gs_], start=True, stop=True),
                                  reads=["xsb", "xdtw"], writes=["ps4"], inc=True)
                            kb.op("dve", lambda e, g=g, gs_=gs_: e.tensor_tensor(
                                out=sT[:, gs_].rearrange("p (h q) -> p h q", h=8), in0=sT[:, gs_].rearrange("p (h q) -> p h q", h=8),
                                in1=dec[:, g * 8:(g + 1) * 8].unsqueeze(2).to_broadcast([128, 8, 64]), op=ALU.mult),
                                reads=["sT", "dec"], writes=["sT"])
                            kb.op("dve", lambda e, gs_=gs_: e.tensor_tensor(out=sT[:, gs_], in0=PS[4][:, :], in1=sT[:, gs_], op=ALU.add),
                                  reads=["ps4", "sT"], writes=["sT"])
                        kb.op("act", lambda e: e.copy(sTb[:, :], sT[:, :]), reads=["sT"], writes=["sTb"])
                        if c == SEQ // 128 - 1:
                            for q4 in range(4):
                                for b4 in range(4):
                                    blk = q4 * 4 + b4
                                    kb.op("pe", lambda e, b4=b4, blk=blk: e.transpose(PS[3][:, b4 * 128:(b4 + 1) * 128], sT[:, blk * 128:(blk + 1) * 128], identf[:, :]),
                                          reads=["sT", "identf"], writes=["ps3"], inc=(b4 == 3))
                                kb.op("act", lambda e, q4=q4: e.copy(snat[q4 % 2][:, :], PS[3][:, :]), reads=["ps3"], writes=[("snat", q4 % 2)])
                                for b4 in range(4):
                                    kb.dma(ssm_p[l, q4 * 8 + 2 * b4:q4 * 8 + 2 * b4 + 2].rearrange("h q n -> (h q) n"),
                                           snat[q4 % 2][:, b4 * 128:(b4 + 1) * 128], reads=[("snat", q4 % 2)], writes=[("ssm_p", l, q4, b4)])
                kb.barrier()

    def s5(l):
        win = W["w_in"][l]
        U0 = 3072
        TWO_PI = 2.0 * np.pi
        MAGIC = 12582912.0
        with contextlib.ExitStack() as pes:
            uT = sbp(pes, "uT", [128, KC * T], BF16)
            uTv = uT[:, :].rearrange("p (k t) -> p k t", k=KC)
            Bpad = sbp(pes, "Bpad", [128, 64 * 128], BF16)
            Bspad = sbp(pes, "Bspad", [128, 64 * 128], BF16)
            Cpad = sbp(pes, "Cpad", [128, 64 * 128], BF16)
            Bpv = Bpad[:, :].rearrange("p (g n) -> p g n", g=64)
            Bsv = Bspad[:, :].rearrange("p (g n) -> p g n", g=64)
            Cpv = Cpad[:, :].rearrange("p (g n) -> p g n", g=64)
            mag = sbp(pes, "mag", [128, 64], F32)
            frc = sbp(pes, "frc", [128, 64], F32)
            dcol = sbp(pes, "dcol", [128, 8], F32)
            tpos = sbp(pes, "tpos", [128, 256], F32)
            tposv = tpos[:, :].rearrange("p (s t) -> p s t", s=2)
            smask = sbp(pes, "smask", [128, 128], F32)
            Pm = sbp(pes, "Pm", [128, 128], F32)
            xst = sbp(pes, "xstate", [128, 64], F32)
            xfs = sbp(pes, "xfs", [128, 64 * 16], F32)
            xfsv = xfs[:, :].rearrange("p (g s) -> p g s", g=64)
            x0T = sbp(pes, "x0T", [128, 16 * 64], F32)
            x0Tv = x0T[:, :].rearrange("p (s g) -> p s g", s=16)
            kb.dma(tposv, CST["tpos"], writes=["tpos"])
            kb.dma(smask[:, :], CST["smask"], writes=["smask"])
            kb.dma(Pm[:, :], CST["Pm"], writes=["Pm"])
            kb.dma(dcol[:, :], W["s5_d"][l].rearrange("(k p) -> p k", p=128), writes=["dcol"])
            kb.op("pool", lambda e: e.memset(xst[:, :], 0.0), writes=[("xst", g) for g in range(64)])
            for j in range(4):
                wu, kwu = load_w([(win[:, U0 + j * 256:U0 + (j + 1) * 256], 0)], KC, 256)
                for (t0, n) in TILES:
                    for sub in range(2):
                        oc = j * 2 + sub
                        bk = (oc % 2)
                        for kc in range(KC):
                            kb.op("pe", lambda e, kc=kc, bk=bk, sub=sub, t0=t0, n=n, wu=wu: e.matmul(
                                PS[bk][:, 0:n], lhsT=wu[:, kc, sub * 128:(sub + 1) * 128], rhs=hTv[:, kc, t0:t0 + n], start=(kc == 0), stop=(kc == KC - 1)),
                                reads=[kwu] + ck("h", t0, n), writes=["ps%d" % bk], inc=(kc == KC - 1))
                        kb.op("act", lambda e, bk=bk, oc=oc, t0=t0, n=n: e.copy(uTv[:, oc, t0:t0 + n], PS[bk][:, 0:n]), reads=["ps%d" % bk], writes=[("uT", oc)])
            with contextlib.ExitStack() as pp:
                def t64(name):
                    return sbp(pp, name, [128, 64], F32)
                aa = sbp(pp, "aa", [64, 256], F32)
                dt = t64("dt"); ar = t64("ar"); ai = t64("ai"); th = t64("th"); t1 = t64("t1"); t2 = t64("t2")
                sn = t64("sn"); cs = t64("cs"); abr = t64("abr"); abi = t64("abi"); den = t64("den")
                fre = t64("fre"); fim = t64("fim"); FP = t64("FP"); FQ = t64("FQ")
                bre2 = sbp(pp, "bre2", [128, 1024], F32)
                bim2 = sbp(pp, "bim2", [128, 1024], F32)
                Bn2 = sbp(pp, "Bn2", [128, 1024], F32)
                Bsn2 = sbp(pp, "Bsn2", [128, 1024], F32)
                tq = sbp(pp, "tq", [128, 1024], F32)
                Ball = sbp(pp, "Ball", [128, 256], F32)
                cT = sbp(pp, "cT", [128, 8 * 128], F32)
                cTv = cT[:, :].rearrange("p (k n) -> p k n", k=8)
                gmask = sbp(pp, "gmask", [128, 8], F32)
                cmask = sbp(pp, "cmask", [128, 1024], F32)
                cmv = cmask[:, :].rearrange("p (g n) -> p g n", g=8)
                kb.dma(gmask[:, :], CST["gmask"], writes=["gmask"])
                kb.dma(cmv, CST["cmask"], writes=["cmask"])
                kb.dma(dt[:, :], W["s5_log_dt"][l:l + 1, :].to_broadcast([128, 64]), writes=["dt"])
                kb.op("act", lambda e: e.activation(out=dt[:, :], in_=dt[:, :], func=AF.Exp), reads=["dt"], writes=["dt"])
                kb.dma(aa[:, 0:64], W["s5_a_re"][l], writes=["aa"])
                kb.dma(aa[:, 64:128], W["s5_a_re"][l], writes=["aa"])
                kb.dma(aa[:, 128:192], W["s5_a_im"][l], writes=["aa"])
                kb.dma(aa[:, 192:256], W["s5_a_im"][l], writes=["aa"])
                for i2 in range(2):
                    kb.op("pe", lambda e, i2=i2: e.transpose(PS[7][:, i2 * 64:(i2 + 1) * 64], aa[0:64, i2 * 128:(i2 + 1) * 128], identf[0:64, 0:64]),
                          reads=["aa", "identf"], writes=["ps7"], inc=(i2 == 1))
                kb.op("act", lambda e: e.copy(ar[:, :], PS[7][:, 0:64]), reads=["ps7"], writes=["ar"])
                kb.op("act", lambda e: e.copy(ai[:, :], PS[7][:, 64:128]), reads=["ps7"], writes=["ai"])
                TT = lambda o, a, b, op, eng="dve": kb.op(eng, lambda e: e.tensor_tensor(out=o[:, :], in0=a[:, :], in1=b[:, :], op=op),
                                                          reads=[id(a), id(b)], writes=[id(o)])
                TS = lambda o, a, s1, op0, s2=None, op1=None: kb.op("dve", (lambda e: e.tensor_scalar(out=o[:, :], in0=a[:, :], scalar1=s1, scalar2=s2, op0=op0, op1=op1)) if op1 is not None
                                                                      else (lambda e: e.tensor_scalar(out=o[:, :], in0=a[:, :], scalar1=s1, scalar2=None, op0=op0)),
                                                                      reads=[id(a)], writes=[id(o)])
                for tns, key in [(dt, "dt"), (ar, "ar"), (ai, "ai")]:
                    kb.writer[id(tns)] = kb.writer.get(key)
                TT(t1, dt, ar, ALU.mult)
                kb.op("act", lambda e: e.activation(out=mag[:, :], in_=t1[:, :], func=AF.Exp), reads=[id(t1)], writes=["mag"])
                TT(th, dt, ai, ALU.mult)
                TS(th, th, 1.0 / TWO_PI, ALU.mult)
                TS(t1, th, MAGIC, ALU.add)
                TS(t1, t1, MAGIC, ALU.subtract)
                kb.op("dve", lambda e: e.tensor_tensor(out=frc[:, :], in0=th[:, :], in1=t1[:, :], op=ALU.subtract), reads=[id(th), id(t1)], writes=["frc"])
                kb.op("act", lambda e: e.activation(out=sn[:, :], in_=frc[:, :], func=AF.Sin, scale=TWO_PI), reads=["frc"], writes=[id(sn)])
                kb.op("dve", lambda e: e.tensor_scalar(out=t2[:, :], in0=frc[:, :], scalar1=0.25, scalar2=None, op0=ALU.add), reads=["frc"], writes=[id(t2)])
                TS(t1, t2, 0.5, ALU.is_gt)
                TT(t2, t2, t1, ALU.subtract)
                kb.op("act", lambda e: e.activation(out=cs[:, :], in_=t2[:, :], func=AF.Sin, scale=TWO_PI), reads=[id(t2)], writes=[id(cs)])
                kb.op("dve", lambda e: e.tensor_tensor(out=abr[:, :], in0=mag[:, :], in1=cs[:, :], op=ALU.mult), reads=["mag", id(cs)], writes=[id(abr)])
                kb.op("dve", lambda e: e.tensor_tensor(out=abi[:, :], in0=mag[:, :], in1=sn[:, :], op=ALU.mult), reads=["mag", id(sn)], writes=[id(abi)])
                TS(abr, abr, -1.0, ALU.add)
                TT(t1, ar, ar, ALU.mult)
                TT(t2, ai, ai, ALU.mult)
                TT(den, t1, t2, ALU.add)
                kb.op("dve", lambda e: e.reciprocal(den[:, :], den[:, :]), reads=[id(den)], writes=[id(den)])
                TT(t1, abr, ar, ALU.mult)
                TT(t2, abi, ai, ALU.mult)
                TT(fre, t1, t2, ALU.add)
                TT(fre, fre, den, ALU.mult)
                TT(t1, abi, ar, ALU.mult)
                TT(t2, abr, ai, ALU.mult)
                TT(fim, t1, t2, ALU.subtract)
                TT(fim, fim, den, ALU.mult)
                kb.op("dve", lambda e: e.tensor_copy(FP[0:64, :], fre[0:64, :]), reads=[id(fre)], writes=[id(FP)])
                kb.op("dve", lambda e: e.tensor_copy(FP[64:128, :], fim[64:128, :]), reads=[id(fim)], writes=[id(FP)])
                kb.op("dve", lambda e: e.tensor_scalar(out=FQ[0:64, :], in0=fim[0:64, :], scalar1=-1.0, scalar2=None, op0=ALU.mult), reads=[id(fim)], writes=[id(FQ)])
                kb.op("dve", lambda e: e.tensor_copy(FQ[64:128, :], fre[64:128, :]), reads=[id(fre)], writes=[id(FQ)])
                for half in range(2):
                    kb.dma(bre2[half * 64:(half + 1) * 64, :].rearrange("p (g c) -> p g c", g=64), W["s5_b_re"][l].rearrange("g n c -> n g c"), writes=["bre2"])
                    kb.dma(bim2[half * 64:(half + 1) * 64, :].rearrange("p (g c) -> p g c", g=64), W["s5_b_im"][l].rearrange("g n c -> n g c"), writes=["bim2"])
                v3 = lambda t: t[:, :].rearrange("p (g c) -> p g c", g=64)
                bc = lambda t: t[:, :].unsqueeze(2).to_broadcast([128, 64, 16])
                kb.op("dve", lambda e: e.tensor_tensor(out=v3(Bn2), in0=v3(bre2), in1=bc(FP), op=ALU.mult), reads=["bre2", id(FP)], writes=["Bn2"])
                kb.op("dve", lambda e: e.tensor_tensor(out=v3(tq), in0=v3(bim2), in1=bc(FQ), op=ALU.mult), reads=["bim2", id(FQ)], writes=["tq"])
                kb.op("dve", lambda e: e.tensor_tensor(out=Bn2[:, :], in0=Bn2[:, :], in1=tq[:, :], op=ALU.add), reads=["Bn2", "tq"], writes=["Bn2"])
                kb.op("dve", lambda e: e.tensor_tensor(out=v3(Bsn2), in0=v3(bim2), in1=bc(FP), op=ALU.mult), reads=["bim2", id(FP)], writes=["Bsn2"])
                kb.op("dve", lambda e: e.tensor_tensor(out=v3(tq), in0=v3(bre2), in1=bc(FQ), op=ALU.mult), reads=["bre2", id(FQ), "Bn2"], writes=["tq"])
                kb.op("dve", lambda e: e.tensor_tensor(out=Bsn2[:, :], in0=Bsn2[:, :], in1=tq[:, :], op=ALU.subtract), reads=["Bsn2", "tq"], writes=["Bsn2"])
                kb.dma(cTv[:, :, 0:64], W["s5_c_re"][l].rearrange("(k g) c n -> (g c) k n", k=8), writes=["cT"])
                kb.dma(cTv[:, :, 64:128], W["s5_c_im"][l].rearrange("(k g) c n -> (g c) k n", k=8), writes=["cT"])
                kb.op("dve", lambda e: e.tensor_scalar(out=cTv[:, :, 64:128], in0=cTv[:, :, 64:128], scalar1=-1.0, scalar2=None, op0=ALU.mult), reads=["cT"], writes=["cT"])
                for gc in range(8):
                    kb.op("pe", lambda e, gc=gc: e.transpose(PS[6][:, 0:128], Bn2[:, gc * 128:(gc + 1) * 128], identf[:, :]), reads=["Bn2", "identf"], writes=["ps6"], inc=False)
                    kb.op("pe", lambda e, gc=gc: e.transpose(PS[6][:, 128:256], Bsn2[:, gc * 128:(gc + 1) * 128], identf[:, :]), reads=["Bsn2", "identf"], writes=["ps6"], inc=True)
                    kb.op("act", lambda e: e.copy(Ball[:, :], PS[6][:, 0:256]), reads=["ps6"], writes=["Ball"])
                    kb.op("pe", lambda e, gc=gc: e.transpose(PS[7][:, 0:128], cTv[:, gc, :], identf[:, :]), reads=["cT", "identf"], writes=["ps7"], inc=True)
                    for g8 in range(8):
                        g = gc * 8 + g8
                        kb.op("dve", lambda e, g=g, g8=g8: e.tensor_scalar(out=Bpv[:, g, :], in0=Ball[:, 0:128], scalar1=gmask[:, g8:g8 + 1], scalar2=None, op0=ALU.mult),
                              reads=["Ball", "gmask"], writes=["Bpad"])
                        kb.op("pool", lambda e, g=g, g8=g8: e.tensor_scalar(out=Bsv[:, g, :], in0=Ball[:, 128:256], scalar1=gmask[:, g8:g8 + 1], scalar2=None, op0=ALU.mult),
                              reads=["Ball", "gmask"], writes=["Bspad"])
                        kb.op("dve", lambda e, g=g, g8=g8: e.tensor_tensor(out=Cpv[:, g, :], in0=PS[7][:, 0:128], in1=cmv[:, g8, :], op=ALU.mult),
                              reads=["ps7", "cmask"], writes=["Cpad"])
                kb.barrier()
            with contextlib.ExitStack() as px:
                s0in = sbp(px, "s0in", [64, 16 * 128], F32)
                s0r = sbp(px, "s0r", [64, 16 * 128], F32)
                kb.dma(s0in[:, :].rearrange("p (s x) -> p s x", s=16), W["state_s5"][l].rearrange("s g n r -> g s (n r)"), writes=["s0in"])
                kb.op("dve", lambda e: e.tensor_copy(s0r[:, :].rearrange("p (s r n) -> p s r n", s=16, r=2), s0in[:, :].rearrange("p (s n r) -> p s r n", s=16, r=2)),
                      reads=["s0in"], writes=["s0r"])
                for s in range(NSS):
                    bk = 6 + (s // 8) % 2
                    kb.op("pe", lambda e, s=s, bk=bk: e.transpose(PS[bk][:, (s % 8) * 64:(s % 8 + 1) * 64], s0r[0:64, s * 128:(s + 1) * 128], identf[0:64, 0:64]),
                          reads=["s0r", "identf"], writes=["ps%d" % bk], inc=(s % 8 == 7))
                    if s % 8 == 7:
                        kb.op("act", lambda e, s=s, bk=bk: e.copy(x0T[:, (s - 7) * 64:(s + 1) * 64], PS[bk][:, :]), reads=["ps%d" % bk], writes=["x0T"])
                kb.barrier()
            with contextlib.ExitStack() as pm:
                cosT = sbp(pm, "cosT", [128, 2 * 1024], F32)
                sinT = sbp(pm, "sinT", [128, 2 * 1024], F32)
                cosv = cosT[:, :].rearrange("p (s g t) -> p s g t", s=2, g=8)
                sinv = sinT[:, :].rearrange("p (s g t) -> p s g t", s=2, g=8)
                ta = sbp(pm, "ta", [128, 1024], F32)
                tb = sbp(pm, "tb", [128, 1024], F32)
                rms_ = sbp(pm, "rms_", [128, 128], F32)
                w1 = [sbp(pm, "w1_%d" % i, [128, 128], F32) for i in range(4)]
                w2 = [sbp(pm, "w2_%d" % i, [128, 128], F32) for i in range(4)]
                zz = [sbp(pm, "zz%d" % i, [128, 128], F32) for i in range(4)]
                x1 = [sbp(pm, "x1_%d" % i, [128, 128], F32) for i in range(6)]
                x2 = [sbp(pm, "x2_%d" % i, [128, 128], F32) for i in range(4)]
                xb = [sbp(pm, "xb%d" % i, [128, 128], BF16) for i in range(4)]
                yvL = [sbp(pm, "yv%d" % i, [128, 128], F32) for i in range(2)]
                ytL = [sbp(pm, "yt%d" % i, [128, 128], F32) for i in range(2)]
                ysgL = [sbp(pm, "ysg%d" % i, [128, 128], F32) for i in range(2)]
                yo = [sbp(pm, "yo%d" % i, [128, 128], BF16) for i in range(2)]
                fin = sbp(pm, "s5fin", [64, 128], F32)
                fin2 = [sbp(pm, "s5fin2_%d" % i, [64, 128], F32) for i in range(2)]
                it = 0
                for gc in range(8):
                    for sel in range(2):
                        kb.op("dve", lambda e, sel=sel, gc=gc: e.tensor_tensor(
                            out=ta[:, :].rearrange("p (g t) -> p g t", g=8), in0=tposv[:, sel:sel + 1, :].to_broadcast([128, 8, 128]),
                            in1=frc[:, gc * 8:(gc + 1) * 8].unsqueeze(2).to_broadcast([128, 8, 128]), op=ALU.mult),
                            reads=["tpos", "frc"], writes=["ta"])
                        kb.op("dve", lambda e: e.tensor_scalar(out=tb[:, :], in0=ta[:, :], scalar1=MAGIC, scalar2=None, op0=ALU.add), reads=["ta"], writes=["tb"])
                        kb.op("dve", lambda e: e.tensor_scalar(out=tb[:, :], in0=tb[:, :], scalar1=MAGIC, scalar2=None, op0=ALU.subtract), reads=["tb"], writes=["tb"])
                        kb.op("dve", lambda e: e.tensor_tensor(out=ta[:, :], in0=ta[:, :], in1=tb[:, :], op=ALU.subtract), reads=["ta", "tb"], writes=["ta"])
                        kb.op("act", lambda e, sel=sel: e.activation(out=sinT[:, sel * 1024:(sel + 1) * 1024], in_=ta[:, :], func=AF.Sin, scale=TWO_PI),
                              reads=["ta"], writes=[("sinT", sel)])
                        kb.op("dve", lambda e: e.tensor_scalar(out=ta[:, :], in0=ta[:, :], scalar1=0.25, scalar2=None, op0=ALU.add), reads=["ta"], writes=["ta"])
                        kb.op("dve", lambda e: e.tensor_scalar(out=tb[:, :], in0=ta[:, :], scalar1=0.5, scalar2=None, op0=ALU.is_gt), reads=["ta"], writes=["tb"])
                        kb.op("dve", lambda e: e.tensor_tensor(out=ta[:, :], in0=ta[:, :], in1=tb[:, :], op=ALU.subtract), reads=["ta", "tb"], writes=["ta"])
                        kb.op("act", lambda e, sel=sel: e.activation(out=cosT[:, sel * 1024:(sel + 1) * 1024], in_=ta[:, :], func=AF.Sin, scale=TWO_PI),
                              reads=["ta"], writes=[("cosT", sel)])
                    items = [(c, g8) for c in range(NCH) for g8 in range(8)]

                    def ctx(k, item):
                        c, g8 = item
                        d = {"c": c, "g8": g8, "g": gc * 8 + g8, "ts": slice(c * 128, (c + 1) * 128), "samp": c * 128 >= SEQ}
                        d["sel"] = 1 if d["samp"] else 0
                        q = k % 4
                        d["pA"], d["kA"] = PS[q][:, 0:128], ("spbank", q)
                        d["pB"], d["kB"] = PS[q][:, 128:256], ("spbank", q)
                        d["pC"], d["kC"] = PS[q][:, 256:384], ("spbank", q)
                        d["w1"], d["kw1"] = w1[k % 4], ("w1", k % 4)
                        d["w2"], d["kw2"] = w2[k % 4], ("w2", k % 4)
                        d["zz"], d["kzz"] = zz[k % 4], ("zz", k % 4)
                        d["x1"], d["kx1"] = x1[k % 6], ("x1", k % 6)
                        d["x2"], d["kx2"] = x2[k % 4], ("x2", k % 4)
                        d["xb"], d["kxb"] = xb[k % 4], ("xb", k % 4)
                        return d

                    def sA(k, item):
                        d = ctx(k, item)
                        kb.op("pe", lambda e: e.matmul(d["pA"], lhsT=Bpv[:, d["g"], :], rhs=uTv[:, gc, d["ts"]], start=True, stop=True),
                              reads=["Bpad", ("uT", gc)], writes=[d["kA"]], inc=True)
                        kb.op("pe", lambda e: e.matmul(d["pB"], lhsT=Bsv[:, d["g"], :], rhs=uTv[:, gc, d["ts"]], start=True, stop=True),
                              reads=["Bspad", ("uT", gc)], writes=[d["kB"]], inc=True)

                    def sB(k, item):
                        d = ctx(k, item)
                        kb.op("dve", lambda e: e.tensor_tensor(out=d["w1"][:, :], in0=d["pA"], in1=cosv[:, d["sel"], d["g8"], :], op=ALU.mult),
                              reads=[d["kA"], ("cosT", d["sel"])], writes=[d["kw1"]])
                        kb.op("dve", lambda e: e.tensor_tensor(out=d["w2"][:, :], in0=d["pB"], in1=sinv[:, d["sel"], d["g8"], :], op=ALU.mult),
                              reads=[d["kB"], ("sinT", d["sel"])], writes=[d["kw2"]])

                    def sC(k, item):
                        d = ctx(k, item)
                        kb.op("pool", lambda e: e.tensor_tensor(out=d["w1"][:, :], in0=d["w1"][:, :], in1=d["w2"][:, :], op=ALU.add),
                              reads=[d["kw1"], d["kw2"]], writes=[d["kw1"]])

                    def sD(k, item):
                        d = ctx(k, item)
                        g = d["g"]
                        if not d["samp"]:
                            kb.op("dve", lambda e: e.tensor_tensor_scan(
                                out=d["zz"][:, :], data0=mag[:, g:g + 1].to_broadcast([128, 128]), data1=d["w1"][:, :], initial=xst[:, g:g + 1],
                                op0=ALU.mult, op1=ALU.add),
                                reads=[d["kw1"], "mag", ("xst", g)], writes=[d["kzz"]])
                        else:
                            kb.op("dve", lambda e: e.scalar_tensor_tensor(
                                out=d["w1"][:, 0:128:8], in0=x0Tv[:, :, g], scalar=mag[:, g:g + 1], in1=d["w1"][:, 0:128:8], op0=ALU.mult, op1=ALU.add),
                                reads=[d["kw1"], "mag", "x0T"], writes=[d["kw1"]])
                            kb.op("dve", lambda e: e.tensor_scalar(out=rms_[:, :], in0=smask[:, :], scalar1=mag[:, g:g + 1], scalar2=None, op0=ALU.mult),
                                  reads=["smask", "mag"], writes=["rms_"])
                            kb.op("dve", lambda e: e.tensor_tensor_scan(
                                out=d["zz"][:, :], data0=rms_[:, :], data1=d["w1"][:, :], initial=0.0, op0=ALU.mult, op1=ALU.add),
                                reads=[d["kw1"], "rms_"], writes=[d["kzz"]])

                    def sE(k, item):
                        d = ctx(k, item)
                        kb.op("pe", lambda e: e.matmul(d["pC"], lhsT=Pm[:, :], rhs=d["zz"][:, :], start=True, stop=True),
                              reads=["Pm", d["kzz"]], writes=[d["kC"]], inc=True)
                        kb.op("pool", lambda e: e.tensor_tensor(out=d["x1"][:, :], in0=d["zz"][:, :], in1=cosv[:, d["sel"], d["g8"], :], op=ALU.mult),
                              reads=[d["kzz"], ("cosT", d["sel"])], writes=[d["kx1"]])

                    def sF(k, item):
                        d = ctx(k, item)
                        kb.op("dve", lambda e: e.tensor_tensor(out=d["x2"][:, :], in0=d["pC"], in1=sinv[:, d["sel"], d["g8"], :], op=ALU.mult),
                              reads=[d["kC"], ("sinT", d["sel"])], writes=[d["kx2"]])

                    def sG(k, item):
                        d = ctx(k, item)
                        kb.op("pool", lambda e: e.tensor_tensor(out=d["x1"][:, :], in0=d["x1"][:, :], in1=d["x2"][:, :], op=ALU.add),
                              reads=[d["kx1"], d["kx2"]], writes=[d["kx1"]])

                    def sH(k, item):
                        d = ctx(k, item)
                        g = d["g"]
                        kb.op("act", lambda e: e.copy(d["xb"][:, :], d["x1"][:, :]), reads=[d["kx1"]], writes=[d["kxb"]])
                        if not d["samp"]:
                            kb.op("act", lambda e: e.copy(xst[:, g:g + 1], d["x1"][:, 127:128]), reads=[d["kx1"]], writes=[("xst", g)])
                        else:
                            kb.op("act", lambda e: e.copy(xfsv[:, g, :], d["x1"][:, 7:128:8]), reads=[d["kx1"]], writes=[("xfs", g)])

                    def sI(k, item):
                        d = ctx(k, item)
                        c, g8, g, ts = d["c"], d["g8"], d["g"], d["ts"]
                        yb = 6 + (c % 2)
                        kb.op("pe", lambda e: e.matmul(PS[yb][:, 0:128], lhsT=Cpv[:, g, :], rhs=d["xb"][:, :], start=(g8 == 0), stop=(g8 == 7)),
                              reads=["Cpad", d["kxb"]], writes=["ps%d" % yb], inc=True)

                    def sJ(k, item):
                        d = ctx(k, item)
                        c, g8, ts = d["c"], d["g8"], d["ts"]
                        if g8 != 7:
                            return
                        yb = 6 + (c % 2)
                        yv, yt, ysg = yvL[c % 2], ytL[c % 2], ysgL[c % 2]
                        kyv, kyt, kys = ("yv", c % 2), ("yt", c % 2), ("ysg", c % 2)
                        kb.op("dve", lambda e: e.scalar_tensor_tensor(out=yv[:, :], in0=uTv[:, gc, ts], scalar=dcol[:, gc:gc + 1], in1=PS[yb][:, 0:128],
                                                                     op0=ALU.mult, op1=ALU.add),
                              reads=["ps%d" % yb, ("uT", gc), "dcol"], writes=[kyv])
                        kb.op("act", lambda e: e.activation(out=yt[:, :], in_=yv[:, :], func=AF.Square), reads=[kyv], writes=[kyt])
                        kb.op("pool", lambda e: e.tensor_scalar(out=yt[:, :], in0=yt[:, :], scalar1=0.044715, scalar2=1.0, op0=ALU.mult, op1=ALU.add), reads=[kyt], writes=[kyt])
                        kb.op("pool", lambda e: e.tensor_tensor(out=yt[:, :], in0=yt[:, :], in1=yv[:, :], op=ALU.mult), reads=[kyt, kyv], writes=[kyt])
                        kb.op("act", lambda e: e.activation(out=ysg[:, :], in_=yt[:, :], func=AF.Sigmoid, scale=float(2.0 * np.sqrt(2.0 / np.pi))), reads=[kyt], writes=[kys])
                        yob = yo[c % 2]
                        kb.op("pool", lambda e: e.tensor_tensor(out=yob[:, :], in0=ysg[:, :], in1=yv[:, :], op=ALU.mult), reads=[kys, kyv], writes=[("yo", c % 2)])
                        kb.dma(brscr[:, gc, ts], yob[:, :], reads=[("yo", c % 2)], writes=[("brscr5", c, gc)])

                    run_pipeline(items, [sA, sB, sC, sD, sE, sF, sG, sH, sI, sJ])
                kb.op("pe", lambda e: e.transpose(PS[7][0:64, 0:128], xst[:, :], identf[:, :]), reads=[("xst", g) for g in range(64)] + ["identf"], writes=["ps7"], inc=True)
                kb.op("dve", lambda e: e.tensor_copy(fin[:, :].rearrange("p (n r) -> p r n", r=2), PS[7][0:64, 0:128].rearrange("p (r n) -> p r n", r=2)),
                      reads=["ps7"], writes=["s5fin"])
                kb.dma(s5_p[l].rearrange("g n r -> g (n r)"), fin[:, :], reads=["s5fin"], writes=[("s5_p", l)])
                for s in range(NSS):
                    kb.op("pe", lambda e, s=s: e.transpose(PS[7][0:64, 0:128], xfsv[:, :, s], identf[:, :]), reads=[("xfs", g) for g in range(64)] + ["identf"], writes=["ps7"], inc=True)
                    f2 = fin2[s % 2]
                    kb.op("dve", lambda e, f2=f2: e.tensor_copy(f2[:, :].rearrange("p (n r) -> p r n", r=2), PS[7][0:64, 0:128].rearrange("p (r n) -> p r n", r=2)),
                          reads=["ps7"], writes=[("fin2", s % 2)])
                    kb.dma(s5_s[l, s].rearrange("g n r -> g (n r)"), f2[:, :], reads=[("fin2", s % 2)], writes=[("s5_s", l, s)])
                kb.barrier()

    for l in range(NL + 1):
        phase_x(l)
        if l < NL:
            if stages.get("ret", True):
                retention(l)
                stage_c(l, "ret", W["ret_w_o"][l], 8, 9248, "retln%d" % l, W["ret_ln_g"][l])
            if stages.get("s5", True):
                s5(l)
                stage_c(l, "s5", W["s5_w_glu"][l], 8, 9248 + 1024, glu=True)
            if stages.get("ssd", True):
                ssd(l)
            if stages.get("ssd", True) and stages.get("ssd_s2", True) and stages.get("ssd_c", True):
                stage_c(l, "ssd", W["ssd_w_o"][l], 16, 9248 + 2048, "ssdn%d" % l, W["ssd_norm"][l])

    kb.finish()
    print("instructions:", kb.ninst, {e: kb.ccnt[e] for e in kb.ccnt})
    return nc, es


_CONSTS = None
STAGES = {"layers": DEPTH, "ffn1": True, "ffn2": True, "ret": True, "ssd": True, "s5": True}
WNAMES = ["ffn1_norm", "ffn1_w_gu", "ffn1_w_down", "ffn2_norm", "ffn2_w_gu", "ffn2_w_down", "mix_norm", "w_in", "ret_ln_g", "ret_w_o", "w_out",
          "ssd_conv_w", "ssd_conv_b", "ssd_dt_bias", "ssd_a_log", "ssd_d", "ssd_norm", "ssd_w_o",
          "s5_a_re", "s5_a_im", "s5_log_dt", "s5_b_re", "s5_b_im", "s5_c_re", "s5_c_im", "s5_d", "s5_w_glu"]


def make_in_map(inp, c, consts):
    xp = np.asarray(inp["x_prompt"])
    xs = np.asarray(inp["x_sample"])
    x_core = np.concatenate([xp[c], xs[c * NSS:(c + 1) * NSS].reshape(NSS * DSEQ, D)], axis=0)
    m = {"x_in": np.ascontiguousarray(x_core)}
    m.update(consts)
    for k in WNAMES:
        m[k] = np.asarray(inp[k])
    m["final_norm"] = np.asarray(inp["final_norm"]).reshape(1, D)
    for k in ["state_ret", "state_ssm", "state_conv", "state_s5"]:
        m[k] = np.ascontiguousarray(np.asarray(inp[k])[:, c * NSS:(c + 1) * NSS])
    return m


def kernel(**inp):
    nc, es = build(STAGES)
    consts = host_consts()
    in_maps = [make_in_map(inp, c, consts) for c in range(NCORES)]
    res = run_bass_kernel_spmd(nc, in_maps, core_ids=list(range(NCORES)))
    es.close()
    R = res.results
    ys = [r["y_out"] for r in R]
    y_prompt = np.stack([y[:SEQ] for y in ys], axis=0)
    y_sample = np.concatenate([y[SEQ:].reshape(NSS, DSEQ, D) for y in ys], axis=0)
    def pstack(k):
        return np.stack([r[k] for r in R], axis=1)

    def scat(k):
        return np.concatenate([r[k] for r in R], axis=1)

    return (y_prompt, y_sample, pstack("ret_p"), scat("ret_s"), pstack("s5_p"), scat("s5_s"),
            pstack("ssm_p"), scat("ssm_s"), pstack("conv_p"), scat("conv_s"))
```

```python
import contextlib
import numpy as np
import concourse.bass as bass
import concourse.mybir as mybir
from concourse.bass_utils import run_bass_kernel_spmd

F32 = mybir.dt.float32
BF16 = mybir.dt.bfloat16
ALU = mybir.AluOpType
AF = mybir.ActivationFunctionType

NCORES = 8
D = 1024
KC = 8
DEPTH = 4
SEQ = 2048
NSS = 16
DSEQ = 8
T = SEQ + NSS * DSEQ
NCH = T // 128
FFN = 2816
EPS = 1e-6
IN_DIM = 12320
TILES = [(0, 512), (512, 512), (1024, 512), (1536, 512), (2048, 128)]
NDS = 40


class KB:
    def __init__(self, nc, es):
        self.nc = nc
        self.E = {"pe": nc.tensor, "act": nc.scalar, "dve": nc.vector, "pool": nc.gpsimd, "sp": nc.sync}
        self.csem = {e: es.enter_context(nc.semaphore("s_" + e)) for e in ["pe", "act", "dve", "pool"]}
        self.ccnt = {e: 0 for e in self.csem}
        self.dsem = [es.enter_context(nc.semaphore("d%d" % i)) for i in range(NDS)]
        self.dcnt = [0] * NDS
        self.dnext = 0
        self.waited = {e: {} for e in self.E}
        self.writer = {}
        self.readers = {}
        self.ninst = 0

    def _wait(self, eng, tok):
        semid, sem, val, src = tok
        if self.waited[eng].get(semid, 0) >= val:
            return
        self.E[eng].wait_ge(sem, val)
        self.waited[eng][semid] = val

    def _sync(self, eng, reads, writes, is_dma=False):
        for k in reads:
            w = self.writer.get(k)
            if w is not None:
                if (not is_dma) and w[3] == eng and eng == "pe":
                    continue
                self._wait(eng, w)
        for k in writes:
            w = self.writer.get(k)
            if w is not None:
                if is_dma or w[3] != eng:
                    self._wait(eng, w)
            for r in self.readers.get(k, {}).values():
                if is_dma or r[3] != eng:
                    self._wait(eng, r)

    def _record(self, tok, reads, writes):
        for k in reads:
            self.readers.setdefault(k, {})[tok[0]] = tok
        for k in writes:
            self.writer[k] = tok
            self.readers[k] = {}

    def op(self, eng, fn, reads=(), writes=(), inc=True):
        self._sync(eng, reads, writes)
        ins = fn(self.E[eng])
        self.ninst += 1
        if inc:
            self.ccnt[eng] += 1
            ins.then_inc(self.csem[eng], 1)
            tok = (eng, self.csem[eng], self.ccnt[eng], eng)
        else:
            tok = (eng, self.csem[eng], self.ccnt[eng] + 1, eng)
        self._record(tok, reads, writes)
        return tok

    def dma(self, out, in_, reads=(), writes=(), q="sp"):
        self._sync(q, reads, writes, is_dma=True)
        i = self.dnext
        self.dnext = (i + 1) % NDS
        sid = "d%d" % i
        if self.dcnt[i] > 0:
            self._wait(q, (sid, self.dsem[i], self.dcnt[i], "dma"))
        self.dcnt[i] += 16
        self.E[q].dma_start(out=out, in_=in_).then_inc(self.dsem[i], 16)
        self.ninst += 1
        tok = (sid, self.dsem[i], self.dcnt[i], "dma")
        self._record(tok, reads, writes)
        return tok

    def finish(self):
        for i in range(NDS):
            if self.dcnt[i] > 0:
                self._wait("sp", ("d%d" % i, self.dsem[i], self.dcnt[i], "dma"))
        for e in ["pe", "act", "dve", "pool"]:
            if self.ccnt[e] > 0:
                self._wait("sp", (e, self.csem[e], self.ccnt[e], e))

    def barrier(self):
        toks = []
        for e in ["pe", "act", "dve", "pool"]:
            if self.ccnt[e] > 0:
                toks.append((e, self.csem[e], self.ccnt[e], e))
        for i in range(NDS):
            if self.dcnt[i] > 0:
                toks.append(("d%d" % i, self.dsem[i], self.dcnt[i], "dma"))
        for eng in ["pe", "act", "dve", "pool", "sp"]:
            for t in toks:
                if t[3] == eng:
                    continue
                self._wait(eng, t)
        self.writer.clear()
        self.readers.clear()


RET_HEADS = 4
GAM = [1.0 - 2.0 ** (-5.0 - h) for h in range(RET_HEADS)]


def host_consts():
    c = {}
    c["ident_f"] = np.eye(128, dtype=np.float32)
    pos = np.concatenate([np.arange(SEQ), np.tile(16384 + np.arange(DSEQ), NSS)]).astype(np.float64)
    inv = 10000.0 ** (-np.arange(64, dtype=np.float64) / 64.0)
    ang = (pos.astype(np.float32)[None, :] * inv.astype(np.float32)[:, None]).astype(np.float32).astype(np.float64)
    cos = np.concatenate([np.cos(ang), np.cos(ang)], axis=0)
    sinS = np.concatenate([-np.sin(ang), np.sin(ang)], axis=0)
    sc = 128.0 ** -0.5
    c["tabqk"] = np.stack([cos, sinS, cos * sc, sinS * sc], axis=1).astype(np.float32)
    idx = np.arange(128)
    qdec = np.zeros((128, 4, 640), np.float32)
    maskT = np.zeros((128, 2, 4, 128), np.float32)
    kdec = np.zeros((128, 2, 4), np.float32)
    sj = idx % 8
    seqj = idx // 8
    for h in range(4):
        g = GAM[h]
        qdec[:, h, 0:512] = np.tile(g ** (idx + 1.0), 4)[None, :]
        qdec[:, h, 512:640] = (g ** (sj + 1.0))[None, :]
        maskT[:, 0, h, :] = (g ** (-(idx[:, None] + 1.0))) * (idx[None, :] >= idx[:, None])
        maskT[:, 1, h, :] = (g ** (-(sj[:, None] + 1.0))) * ((sj[None, :] >= sj[:, None]) & (seqj[None, :] == seqj[:, None]))
        kdec[:, 0, h] = g ** (127.0 - idx)
        kdec[:, 1, h] = g ** (7.0 - sj)
    c["qdec"] = qdec
    c["maskT"] = maskT
    c["kdec"] = kdec
    c["rowmask"] = (seqj[:, None] == np.arange(16)[None, :]).astype(np.float32)
    same = (seqj[:, None] == seqj[None, :])
    le = (idx[:, None] <= idx[None, :])
    c["triu"] = np.stack([le, le & same], axis=1).astype(np.float32)
    c["negm"] = np.stack([np.where(le, 0.0, -30000.0), np.where(le & same, 0.0, -30000.0)], axis=1).astype(np.float32)
    c["ss"] = np.stack([np.ones((128, 128)), same], axis=1).astype(np.float32)
    tp = np.stack([idx + 1.0, sj + 1.0], axis=0)
    c["tpos"] = np.repeat(tp[None, :, :], 128, axis=0).astype(np.float32)
    c["smask"] = np.repeat((sj != 0)[None, :], 128, axis=0).astype(np.float32)
    pm = np.zeros((128, 128), np.float32)
    for m_ in range(64):
        pm[m_ + 64, m_] = -1.0
        pm[m_, m_ + 64] = 1.0
    c["Pm"] = pm
    c["gmask"] = ((idx[:, None] // 16) == np.arange(8)[None, :]).astype(np.float32)
    c["cmask"] = np.repeat(((idx[None, :] // 16) == np.arange(8)[:, None])[None, :, :], 128, axis=0).astype(np.float32)
    c["rm"] = np.repeat((seqj[:, None] == np.arange(16)[None, :])[:, :, None], 128, axis=2).astype(np.float32)
    return c


def build(stages):
    nc = bass.Bass("TRN2", target_bir_lowering=False)
    es = contextlib.ExitStack()
    es.enter_context(nc.allow_non_contiguous_dma(reason="small param / layout loads"))
    try:
        es.enter_context(nc.allow_low_precision(reason="bf16 matmul operands by design"))
    except Exception:
        pass
    NL = stages.get("layers", DEPTH)

    def din(name, shape, dt=F32):
        return nc.dram_tensor(name, list(shape), dt, kind="ExternalInput").ap()

    def dout(name, shape, dt=F32):
        return nc.dram_tensor(name, list(shape), dt, kind="ExternalOutput").ap()

    def dscr(name, shape, dt=F32):
        return nc.dram_tensor(name, list(shape), dt, kind="Internal").ap()

    x_in = din("x_in", [T, D])
    CST = {"ident_f": din("ident_f", [128, 128]), "tabqk": din("tabqk", [128, 4, T]), "qdec": din("qdec", [128, 4, 640]),
           "maskT": din("maskT", [128, 2, 4, 128]), "kdec": din("kdec", [128, 2, 4]), "rowmask": din("rowmask", [128, 16]),
           "triu": din("triu", [128, 2, 128]), "negm": din("negm", [128, 2, 128]), "ss": din("ss", [128, 2, 128]), "rm": din("rm", [128, 16, 128]),
           "tpos": din("tpos", [128, 2, 128]), "smask": din("smask", [128, 128]), "Pm": din("Pm", [128, 128]),
           "gmask": din("gmask", [128, 8]), "cmask": din("cmask", [128, 8, 128])}
    W = {}
    for nm, shp in [("ffn1_norm", [DEPTH, D]), ("ffn1_w_gu", [DEPTH, D, 2 * FFN]), ("ffn1_w_down", [DEPTH, FFN, D]),
                    ("ffn2_norm", [DEPTH, D]), ("ffn2_w_gu", [DEPTH, D, 2 * FFN]), ("ffn2_w_down", [DEPTH, FFN, D]),
                    ("final_norm", [1, D]), ("mix_norm", [DEPTH, D]), ("w_in", [DEPTH, D, IN_DIM]),
                    ("ret_ln_g", [DEPTH, D]), ("ret_w_o", [DEPTH, D, D]), ("w_out", [DEPTH, D, D]),
                    ("state_ret", [DEPTH, NSS, 4, 128, 256]),
                    ("s5_a_re", [DEPTH, 64, 64]), ("s5_a_im", [DEPTH, 64, 64]), ("s5_log_dt", [DEPTH, 64]),
                    ("s5_b_re", [DEPTH, 64, 64, 16]), ("s5_b_im", [DEPTH, 64, 64, 16]), ("s5_c_re", [DEPTH, 64, 16, 64]),
                    ("s5_c_im", [DEPTH, 64, 16, 64]), ("s5_d", [DEPTH, D]), ("s5_w_glu", [DEPTH, D, 2 * D]),
                    ("state_s5", [DEPTH, NSS, 64, 64, 2]),
                    ("ssd_conv_w", [DEPTH, 4, 3072]), ("ssd_conv_b", [DEPTH, 3072]), ("ssd_dt_bias", [DEPTH, 32]),
                    ("ssd_a_log", [DEPTH, 32]), ("ssd_d", [DEPTH, 32]), ("ssd_norm", [DEPTH, 2048]), ("ssd_w_o", [DEPTH, 2048, D]),
                    ("state_ssm", [DEPTH, NSS, 32, 64, 128]), ("state_conv", [DEPTH, NSS, 3, 3072])]:
        W[nm] = din(nm, shp)
    y_out = dout("y_out", [T, D])
    ret_p = dout("ret_p", [DEPTH, 4, 128, 256])
    ret_s = dout("ret_s", [DEPTH, NSS, 4, 128, 256])
    s5_p = dout("s5_p", [DEPTH, 64, 64, 2])
    s5_s = dout("s5_s", [DEPTH, NSS, 64, 64, 2])
    ssm_p = dout("ssm_p", [DEPTH, 32, 64, 128])
    ssm_s = dout("ssm_s", [DEPTH, NSS, 32, 64, 128])
    conv_p = dout("conv_p", [DEPTH, 3, 3072])
    conv_s = dout("conv_s", [DEPTH, NSS, 3, 3072])
    xsb_scr = dscr("xsb_scr", [T, 2560], BF16)
    bc_scr = dscr("bc_scr", [128, 8, T], BF16)
    xscr = dscr("xscr", [128, KC, T])
    brscr = dscr("brscr", [128, 16, T], BF16)

    kb = KB(nc, es)

    uniq = [0]

    def sbp(stack, name, shape, dt):
        uniq[0] += 1
        return stack.enter_context(nc.sbuf_tensor("sb%d_%s" % (uniq[0], name), list(shape), dt))

    hT = sbp(es, "hT", [128, KC * T], BF16)
    hTv = hT[:, :].rearrange("p (k t) -> p k t", k=KC)
    identf = sbp(es, "identf", [128, 128], F32)
    identb = sbp(es, "identb", [128, 128], BF16)
    ones_bf = sbp(es, "ones_bf", [128, 128], BF16)
    gcols = sbp(es, "gcols", [128, 24 * 16], F32)
    gcv = gcols[:, :].rearrange("p (s k) -> p s k", k=16)
    sqb = sbp(es, "sqb", [128, KC * 512], BF16)
    sqv = sqb[:, :].rearrange("p (k t) -> p k t", k=KC)
    sdb = sbp(es, "sdb", [128, 512], F32)
    rstd = sbp(es, "rstd", [128, 512], F32)
    wst = [sbp(es, "wst%d" % i, [128, 2048], F32) for i in range(2)]
    wbf = [sbp(es, "wbf%d" % i, [128, 2048], BF16) for i in range(2)]
    PS = [es.enter_context(nc.psum_tensor("ps%d" % i, [128, 512], F32)) for i in range(8)]

    kb.dma(identf[:, :], CST["ident_f"][:, :], writes=["identf"])
    kb.op("dve", lambda e: e.tensor_copy(identb[:, :], identf[:, :]), reads=["identf"], writes=["identb"])
    kb.op("dve", lambda e: e.memset(ones_bf[:, :], 1.0), writes=["ones_bf"])

    gslot = {}

    def load_gain(name, ap_row, nk=KC):
        s = len(gslot) % 24
        gslot[name] = s
        kb.dma(gcv[:, s, 0:nk], ap_row.rearrange("(k p) -> p k", p=128), writes=[("g", s)])
        return s

    def ck(name, t0, n):
        return [(name, c) for c in range(t0 // 128, (t0 + n) // 128)]

    wslot = [0]

    def load_w(pieces, kcn, ncols, dst=None, dstkey=None, rowscale=None):
        s = wslot[0] % len(wst)
        wslot[0] = (s + 1) % len(wst)
        st = wst[s][:, 0:kcn * ncols].rearrange("p (k n) -> p k n", k=kcn)
        for (src, off) in pieces:
            wdt = src.shape[1]
            kb.dma(st[:, :, off:off + wdt], src.rearrange("(k p) n -> p k n", p=128), writes=[("wst", s)])
        if dst is None:
            bf = wbf[s][:, 0:kcn * ncols].rearrange("p (k n) -> p k n", k=kcn)
            key = ("wbf", s)
        else:
            bf = dst
            key = dstkey
        if rowscale is None:
            kb.op("pool", lambda e: e.tensor_copy(bf, st), reads=[("wst", s)], writes=[key])
        else:
            for k in range(kcn):
                kb.op("pool", lambda e, k=k: e.tensor_scalar(out=bf[:, k, :], in0=st[:, k, :], scalar1=gcv[:, rowscale, k:k + 1],
                                                           scalar2=None, op0=ALU.mult),
                      reads=[("wst", s), ("g", rowscale)], writes=[key])
        return bf, key

    def rmsnorm(xTv, gs, dst_fn, dst_keys_fn):
        for (t0, n) in TILES:
            for kc in range(KC):
                kb.op("act", lambda e, kc=kc, t0=t0, n=n: e.activation(
                    out=sqv[:, kc, 0:n], in_=xTv[:, kc, t0:t0 + n], func=AF.Square),
                    reads=ck("x", t0, n), writes=[("sq", kc)])
            for kc in range(KC):
                kb.op("pe", lambda e, kc=kc, n=n: e.matmul(
                    PS[6][:, 0:n], lhsT=ones_bf[:, :], rhs=sqv[:, kc, 0:n], start=(kc == 0), stop=(kc == KC - 1)),
                    reads=["ones_bf", ("sq", kc)], writes=["ps6"], inc=(kc == KC - 1))
            kb.op("act", lambda e, n=n: e.activation(
                out=sdb[:, 0:n], in_=PS[6][:, 0:n], func=AF.Sqrt, scale=1.0 / D, bias=EPS),
                reads=["ps6"], writes=["sdb"])
            kb.op("dve", lambda e, n=n: e.reciprocal(rstd[:, 0:n], sdb[:, 0:n]), reads=["sdb"], writes=["rstd"])
            for kc in range(KC):
                kb.op("dve", lambda e, kc=kc, t0=t0, n=n: e.scalar_tensor_tensor(
                    out=dst_fn(kc, t0, n), in0=xTv[:, kc, t0:t0 + n], scalar=gcv[:, gs, kc:kc + 1],
                    in1=rstd[:, 0:n], op0=ALU.mult, op1=ALU.mult),
                    reads=ck("x", t0, n) + ["rstd", ("g", gs)], writes=dst_keys_fn(kc, t0, n))

    def norm_to_h(xTv, gs):
        rmsnorm(xTv, gs, lambda kc, t0, n: hTv[:, kc, t0:t0 + n], lambda kc, t0, n: ck("h", t0, n))

    def ffn(P, xTv, l, pre):
        actb = P["actb"]
        sgb = P["sgb"]
        gs = load_gain("%s_norm%d" % (pre, l), W[pre + "_norm"][l])
        norm_to_h(xTv, gs)
        wgu = W[pre + "_w_gu"][l]
        wdn = W[pre + "_w_down"][l]
        NHG = FFN // 256
        gu_bank = 0
        dn_bank = 0
        for hg in range(NHG):
            ab = actb[hg % 2]
            abv = ab[:, :].rearrange("p (b t) -> p b t", b=2)
            kab = ("actb", hg % 2)
            wg, kwg = load_w([(wgu[:, hg * 256:(hg + 1) * 256], 0)], KC, 256)
            wu, kwu = load_w([(wgu[:, FFN + hg * 256:FFN + (hg + 1) * 256], 0)], KC, 256)
            for (t0, n) in TILES:
                for blk in range(2):
                    bg = gu_bank % 4
                    bu = (gu_bank + 1) % 4
                    gu_bank += 2
                    for kc in range(KC):
                        kb.op("pe", lambda e, kc=kc, bg=bg, blk=blk, t0=t0, n=n, wg=wg: e.matmul(
                            PS[bg][:, 0:n], lhsT=wg[:, kc, blk * 128:(blk + 1) * 128], rhs=hTv[:, kc, t0:t0 + n],
                            start=(kc == 0), stop=(kc == KC - 1)),
                            reads=[kwg] + ck("h", t0, n), writes=["ps%d" % bg], inc=(kc == KC - 1))
                    for kc in range(KC):
                        kb.op("pe", lambda e, kc=kc, bu=bu, blk=blk, t0=t0, n=n, wu=wu: e.matmul(
                            PS[bu][:, 0:n], lhsT=wu[:, kc, blk * 128:(blk + 1) * 128], rhs=hTv[:, kc, t0:t0 + n],
                            start=(kc == 0), stop=(kc == KC - 1)),
                            reads=[kwu] + ck("h", t0, n), writes=["ps%d" % bu], inc=(kc == KC - 1))
                    sg = sgb[(gu_bank // 2) % 2]
                    ksg = ("sgb", (gu_bank // 2) % 2)
                    kb.op("act", lambda e, bg=bg, n=n, sg=sg: e.activation(
                        out=sg[:, 0:n], in_=PS[bg][:, 0:n], func=AF.Silu),
                        reads=["ps%d" % bg], writes=[ksg])
                    kb.op("dve", lambda e, bu=bu, n=n, sg=sg, blk=blk, t0=t0, abv=abv: e.tensor_tensor(
                        out=abv[:, blk, t0:t0 + n], in0=PS[bu][:, 0:n], in1=sg[:, 0:n], op=ALU.mult),
                        reads=["ps%d" % bu, ksg], writes=[kab])
            wd, kwd = load_w([(wdn[hg * 256:(hg + 1) * 256, :], 0)], 2, D)
            for (t0, n) in TILES:
                for oc in range(KC):
                    bo = 4 + (dn_bank % 2)
                    dn_bank += 1
                    for blk in range(2):
                        kb.op("pe", lambda e, blk=blk, bo=bo, oc=oc, t0=t0, n=n, wd=wd, abv=abv: e.matmul(
                            PS[bo][:, 0:n], lhsT=wd[:, blk, oc * 128:(oc + 1) * 128], rhs=abv[:, blk, t0:t0 + n],
                            start=(blk == 0), stop=(blk == 1)),
                            reads=[kwd, kab], writes=["ps%d" % bo], inc=(blk == 1))
                    kb.op("dve", lambda e, bo=bo, oc=oc, t0=t0, n=n: e.scalar_tensor_tensor(
                        out=xTv[:, oc, t0:t0 + n], in0=PS[bo][:, 0:n], scalar=0.5, in1=xTv[:, oc, t0:t0 + n],
                        op0=ALU.mult, op1=ALU.add),
                        reads=["ps%d" % bo] + ck("x", t0, n), writes=ck("x", t0, n))

    def run_pipeline(items, stage_fns):
        n = len(items)
        S = len(stage_fns)
        for step in range(n + S - 1):
            for si, fn in enumerate(stage_fns):
                k = step - si
                if 0 <= k < n:
                    fn(k, items[k])

    def phase_x(l):
        with contextlib.ExitStack() as pes:
            xT = sbp(pes, "xT", [128, KC * T], F32)
            xTv = xT[:, :].rearrange("p (k t) -> p k t", k=KC)
            P = {"actb": [sbp(pes, "actb%d" % i, [128, 2 * T], BF16) for i in range(2)],
                 "sgb": [sbp(pes, "sgb%d" % i, [128, 512], BF16) for i in range(2)]}
            iobuf = [sbp(pes, "iobuf%d" % i, [128, D], F32) for i in range(2)]
            for i in range(2, 4):
                wst.append(sbp(pes, "wstx%d" % i, [128, 2048], F32))
                wbf.append(sbp(pes, "wbfx%d" % i, [128, 2048], BF16))
            if l == 0:
                for c in range(NCH):
                    io = iobuf[c % 2]
                    kio = "io%d" % (c % 2)
                    kb.dma(io[:, :], x_in[c * 128:(c + 1) * 128, :], writes=[kio])
                    for half in range(2):
                        bank = PS[6 + half]
                        kbk = "ps%d" % (6 + half)
                        for j in range(4):
                            kc = half * 4 + j
                            kb.op("pe", lambda e, j=j, kc=kc, bank=bank, io=io: e.transpose(
                                bank[:, j * 128:(j + 1) * 128], io[:, kc * 128:(kc + 1) * 128], identf[:, :]),
                                reads=[kio, "identf"], writes=[kbk], inc=(j == 3))
                        if half == 0:
                            kb.op("act", lambda e, bank=bank, c=c: e.copy(
                                xTv[:, 0:4, c * 128:(c + 1) * 128], bank[:, :].rearrange("p (k t) -> p k t", k=4)),
                                reads=[kbk], writes=[("x", c)])
                        else:
                            kb.op("dve", lambda e, bank=bank, c=c: e.tensor_copy(
                                xTv[:, 4:8, c * 128:(c + 1) * 128], bank[:, :].rearrange("p (k t) -> p k t", k=4)),
                                reads=[kbk], writes=[("x", c)])
            else:
                for (t0, n) in TILES:
                    kb.dma(xTv[:, :, t0:t0 + n], xscr[:, :, t0:t0 + n], reads=[("xscr", t0)], writes=ck("x", t0, n))
                if stages.get("ffn2", True):
                    ffn(P, xTv, l - 1, "ffn2")
            if l < NL:
                if stages.get("ffn1", True):
                    ffn(P, xTv, l, "ffn1")
                gs = load_gain("mix%d" % l, W["mix_norm"][l])
                norm_to_h(xTv, gs)
                for (t0, n) in TILES:
                    kb.dma(xscr[:, :, t0:t0 + n], xTv[:, :, t0:t0 + n], reads=ck("x", t0, n), writes=[("xscr", t0)])
            else:
                gs = load_gain("final", W["final_norm"][0])
                finb = sbp(pes, "finb", [128, 4096], F32)
                fin = finb[:, :].rearrange("p (k t) -> p k t", k=KC)
                for (t0, n) in TILES:
                    rm_tiles = [(t0, n)]
                    for kc in range(KC):
                        kb.op("act", lambda e, kc=kc, t0=t0, n=n: e.activation(
                            out=sqv[:, kc, 0:n], in_=xTv[:, kc, t0:t0 + n], func=AF.Square),
                            reads=ck("x", t0, n), writes=[("sq", kc)])
                    for kc in range(KC):
                        kb.op("pe", lambda e, kc=kc, n=n: e.matmul(
                            PS[6][:, 0:n], lhsT=ones_bf[:, :], rhs=sqv[:, kc, 0:n], start=(kc == 0), stop=(kc == KC - 1)),
                            reads=["ones_bf", ("sq", kc)], writes=["ps6"], inc=(kc == KC - 1))
                    kb.op("act", lambda e, n=n: e.activation(
                        out=sdb[:, 0:n], in_=PS[6][:, 0:n], func=AF.Sqrt, scale=1.0 / D, bias=EPS),
                        reads=["ps6"], writes=["sdb"])
                    kb.op("dve", lambda e, n=n: e.reciprocal(rstd[:, 0:n], sdb[:, 0:n]), reads=["sdb"], writes=["rstd"])
                    for kc in range(KC):
                        kb.op("dve", lambda e, kc=kc, t0=t0, n=n: e.scalar_tensor_tensor(
                            out=fin[:, kc, 0:n], in0=xTv[:, kc, t0:t0 + n], scalar=gcv[:, gs, kc:kc + 1],
                            in1=rstd[:, 0:n], op0=ALU.mult, op1=ALU.mult),
                            reads=ck("x", t0, n) + ["rstd", ("g", gs)], writes=["fin"])
                    for c in range(n // 128):
                        io = iobuf[c % 2]
                        kio = "io%d" % (c % 2)
                        for half in range(2):
                            bank = PS[half]
                            kbk = "ps%d" % half
                            for j in range(4):
                                kc = half * 4 + j
                                kb.op("pe", lambda e, j=j, kc=kc, bank=bank, c=c: e.transpose(
                                    bank[:, j * 128:(j + 1) * 128], fin[:, kc, c * 128:(c + 1) * 128], identf[:, :]),
                                    reads=["fin", "identf"], writes=[kbk], inc=(j == 3))
                            if half == 0:
                                kb.op("act", lambda e, bank=bank, io=io: e.copy(io[:, 0:512], bank[:, :]),
                                      reads=[kbk], writes=[(kio, half)])
                            else:
                                kb.op("dve", lambda e, bank=bank, io=io: e.tensor_copy(io[:, 512:1024], bank[:, :]),
                                      reads=[kbk], writes=[(kio, half)])
                        kb.dma(y_out[t0 + c * 128:t0 + (c + 1) * 128, :], io[:, :], reads=[(kio, 0), (kio, 1)],
                               writes=[("yout", t0, c)])
            kb.barrier()
            del wst[2:]
            del wbf[2:]
            wslot[0] = 0

    def retention(l):
        win = W["w_in"][l]
        with contextlib.ExitStack() as pes:
            Wv = sbp(pes, "Wv", [128, KC * 1024], BF16)
            Wg = sbp(pes, "Wg", [128, KC * 1024], BF16)
            Wvv = Wv[:, :].rearrange("p (k n) -> p k n", k=KC)
            Wgv = Wg[:, :].rearrange("p (k n) -> p k n", k=KC)
            qd = sbp(pes, "qd", [128, 4 * 512], BF16)
            kT = sbp(pes, "kT", [128, 4 * 512], BF16)
            qdv = qd[:, :].rearrange("p (h t) -> p h t", h=4)
            kTv = kT[:, :].rearrange("p (h t) -> p h t", h=4)
            tab = [sbp(pes, "tab%d" % i, [128, 4 * 512], F32) for i in range(1)]
            qdec = sbp(pes, "qdec", [128, 4 * 640], F32)
            qdecv = qdec[:, :].rearrange("p (h t) -> p h t", h=4)
            maskT = sbp(pes, "maskT", [128, 2 * 512], F32)
            maskTv = maskT[:, :].rearrange("p (s n) -> p s n", s=2)
            kdec = sbp(pes, "kdec", [128, 8], F32)
            rowmask = sbp(pes, "rowmask", [128, 16], F32)
            tmpA = sbp(pes, "tmpA", [128, 512], F32)
            tmpB = sbp(pes, "tmpB", [128, 512], F32)
            tmpC = sbp(pes, "tmpC", [128, 512], F32)
            tmpD = sbp(pes, "tmpD", [128, 512], F32)
            v_tm = sbp(pes, "v_tm", [128, 1024], BF16)
            sg = sbp(pes, "sg", [128, 1024], BF16)
            kd_tm = sbp(pes, "kd_tm", [128, 512], BF16)
            kdm = sbp(pes, "kdm", [128, 512], BF16)
            sc = sbp(pes, "sc", [128, 512], BF16)
            S_f = sbp(pes, "S_f", [128, 1024], F32)
            S_b = sbp(pes, "S_b", [128, 1024], BF16)
            S0f = [sbp(pes, "S0f%d" % i, [128, 1024], F32) for i in range(2)]
            S0b = [sbp(pes, "S0b%d" % i, [128, 1024], BF16) for i in range(2)]
            Snew = [sbp(pes, "Snew%d" % i, [128, 1024], F32) for i in range(2)]
            qblk = sbp(pes, "qblk", [128, 4 * 2048], BF16)
            qblkv = qblk[:, :].rearrange("p (h s t) -> p h s t", h=4, s=16)
            bst = sbp(pes, "bst", [128, 4 * 6], F32)
            mv = sbp(pes, "mv", [128, 4 * 2], F32)
            mvv = mv[:, :].rearrange("p (h t) -> p h t", h=4)
            sdv = sbp(pes, "sdv", [128, 4], F32)
            rsv = sbp(pes, "rsv", [128, 4], F32)
            on = sbp(pes, "on", [128, 1024], BF16)
            og = sbp(pes, "og", [128, 1024], BF16)
            ogT = [sbp(pes, "ogT%d" % i, [128, 1024], BF16) for i in range(2)]

            kb.dma(qdecv, CST["qdec"], writes=["qdec"])
            kb.dma(maskT[:, :].rearrange("p (s h n) -> p s h n", s=2, h=4), CST["maskT"], writes=["maskT"])
            kb.dma(kdec[:, :].rearrange("p (s h) -> p s h", s=2), CST["kdec"], writes=["kdec"])
            kb.dma(rowmask[:, :], CST["rowmask"], writes=["rowmask"])
            kb.op("pool", lambda e: e.memset(qblk[:, :], 0.0), writes=["qblk"])
            kb.op("pool", lambda e: e.memset(S_f[:, :], 0.0), writes=["S_f"])
            kb.op("pool", lambda e: e.memset(S_b[:, :], 0.0), writes=["S_b"])
            for j in range(4):
                load_w([(win[:, 1024 + j * 256:1024 + (j + 1) * 256], 0)], KC, 256, dst=Wvv[:, :, j * 256:(j + 1) * 256], dstkey="Wv")
            for j in range(4):
                load_w([(win[:, 2048 + j * 256:2048 + (j + 1) * 256], 0)], KC, 256, dst=Wgv[:, :, j * 256:(j + 1) * 256], dstkey="Wg")

            for ti, (t0, n) in enumerate(TILES):
                tb = tab[0]
                tbv = tb[:, :].rearrange("p (s t) -> p s t", s=4)
                ktb = ("tab", 0)
                kb.dma(tbv[:, :, 0:n], CST["tabqk"][:, :, t0:t0 + n], writes=[ktb])
                samp = (t0 >= SEQ)
                sel = 1 if samp else 0
                qoff = 512 if samp else 0
                for h in range(4):
                    c0 = h * 128
                    k0 = 512 + h * 128
                    wq, kwq = load_w([(win[:, c0:c0 + 128], 0), (win[:, c0 + 64:c0 + 128], 128), (win[:, c0:c0 + 64], 192)], KC, 256)
                    wk, kwk = load_w([(win[:, k0:k0 + 128], 0), (win[:, k0 + 64:k0 + 128], 128), (win[:, k0:k0 + 64], 192)], KC, 256)
                    for b in range(4):
                        ww, kww = (wq, kwq) if b < 2 else (wk, kwk)
                        for kc in range(KC):
                            kb.op("pe", lambda e, kc=kc, b=b, ww=ww, t0=t0, n=n: e.matmul(
                                PS[b][:, 0:n], lhsT=ww[:, kc, (b % 2) * 128:(b % 2 + 1) * 128], rhs=hTv[:, kc, t0:t0 + n],
                                start=(kc == 0), stop=(kc == KC - 1)),
                                reads=[kww] + ck("h", t0, n), writes=["ps%d" % b], inc=(kc == KC - 1))
                    kb.op("dve", lambda e, n=n, tbv=tbv: e.tensor_tensor(out=tmpA[:, 0:n], in0=PS[0][:, 0:n], in1=tbv[:, 0, 0:n], op=ALU.mult),
                          reads=["ps0", ktb], writes=["tmpA"])
                    kb.op("dve", lambda e, n=n, tbv=tbv: e.tensor_tensor(out=tmpB[:, 0:n], in0=PS[1][:, 0:n], in1=tbv[:, 1, 0:n], op=ALU.mult),
                          reads=["ps1", ktb], writes=["tmpB"])
                    kb.op("pool", lambda e, n=n: e.tensor_tensor(out=tmpA[:, 0:n], in0=tmpA[:, 0:n], in1=tmpB[:, 0:n], op=ALU.add),
                          reads=["tmpA", "tmpB"], writes=["tmpA"])
                    kb.op("pool", lambda e, n=n, h=h, qoff=qoff: e.tensor_tensor(
                        out=qdv[:, h, 0:n], in0=tmpA[:, 0:n], in1=qdecv[:, h, qoff:qoff + n], op=ALU.mult),
                        reads=["tmpA", "qdec"], writes=[("qd", h)])
                    kb.op("dve", lambda e, n=n, tbv=tbv: e.tensor_tensor(out=tmpC[:, 0:n], in0=PS[2][:, 0:n], in1=tbv[:, 2, 0:n], op=ALU.mult),
                          reads=["ps2", ktb], writes=["tmpC"])
                    kb.op("dve", lambda e, n=n, tbv=tbv: e.tensor_tensor(out=tmpD[:, 0:n], in0=PS[3][:, 0:n], in1=tbv[:, 3, 0:n], op=ALU.mult),
                          reads=["ps3", ktb], writes=["tmpD"])
                    kb.op("pool", lambda e, n=n, h=h: e.tensor_tensor(out=kTv[:, h, 0:n], in0=tmpC[:, 0:n], in1=tmpD[:, 0:n], op=ALU.add),
                          reads=["tmpC", "tmpD"], writes=[("kT", h)])
                QD = [("qd", h) for h in range(4)]
                KT = [("kT", h) for h in range(4)]
                for ci in range(n // 128):
                    c = t0 // 128 + ci
                    cs = slice(ci * 128, (ci + 1) * 128)
                    ts = slice(t0 + ci * 128, t0 + (ci + 1) * 128)
                    for half in range(2):
                        for kc in range(KC):
                            kb.op("pe", lambda e, kc=kc, half=half, ts=ts: e.matmul(
                                PS[half][:, :], lhsT=hTv[:, kc, ts], rhs=Wvv[:, kc, half * 512:(half + 1) * 512],
                                start=(kc == 0), stop=(kc == KC - 1)),
                                reads=["Wv", ("h", c)], writes=["ps%d" % half], inc=(kc == KC - 1))
                        kb.op("act", lambda e, half=half: e.copy(v_tm[:, half * 512:(half + 1) * 512], PS[half][:, :]),
                              reads=["ps%d" % half], writes=[("v_tm", half)])
                    for half in range(2):
                        for kc in range(KC):
                            kb.op("pe", lambda e, kc=kc, half=half, ts=ts: e.matmul(
                                PS[2 + half][:, :], lhsT=hTv[:, kc, ts], rhs=Wgv[:, kc, half * 512:(half + 1) * 512],
                                start=(kc == 0), stop=(kc == KC - 1)),
                                reads=["Wg", ("h", c)], writes=["ps%d" % (2 + half)], inc=(kc == KC - 1))
                        kb.op("act", lambda e, half=half: e.activation(out=sg[:, half * 512:(half + 1) * 512], in_=PS[2 + half][:, :], func=AF.Silu),
                              reads=["ps%d" % (2 + half)], writes=[("sg", half)])
                    VT = [("v_tm", 0), ("v_tm", 1)]
                    p4 = PS[4][:, :].bitcast(BF16)
                    for h in range(4):
                        kb.op("pe", lambda e, h=h, cs=cs: e.transpose(p4[:, h * 128:(h + 1) * 128], kTv[:, h, cs], identb[:, :]),
                              reads=[("kT", h), "identb"], writes=["ps4"], inc=(h == 3))
                    for h in range(4):
                        kb.op("dve", lambda e, h=h, sel=sel: e.tensor_scalar(
                            out=kd_tm[:, h * 128:(h + 1) * 128], in0=p4[:, h * 128:(h + 1) * 128],
                            scalar1=kdec[:, sel * 4 + h:sel * 4 + h + 1], scalar2=None, op0=ALU.mult),
                            reads=["ps4", "kdec"], writes=["kd_tm"])
                    for h in range(4):
                        kb.op("pe", lambda e, h=h, cs=cs: e.matmul(PS[5][:, h * 128:(h + 1) * 128], lhsT=kTv[:, h, cs], rhs=qdv[:, h, cs],
                                                                 start=True, stop=True),
                              reads=[("kT", h), ("qd", h)], writes=["ps5"], inc=(h == 3))
                    kb.op("dve", lambda e, sel=sel: e.tensor_tensor(out=sc[:, :], in0=PS[5][:, :], in1=maskTv[:, sel, :], op=ALU.mult),
                          reads=["ps5", "maskT"], writes=["sc"])
                    if not samp:
                        for h in range(4):
                            ob = PS[6 + h // 2][:, (h % 2) * 256:(h % 2) * 256 + 256]
                            kb.op("pe", lambda e, h=h, ob=ob: e.matmul(ob, lhsT=sc[:, h * 128:(h + 1) * 128], rhs=v_tm[:, h * 256:(h + 1) * 256],
                                                                     start=True, stop=False),
                                  reads=["sc"] + VT, writes=["ps%d" % (6 + h // 2)], inc=False)
                            kb.op("pe", lambda e, h=h, ob=ob, cs=cs: e.matmul(ob, lhsT=qdv[:, h, cs], rhs=S_b[:, h * 256:(h + 1) * 256],
                                                                            start=False, stop=True),
                                  reads=[("qd", h), "S_b"], writes=["ps%d" % (6 + h // 2)], inc=True)
                        for h in range(4):
                            sbk = PS[h // 2][:, (h % 2) * 256:(h % 2) * 256 + 256]
                            kb.op("pe", lambda e, h=h, sbk=sbk: e.matmul(sbk, lhsT=kd_tm[:, h * 128:(h + 1) * 128], rhs=v_tm[:, h * 256:(h + 1) * 256],
                                                                       start=True, stop=True),
                                  reads=["kd_tm"] + VT, writes=["ps%d" % (h // 2)], inc=True)
                            kb.op("dve", lambda e, h=h, sbk=sbk: e.scalar_tensor_tensor(
                                out=S_f[:, h * 256:(h + 1) * 256], in0=S_f[:, h * 256:(h + 1) * 256], scalar=float(GAM[h] ** 128),
                                in1=sbk, op0=ALU.mult, op1=ALU.add),
                                reads=["ps%d" % (h // 2), "S_f"], writes=["S_f"])
                        kb.op("act", lambda e: e.copy(S_b[:, :], S_f[:, :]), reads=["S_f"], writes=["S_b"])
                        if c == SEQ // 128 - 1:
                            kb.dma(ret_p[l].rearrange("h d e -> d h e"), S_f[:, :].rearrange("p (h e) -> p h e", h=4),
                                   reads=["S_f"], writes=[("ret_p", l)])
                    else:
                        for h in range(4):
                            ob = PS[6 + h // 2][:, (h % 2) * 256:(h % 2) * 256 + 256]
                            for s in range(NSS):
                                kb.op("pool", lambda e, h=h, s=s: e.tensor_copy(qblkv[:, h, s, s * 8:(s + 1) * 8], qdv[:, h, s * 8:(s + 1) * 8]),
                                      reads=[("qd", h)], writes=["qblk"])
                            kb.op("pe", lambda e, h=h, ob=ob: e.matmul(ob, lhsT=sc[:, h * 128:(h + 1) * 128], rhs=v_tm[:, h * 256:(h + 1) * 256],
                                                                     start=True, stop=False),
                                  reads=["sc"] + VT, writes=["ps%d" % (6 + h // 2)], inc=False)
                            for s in range(NSS):
                                i2 = (h * NSS + s) % 2
                                s0f = S0f[i2]
                                s0b = S0b[i2]
                                sn = Snew[i2]
                                kb.dma(s0f[:, 0:256], W["state_ret"][l, s, h], writes=[("S0f", i2)])
                                kb.op("act", lambda e, s0f=s0f, s0b=s0b: e.copy(s0b[:, 0:256], s0f[:, 0:256]), reads=[("S0f", i2)], writes=[("S0b", i2)])
                                kb.op("pe", lambda e, h=h, ob=ob, s=s, s0b=s0b: e.matmul(
                                    ob, lhsT=qblkv[:, h, s, :], rhs=s0b[:, 0:256], start=False, stop=(s == NSS - 1)),
                                    reads=["qblk", ("S0b", i2)], writes=["ps%d" % (6 + h // 2)], inc=True)
                                kb.op("dve", lambda e, s=s, h=h: e.tensor_scalar(out=kdm[:, 0:128], in0=kd_tm[:, h * 128:(h + 1) * 128],
                                                                               scalar1=rowmask[:, s:s + 1], scalar2=None, op0=ALU.mult),
                                      reads=["kd_tm", "rowmask"], writes=["kdm"])
                                sbk = PS[i2][:, 0:256]
                                kb.op("pe", lambda e, h=h, sbk=sbk: e.matmul(sbk, lhsT=kdm[:, 0:128], rhs=v_tm[:, h * 256:(h + 1) * 256],
                                                                           start=True, stop=True),
                                      reads=["kdm"] + VT, writes=["ps%d" % i2], inc=True)
                                kb.op("dve", lambda e, h=h, sbk=sbk, s0f=s0f, sn=sn: e.scalar_tensor_tensor(
                                    out=sn[:, 0:256], in0=s0f[:, 0:256], scalar=float(GAM[h] ** 8),
                                    in1=sbk, op0=ALU.mult, op1=ALU.add),
                                    reads=["ps%d" % i2, ("S0f", i2)], writes=[("Snew", i2)])
                                kb.dma(ret_s[l, s, h], sn[:, 0:256], reads=[("Snew", i2)], writes=[("ret_s", l, s, h)])
                    for h in range(4):
                        ob = PS[6 + h // 2][:, (h % 2) * 256:(h % 2) * 256 + 256]
                        kb.op("dve", lambda e, h=h, ob=ob: e.bn_stats(bst[:, h * 6:(h + 1) * 6], ob), reads=["ps%d" % (6 + h // 2)], writes=[("bst", h)])
                        kb.op("dve", lambda e, h=h: e.bn_aggr(mv[:, h * 2:(h + 1) * 2], bst[:, h * 6:(h + 1) * 6]), reads=[("bst", h)], writes=[("mv", h)])
                    MV = [("mv", h) for h in range(4)]
                    kb.op("act", lambda e: e.activation(out=sdv[:, :], in_=mvv[:, :, 1], func=AF.Sqrt, bias=EPS, scale=1.0), reads=MV, writes=["sdv"])
                    kb.op("dve", lambda e: e.reciprocal(rsv[:, :], sdv[:, :]), reads=["sdv"], writes=["rsv"])
                    for h in range(4):
                        ob = PS[6 + h // 2][:, (h % 2) * 256:(h % 2) * 256 + 256]
                        kb.op("dve", lambda e, h=h, ob=ob: e.tensor_scalar(
                            out=on[:, h * 256:(h + 1) * 256], in0=ob, scalar1=mvv[:, h, 0:1], scalar2=rsv[:, h:h + 1],
                            op0=ALU.subtract, op1=ALU.mult),
                            reads=["ps%d" % (6 + h // 2), "rsv"] + MV, writes=["on"])
                    kb.op("pool", lambda e: e.tensor_tensor(out=og[:, :], in0=on[:, :], in1=sg[:, :], op=ALU.mult),
                          reads=["on", ("sg", 0), ("sg", 1)], writes=["og"])
                    ogt = ogT[c % 2]
                    for kc in range(KC):
                        kb.op("pe", lambda e, kc=kc: e.transpose(p4[:, kc * 128:(kc + 1) * 128], og[:, kc * 128:(kc + 1) * 128], identb[:, :]),
                              reads=["og", "identb"], writes=["ps4"], inc=(kc == KC - 1))
                    kb.op("act", lambda e, ogt=ogt: e.copy(ogt[:, :], p4[:, :]), reads=["ps4"], writes=[("ogT", c % 2)])
                    kb.dma(brscr[:, 0:KC, ts], ogt[:, :].rearrange("p (k t) -> p k t", k=KC), reads=[("ogT", c % 2)], writes=[("brscr", c)])
            kb.barrier()

    def stage_c(l, branch, wo_ap, kcb, gate_col0, rowscale_name=None, rowscale_ap=None, glu=False):
        win = W["w_in"][l]
        with contextlib.ExitStack() as pes:
            nco = 2048 if glu else 1024
            Wo = sbp(pes, "Wo", [128, kcb * nco], BF16)
            Wov = Wo[:, :].rearrange("p (k n) -> p k n", k=kcb)
            tglu = [sbp(pes, "tglu%d" % i, [128, 512], F32) for i in range(2)] if glu else None
            Wgt = sbp(pes, "Wgt", [128, KC * 1024], BF16)
            Wgtv = Wgt[:, :].rearrange("p (k n) -> p k n", k=KC)
            Wout = sbp(pes, "Wout", [128, KC * 1024], BF16)
            Woutv = Wout[:, :].rearrange("p (k n) -> p k n", k=KC)
            nbr = 2 if kcb == 8 else 1
            brt = [sbp(pes, "brt%d" % i, [128, kcb * 512], BF16) for i in range(nbr)]
            xt = [sbp(pes, "xt%d" % i, [128, KC * 512], F32) for i in range(2)]
            mt = sbp(pes, "mt", [128, KC * 512], BF16)
            mtv = mt[:, :].rearrange("p (k t) -> p k t", k=KC)
            sig = [sbp(pes, "sig%d" % i, [128, 512], F32) for i in range(2)]
            rs = None
            if rowscale_ap is not None:
                for k0 in range(0, kcb, KC):
                    pass
                rs = load_gain(rowscale_name, rowscale_ap, kcb)
            for k0 in range(0, kcb, 2):
                for j in range(nco // 256):
                    if rs is None:
                        load_w([(wo_ap[k0 * 128:(k0 + 2) * 128, j * 256:(j + 1) * 256], 0)], 2, 256,
                               dst=Wov[:, k0:k0 + 2, j * 256:(j + 1) * 256], dstkey="Wo")
                    else:
                        s = wslot[0] % len(wst)
                        wslot[0] = (s + 1) % len(wst)
                        st = wst[s][:, 0:512].rearrange("p (k n) -> p k n", k=2)
                        kb.dma(st, wo_ap[k0 * 128:(k0 + 2) * 128, j * 256:(j + 1) * 256].rearrange("(k p) n -> p k n", p=128), writes=[("wst", s)])
                        for kk in range(2):
                            kb.op("pool", lambda e, kk=kk, st=st, k0=k0, j=j: e.tensor_scalar(
                                out=Wov[:, k0 + kk, j * 256:(j + 1) * 256], in0=st[:, kk, :], scalar1=gcv[:, rs, k0 + kk:k0 + kk + 1],
                                scalar2=None, op0=ALU.mult),
                                reads=[("wst", s), ("g", rs)], writes=["Wo"])
            for j in range(4):
                load_w([(win[:, gate_col0 + j * 256:gate_col0 + (j + 1) * 256], 0)], KC, 256, dst=Wgtv[:, :, j * 256:(j + 1) * 256], dstkey="Wgt")
            for j in range(4):
                load_w([(W["w_out"][l][:, j * 256:(j + 1) * 256], 0)], KC, 256, dst=Woutv[:, :, j * 256:(j + 1) * 256], dstkey="Wout")
            bank = 0
            for ti, (t0, n) in enumerate(TILES):
                b_t = brt[ti % nbr]
                b_v = b_t[:, :].rearrange("p (k t) -> p k t", k=kcb)
                x_t = xt[ti % 2]
                x_v = x_t[:, :].rearrange("p (k t) -> p k t", k=KC)
                kb.dma(b_v[:, :, 0:n], brscr[:, 0:kcb, t0:t0 + n], reads=ck("brscr", t0, n), writes=[("brt", ti % nbr)])
                kb.dma(x_v[:, :, 0:n], xscr[:, :, t0:t0 + n], reads=[("xscr", t0)], writes=[("xt", ti % 2)])
                for oc in range(KC):
                    by = bank % 4
                    bg = (bank + 1) % 4
                    bank += 2
                    for kc in range(kcb):
                        kb.op("pe", lambda e, kc=kc, oc=oc, by=by, b_v=b_v, n=n: e.matmul(
                            PS[by][:, 0:n], lhsT=Wov[:, kc, oc * 128:(oc + 1) * 128], rhs=b_v[:, kc, 0:n], start=(kc == 0), stop=(kc == kcb - 1)),
                            reads=["Wo", ("brt", ti % nbr)], writes=["ps%d" % by], inc=(kc == kcb - 1))
                    for kc in range(KC):
                        kb.op("pe", lambda e, kc=kc, oc=oc, bg=bg, t0=t0, n=n: e.matmul(
                            PS[bg][:, 0:n], lhsT=Wgtv[:, kc, oc * 128:(oc + 1) * 128], rhs=hTv[:, kc, t0:t0 + n], start=(kc == 0), stop=(kc == KC - 1)),
                            reads=["Wgt"] + ck("h", t0, n), writes=["ps%d" % bg], inc=(kc == KC - 1))
                    sgm = sig[oc % 2]
                    if glu:
                        b2 = 6 + (oc % 2)
                        for kc in range(kcb):
                            kb.op("pe", lambda e, kc=kc, oc=oc, b2=b2, b_v=b_v, n=n: e.matmul(
                                PS[b2][:, 0:n], lhsT=Wov[:, kc, 1024 + oc * 128:1024 + (oc + 1) * 128], rhs=b_v[:, kc, 0:n], start=(kc == 0), stop=(kc == kcb - 1)),
                                reads=["Wo", ("brt", ti % nbr)], writes=["ps%d" % b2], inc=(kc == kcb - 1))
                        tg = tglu[oc % 2]
                        kb.op("act", lambda e, b2=b2, n=n, tg=tg: e.activation(out=tg[:, 0:n], in_=PS[b2][:, 0:n], func=AF.Sigmoid),
                              reads=["ps%d" % b2], writes=[("tglu", oc % 2)])
                        kb.op("dve", lambda e, by=by, n=n, tg=tg: e.tensor_tensor(out=tg[:, 0:n], in0=PS[by][:, 0:n], in1=tg[:, 0:n], op=ALU.mult),
                              reads=["ps%d" % by, ("tglu", oc % 2)], writes=[("tglu", oc % 2)])
                        kb.op("act", lambda e, bg=bg, n=n, sgm=sgm: e.activation(out=sgm[:, 0:n], in_=PS[bg][:, 0:n], func=AF.Sigmoid),
                              reads=["ps%d" % bg], writes=[("sig", oc % 2)])
                        kb.op("dve", lambda e, n=n, sgm=sgm, oc=oc, tg=tg: e.tensor_tensor(out=mtv[:, oc, 0:n], in0=tg[:, 0:n], in1=sgm[:, 0:n], op=ALU.mult),
                              reads=[("tglu", oc % 2), ("sig", oc % 2)], writes=[("mt", oc)])
                    else:
                        kb.op("act", lambda e, bg=bg, n=n, sgm=sgm: e.activation(out=sgm[:, 0:n], in_=PS[bg][:, 0:n], func=AF.Sigmoid),
                              reads=["ps%d" % bg], writes=[("sig", oc % 2)])
                        kb.op("dve", lambda e, by=by, n=n, sgm=sgm, oc=oc: e.tensor_tensor(out=mtv[:, oc, 0:n], in0=PS[by][:, 0:n], in1=sgm[:, 0:n], op=ALU.mult),
                              reads=["ps%d" % by, ("sig", oc % 2)], writes=[("mt", oc)])
                MT = [("mt", oc) for oc in range(KC)]
                for oc in range(KC):
                    bo = 4 + (oc % 2)
                    for kc in range(KC):
                        kb.op("pe", lambda e, kc=kc, oc=oc, bo=bo, n=n: e.matmul(
                            PS[bo][:, 0:n], lhsT=Woutv[:, kc, oc * 128:(oc + 1) * 128], rhs=mtv[:, kc, 0:n], start=(kc == 0), stop=(kc == KC - 1)),
                            reads=["Wout"] + MT, writes=["ps%d" % bo], inc=(kc == KC - 1))
                    kb.op("dve", lambda e, oc=oc, bo=bo, n=n, x_v=x_v: e.tensor_tensor(out=x_v[:, oc, 0:n], in0=PS[bo][:, 0:n], in1=x_v[:, oc, 0:n], op=ALU.add),
                          reads=["ps%d" % bo, ("xt", ti % 2)], writes=[("xt", ti % 2)])
                kb.dma(xscr[:, :, t0:t0 + n], x_v[:, :, 0:n], reads=[("xt", ti % 2)], writes=[("xscr", t0)])
            kb.barrier()

    def ssd(l):
        win = W["w_in"][l]
        XB0, Z0, DT0 = 6144, 4096, 9216
        Ident = AF.Identity
        with contextlib.ExitStack() as pes:
            cw = sbp(pes, "cw", [128, 24 * 5], F32)
            cwv = cw[:, :].rearrange("p (f k) -> p f k", k=5)
            cb = None
            with contextlib.ExitStack() as p0:
                cwt = sbp(p0, "cwt", [5, 3072], F32)
                kb.dma(cwt[0:4, :], W["ssd_conv_w"][l], writes=["cwt"])
                kb.dma(cwt[4:5, :], W["ssd_conv_b"][l:l + 1, :], writes=["cwt"])
                for fc in range(24):
                    kb.op("pe", lambda e, fc=fc: e.transpose(PS[7][:, fc * 5:(fc + 1) * 5], cwt[0:5, fc * 128:(fc + 1) * 128], identf[0:5, 0:5]),
                          reads=["cwt", "identf"], writes=["ps7"], inc=(fc == 23))
                kb.op("act", lambda e: e.copy(cw[:, :], PS[7][:, 0:120]), reads=["ps7"], writes=["cw"])
                kb.barrier()
            with contextlib.ExitStack() as p1:
                cin = sbp(p1, "cin", [48, 3072], F32)
                histT = sbp(p1, "histT", [128, 24 * 48], F32)
                hv = histT[:, :].rearrange("p (f s r) -> p f s r", f=24, s=16)
                tailT = sbp(p1, "tailT", [128, 24 * 48], F32)
                tv = tailT[:, :].rearrange("p (f s r) -> p f s r", f=24, s=16)
                tailP = sbp(p1, "tailP", [128, 72], F32)
                cout = sbp(p1, "cout", [48, 3072], F32)
                raw = [sbp(p1, "raw%d" % i, [128, 515], F32) for i in range(6)]
                accs = [sbp(p1, "acc%d" % i, [128, 512], F32) for i in range(4)]
                xc = [sbp(p1, "xc%d" % i, [128, 512], BF16) for i in range(3)]
                xst = [sbp(p1, "xst%d" % i, [128, 512], BF16) for i in range(3)]
                kb.dma(cin[:, :], W["state_conv"][l].rearrange("s r f -> (s r) f"), writes=["cin"])
                for fc in range(24):
                    bi = 6 + (fc // 4) % 2
                    j = fc % 4
                    kb.op("pe", lambda e, bi=bi, j=j, fc=fc: e.transpose(PS[bi][:, j * 48:(j + 1) * 48], cin[0:48, fc * 128:(fc + 1) * 128], identf[0:48, 0:48]),
                          reads=["cin", "identf"], writes=["ps%d" % bi], inc=(j == 3))
                    if j == 3:
                        kb.op("act", lambda e, bi=bi, fc=fc: e.copy(histT[:, (fc - 3) * 48:(fc + 1) * 48], PS[bi][:, 0:192]),
                              reads=["ps%d" % bi], writes=["histT"])
                NR = 6
                items = [(fcp, sub, ti) for fcp in range(12) for sub in range(2) for ti in range(len(TILES))]
                wcache = {}
                tbanks = [PS[5][:, :].bitcast(BF16), PS[6][:, :].bitcast(BF16)]

                def c1(k, item):
                    fcp, sub, ti = item
                    t0, n = TILES[ti]
                    fc = fcp * 2 + sub
                    d = {"fc": fc, "sub": sub, "t0": t0, "n": n, "samp": t0 >= SEQ, "fcp": fcp,
                         "bank": PS[k % 4], "kbank": "ps%d" % (k % 4),
                         "R": raw[k % NR], "kR": ("raw", k % NR), "prevR": raw[(k - 1) % NR], "kprev": ("raw", (k - 1) % NR),
                         "acc": accs[k % 4], "kacc": ("acc", k % 4), "xc": xc[k % 3], "kxc": ("xc", k % 3),
                         "xst": xst[k % 3], "kxs": ("xst", k % 3), "tb": tbanks[k % 2], "ktb": "ps%d" % (5 + k % 2)}
                    if d["samp"]:
                        R3 = d["R"][:, 0:176].rearrange("p (s c) -> p s c", c=11)
                        d["R3"] = R3
                        d["X"] = [R3[:, :, kk:kk + 8] for kk in range(4)]
                        d["A"] = d["acc"][:, 0:128].rearrange("p (s c) -> p s c", c=8)
                    else:
                        d["X"] = [d["R"][:, kk:kk + n] for kk in range(4)]
                        d["A"] = d["acc"][:, 0:n]
                    return d

                def p0(k, item):
                    d = c1(k, item)
                    if d["sub"] == 0 and d["t0"] == 0:
                        wcache[d["fcp"]] = load_w([(win[:, XB0 + d["fcp"] * 256:XB0 + (d["fcp"] + 1) * 256], 0)], KC, 256)
                    wx, kwx = wcache[d["fcp"]]
                    for kc in range(KC):
                        kb.op("pe", lambda e, kc=kc: e.matmul(
                            d["bank"][:, 0:d["n"]], lhsT=wx[:, kc, d["sub"] * 128:(d["sub"] + 1) * 128], rhs=hTv[:, kc, d["t0"]:d["t0"] + d["n"]],
                            start=(kc == 0), stop=(kc == KC - 1)),
                            reads=[kwx] + ck("h", d["t0"], d["n"]), writes=[d["kbank"]], inc=(kc == KC - 1))

                def p1(k, item):
                    d = c1(k, item)
                    R, n = d["R"], d["n"]
                    if not d["samp"]:
                        if d["t0"] == 0:
                            kb.op("pool", lambda e: e.memset(R[:, 0:3], 0.0), writes=[d["kR"]])
                        else:
                            kb.op("pool", lambda e: e.tensor_copy(R[:, 0:3], d["prevR"][:, 512:515]), reads=[d["kprev"]], writes=[d["kR"]])
                        kb.op("act", lambda e: e.copy(R[:, 3:3 + n], d["bank"][:, 0:n]), reads=[d["kbank"]], writes=[d["kR"]])
                    else:
                        kb.op("pool", lambda e: e.tensor_copy(d["R3"][:, :, 0:3], hv[:, d["fc"]]), reads=["histT"], writes=[d["kR"]])
                        kb.op("act", lambda e: e.copy(d["R3"][:, :, 3:11], d["bank"][:, 0:128].rearrange("p (s c) -> p s c", c=8)),
                              reads=[d["kbank"]], writes=[d["kR"]])

                def p2(k, item):
                    d = c1(k, item)
                    fc = d["fc"]
                    kb.op("act", lambda e: e.activation(out=d["A"], in_=d["X"][3], func=Ident, scale=cwv[:, fc, 3:4], bias=cwv[:, fc, 4:5]),
                          reads=[d["kR"], "cw"], writes=[d["kacc"]])

                def p3(k, item):
                    d = c1(k, item)
                    fc = d["fc"]
                    for kk in (2, 1, 0):
                        kb.op("dve", lambda e, kk=kk: e.scalar_tensor_tensor(
                            out=d["A"], in0=d["X"][kk], scalar=cwv[:, fc, kk:kk + 1], in1=d["A"], op0=ALU.mult, op1=ALU.add),
                            reads=[d["kR"], "cw", d["kacc"]], writes=[d["kacc"]])

                def p4(k, item):
                    d = c1(k, item)
                    fc, n = d["fc"], d["n"]
                    kb.op("act", lambda e: e.activation(out=d["xc"][:, 0:n], in_=d["acc"][:, 0:n], func=AF.Silu),
                          reads=[d["kacc"]], writes=[d["kxc"]])
                    if d["t0"] == 1536:
                        kb.op("pool", lambda e: e.tensor_copy(tailP[:, fc * 3:(fc + 1) * 3], d["R"][:, 512:515]), reads=[d["kR"]], writes=["tailP"])
                    if d["samp"]:
                        kb.op("pool", lambda e: e.tensor_copy(tv[:, fc], d["R3"][:, :, 8:11]), reads=[d["kR"]], writes=["tailT"])

                def p5_(k, item):
                    d = c1(k, item)
                    fc, n, t0 = d["fc"], d["n"], d["t0"]
                    if fc >= 16:
                        kb.dma(bc_scr[:, fc - 16, t0:t0 + n], d["xc"][:, 0:n], reads=[d["kxc"]], writes=[("bc_scr", fc, t0)])
                    if fc < 20:
                        nci = n // 128
                        for ci in range(nci):
                            kb.op("pe", lambda e, ci=ci: e.transpose(d["tb"][:, ci * 128:(ci + 1) * 128], d["xc"][:, ci * 128:(ci + 1) * 128], identb[:, :]),
                                  reads=[d["kxc"], "identb"], writes=[d["ktb"]], inc=(ci == nci - 1))

                def p6(k, item):
                    d = c1(k, item)
                    fc, n, t0 = d["fc"], d["n"], d["t0"]
                    if fc < 20:
                        kb.op("act", lambda e: e.copy(d["xst"][:, 0:n], d["tb"][:, 0:n]), reads=[d["ktb"]], writes=[d["kxs"]])
                        kb.dma(xsb_scr[t0:t0 + n, fc * 128:(fc + 1) * 128].rearrange("(c p) f -> p c f", p=128),
                               d["xst"][:, 0:n].rearrange("p (c f) -> p c f", f=128), reads=[d["kxs"]], writes=[("xsb_scr", fc, t0)])

                run_pipeline(items, [p0, p1, p2, p3, p4, p5_, p6])
                for fc in range(24):
                    bi = 6 + (fc // 4) % 2
                    j = fc % 4
                    kb.op("pe", lambda e, bi=bi, j=j, fc=fc: e.transpose(PS[bi][0:3, j * 128:(j + 1) * 128], tailP[:, fc * 3:(fc + 1) * 3], identf[:, :]),
                          reads=["tailP", "identf"], writes=["ps%d" % bi], inc=(j == 3))
                    if j == 3:
                        kb.op("act", lambda e, bi=bi, fc=fc: e.copy(cin[0:3, (fc - 3) * 128:(fc + 1) * 128], PS[bi][0:3, 0:512]),
                              reads=["ps%d" % bi, "histT"], writes=["cin"])
                kb.dma(conv_p[l], cin[0:3, :], reads=["cin"], writes=[("conv_p", l)])
                for fc in range(24):
                    bi = 6 + (fc // 4) % 2
                    j = fc % 4
                    kb.op("pe", lambda e, bi=bi, j=j, fc=fc: e.transpose(PS[bi][0:48, j * 128:(j + 1) * 128], tailT[:, fc * 48:(fc + 1) * 48], identf[:, :]),
                          reads=["tailT", "identf"], writes=["ps%d" % bi], inc=(j == 3))
                    if j == 3:
                        kb.op("act", lambda e, bi=bi, fc=fc: e.copy(cout[0:48, (fc - 3) * 128:(fc + 1) * 128], PS[bi][0:48, 0:512]),
                              reads=["ps%d" % bi], writes=["cout"])
                kb.dma(conv_s[l].rearrange("s r f -> (s r) f"), cout[:, :], reads=["cout"], writes=[("conv_s", l)])
                kb.barrier()

            if not stages.get("ssd_s2", True):
                return
            with contextlib.ExitStack() as p2:
                Wz = sbp(p2, "Wz", [128, KC * 2048], BF16)
                Wzv = Wz[:, :].rearrange("p (k n) -> p k n", k=KC)
                Wdt = sbp(p2, "Wdt", [128, KC * 32], BF16)
                Wdtv = Wdt[:, :].rearrange("p (k n) -> p k n", k=KC)
                triu = sbp(p2, "triu", [128, 256], F32)
                triuv = triu[:, :].rearrange("p (s n) -> p s n", s=2)
                negm = sbp(p2, "negm", [128, 256], F32)
                negmv = negm[:, :].rearrange("p (s n) -> p s n", s=2)
                ssm = sbp(p2, "ssm", [128, 256], F32)
                ssv = ssm[:, :].rearrange("p (s n) -> p s n", s=2)
                rm = sbp(p2, "rm", [128, 2048], F32)
                rmv = rm[:, :].rearrange("p (s n) -> p s n", s=16)
                rowmask = sbp(p2, "rowmask2", [128, 16], F32)
                dtb = sbp(p2, "dtb", [128, 32], F32)
                a_t = sbp(p2, "a_t", [128, 32], F32)
                dsk = sbp(p2, "dsk", [128, 32], F32)
                sT = sbp(p2, "sT", [128, 2048], F32)
                sTb = sbp(p2, "sTb", [128, 2048], BF16)
                xsb = sbp(p2, "xsb", [128, 2560], BF16)
                bct = sbp(p2, "bct", [128, 1024], BF16)
                bcv = bct[:, :].rearrange("p (g t) -> p g t", g=8)
                dtt = sbp(p2, "dtt", [128, 32], F32)
                ex1 = sbp(p2, "ex1", [128, 32], F32)
                dt_ = sbp(p2, "dt_", [128, 32], F32)
                dA = sbp(p2, "dA", [128, 32], F32)
                cum = sbp(p2, "cum", [128, 32], F32)
                wtmp = sbp(p2, "wtmp", [128, 32], F32)
                wend = sbp(p2, "wend", [128, 32], F32)
                dec = sbp(p2, "dec", [128, 32], F32)
                decs = sbp(p2, "decs", [128, 512], F32)
                decsv = decs[:, :].rearrange("p (s h) -> p s h", s=16)
                ecum = sbp(p2, "ecum", [128, 32], F32)
                xdt = sbp(p2, "xdt", [128, 2048], BF16)
                xsD = sbp(p2, "xsD", [128, 2048], BF16)
                xdtw = sbp(p2, "xdtw", [128, 2048], BF16)
                scT = sbp(p2, "scT", [128, 512], BF16)
                yoff = sbp(p2, "yoff", [128, 2048], F32)
                yy = sbp(p2, "yy", [128, 2048], F32)
                zs = sbp(p2, "zs", [128, 512], F32)
                seg = [sbp(p2, "seg%d" % i, [128, 512], F32) for i in range(2)]
                Lm = [sbp(p2, "Lm%d" % i, [128, 512], BF16) for i in range(2)]
                Mm = [sbp(p2, "Mm%d" % i, [128, 512], BF16) for i in range(2)]
                bst = sbp(p2, "bst2", [128, 24], F32)
                mv = sbp(p2, "mv2", [128, 8], F32)
                mvv = mv[:, :].rearrange("p (g t) -> p g t", g=4)
                ms = sbp(p2, "ms", [128, 4], F32)
                sdv = sbp(p2, "sdv2", [128, 4], F32)
                rsv = sbp(p2, "rsv2", [128, 4], F32)
                yn = sbp(p2, "yn", [128, 2048], BF16)
                ynT = sbp(p2, "ynT", [128, 2048], BF16)
                Cblk = sbp(p2, "Cblk", [128, 2048], BF16)
                Cbv = Cblk[:, :].rearrange("p (s t) -> p s t", s=16)
                s0 = [sbp(p2, "s0_%d" % i, [128, 512], F32) for i in range(2)]
                s0T = [sbp(p2, "s0T%d" % i, [128, 512], F32) for i in range(2)]
                s0Tb = [sbp(p2, "s0Tb%d" % i, [128, 512], BF16) for i in range(2)]
                Bm = [sbp(p2, "Bm%d" % i, [128, 128], BF16) for i in range(2)]
                snat = [sbp(p2, "snat%d" % i, [128, 512], F32) for i in range(2)]
                print("S2 sbuf remaining", nc.sbuf_bytes_remaining)

                kb.dma(triuv, CST["triu"], writes=["triu"])
                kb.dma(negmv, CST["negm"], writes=["negm"])
                kb.dma(ssv, CST["ss"], writes=["ss"])
                kb.dma(rmv, CST["rm"], writes=["rm"])
                kb.dma(rowmask[:, :], CST["rowmask"], writes=["rowmask"])
                kb.dma(dtb[:, :], W["ssd_dt_bias"][l:l + 1, :].to_broadcast([128, 32]), writes=["dtb"])
                kb.dma(a_t[:, :], W["ssd_a_log"][l:l + 1, :].to_broadcast([128, 32]), writes=["a_t"])
                kb.dma(dsk[:, :], W["ssd_d"][l:l + 1, :].to_broadcast([128, 32]), writes=["dsk"])
                kb.op("act", lambda e: e.activation(out=a_t[:, :], in_=a_t[:, :], func=AF.Exp), reads=["a_t"], writes=["a_t"])
                kb.op("dve", lambda e: e.tensor_scalar(out=a_t[:, :], in0=a_t[:, :], scalar1=-1.0, scalar2=None, op0=ALU.mult), reads=["a_t"], writes=["a_t"])
                kb.op("pool", lambda e: e.memset(sT[:, :], 0.0), writes=["sT"])
                kb.op("pool", lambda e: e.memset(sTb[:, :], 0.0), writes=["sTb"])
                kb.op("pool", lambda e: e.memset(Cblk[:, :], 0.0), writes=["Cblk"])
                for j in range(8):
                    load_w([(win[:, Z0 + j * 256:Z0 + (j + 1) * 256], 0)], KC, 256, dst=Wzv[:, :, j * 256:(j + 1) * 256], dstkey="Wz")
                load_w([(win[:, DT0:DT0 + 32], 0)], KC, 32, dst=Wdtv, dstkey="Wdt")
                p5 = PS[5][:, :].bitcast(BF16)
                hcount = 0
                for c in stages.get("ssd_chunks", list(range(NCH))):
                    ts = slice(c * 128, (c + 1) * 128)
                    samp = c * 128 >= SEQ
                    sel = 1 if samp else 0
                    kb.dma(xsb[:, :], xsb_scr[ts, :], reads=[("xsb_scr", fc, (c * 128 // 512) * 512) for fc in range(20)], writes=["xsb"])
                    kb.dma(bcv, bc_scr[:, :, ts], reads=[("bc_scr", fc, (c * 128 // 512) * 512) for fc in range(16, 24)], writes=["bct"])
                    for kc in range(KC):
                        kb.op("pe", lambda e, kc=kc, ts=ts: e.matmul(PS[0][:, 0:32], lhsT=hTv[:, kc, ts], rhs=Wdtv[:, kc, :], start=(kc == 0), stop=(kc == KC - 1)),
                              reads=["Wdt", ("h", c)], writes=["ps0"], inc=(kc == KC - 1))
                    kb.op("dve", lambda e: e.tensor_tensor(out=dtt[:, :], in0=PS[0][:, 0:32], in1=dtb[:, :], op=ALU.add), reads=["ps0", "dtb"], writes=["dtt"])
                    kb.op("act", lambda e: e.activation(out=ex1[:, :], in_=dtt[:, :], func=AF.Exp), reads=["dtt"], writes=["ex1"])
                    kb.op("act", lambda e: e.activation(out=dt_[:, :], in_=ex1[:, :], func=AF.Ln, bias=1.0, scale=1.0), reads=["ex1"], writes=["dt_"])
                    kb.op("dve", lambda e: e.tensor_tensor(out=dA[:, :], in0=dt_[:, :], in1=a_t[:, :], op=ALU.mult), reads=["dt_", "a_t"], writes=["dA"])
                    kb.op("pe", lambda e, sel=sel: e.matmul(PS[0][:, 32:64], lhsT=triuv[:, sel, :], rhs=dA[:, :], start=True, stop=True),
                          reads=["triu", "dA"], writes=["ps0"], inc=True)
                    kb.op("act", lambda e: e.copy(cum[:, :], PS[0][:, 32:64]), reads=["ps0"], writes=["cum"])
                    kb.op("pe", lambda e, sel=sel: e.matmul(PS[0][:, 64:96], lhsT=ssv[:, sel, :], rhs=dA[:, :], start=True, stop=True),
                          reads=["ss", "dA"], writes=["ps0"], inc=True)
                    kb.op("dve", lambda e: e.tensor_tensor(out=wtmp[:, :], in0=PS[0][:, 64:96], in1=cum[:, :], op=ALU.subtract), reads=["ps0", "cum"], writes=["wtmp"])
                    kb.op("act", lambda e: e.activation(out=wend[:, :], in_=wtmp[:, :], func=AF.Exp), reads=["wtmp"], writes=["wend"])
                    kb.op("act", lambda e: e.activation(out=dec[:, :], in_=PS[0][:, 64:96], func=AF.Exp), reads=["ps0"], writes=["dec"])
                    kb.op("act", lambda e: e.activation(out=ecum[:, :], in_=cum[:, :], func=AF.Exp), reads=["cum"], writes=["ecum"])
                    xs3 = xsb[:, 0:2048].rearrange("p (h q) -> p h q", h=32)
                    kb.op("dve", lambda e, xs3=xs3: e.tensor_tensor(out=xdt[:, :].rearrange("p (h q) -> p h q", h=32), in0=xs3,
                                                                  in1=dt_[:, :].unsqueeze(2).to_broadcast([128, 32, 64]), op=ALU.mult),
                          reads=["xsb", "dt_"], writes=["xdt"])
                    kb.op("pool", lambda e, xs3=xs3: e.tensor_tensor(out=xsD[:, :].rearrange("p (h q) -> p h q", h=32), in0=xs3,
                                                                   in1=dsk[:, :].unsqueeze(2).to_broadcast([128, 32, 64]), op=ALU.mult),
                          reads=["xsb", "dsk"], writes=["xsD"])
                    kb.op("pool", lambda e: e.tensor_tensor(out=xdtw[:, :].rearrange("p (h q) -> p h q", h=32), in0=xdt[:, :].rearrange("p (h q) -> p h q", h=32),
                                                          in1=wend[:, :].unsqueeze(2).to_broadcast([128, 32, 64]), op=ALU.mult),
                          reads=["xdt", "wend"], writes=["xdtw"])
                    for g in range(4):
                        kb.op("pe", lambda e, g=g: e.matmul(PS[1][:, g * 128:(g + 1) * 128], lhsT=bcv[:, g, :], rhs=bcv[:, 4 + g, :], start=True, stop=True),
                              reads=["bct"], writes=["ps1"], inc=(g == 3))
                    kb.op("act", lambda e: e.copy(scT[:, :], PS[1][:, :]), reads=["ps1"], writes=["scT"])
                    if samp:
                        for s in range(NSS):
                            kb.op("pe", lambda e, s=s: e.matmul(PS[1][:, s * 32:(s + 1) * 32], lhsT=rmv[:, s, :], rhs=dA[:, :], start=True, stop=True),
                                  reads=["rm", "dA", "scT"], writes=["ps1"], inc=(s == NSS - 1))
                        kb.op("act", lambda e: e.activation(out=decs[:, :], in_=PS[1][:, :], func=AF.Exp), reads=["ps1"], writes=["decs"])
                    for g in range(4):
                        gs_ = slice(g * 512, (g + 1) * 512)
                        if not samp:
                            kb.op("pe", lambda e, g=g, gs_=gs_: e.matmul(PS[2][:, :], lhsT=bcv[:, 4 + g, :], rhs=sTb[:, gs_], start=True, stop=True),
                                  reads=["bct", "sTb"], writes=["ps2"], inc=True)
                        else:
                            for s in range(NSS):
                                kb.op("pool", lambda e, s=s, g=g: e.tensor_copy(Cbv[:, s, s * 8:(s + 1) * 8], bcv[:, 4 + g, s * 8:(s + 1) * 8]),
                                      reads=["bct"], writes=["Cblk"])
                            dbg = stages.get("dbg", 9)
                            for s in (range(stages.get("smp_ns", NSS)) if dbg >= 2 else []):
                                i2 = s % 2
                                for b4 in range(4):
                                    kb.dma(s0[i2][:, b4 * 128:(b4 + 1) * 128],
                                           W["state_ssm"][l, s, g * 8 + 2 * b4:g * 8 + 2 * b4 + 2].rearrange("h q n -> (h q) n"), writes=[("s0", i2)])
                                if dbg < 2.2:
                                    continue
                                for b4 in range(4):
                                    kb.op("pe", lambda e, b4=b4, i2=i2: e.transpose(PS[3][:, b4 * 128:(b4 + 1) * 128], s0[i2][:, b4 * 128:(b4 + 1) * 128], identf[:, :]),
                                          reads=[("s0", i2), "identf"], writes=["ps3"], inc=(b4 == 3))
                                if dbg < 2.4:
                                    continue
                                kb.op("dve", lambda e, i2=i2: e.tensor_copy(s0T[i2][:, :], PS[3][:, :]), reads=["ps3"], writes=[("s0T", i2)])
                                if dbg < 2.6:
                                    continue
                                kb.op("act", lambda e, i2=i2: e.copy(s0Tb[i2][:, :], s0T[i2][:, :]), reads=[("s0T", i2)], writes=[("s0Tb", i2)])
                                if dbg < 3:
                                    continue
                                kb.op("pe", lambda e, s=s, i2=i2: e.matmul(PS[2][:, :], lhsT=Cbv[:, s, :], rhs=s0Tb[i2][:, :], start=(s == 0), stop=(s == NSS - 1)),
                                      reads=["Cblk", ("s0Tb", i2)], writes=["ps2"], inc=True)
                                if dbg < 4:
                                    continue
                                kb.op("dve", lambda e, s=s, g=g, i2=i2: e.tensor_scalar(out=Bm[i2][:, :], in0=xsb[:, 2048 + g * 128:2048 + (g + 1) * 128],
                                                                                   scalar1=rowmask[:, s:s + 1], scalar2=None, op0=ALU.mult),
                                      reads=["xsb", "rowmask"], writes=[("Bm", i2)])
                                kb.op("pe", lambda e, i2=i2, gs_=gs_: e.matmul(PS[4][:, :], lhsT=Bm[i2][:, :], rhs=xdtw[:, gs_], start=True, stop=True),
                                      reads=[("Bm", i2), "xdtw"], writes=["ps4"], inc=True)
                                kb.op("dve", lambda e, i2=i2, s=s, g=g: e.tensor_tensor(
                                    out=s0T[i2][:, :].rearrange("p (h q) -> p h q", h=8), in0=s0T[i2][:, :].rearrange("p (h q) -> p h q", h=8),
                                    in1=decsv[:, s, g * 8:(g + 1) * 8].unsqueeze(2).to_broadcast([128, 8, 64]), op=ALU.mult),
                                    reads=[("s0T", i2), "decs"], writes=[("s0T", i2)])
                                kb.op("dve", lambda e, i2=i2: e.tensor_tensor(out=s0T[i2][:, :], in0=PS[4][:, :], in1=s0T[i2][:, :], op=ALU.add),
                                      reads=["ps4", ("s0T", i2)], writes=[("s0T", i2)])
                                if dbg < 5:
                                    continue
                                for b4 in range(4):
                                    kb.op("pe", lambda e, b4=b4, i2=i2: e.transpose(PS[3][:, b4 * 128:(b4 + 1) * 128], s0T[i2][:, b4 * 128:(b4 + 1) * 128], identf[:, :]),
                                          reads=[("s0T", i2), "identf"], writes=["ps3"], inc=(b4 == 3))
                                kb.op("act", lambda e, i2=i2: e.copy(snat[i2][:, :], PS[3][:, :]), reads=["ps3"], writes=[("snat", i2)])
                                for b4 in range(4):
                                    kb.dma(ssm_s[l, s, g * 8 + 2 * b4:g * 8 + 2 * b4 + 2].rearrange("h q n -> (h q) n"),
                                           snat[i2][:, b4 * 128:(b4 + 1) * 128], reads=[("snat", i2)], writes=[("ssm_s", l, s, g, b4)])
                        if samp and stages.get("dbg", 9) < 3:
                            kb.op("pe", lambda e, g=g, gs_=gs_: e.matmul(PS[2][:, :], lhsT=bcv[:, 4 + g, :], rhs=sTb[:, gs_], start=True, stop=True),
                                  reads=["bct", "sTb"], writes=["ps2"], inc=True)
                        kb.op("dve", lambda e, g=g, gs_=gs_: e.tensor_tensor(
                            out=yoff[:, gs_].rearrange("p (h q) -> p h q", h=8), in0=PS[2][:, :].rearrange("p (h q) -> p h q", h=8),
                            in1=ecum[:, g * 8:(g + 1) * 8].unsqueeze(2).to_broadcast([128, 8, 64]), op=ALU.mult),
                            reads=["ps2", "ecum"], writes=[("yoff", g)])
                    for hq in range(8):
                        h0 = hq * 4
                        g = h0 // 8
                        i2 = hcount % 2
                        hcount += 1
                        cbk = 6 + i2
                        v4 = lambda ap: ap.rearrange("p (j t) -> p j t", j=4)
                        for j in range(4):
                            h = h0 + j
                            kb.op("pe", lambda e, h=h, j=j, cbk=cbk, sel=sel: e.matmul(PS[cbk][:, j * 128:(j + 1) * 128], lhsT=dA[:, h:h + 1].to_broadcast([128, 128]),
                                                                                    rhs=triuv[:, sel, :], start=True, stop=True),
                                  reads=["dA", "triu"], writes=["ps%d" % cbk], inc=(j == 3))
                        kb.op("dve", lambda e, h0=h0, cbk=cbk, i2=i2: e.tensor_tensor(
                            out=v4(seg[i2][:, :]), in0=v4(PS[cbk][:, :]), in1=cum[:, h0:h0 + 4].unsqueeze(2).to_broadcast([128, 4, 128]), op=ALU.subtract),
                            reads=["ps%d" % cbk, "cum"], writes=[("seg", i2)])
                        kb.op("pool", lambda e, i2=i2, sel=sel: e.tensor_tensor(
                            out=v4(seg[i2][:, :]), in0=v4(seg[i2][:, :]), in1=negmv[:, sel:sel + 1, :].to_broadcast([128, 4, 128]), op=ALU.add),
                            reads=[("seg", i2), "negm"], writes=[("seg", i2)])
                        kb.op("act", lambda e, i2=i2: e.activation(out=Lm[i2][:, :], in_=seg[i2][:, :], func=AF.Exp), reads=[("seg", i2)], writes=[("Lm", i2)])
                        kb.op("pool", lambda e, i2=i2, g=g: e.tensor_tensor(
                            out=v4(Mm[i2][:, :]), in0=v4(Lm[i2][:, :]), in1=scT[:, g * 128:(g + 1) * 128].unsqueeze(1).to_broadcast([128, 4, 128]), op=ALU.mult),
                            reads=[("Lm", i2), "scT"], writes=[("Mm", i2)])
                        ybk = 2 + (g % 2)
                        for j in range(4):
                            h = h0 + j
                            yo_ = PS[ybk][:, (h % 8) * 64:(h % 8 + 1) * 64]
                            kb.op("pe", lambda e, yo_=yo_, i2=i2, h=h, j=j: e.matmul(yo_, lhsT=Mm[i2][:, j * 128:(j + 1) * 128], rhs=xdt[:, h * 64:(h + 1) * 64], start=True, stop=False),
                                  reads=[("Mm", i2), "xdt"] + [("yoff", gg) for gg in range(4)], writes=["ps%d" % ybk], inc=False)
                            kb.op("pe", lambda e, yo_=yo_, h=h: e.matmul(yo_, lhsT=identb[:, :], rhs=xsD[:, h * 64:(h + 1) * 64], start=False, stop=True),
                                  reads=["identb", "xsD"], writes=["ps%d" % ybk], inc=True)
                        if h0 % 8 == 4:
                            kb.op("dve", lambda e, g=g, ybk=ybk: e.tensor_tensor(out=yy[:, g * 512:(g + 1) * 512], in0=PS[ybk][:, :], in1=yoff[:, g * 512:(g + 1) * 512], op=ALU.add),
                                  reads=["ps%d" % ybk, ("yoff", g)], writes=[("yy", g)])
                    for g in range(4):
                        zb = 0 + (g % 2)
                        for kc in range(KC):
                            kb.op("pe", lambda e, kc=kc, g=g, zb=zb, ts=ts: e.matmul(PS[zb][:, :], lhsT=hTv[:, kc, ts], rhs=Wzv[:, kc, g * 512:(g + 1) * 512],
                                                                                  start=(kc == 0), stop=(kc == KC - 1)),
                                  reads=["Wz", ("h", c), "cum", "wtmp", "dec"], writes=["ps%d" % zb], inc=(kc == KC - 1))
                        kb.op("act", lambda e, zb=zb: e.activation(out=zs[:, :], in_=PS[zb][:, :], func=AF.Silu), reads=["ps%d" % zb], writes=["zs"])
                        kb.op("dve", lambda e, g=g: e.tensor_tensor(out=yy[:, g * 512:(g + 1) * 512], in0=yy[:, g * 512:(g + 1) * 512], in1=zs[:, :], op=ALU.mult),
                              reads=[("yy", g), "zs"], writes=[("yy", g)])
                        kb.op("dve", lambda e, g=g: e.bn_stats(bst[:, g * 6:(g + 1) * 6], yy[:, g * 512:(g + 1) * 512]), reads=[("yy", g)], writes=[("bst", g)])
                        kb.op("dve", lambda e, g=g: e.bn_aggr(mv[:, g * 2:(g + 1) * 2], bst[:, g * 6:(g + 1) * 6]), reads=[("bst", g)], writes=[("mv", g)])
                    MV = [("mv", g) for g in range(4)]
                    kb.op("dve", lambda e: e.scalar_tensor_tensor(out=ms[:, :], in0=mvv[:, :, 0], scalar=1.0, in1=mvv[:, :, 0], op0=ALU.mult, op1=ALU.mult),
                          reads=MV, writes=["ms"])
                    kb.op("dve", lambda e: e.tensor_tensor(out=ms[:, :], in0=ms[:, :], in1=mvv[:, :, 1], op=ALU.add), reads=MV + ["ms"], writes=["ms"])
                    kb.op("act", lambda e: e.activation(out=sdv[:, :], in_=ms[:, :], func=AF.Sqrt, bias=EPS, scale=1.0), reads=["ms"], writes=["sdv"])
                    kb.op("dve", lambda e: e.reciprocal(rsv[:, :], sdv[:, :]), reads=["sdv"], writes=["rsv"])
                    for g in range(4):
                        kb.op("dve", lambda e, g=g: e.tensor_scalar(out=yn[:, g * 512:(g + 1) * 512], in0=yy[:, g * 512:(g + 1) * 512], scalar1=rsv[:, g:g + 1],
                                                                  scalar2=None, op0=ALU.mult),
                              reads=[("yy", g), "rsv"], writes=["yn"])
                    for half in range(2):
                        for j in range(8):
                            kc = half * 8 + j
                            kb.op("pe", lambda e, kc=kc, j=j: e.transpose(p5[:, j * 128:(j + 1) * 128], yn[:, kc * 128:(kc + 1) * 128], identb[:, :]),
                                  reads=["yn", "identb"], writes=["ps5"], inc=(j == 7))
                        kb.op("act", lambda e, half=half: e.copy(ynT[:, half * 1024:(half + 1) * 1024], p5[:, :]), reads=["ps5"], writes=[("ynT", half)])
                    kb.dma(brscr[:, 0:16, ts], ynT[:, :].rearrange("p (k t) -> p k t", k=16), reads=[("ynT", 0), ("ynT", 1)], writes=[("brscr", c)])
                    if not samp:
                        for g in range(4):
                            gs_ = slice(g * 512, (g + 1) * 512)
                            kb.op("pe", lambda e, g=g, gs_=gs_: e.matmul(PS[4][:, :], lhsT=xsb[:, 2048 + g * 128:2048 + (g + 1) * 128], rhs=xdtw[:, gs_], start=True, stop=True),
                                  reads=["xsb", "xdtw"], writes=["ps4"], inc=True)
                            kb.op("dve", lambda e, g=g, gs_=gs_: e.tensor_tensor(
                                out=sT[:, gs_].rearrange("p (h q) -> p h q", h=8), in0=sT[:, gs_].rearrange("p (h q) -> p h q", h=8),
                                in1=dec[:, g * 8:(g + 1) * 8].unsqueeze(2).to_broadcast([128, 8, 64]), op=ALU.mult),
                                reads=["sT", "dec"], writes=["sT"])
                            kb.op("dve", lambda e, gs_=gs_: e.tensor_tensor(out=sT[:, gs_], in0=PS[4][:, :], in1=sT[:, gs_], op=ALU.add),
                                  reads=["ps4", "sT"], writes=["sT"])
                        kb.op("act", lambda e: e.copy(sTb[:, :], sT[:, :]), reads=["sT"], writes=["sTb"])
                        if c == SEQ // 128 - 1:
                            for q4 in range(4):
                                for b4 in range(4):
                                    blk = q4 * 4 + b4
                                    kb.op("pe", lambda e, b4=b4, blk=blk: e.transpose(PS[3][:, b4 * 128:(b4 + 1) * 128], sT[:, blk * 128:(blk + 1) * 128], identf[:, :]),
                                          reads=["sT", "identf"], writes=["ps3"], inc=(b4 == 3))
                                kb.op("act", lambda e, q4=q4: e.copy(snat[q4 % 2][:, :], PS[3][:, :]), reads=["ps3"], writes=[("snat", q4 % 2)])
                                for b4 in range(4):
                                    kb.dma(ssm_p[l, q4 * 8 + 2 * b4:q4 * 8 + 2 * b4 + 2].rearrange("h q n -> (h q) n"),
                                           snat[q4 % 2][:, b4 * 128:(b4 + 1) * 128], reads=[("snat", q4 % 2)], writes=[("ssm_p", l, q4, b4)])
                kb.barrier()

    def s5(l):
        win = W["w_in"][l]
        U0 = 3072
        TWO_PI = 2.0 * np.pi
        MAGIC = 12582912.0
        with contextlib.ExitStack() as pes:
            uT = sbp(pes, "uT", [128, KC * T], BF16)
            uTv = uT[:, :].rearrange("p (k t) -> p k t", k=KC)
            Bpad = sbp(pes, "Bpad", [128, 64 * 128], BF16)
            Bspad = sbp(pes, "Bspad", [128, 64 * 128], BF16)
            Cpad = sbp(pes, "Cpad", [128, 64 * 128], BF16)
            Bpv = Bpad[:, :].rearrange("p (g n) -> p g n", g=64)
            Bsv = Bspad[:, :].rearrange("p (g n) -> p g n", g=64)
            Cpv = Cpad[:, :].rearrange("p (g n) -> p g n", g=64)
            mag = sbp(pes, "mag", [128, 64], F32)
            frc = sbp(pes, "frc", [128, 64], F32)
            dcol = sbp(pes, "dcol", [128, 8], F32)
            tpos = sbp(pes, "tpos", [128, 256], F32)
            tposv = tpos[:, :].rearrange("p (s t) -> p s t", s=2)
            smask = sbp(pes, "smask", [128, 128], F32)
            Pm = sbp(pes, "Pm", [128, 128], F32)
            xst = sbp(pes, "xstate", [128, 64], F32)
            xfs = sbp(pes, "xfs", [128, 64 * 16], F32)
            xfsv = xfs[:, :].rearrange("p (g s) -> p g s", g=64)
            x0T = sbp(pes, "x0T", [128, 16 * 64], F32)
            x0Tv = x0T[:, :].rearrange("p (s g) -> p s g", s=16)
            kb.dma(tposv, CST["tpos"], writes=["tpos"])
            kb.dma(smask[:, :], CST["smask"], writes=["smask"])
            kb.dma(Pm[:, :], CST["Pm"], writes=["Pm"])
            kb.dma(dcol[:, :], W["s5_d"][l].rearrange("(k p) -> p k", p=128), writes=["dcol"])
            kb.op("pool", lambda e: e.memset(xst[:, :], 0.0), writes=[("xst", g) for g in range(64)])
            for j in range(4):
                wu, kwu = load_w([(win[:, U0 + j * 256:U0 + (j + 1) * 256], 0)], KC, 256)
                for (t0, n) in TILES:
                    for sub in range(2):
                        oc = j * 2 + sub
                        bk = (oc % 2)
                        for kc in range(KC):
                            kb.op("pe", lambda e, kc=kc, bk=bk, sub=sub, t0=t0, n=n, wu=wu: e.matmul(
                                PS[bk][:, 0:n], lhsT=wu[:, kc, sub * 128:(sub + 1) * 128], rhs=hTv[:, kc, t0:t0 + n], start=(kc == 0), stop=(kc == KC - 1)),
                                reads=[kwu] + ck("h", t0, n), writes=["ps%d" % bk], inc=(kc == KC - 1))
                        kb.op("act", lambda e, bk=bk, oc=oc, t0=t0, n=n: e.copy(uTv[:, oc, t0:t0 + n], PS[bk][:, 0:n]), reads=["ps%d" % bk], writes=[("uT", oc)])
            with contextlib.ExitStack() as pp:
                def t64(name):
                    return sbp(pp, name, [128, 64], F32)
                aa = sbp(pp, "aa", [64, 256], F32)
                dt = t64("dt"); ar = t64("ar"); ai = t64("ai"); th = t64("th"); t1 = t64("t1"); t2 = t64("t2")
                sn = t64("sn"); cs = t64("cs"); abr = t64("abr"); abi = t64("abi"); den = t64("den")
                fre = t64("fre"); fim = t64("fim"); FP = t64("FP"); FQ = t64("FQ")
                bre2 = sbp(pp, "bre2", [128, 1024], F32)
                bim2 = sbp(pp, "bim2", [128, 1024], F32)
                Bn2 = sbp(pp, "Bn2", [128, 1024], F32)
                Bsn2 = sbp(pp, "Bsn2", [128, 1024], F32)
                tq = sbp(pp, "tq", [128, 1024], F32)
                Ball = sbp(pp, "Ball", [128, 256], F32)
                cT = sbp(pp, "cT", [128, 8 * 128], F32)
                cTv = cT[:, :].rearrange("p (k n) -> p k n", k=8)
                gmask = sbp(pp, "gmask", [128, 8], F32)
                cmask = sbp(pp, "cmask", [128, 1024], F32)
                cmv = cmask[:, :].rearrange("p (g n) -> p g n", g=8)
                kb.dma(gmask[:, :], CST["gmask"], writes=["gmask"])
                kb.dma(cmv, CST["cmask"], writes=["cmask"])
                kb.dma(dt[:, :], W["s5_log_dt"][l:l + 1, :].to_broadcast([128, 64]), writes=["dt"])
                kb.op("act", lambda e: e.activation(out=dt[:, :], in_=dt[:, :], func=AF.Exp), reads=["dt"], writes=["dt"])
                kb.dma(aa[:, 0:64], W["s5_a_re"][l], writes=["aa"])
                kb.dma(aa[:, 64:128], W["s5_a_re"][l], writes=["aa"])
                kb.dma(aa[:, 128:192], W["s5_a_im"][l], writes=["aa"])
                kb.dma(aa[:, 192:256], W["s5_a_im"][l], writes=["aa"])
                for i2 in range(2):
                    kb.op("pe", lambda e, i2=i2: e.transpose(PS[7][:, i2 * 64:(i2 + 1) * 64], aa[0:64, i2 * 128:(i2 + 1) * 128], identf[0:64, 0:64]),
                          reads=["aa", "identf"], writes=["ps7"], inc=(i2 == 1))
                kb.op("act", lambda e: e.copy(ar[:, :], PS[7][:, 0:64]), reads=["ps7"], writes=["ar"])
                kb.op("act", lambda e: e.copy(ai[:, :], PS[7][:, 64:128]), reads=["ps7"], writes=["ai"])
                TT = lambda o, a, b, op, eng="dve": kb.op(eng, lambda e: e.tensor_tensor(out=o[:, :], in0=a[:, :], in1=b[:, :], op=op),
                                                          reads=[id(a), id(b)], writes=[id(o)])
                TS = lambda o, a, s1, op0, s2=None, op1=None: kb.op("dve", (lambda e: e.tensor_scalar(out=o[:, :], in0=a[:, :], scalar1=s1, scalar2=s2, op0=op0, op1=op1)) if op1 is not None
                                                                      else (lambda e: e.tensor_scalar(out=o[:, :], in0=a[:, :], scalar1=s1, scalar2=None, op0=op0)),
                                                                      reads=[id(a)], writes=[id(o)])
                for tns, key in [(dt, "dt"), (ar, "ar"), (ai, "ai")]:
                    kb.writer[id(tns)] = kb.writer.get(key)
                TT(t1, dt, ar, ALU.mult)
                kb.op("act", lambda e: e.activation(out=mag[:, :], in_=t1[:, :], func=AF.Exp), reads=[id(t1)], writes=["mag"])
                TT(th, dt, ai, ALU.mult)
                TS(th, th, 1.0 / TWO_PI, ALU.mult)
                TS(t1, th, MAGIC, ALU.add)
                TS(t1, t1, MAGIC, ALU.subtract)
                kb.op("dve", lambda e: e.tensor_tensor(out=frc[:, :], in0=th[:, :], in1=t1[:, :], op=ALU.subtract), reads=[id(th), id(t1)], writes=["frc"])
                kb.op("act", lambda e: e.activation(out=sn[:, :], in_=frc[:, :], func=AF.Sin, scale=TWO_PI), reads=["frc"], writes=[id(sn)])
                kb.op("dve", lambda e: e.tensor_scalar(out=t2[:, :], in0=frc[:, :], scalar1=0.25, scalar2=None, op0=ALU.add), reads=["frc"], writes=[id(t2)])
                TS(t1, t2, 0.5, ALU.is_gt)
                TT(t2, t2, t1, ALU.subtract)
                kb.op("act", lambda e: e.activation(out=cs[:, :], in_=t2[:, :], func=AF.Sin, scale=TWO_PI), reads=[id(t2)], writes=[id(cs)])
                kb.op("dve", lambda e: e.tensor_tensor(out=abr[:, :], in0=mag[:, :], in1=cs[:, :], op=ALU.mult), reads=["mag", id(cs)], writes=[id(abr)])
                kb.op("dve", lambda e: e.tensor_tensor(out=abi[:, :], in0=mag[:, :], in1=sn[:, :], op=ALU.mult), reads=["mag", id(sn)], writes=[id(abi)])
                TS(abr, abr, -1.0, ALU.add)
                TT(t1, ar, ar, ALU.mult)
                TT(t2, ai, ai, ALU.mult)
                TT(den, t1, t2, ALU.add)
                kb.op("dve", lambda e: e.reciprocal(den[:, :], den[:, :]), reads=[id(den)], writes=[id(den)])
                TT(t1, abr, ar, ALU.mult)
                TT(t2, abi, ai, ALU.mult)
                TT(fre, t1, t2, ALU.add)
                TT(fre, fre, den, ALU.mult)
                TT(t1, abi, ar, ALU.mult)
                TT(t2, abr, ai, ALU.mult)
                TT(fim, t1, t2, ALU.subtract)
                TT(fim, fim, den, ALU.mult)
                kb.op("dve", lambda e: e.tensor_copy(FP[0:64, :], fre[0:64, :]), reads=[id(fre)], writes=[id(FP)])
                kb.op("dve", lambda e: e.tensor_copy(FP[64:128, :], fim[64:128, :]), reads=[id(fim)], writes=[id(FP)])
                kb.op("dve", lambda e: e.tensor_scalar(out=FQ[0:64, :], in0=fim[0:64, :], scalar1=-1.0, scalar2=None, op0=ALU.mult), reads=[id(fim)], writes=[id(FQ)])
                kb.op("dve", lambda e: e.tensor_copy(FQ[64:128, :], fre[64:128, :]), reads=[id(fre)], writes=[id(FQ)])
                for half in range(2):
                    kb.dma(bre2[half * 64:(half + 1) * 64, :].rearrange("p (g c) -> p g c", g=64), W["s5_b_re"][l].rearrange("g n c -> n g c"), writes=["bre2"])
                    kb.dma(bim2[half * 64:(half + 1) * 64, :].rearrange("p (g c) -> p g c", g=64), W["s5_b_im"][l].rearrange("g n c -> n g c"), writes=["bim2"])
                v3 = lambda t: t[:, :].rearrange("p (g c) -> p g c", g=64)
                bc = lambda t: t[:, :].unsqueeze(2).to_broadcast([128, 64, 16])
                kb.op("dve", lambda e: e.tensor_tensor(out=v3(Bn2), in0=v3(bre2), in1=bc(FP), op=ALU.mult), reads=["bre2", id(FP)], writes=["Bn2"])
                kb.op("dve", lambda e: e.tensor_tensor(out=v3(tq), in0=v3(bim2), in1=bc(FQ), op=ALU.mult), reads=["bim2", id(FQ)], writes=["tq"])
                kb.op("dve", lambda e: e.tensor_tensor(out=Bn2[:, :], in0=Bn2[:, :], in1=tq[:, :], op=ALU.add), reads=["Bn2", "tq"], writes=["Bn2"])
                kb.op("dve", lambda e: e.tensor_tensor(out=v3(Bsn2), in0=v3(bim2), in1=bc(FP), op=ALU.mult), reads=["bim2", id(FP)], writes=["Bsn2"])
                kb.op("dve", lambda e: e.tensor_tensor(out=v3(tq), in0=v3(bre2), in1=bc(FQ), op=ALU.mult), reads=["bre2", id(FQ), "Bn2"], writes=["tq"])
                kb.op("dve", lambda e: e.tensor_tensor(out=Bsn2[:, :], in0=Bsn2[:, :], in1=tq[:, :], op=ALU.subtract), reads=["Bsn2", "tq"], writes=["Bsn2"])
                kb.dma(cTv[:, :, 0:64], W["s5_c_re"][l].rearrange("(k g) c n -> (g c) k n", k=8), writes=["cT"])
                kb.dma(cTv[:, :, 64:128], W["s5_c_im"][l].rearrange("(k g) c n -> (g c) k n", k=8), writes=["cT"])
                kb.op("dve", lambda e: e.tensor_scalar(out=cTv[:, :, 64:128], in0=cTv[:, :, 64:128], scalar1=-1.0, scalar2=None, op0=ALU.mult), reads=["cT"], writes=["cT"])
                for gc in range(8):
                    kb.op("pe", lambda e, gc=gc: e.transpose(PS[6][:, 0:128], Bn2[:, gc * 128:(gc + 1) * 128], identf[:, :]), reads=["Bn2", "identf"], writes=["ps6"], inc=False)
                    kb.op("pe", lambda e, gc=gc: e.transpose(PS[6][:, 128:256], Bsn2[:, gc * 128:(gc + 1) * 128], identf[:, :]), reads=["Bsn2", "identf"], writes=["ps6"], inc=True)
                    kb.op("act", lambda e: e.copy(Ball[:, :], PS[6][:, 0:256]), reads=["ps6"], writes=["Ball"])
                    kb.op("pe", lambda e, gc=gc: e.transpose(PS[7][:, 0:128], cTv[:, gc, :], identf[:, :]), reads=["cT", "identf"], writes=["ps7"], inc=True)
                    for g8 in range(8):
                        g = gc * 8 + g8
                        kb.op("dve", lambda e, g=g, g8=g8: e.tensor_scalar(out=Bpv[:, g, :], in0=Ball[:, 0:128], scalar1=gmask[:, g8:g8 + 1], scalar2=None, op0=ALU.mult),
                              reads=["Ball", "gmask"], writes=["Bpad"])
                        kb.op("pool", lambda e, g=g, g8=g8: e.tensor_scalar(out=Bsv[:, g, :], in0=Ball[:, 128:256], scalar1=gmask[:, g8:g8 + 1], scalar2=None, op0=ALU.mult),
                              reads=["Ball", "gmask"], writes=["Bspad"])
                        kb.op("dve", lambda e, g=g, g8=g8: e.tensor_tensor(out=Cpv[:, g, :], in0=PS[7][:, 0:128], in1=cmv[:, g8, :], op=ALU.mult),
                              reads=["ps7", "cmask"], writes=["Cpad"])
                kb.barrier()
            with contextlib.ExitStack() as px:
                s0in = sbp(px, "s0in", [64, 16 * 128], F32)
                s0r = sbp(px, "s0r", [64, 16 * 128], F32)
                kb.dma(s0in[:, :].rearrange("p (s x) -> p s x", s=16), W["state_s5"][l].rearrange("s g n r -> g s (n r)"), writes=["s0in"])
                kb.op("dve", lambda e: e.tensor_copy(s0r[:, :].rearrange("p (s r n) -> p s r n", s=16, r=2), s0in[:, :].rearrange("p (s n r) -> p s r n", s=16, r=2)),
                      reads=["s0in"], writes=["s0r"])
                for s in range(NSS):
                    bk = 6 + (s // 8) % 2
                    kb.op("pe", lambda e, s=s, bk=bk: e.transpose(PS[bk][:, (s % 8) * 64:(s % 8 + 1) * 64], s0r[0:64, s * 128:(s + 1) * 128], identf[0:64, 0:64]),
                          reads=["s0r", "identf"], writes=["ps%d" % bk], inc=(s % 8 == 7))
                    if s % 8 == 7:
                        kb.op("act", lambda e, s=s, bk=bk: e.copy(x0T[:, (s - 7) * 64:(s + 1) * 64], PS[bk][:, :]), reads=["ps%d" % bk], writes=["x0T"])
                kb.barrier()
            with contextlib.ExitStack() as pm:
                cosT = sbp(pm, "cosT", [128, 2 * 1024], F32)
                sinT = sbp(pm, "sinT", [128, 2 * 1024], F32)
                cosv = cosT[:, :].rearrange("p (s g t) -> p s g t", s=2, g=8)
                sinv = sinT[:, :].rearrange("p (s g t) -> p s g t", s=2, g=8)
                ta = sbp(pm, "ta", [128, 1024], F32)
                tb = sbp(pm, "tb", [128, 1024], F32)
                rms_ = sbp(pm, "rms_", [128, 128], F32)
                w1 = [sbp(pm, "w1_%d" % i, [128, 128], F32) for i in range(4)]
                w2 = [sbp(pm, "w2_%d" % i, [128, 128], F32) for i in range(4)]
                zz = [sbp(pm, "zz%d" % i, [128, 128], F32) for i in range(4)]
                x1 = [sbp(pm, "x1_%d" % i, [128, 128], F32) for i in range(6)]
                x2 = [sbp(pm, "x2_%d" % i, [128, 128], F32) for i in range(4)]
                xb = [sbp(pm, "xb%d" % i, [128, 128], BF16) for i in range(4)]
                yvL = [sbp(pm, "yv%d" % i, [128, 128], F32) for i in range(2)]
                ytL = [sbp(pm, "yt%d" % i, [128, 128], F32) for i in range(2)]
                ysgL = [sbp(pm, "ysg%d" % i, [128, 128], F32) for i in range(2)]
                yo = [sbp(pm, "yo%d" % i, [128, 128], BF16) for i in range(2)]
                fin = sbp(pm, "s5fin", [64, 128], F32)
                fin2 = [sbp(pm, "s5fin2_%d" % i, [64, 128], F32) for i in range(2)]
                it = 0
                for gc in range(8):
                    for sel in range(2):
                        kb.op("dve", lambda e, sel=sel, gc=gc: e.tensor_tensor(
                            out=ta[:, :].rearrange("p (g t) -> p g t", g=8), in0=tposv[:, sel:sel + 1, :].to_broadcast([128, 8, 128]),
                            in1=frc[:, gc * 8:(gc + 1) * 8].unsqueeze(2).to_broadcast([128, 8, 128]), op=ALU.mult),
                            reads=["tpos", "frc"], writes=["ta"])
                        kb.op("dve", lambda e: e.tensor_scalar(out=tb[:, :], in0=ta[:, :], scalar1=MAGIC, scalar2=None, op0=ALU.add), reads=["ta"], writes=["tb"])
                        kb.op("dve", lambda e: e.tensor_scalar(out=tb[:, :], in0=tb[:, :], scalar1=MAGIC, scalar2=None, op0=ALU.subtract), reads=["tb"], writes=["tb"])
                        kb.op("dve", lambda e: e.tensor_tensor(out=ta[:, :], in0=ta[:, :], in1=tb[:, :], op=ALU.subtract), reads=["ta", "tb"], writes=["ta"])
                        kb.op("act", lambda e, sel=sel: e.activation(out=sinT[:, sel * 1024:(sel + 1) * 1024], in_=ta[:, :], func=AF.Sin, scale=TWO_PI),
                              reads=["ta"], writes=[("sinT", sel)])
                        kb.op("dve", lambda e: e.tensor_scalar(out=ta[:, :], in0=ta[:, :], scalar1=0.25, scalar2=None, op0=ALU.add), reads=["ta"], writes=["ta"])
                        kb.op("dve", lambda e: e.tensor_scalar(out=tb[:, :], in0=ta[:, :], scalar1=0.5, scalar2=None, op0=ALU.is_gt), reads=["ta"], writes=["tb"])
                        kb.op("dve", lambda e: e.tensor_tensor(out=ta[:, :], in0=ta[:, :], in1=tb[:, :], op=ALU.subtract), reads=["ta", "tb"], writes=["ta"])
                        kb.op("act", lambda e, sel=sel: e.activation(out=cosT[:, sel * 1024:(sel + 1) * 1024], in_=ta[:, :], func=AF.Sin, scale=TWO_PI),
                              reads=["ta"], writes=[("cosT", sel)])
                    items = [(c, g8) for c in range(NCH) for g8 in range(8)]

                    def ctx(k, item):
                        c, g8 = item
                        d = {"c": c, "g8": g8, "g": gc * 8 + g8, "ts": slice(c * 128, (c + 1) * 128), "samp": c * 128 >= SEQ}
                        d["sel"] = 1 if d["samp"] else 0
                        q = k % 4
                        d["pA"], d["kA"] = PS[q][:, 0:128], ("spbank", q)
                        d["pB"], d["kB"] = PS[q][:, 128:256], ("spbank", q)
                        d["pC"], d["kC"] = PS[q][:, 256:384], ("spbank", q)
                        d["w1"], d["kw1"] = w1[k % 4], ("w1", k % 4)
                        d["w2"], d["kw2"] = w2[k % 4], ("w2", k % 4)
                        d["zz"], d["kzz"] = zz[k % 4], ("zz", k % 4)
                        d["x1"], d["kx1"] = x1[k % 6], ("x1", k % 6)
                        d["x2"], d["kx2"] = x2[k % 4], ("x2", k % 4)
                        d["xb"], d["kxb"] = xb[k % 4], ("xb", k % 4)
                        return d

                    def sA(k, item):
                        d = ctx(k, item)
                        kb.op("pe", lambda e: e.matmul(d["pA"], lhsT=Bpv[:, d["g"], :], rhs=uTv[:, gc, d["ts"]], start=True, stop=True),
                              reads=["Bpad", ("uT", gc)], writes=[d["kA"]], inc=True)
                        kb.op("pe", lambda e: e.matmul(d["pB"], lhsT=Bsv[:, d["g"], :], rhs=uTv[:, gc, d["ts"]], start=True, stop=True),
                              reads=["Bspad", ("uT", gc)], writes=[d["kB"]], inc=True)

                    def sB(k, item):
                        d = ctx(k, item)
                        kb.op("dve", lambda e: e.tensor_tensor(out=d["w1"][:, :], in0=d["pA"], in1=cosv[:, d["sel"], d["g8"], :], op=ALU.mult),
                              reads=[d["kA"], ("cosT", d["sel"])], writes=[d["kw1"]])
                        kb.op("dve", lambda e: e.tensor_tensor(out=d["w2"][:, :], in0=d["pB"], in1=sinv[:, d["sel"], d["g8"], :], op=ALU.mult),
                              reads=[d["kB"], ("sinT", d["sel"])], writes=[d["kw2"]])

                    def sC(k, item):
                        d = ctx(k, item)
                        kb.op("pool", lambda e: e.tensor_tensor(out=d["w1"][:, :], in0=d["w1"][:, :], in1=d["w2"][:, :], op=ALU.add),
                              reads=[d["kw1"], d["kw2"]], writes=[d["kw1"]])

                    def sD(k, item):
                        d = ctx(k, item)
                        g = d["g"]
                        if not d["samp"]:
                            kb.op("dve", lambda e: e.tensor_tensor_scan(
                                out=d["zz"][:, :], data0=mag[:, g:g + 1].to_broadcast([128, 128]), data1=d["w1"][:, :], initial=xst[:, g:g + 1],
                                op0=ALU.mult, op1=ALU.add),
                                reads=[d["kw1"], "mag", ("xst", g)], writes=[d["kzz"]])
                        else:
                            kb.op("dve", lambda e: e.scalar_tensor_tensor(
                                out=d["w1"][:, 0:128:8], in0=x0Tv[:, :, g], scalar=mag[:, g:g + 1], in1=d["w1"][:, 0:128:8], op0=ALU.mult, op1=ALU.add),
                                reads=[d["kw1"], "mag", "x0T"], writes=[d["kw1"]])
                            kb.op("dve", lambda e: e.tensor_scalar(out=rms_[:, :], in0=smask[:, :], scalar1=mag[:, g:g + 1], scalar2=None, op0=ALU.mult),
                                  reads=["smask", "mag"], writes=["rms_"])
                            kb.op("dve", lambda e: e.tensor_tensor_scan(
                                out=d["zz"][:, :], data0=rms_[:, :], data1=d["w1"][:, :], initial=0.0, op0=ALU.mult, op1=ALU.add),
                                reads=[d["kw1"], "rms_"], writes=[d["kzz"]])

                    def sE(k, item):
                        d = ctx(k, item)
                        kb.op("pe", lambda e: e.matmul(d["pC"], lhsT=Pm[:, :], rhs=d["zz"][:, :], start=True, stop=True),
                              reads=["Pm", d["kzz"]], writes=[d["kC"]], inc=True)
                        kb.op("pool", lambda e: e.tensor_tensor(out=d["x1"][:, :], in0=d["zz"][:, :], in1=cosv[:, d["sel"], d["g8"], :], op=ALU.mult),
                              reads=[d["kzz"], ("cosT", d["sel"])], writes=[d["kx1"]])

                    def sF(k, item):
                        d = ctx(k, item)
                        kb.op("dve", lambda e: e.tensor_tensor(out=d["x2"][:, :], in0=d["pC"], in1=sinv[:, d["sel"], d["g8"], :], op=ALU.mult),
                              reads=[d["kC"], ("sinT", d["sel"])], writes=[d["kx2"]])

                    def sG(k, item):
                        d = ctx(k, item)
                        kb.op("pool", lambda e: e.tensor_tensor(out=d["x1"][:, :], in0=d["x1"][:, :], in1=d["x2"][:, :], op=ALU.add),
                              reads=[d["kx1"], d["kx2"]], writes=[d["kx1"]])

                    def sH(k, item):
                        d = ctx(k, item)
                        g = d["g"]
                        kb.op("act", lambda e: e.copy(d["xb"][:, :], d["x1"][:, :]), reads=[d["kx1"]], writes=[d["kxb"]])
                        if not d["samp"]:
                            kb.op("act", lambda e: e.copy(xst[:, g:g + 1], d["x1"][:, 127:128]), reads=[d["kx1"]], writes=[("xst", g)])
                        else:
                            kb.op("act", lambda e: e.copy(xfsv[:, g, :], d["x1"][:, 7:128:8]), reads=[d["kx1"]], writes=[("xfs", g)])

                    def sI(k, item):
                        d = ctx(k, item)
                        c, g8, g, ts = d["c"], d["g8"], d["g"], d["ts"]
                        yb = 6 + (c % 2)
                        kb.op("pe", lambda e: e.matmul(PS[yb][:, 0:128], lhsT=Cpv[:, g, :], rhs=d["xb"][:, :], start=(g8 == 0), stop=(g8 == 7)),
                              reads=["Cpad", d["kxb"]], writes=["ps%d" % yb], inc=True)

                    def sJ(k, item):
                        d = ctx(k, item)
                        c, g8, ts = d["c"], d["g8"], d["ts"]
                        if g8 != 7:
                            return
                        yb = 6 + (c % 2)
                        yv, yt, ysg = yvL[c % 2], ytL[c % 2], ysgL[c % 2]
                        kyv, kyt, kys = ("yv", c % 2), ("yt", c % 2), ("ysg", c % 2)
                        kb.op("dve", lambda e: e.scalar_tensor_tensor(out=yv[:, :], in0=uTv[:, gc, ts], scalar=dcol[:, gc:gc + 1], in1=PS[yb][:, 0:128],
                                                                     op0=ALU.mult, op1=ALU.add),
                              reads=["ps%d" % yb, ("uT", gc), "dcol"], writes=[kyv])
                        kb.op("act", lambda e: e.activation(out=yt[:, :], in_=yv[:, :], func=AF.Square), reads=[kyv], writes=[kyt])
                        kb.op("pool", lambda e: e.tensor_scalar(out=yt[:, :], in0=yt[:, :], scalar1=0.044715, scalar2=1.0, op0=ALU.mult, op1=ALU.add), reads=[kyt], writes=[kyt])
                        kb.op("pool", lambda e: e.tensor_tensor(out=yt[:, :], in0=yt[:, :], in1=yv[:, :], op=ALU.mult), reads=[kyt, kyv], writes=[kyt])
                        kb.op("act", lambda e: e.activation(out=ysg[:, :], in_=yt[:, :], func=AF.Sigmoid, scale=float(2.0 * np.sqrt(2.0 / np.pi))), reads=[kyt], writes=[kys])
                        yob = yo[c % 2]
                        kb.op("pool", lambda e: e.tensor_tensor(out=yob[:, :], in0=ysg[:, :], in1=yv[:, :], op=ALU.mult), reads=[kys, kyv], writes=[("yo", c % 2)])
                        kb.dma(brscr[:, gc, ts], yob[:, :], reads=[("yo", c % 2)], writes=[("brscr5", c, gc)])

                    run_pipeline(items, [sA, sB, sC, sD, sE, sF, sG, sH, sI, sJ])
                kb.op("pe", lambda e: e.transpose(PS[7][0:64, 0:128], xst[:, :], identf[:, :]), reads=[("xst", g) for g in range(64)] + ["identf"], writes=["ps7"], inc=True)
                kb.op("dve", lambda e: e.tensor_copy(fin[:, :].rearrange("p (n r) -> p r n", r=2), PS[7][0:64, 0:128].rearrange("p (r n) -> p r n", r=2)),
                      reads=["ps7"], writes=["s5fin"])
                kb.dma(s5_p[l].rearrange("g n r -> g (n r)"), fin[:, :], reads=["s5fin"], writes=[("s5_p", l)])
                for s in range(NSS):
                    kb.op("pe", lambda e, s=s: e.transpose(PS[7][0:64, 0:128], xfsv[:, :, s], identf[:, :]), reads=[("xfs", g) for g in range(64)] + ["identf"], writes=["ps7"], inc=True)
                    f2 = fin2[s % 2]
                    kb.op("dve", lambda e, f2=f2: e.tensor_copy(f2[:, :].rearrange("p (n r) -> p r n", r=2), PS[7][0:64, 0:128].rearrange("p (r n) -> p r n", r=2)),
                          reads=["ps7"], writes=[("fin2", s % 2)])
                    kb.dma(s5_s[l, s].rearrange("g n r -> g (n r)"), f2[:, :], reads=[("fin2", s % 2)], writes=[("s5_s", l, s)])
                kb.barrier()

    for l in range(NL + 1):
        phase_x(l)
        if l < NL:
            if stages.get("ret", True):
                retention(l)
                stage_c(l, "ret", W["ret_w_o"][l], 8, 9248, "retln%d" % l, W["ret_ln_g"][l])
            if stages.get("s5", True):
                s5(l)
                stage_c(l, "s5", W["s5_w_glu"][l], 8, 9248 + 1024, glu=True)
            if stages.get("ssd", True):
                ssd(l)
            if stages.get("ssd", True) and stages.get("ssd_s2", True) and stages.get("ssd_c", True):
                stage_c(l, "ssd", W["ssd_w_o"][l], 16, 9248 + 2048, "ssdn%d" % l, W["ssd_norm"][l])

    kb.finish()
    print("instructions:", kb.ninst, {e: kb.ccnt[e] for e in kb.ccnt})
    return nc, es


_CONSTS = None
STAGES = {"layers": DEPTH, "ffn1": True, "ffn2": True, "ret": True, "ssd": True, "s5": True}
WNAMES = ["ffn1_norm", "ffn1_w_gu", "ffn1_w_down", "ffn2_norm", "ffn2_w_gu", "ffn2_w_down", "mix_norm", "w_in", "ret_ln_g", "ret_w_o", "w_out",
          "ssd_conv_w", "ssd_conv_b", "ssd_dt_bias", "ssd_a_log", "ssd_d", "ssd_norm", "ssd_w_o",
          "s5_a_re", "s5_a_im", "s5_log_dt", "s5_b_re", "s5_b_im", "s5_c_re", "s5_c_im", "s5_d", "s5_w_glu"]


def make_in_map(inp, c, consts):
    xp = np.asarray(inp["x_prompt"])
    xs = np.asarray(inp["x_sample"])
    x_core = np.concatenate([xp[c], xs[c * NSS:(c + 1) * NSS].reshape(NSS * DSEQ, D)], axis=0)
    m = {"x_in": np.ascontiguousarray(x_core)}
    m.update(consts)
    for k in WNAMES:
        m[k] = np.asarray(inp[k])
    m["final_norm"] = np.asarray(inp["final_norm"]).reshape(1, D)
    for k in ["state_ret", "state_ssm", "state_conv", "state_s5"]:
        m[k] = np.ascontiguousarray(np.asarray(inp[k])[:, c * NSS:(c + 1) * NSS])
    return m


def kernel(**inp):
    nc, es = build(STAGES)
    consts = host_consts()
    in_maps = [make_in_map(inp, c, consts) for c in range(NCORES)]
    res = run_bass_kernel_spmd(nc, in_maps, core_ids=list(range(NCORES)))
    es.close()
    R = res.results
    ys = [r["y_out"] for r in R]
    y_prompt = np.stack([y[:SEQ] for y in ys], axis=0)
    y_sample = np.concatenate([y[SEQ:].reshape(NSS, DSEQ, D) for y in ys], axis=0)
    def pstack(k):
        return np.stack([r[k] for r in R], axis=1)

    def scat(k):
        return np.concatenate([r[k] for r in R], axis=1)

    return (y_prompt, y_sample, pstack("ret_p"), scat("ret_s"), pstack("s5_p"), scat("s5_s"),
            pstack("ssm_p"), scat("ssm_s"), pstack("conv_p"), scat("conv_s"))
```

```python
import contextlib
import numpy as np
import concourse.bass as bass
import concourse.mybir as mybir
from concourse.bass_utils import run_bass_kernel_spmd

F32 = mybir.dt.float32
BF16 = mybir.dt.bfloat16
ALU = mybir.AluOpType
AF = mybir.ActivationFunctionType

NCORES = 8
D = 1024
KC = 8
DEPTH = 4
SEQ = 2048
NSS = 16
DSEQ = 8
T = SEQ + NSS * DSEQ
NCH = T // 128
FFN = 2816
EPS = 1e-6
IN_DIM = 12320
TILES = [(0, 512), (512, 512), (1024, 512), (1536, 512), (2048, 128)]
NDS = 40


class KB:
    def __init__(self, nc, es):
        self.nc = nc
        self.E = {"pe": nc.tensor, "act": nc.scalar, "dve": nc.vector, "pool": nc.gpsimd, "sp": nc.sync}
        self.csem = {e: es.enter_context(nc.semaphore("s_" + e)) for e in ["pe", "act", "dve", "pool"]}
        self.ccnt = {e: 0 for e in self.csem}
        self.dsem = [es.enter_context(nc.semaphore("d%d" % i)) for i in range(NDS)]
        self.dcnt = [0] * NDS
        self.dnext = 0
        self.waited = {e: {} for e in self.E}
        self.writer = {}
        self.readers = {}
        self.ninst = 0

    def _wait(self, eng, tok):
        semid, sem, val, src = tok
        if self.waited[eng].get(semid, 0) >= val:
            return
        self.E[eng].wait_ge(sem, val)
        self.waited[eng][semid] = val

    def _sync(self, eng, reads, writes, is_dma=False):
        for k in reads:
            w = self.writer.get(k)
            if w is not None:
                if (not is_dma) and w[3] == eng and eng == "pe":
                    continue
                self._wait(eng, w)
        for k in writes:
            w = self.writer.get(k)
            if w is not None:
                if is_dma or w[3] != eng:
                    self._wait(eng, w)
            for r in self.readers.get(k, {}).values():
                if is_dma or r[3] != eng:
                    self._wait(eng, r)

    def _record(self, tok, reads, writes):
        for k in reads:
            self.readers.setdefault(k, {})[tok[0]] = tok
        for k in writes:
            self.writer[k] = tok
            self.readers[k] = {}

    def op(self, eng, fn, reads=(), writes=(), inc=True):
        self._sync(eng, reads, writes)
        ins = fn(self.E[eng])
        self.ninst += 1
        if inc:
            self.ccnt[eng] += 1
            ins.then_inc(self.csem[eng], 1)
            tok = (eng, self.csem[eng], self.ccnt[eng], eng)
        else:
            tok = (eng, self.csem[eng], self.ccnt[eng] + 1, eng)
        self._record(tok, reads, writes)
        return tok

    def dma(self, out, in_, reads=(), writes=(), q="sp"):
        self._sync(q, reads, writes, is_dma=True)
        i = self.dnext
        self.dnext = (i + 1) % NDS
        sid = "d%d" % i
        if self.dcnt[i] > 0:
            self._wait(q, (sid, self.dsem[i], self.dcnt[i], "dma"))
        self.dcnt[i] += 16
        self.E[q].dma_start(out=out, in_=in_).then_inc(self.dsem[i], 16)
        self.ninst += 1
        tok = (sid, self.dsem[i], self.dcnt[i], "dma")
        self._record(tok, reads, writes)
        return tok

    def finish(self):
        for i in range(NDS):
            if self.dcnt[i] > 0:
                self._wait("sp", ("d%d" % i, self.dsem[i], self.dcnt[i], "dma"))
        for e in ["pe", "act", "dve", "pool"]:
            if self.ccnt[e] > 0:
                self._wait("sp", (e, self.csem[e], self.ccnt[e], e))

    def barrier(self):
        toks = []
        for e in ["pe", "act", "dve", "pool"]:
            if self.ccnt[e] > 0:
                toks.append((e, self.csem[e], self.ccnt[e], e))
        for i in range(NDS):
            if self.dcnt[i] > 0:
                toks.append(("d%d" % i, self.dsem[i], self.dcnt[i], "dma"))
        for eng in ["pe", "act", "dve", "pool", "sp"]:
            for t in toks:
                if t[3] == eng:
                    continue
                self._wait(eng, t)
        self.writer.clear()
        self.readers.clear()


RET_HEADS = 4
GAM = [1.0 - 2.0 ** (-5.0 - h) for h in range(RET_HEADS)]


def host_consts():
    c = {}
    c["ident_f"] = np.eye(128, dtype=np.float32)
    pos = np.concatenate([np.arange(SEQ), np.tile(16384 + np.arange(DSEQ), NSS)]).astype(np.float64)
    inv = 10000.0 ** (-np.arange(64, dtype=np.float64) / 64.0)
    ang = (pos.astype(np.float32)[None, :] * inv.astype(np.float32)[:, None]).astype(np.float32).astype(np.float64)
    cos = np.concatenate([np.cos(ang), np.cos(ang)], axis=0)
    sinS = np.concatenate([-np.sin(ang), np.sin(ang)], axis=0)
    sc = 128.0 ** -0.5
    c["tabqk"] = np.stack([cos, sinS, cos * sc, sinS * sc], axis=1).astype(np.float32)
    idx = np.arange(128)
    qdec = np.zeros((128, 4, 640), np.float32)
    maskT = np.zeros((128, 2, 4, 128), np.float32)
    kdec = np.zeros((128, 2, 4), np.float32)
    sj = idx % 8
    seqj = idx // 8
    for h in range(4):
        g = GAM[h]
        qdec[:, h, 0:512] = np.tile(g ** (idx + 1.0), 4)[None, :]
        qdec[:, h, 512:640] = (g ** (sj + 1.0))[None, :]
        maskT[:, 0, h, :] = (g ** (-(idx[:, None] + 1.0))) * (idx[None, :] >= idx[:, None])
        maskT[:, 1, h, :] = (g ** (-(sj[:, None] + 1.0))) * ((sj[None, :] >= sj[:, None]) & (seqj[None, :] == seqj[:, None]))
        kdec[:, 0, h] = g ** (127.0 - idx)
        kdec[:, 1, h] = g ** (7.0 - sj)
    c["qdec"] = qdec
    c["maskT"] = maskT
    c["kdec"] = kdec
    c["rowmask"] = (seqj[:, None] == np.arange(16)[None, :]).astype(np.float32)
    same = (seqj[:, None] == seqj[None, :])
    le = (idx[:, None] <= idx[None, :])
    c["triu"] = np.stack([le, le & same], axis=1).astype(np.float32)
    c["negm"] = np.stack([np.where(le, 0.0, -30000.0), np.where(le & same, 0.0, -30000.0)], axis=1).astype(np.float32)
    c["ss"] = np.stack([np.ones((128, 128)), same], axis=1).astype(np.float32)
    tp = np.stack([idx + 1.0, sj + 1.0], axis=0)
    c["tpos"] = np.repeat(tp[None, :, :], 128, axis=0).astype(np.float32)
    c["smask"] = np.repeat((sj != 0)[None, :], 128, axis=0).astype(np.float32)
    pm = np.zeros((128, 128), np.float32)
    for m_ in range(64):
        pm[m_ + 64, m_] = -1.0
        pm[m_, m_ + 64] = 1.0
    c["Pm"] = pm
    c["gmask"] = ((idx[:, None] // 16) == np.arange(8)[None, :]).astype(np.float32)
    c["cmask"] = np.repeat(((idx[None, :] // 16) == np.arange(8)[:, None])[None, :, :], 128, axis=0).astype(np.float32)
    c["rm"] = np.repeat((seqj[:, None] == np.arange(16)[None, :])[:, :, None], 128, axis=2).astype(np.float32)
    return c


def build(stages):
    nc = bass.Bass("TRN2", target_bir_lowering=False)
    es = contextlib.ExitStack()
    es.enter_context(nc.allow_non_contiguous_dma(reason="small param / layout loads"))
    try:
        es.enter_context(nc.allow_low_precision(reason="bf16 matmul operands by design"))
    except Exception:
        pass
    NL = stages.get("layers", DEPTH)

    def din(name, shape, dt=F32):
        return nc.dram_tensor(name, list(shape), dt, kind="ExternalInput").ap()

    def dout(name, shape, dt=F32):
        return nc.dram_tensor(name, list(shape), dt, kind="ExternalOutput").ap()

    def dscr(name, shape, dt=F32):
        return nc.dram_tensor(name, list(shape), dt, kind="Internal").ap()

    x_in = din("x_in", [T, D])
    CST = {"ident_f": din("ident_f", [128, 128]), "tabqk": din("tabqk", [128, 4, T]), "qdec": din("qdec", [128, 4, 640]),
           "maskT": din("maskT", [128, 2, 4, 128]), "kdec": din("kdec", [128, 2, 4]), "rowmask": din("rowmask", [128, 16]),
           "triu": din("triu", [128, 2, 128]), "negm": din("negm", [128, 2, 128]), "ss": din("ss", [128, 2, 128]), "rm": din("rm", [128, 16, 128]),
           "tpos": din("tpos", [128, 2, 128]), "smask": din("smask", [128, 128]), "Pm": din("Pm", [128, 128]),
           "gmask": din("gmask", [128, 8]), "cmask": din("cmask", [128, 8, 128])}
    W = {}
    for nm, shp in [("ffn1_norm", [DEPTH, D]), ("ffn1_w_gu", [DEPTH, D, 2 * FFN]), ("ffn1_w_down", [DEPTH, FFN, D]),
                    ("ffn2_norm", [DEPTH, D]), ("ffn2_w_gu", [DEPTH, D, 2 * FFN]), ("ffn2_w_down", [DEPTH, FFN, D]),
                    ("final_norm", [1, D]), ("mix_norm", [DEPTH, D]), ("w_in", [DEPTH, D, IN_DIM]),
                    ("ret_ln_g", [DEPTH, D]), ("ret_w_o", [DEPTH, D, D]), ("w_out", [DEPTH, D, D]),
                    ("state_ret", [DEPTH, NSS, 4, 128, 256]),
                    ("s5_a_re", [DEPTH, 64, 64]), ("s5_a_im", [DEPTH, 64, 64]), ("s5_log_dt", [DEPTH, 64]),
                    ("s5_b_re", [DEPTH, 64, 64, 16]), ("s5_b_im", [DEPTH, 64, 64, 16]), ("s5_c_re", [DEPTH, 64, 16, 64]),
                    ("s5_c_im", [DEPTH, 64, 16, 64]), ("s5_d", [DEPTH, D]), ("s5_w_glu", [DEPTH, D, 2 * D]),
                    ("state_s5", [DEPTH, NSS, 64, 64, 2]),
                    ("ssd_conv_w", [DEPTH, 4, 3072]), ("ssd_conv_b", [DEPTH, 3072]), ("ssd_dt_bias", [DEPTH, 32]),
                    ("ssd_a_log", [DEPTH, 32]), ("ssd_d", [DEPTH, 32]), ("ssd_norm", [DEPTH, 2048]), ("ssd_w_o", [DEPTH, 2048, D]),
                    ("state_ssm", [DEPTH, NSS, 32, 64, 128]), ("state_conv", [DEPTH, NSS, 3, 3072])]:
        W[nm] = din(nm, shp)
    y_out = dout("y_out", [T, D])
    ret_p = dout("ret_p", [DEPTH, 4, 128, 256])
    ret_s = dout("ret_s", [DEPTH, NSS, 4, 128, 256])
    s5_p = dout("s5_p", [DEPTH, 64, 64, 2])
    s5_s = dout("s5_s", [DEPTH, NSS, 64, 64, 2])
    ssm_p = dout("ssm_p", [DEPTH, 32, 64, 128])
    ssm_s = dout("ssm_s", [DEPTH, NSS, 32, 64, 128])
    conv_p = dout("conv_p", [DEPTH, 3, 3072])
    conv_s = dout("conv_s", [DEPTH, NSS, 3, 3072])
    xsb_scr = dscr("xsb_scr", [T, 2560], BF16)
    bc_scr = dscr("bc_scr", [128, 8, T], BF16)
    xscr = dscr("xscr", [128, KC, T])
    brscr = dscr("brscr", [128, 16, T], BF16)

    kb = KB(nc, es)

    uniq = [0]

    def sbp(stack, name, shape, dt):
        uniq[0] += 1
        return stack.enter_context(nc.sbuf_tensor("sb%d_%s" % (uniq[0], name), list(shape), dt))

    hT = sbp(es, "hT", [128, KC * T], BF16)
    hTv = hT[:, :].rearrange("p (k t) -> p k t", k=KC)
    identf = sbp(es, "identf", [128, 128], F32)
    identb = sbp(es, "identb", [128, 128], BF16)
    ones_bf = sbp(es, "ones_bf", [128, 128], BF16)
    gcols = sbp(es, "gcols", [128, 24 * 16], F32)
    gcv = gcols[:, :].rearrange("p (s k) -> p s k", k=16)
    sqb = sbp(es, "sqb", [128, KC * 512], BF16)
    sqv = sqb[:, :].rearrange("p (k t) -> p k t", k=KC)
    sdb = sbp(es, "sdb", [128, 512], F32)
    rstd = sbp(es, "rstd", [128, 512], F32)
    wst = [sbp(es, "wst%d" % i, [128, 2048], F32) for i in range(2)]
    wbf = [sbp(es, "wbf%d" % i, [128, 2048], BF16) for i in range(2)]
    PS = [es.enter_context(nc.psum_tensor("ps%d" % i, [128, 512], F32)) for i in range(8)]

    kb.dma(identf[:, :], CST["ident_f"][:, :], writes=["identf"])
    kb.op("dve", lambda e: e.tensor_copy(identb[:, :], identf[:, :]), reads=["identf"], writes=["identb"])
    kb.op("dve", lambda e: e.memset(ones_bf[:, :], 1.0), writes=["ones_bf"])

    gslot = {}

    def load_gain(name, ap_row, nk=KC):
        s = len(gslot) % 24
        gslot[name] = s
        kb.dma(gcv[:, s, 0:nk], ap_row.rearrange("(k p) -> p k", p=128), writes=[("g", s)])
        return s

    def ck(name, t0, n):
        return [(name, c) for c in range(t0 // 128, (t0 + n) // 128)]

    wslot = [0]

    def load_w(pieces, kcn, ncols, dst=None, dstkey=None, rowscale=None):
        s = wslot[0] % len(wst)
        wslot[0] = (s + 1) % len(wst)
        st = wst[s][:, 0:kcn * ncols].rearrange("p (k n) -> p k n", k=kcn)
        for (src, off) in pieces:
            wdt = src.shape[1]
            kb.dma(st[:, :, off:off + wdt], src.rearrange("(k p) n -> p k n", p=128), writes=[("wst", s)])
        if dst is None:
            bf = wbf[s][:, 0:kcn * ncols].rearrange("p (k n) -> p k n", k=kcn)
            key = ("wbf", s)
        else:
            bf = dst
            key = dstkey
        if rowscale is None:
            kb.op("pool", lambda e: e.tensor_copy(bf, st), reads=[("wst", s)], writes=[key])
        else:
            for k in range(kcn):
                kb.op("pool", lambda e, k=k: e.tensor_scalar(out=bf[:, k, :], in0=st[:, k, :], scalar1=gcv[:, rowscale, k:k + 1],
                                                           scalar2=None, op0=ALU.mult),
                      reads=[("wst", s), ("g", rowscale)], writes=[key])
        return bf, key

    def rmsnorm(xTv, gs, dst_fn, dst_keys_fn):
        for (t0, n) in TILES:
            for kc in range(KC):
                kb.op("act", lambda e, kc=kc, t0=t0, n=n: e.activation(
                    out=sqv[:, kc, 0:n], in_=xTv[:, kc, t0:t0 + n], func=AF.Square),
                    reads=ck("x", t0, n), writes=[("sq", kc)])
            for kc in range(KC):
                kb.op("pe", lambda e, kc=kc, n=n: e.matmul(
                    PS[6][:, 0:n], lhsT=ones_bf[:, :], rhs=sqv[:, kc, 0:n], start=(kc == 0), stop=(kc == KC - 1)),
                    reads=["ones_bf", ("sq", kc)], writes=["ps6"], inc=(kc == KC - 1))
            kb.op("act", lambda e, n=n: e.activation(
                out=sdb[:, 0:n], in_=PS[6][:, 0:n], func=AF.Sqrt, scale=1.0 / D, bias=EPS),
                reads=["ps6"], writes=["sdb"])
            kb.op("dve", lambda e, n=n: e.reciprocal(rstd[:, 0:n], sdb[:, 0:n]), reads=["sdb"], writes=["rstd"])
            for kc in range(KC):
                kb.op("dve", lambda e, kc=kc, t0=t0, n=n: e.scalar_tensor_tensor(
                    out=dst_fn(kc, t0, n), in0=xTv[:, kc, t0:t0 + n], scalar=gcv[:, gs, kc:kc + 1],
                    in1=rstd[:, 0:n], op0=ALU.mult, op1=ALU.mult),
                    reads=ck("x", t0, n) + ["rstd", ("g", gs)], writes=dst_keys_fn(kc, t0, n))

    def norm_to_h(xTv, gs):
        rmsnorm(xTv, gs, lambda kc, t0, n: hTv[:, kc, t0:t0 + n], lambda kc, t0, n: ck("h", t0, n))

    def ffn(P, xTv, l, pre):
        actb = P["actb"]
        sgb = P["sgb"]
        gs = load_gain("%s_norm%d" % (pre, l), W[pre + "_norm"][l])
        norm_to_h(xTv, gs)
        wgu = W[pre + "_w_gu"][l]
        wdn = W[pre + "_w_down"][l]
        NHG = FFN // 256
        gu_bank = 0
        dn_bank = 0
        for hg in range(NHG):
            ab = actb[hg % 2]
            abv = ab[:, :].rearrange("p (b t) -> p b t", b=2)
            kab = ("actb", hg % 2)
            wg, kwg = load_w([(wgu[:, hg * 256:(hg + 1) * 256], 0)], KC, 256)
            wu, kwu = load_w([(wgu[:, FFN + hg * 256:FFN + (hg + 1) * 256], 0)], KC, 256)
            for (t0, n) in TILES:
                for blk in range(2):
                    bg = gu_bank % 4
                    bu = (gu_bank + 1) % 4
                    gu_bank += 2
                    for kc in range(KC):
                        kb.op("pe", lambda e, kc=kc, bg=bg, blk=blk, t0=t0, n=n, wg=wg: e.matmul(
                            PS[bg][:, 0:n], lhsT=wg[:, kc, blk * 128:(blk + 1) * 128], rhs=hTv[:, kc, t0:t0 + n],
                            start=(kc == 0), stop=(kc == KC - 1)),
                            reads=[kwg] + ck("h", t0, n), writes=["ps%d" % bg], inc=(kc == KC - 1))
                    for kc in range(KC):
                        kb.op("pe", lambda e, kc=kc, bu=bu, blk=blk, t0=t0, n=n, wu=wu: e.matmul(
                            PS[bu][:, 0:n], lhsT=wu[:, kc, blk * 128:(blk + 1) * 128], rhs=hTv[:, kc, t0:t0 + n],
                            start=(kc == 0), stop=(kc == KC - 1)),
                            reads=[kwu] + ck("h", t0, n), writes=["ps%d" % bu], inc=(kc == KC - 1))
                    sg = sgb[(gu_bank // 2) % 2]
                    ksg = ("sgb", (gu_bank // 2) % 2)
                    kb.op("act", lambda e, bg=bg, n=n, sg=sg: e.activation(
                        out=sg[:, 0:n], in_=PS[bg][:, 0:n], func=AF.Silu),
                        reads=["ps%d" % bg], writes=[ksg])
                    kb.op("dve", lambda e, bu=bu, n=n, sg=sg, blk=blk, t0=t0, abv=abv: e.tensor_tensor(
                        out=abv[:, blk, t0:t0 + n], in0=PS[bu][:, 0:n], in1=sg[:, 0:n], op=ALU.mult),
                        reads=["ps%d" % bu, ksg], writes=[kab])
            wd, kwd = load_w([(wdn[hg * 256:(hg + 1) * 256, :], 0)], 2, D)
            for (t0, n) in TILES:
                for oc in range(KC):
                    bo = 4 + (dn_bank % 2)
                    dn_bank += 1
                    for blk in range(2):
                        kb.op("pe", lambda e, blk=blk, bo=bo, oc=oc, t0=t0, n=n, wd=wd, abv=abv: e.matmul(
                            PS[bo][:, 0:n], lhsT=wd[:, blk, oc * 128:(oc + 1) * 128], rhs=abv[:, blk, t0:t0 + n],
                            start=(blk == 0), stop=(blk == 1)),
                            reads=[kwd, kab], writes=["ps%d" % bo], inc=(blk == 1))
                    kb.op("dve", lambda e, bo=bo, oc=oc, t0=t0, n=n: e.scalar_tensor_tensor(
                        out=xTv[:, oc, t0:t0 + n], in0=PS[bo][:, 0:n], scalar=0.5, in1=xTv[:, oc, t0:t0 + n],
                        op0=ALU.mult, op1=ALU.add),
                        reads=["ps%d" % bo] + ck("x", t0, n), writes=ck("x", t0, n))

    def run_pipeline(items, stage_fns):
        n = len(items)
        S = len(stage_fns)
        for step in range(n + S - 1):
            for si, fn in enumerate(stage_fns):
                k = step - si
                if 0 <= k < n:
                    fn(k, items[k])

    def phase_x(l):
        with contextlib.ExitStack() as pes:
            xT = sbp(pes, "xT", [128, KC * T], F32)
            xTv = xT[:, :].rearrange("p (k t) -> p k t", k=KC)
            P = {"actb": [sbp(pes, "actb%d" % i, [128, 2 * T], BF16) for i in range(2)],
                 "sgb": [sbp(pes, "sgb%d" % i, [128, 512], BF16) for i in range(2)]}
            iobuf = [sbp(pes, "iobuf%d" % i, [128, D], F32) for i in range(2)]
            for i in range(2, 4):
                wst.append(sbp(pes, "wstx%d" % i, [128, 2048], F32))
                wbf.append(sbp(pes, "wbfx%d" % i, [128, 2048], BF16))
            if l == 0:
                for c in range(NCH):
                    io = iobuf[c % 2]
                    kio = "io%d" % (c % 2)
                    kb.dma(io[:, :], x_in[c * 128:(c + 1) * 128, :], writes=[kio])
                    for half in range(2):
                        bank = PS[6 + half]
                        kbk = "ps%d" % (6 + half)
                        for j in range(4):
                            kc = half * 4 + j
                            kb.op("pe", lambda e, j=j, kc=kc, bank=bank, io=io: e.transpose(
                                bank[:, j * 128:(j + 1) * 128], io[:, kc * 128:(kc + 1) * 128], identf[:, :]),
                                reads=[kio, "identf"], writes=[kbk], inc=(j == 3))
                        if half == 0:
                            kb.op("act", lambda e, bank=bank, c=c: e.copy(
                                xTv[:, 0:4, c * 128:(c + 1) * 128], bank[:, :].rearrange("p (k t) -> p k t", k=4)),
                                reads=[kbk], writes=[("x", c)])
                        else:
                            kb.op("dve", lambda e, bank=bank, c=c: e.tensor_copy(
                                xTv[:, 4:8, c * 128:(c + 1) * 128], bank[:, :].rearrange("p (k t) -> p k t", k=4)),
                                reads=[kbk], writes=[("x", c)])
            else:
                for (t0, n) in TILES:
                    kb.dma(xTv[:, :, t0:t0 + n], xscr[:, :, t0:t0 + n], reads=[("xscr", t0)], writes=ck("x", t0, n))
                if stages.get("ffn2", True):
                    ffn(P, xTv, l - 1, "ffn2")
            if l < NL:
                if stages.get("ffn1", True):
                    ffn(P, xTv, l, "ffn1")
                gs = load_gain("mix%d" % l, W["mix_norm"][l])
                norm_to_h(xTv, gs)
                for (t0, n) in TILES:
                    kb.dma(xscr[:, :, t0:t0 + n], xTv[:, :, t0:t0 + n], reads=ck("x", t0, n), writes=[("xscr", t0)])
            else:
                gs = load_gain("final", W["final_norm"][0])
                finb = sbp(pes, "finb", [128, 4096], F32)
                fin = finb[:, :].rearrange("p (k t) -> p k t", k=KC)
                for (t0, n) in TILES:
                    rm_tiles = [(t0, n)]
                    for kc in range(KC):
                        kb.op("act", lambda e, kc=kc, t0=t0, n=n: e.activation(
                            out=sqv[:, kc, 0:n], in_=xTv[:, kc, t0:t0 + n], func=AF.Square),
                            reads=ck("x", t0, n), writes=[("sq", kc)])
                    for kc in range(KC):
                        kb.op("pe", lambda e, kc=kc, n=n: e.matmul(
                            PS[6][:, 0:n], lhsT=ones_bf[:, :], rhs=sqv[:, kc, 0:n], start=(kc == 0), stop=(kc == KC - 1)),
                            reads=["ones_bf", ("sq", kc)], writes=["ps6"], inc=(kc == KC - 1))
                    kb.op("act", lambda e, n=n: e.activation(
                        out=sdb[:, 0:n], in_=PS[6][:, 0:n], func=AF.Sqrt, scale=1.0 / D, bias=EPS),
                        reads=["ps6"], writes=["sdb"])
                    kb.op("dve", lambda e, n=n: e.reciprocal(rstd[:, 0:n], sdb[:, 0:n]), reads=["sdb"], writes=["rstd"])
                    for kc in range(KC):
                        kb.op("dve", lambda e, kc=kc, t0=t0, n=n: e.scalar_tensor_tensor(
                            out=fin[:, kc, 0:n], in0=xTv[:, kc, t0:t0 + n], scalar=gcv[:, gs, kc:kc + 1],
                            in1=rstd[:, 0:n], op0=ALU.mult, op1=ALU.mult),
                            reads=ck("x", t0, n) + ["rstd", ("g", gs)], writes=["fin"])
                    for c in range(n // 128):
                        io = iobuf[c % 2]
                        kio = "io%d" % (c % 2)
                        for half in range(2):
                            bank = PS[half]
                            kbk = "ps%d" % half
                            for j in range(4):
                                kc = half * 4 + j
                                kb.op("pe", lambda e, j=j, kc=kc, bank=bank, c=c: e.transpose(
                                    bank[:, j * 128:(j + 1) * 128], fin[:, kc, c * 128:(c + 1) * 128], identf[:, :]),
                                    reads=["fin", "identf"], writes=[kbk], inc=(j == 3))
                            if half == 0:
                                kb.op("act", lambda e, bank=bank, io=io: e.copy(io[:, 0:512], bank[:, :]),
                                      reads=[kbk], writes=[(kio, half)])
                            else:
                                kb.op("dve", lambda e, bank=bank, io=io: e.tensor_copy(io[:, 512:1024], bank[:, :]),
                                      reads=[kbk], writes=[(kio, half)])
                        kb.dma(y_out[t0 + c * 128:t0 + (c + 1) * 128, :], io[:, :], reads=[(kio, 0), (kio, 1)],
                               writes=[("yout", t0, c)])
            kb.barrier()
            del wst[2:]
            del wbf[2:]
            wslot[0] = 0

    def retention(l):
        win = W["w_in"][l]
        with contextlib.ExitStack() as pes:
            Wv = sbp(pes, "Wv", [128, KC * 1024], BF16)
            Wg = sbp(pes, "Wg", [128, KC * 1024], BF16)
            Wvv = Wv[:, :].rearrange("p (k n) -> p k n", k=KC)
            Wgv = Wg[:, :].rearrange("p (k n) -> p k n", k=KC)
            qd = sbp(pes, "qd", [128, 4 * 512], BF16)
            kT = sbp(pes, "kT", [128, 4 * 512], BF16)
            qdv = qd[:, :].rearrange("p (h t) -> p h t", h=4)
            kTv = kT[:, :].rearrange("p (h t) -> p h t", h=4)
            tab = [sbp(pes, "tab%d" % i, [128, 4 * 512], F32) for i in range(1)]
            qdec = sbp(pes, "qdec", [128, 4 * 640], F32)
            qdecv = qdec[:, :].rearrange("p (h t) -> p h t", h=4)
            maskT = sbp(pes, "maskT", [128, 2 * 512], F32)
            maskTv = maskT[:, :].rearrange("p (s n) -> p s n", s=2)
            kdec = sbp(pes, "kdec", [128, 8], F32)
            rowmask = sbp(pes, "rowmask", [128, 16], F32)
            tmpA = sbp(pes, "tmpA", [128, 512], F32)
            tmpB = sbp(pes, "tmpB", [128, 512], F32)
            tmpC = sbp(pes, "tmpC", [128, 512], F32)
            tmpD = sbp(pes, "tmpD", [128, 512], F32)
            v_tm = sbp(pes, "v_tm", [128, 1024], BF16)
            sg = sbp(pes, "sg", [128, 1024], BF16)
            kd_tm = sbp(pes, "kd_tm", [128, 512], BF16)
            kdm = sbp(pes, "kdm", [128, 512], BF16)
            sc = sbp(pes, "sc", [128, 512], BF16)
            S_f = sbp(pes, "S_f", [128, 1024], F32)
            S_b = sbp(pes, "S_b", [128, 1024], BF16)
            S0f = [sbp(pes, "S0f%d" % i, [128, 1024], F32) for i in range(2)]
            S0b = [sbp(pes, "S0b%d" % i, [128, 1024], BF16) for i in range(2)]
            Snew = [sbp(pes, "Snew%d" % i, [128, 1024], F32) for i in range(2)]
            qblk = sbp(pes, "qblk", [128, 4 * 2048], BF16)
            qblkv = qblk[:, :].rearrange("p (h s t) -> p h s t", h=4, s=16)
            bst = sbp(pes, "bst", [128, 4 * 6], F32)
            mv = sbp(pes, "mv", [128, 4 * 2], F32)
            mvv = mv[:, :].rearrange("p (h t) -> p h t", h=4)
            sdv = sbp(pes, "sdv", [128, 4], F32)
            rsv = sbp(pes, "rsv", [128, 4], F32)
            on = sbp(pes, "on", [128, 1024], BF16)
            og = sbp(pes, "og", [128, 1024], BF16)
            ogT = [sbp(pes, "ogT%d" % i, [128, 1024], BF16) for i in range(2)]

            kb.dma(qdecv, CST["qdec"], writes=["qdec"])
            kb.dma(maskT[:, :].rearrange("p (s h n) -> p s h n", s=2, h=4), CST["maskT"], writes=["maskT"])
            kb.dma(kdec[:, :].rearrange("p (s h) -> p s h", s=2), CST["kdec"], writes=["kdec"])
            kb.dma(rowmask[:, :], CST["rowmask"], writes=["rowmask"])
            kb.op("pool", lambda e: e.memset(qblk[:, :], 0.0), writes=["qblk"])
            kb.op("pool", lambda e: e.memset(S_f[:, :], 0.0), writes=["S_f"])
            kb.op("pool", lambda e: e.memset(S_b[:, :], 0.0), writes=["S_b"])
            for j in range(4):
                load_w([(win[:, 1024 + j * 256:1024 + (j + 1) * 256], 0)], KC, 256, dst=Wvv[:, :, j * 256:(j + 1) * 256], dstkey="Wv")
            for j in range(4):
                load_w([(win[:, 2048 + j * 256:2048 + (j + 1) * 256], 0)], KC, 256, dst=Wgv[:, :, j * 256:(j + 1) * 256], dstkey="Wg")

            for ti, (t0, n) in enumerate(TILES):
                tb = tab[0]
                tbv = tb[:, :].rearrange("p (s t) -> p s t", s=4)
                ktb = ("tab", 0)
                kb.dma(tbv[:, :, 0:n], CST["tabqk"][:, :, t0:t0 + n], writes=[ktb])
                samp = (t0 >= SEQ)
                sel = 1 if samp else 0
                qoff = 512 if samp else 0
                for h in range(4):
                    c0 = h * 128
                    k0 = 512 + h * 128
                    wq, kwq = load_w([(win[:, c0:c0 + 128], 0), (win[:, c0 + 64:c0 + 128], 128), (win[:, c0:c0 + 64], 192)], KC, 256)
                    wk, kwk = load_w([(win[:, k0:k0 + 128], 0), (win[:, k0 + 64:k0 + 128], 128), (win[:, k0:k0 + 64], 192)], KC, 256)
                    for b in range(4):
                        ww, kww = (wq, kwq) if b < 2 else (wk, kwk)
                        for kc in range(KC):
                            kb.op("pe", lambda e, kc=kc, b=b, ww=ww, t0=t0, n=n: e.matmul(
                                PS[b][:, 0:n], lhsT=ww[:, kc, (b % 2) * 128:(b % 2 + 1) * 128], rhs=hTv[:, kc, t0:t0 + n],
                                start=(kc == 0), stop=(kc == KC - 1)),
                                reads=[kww] + ck("h", t0, n), writes=["ps%d" % b], inc=(kc == KC - 1))
                    kb.op("dve", lambda e, n=n, tbv=tbv: e.tensor_tensor(out=tmpA[:, 0:n], in0=PS[0][:, 0:n], in1=tbv[:, 0, 0:n], op=ALU.mult),
                          reads=["ps0", ktb], writes=["tmpA"])
                    kb.op("dve", lambda e, n=n, tbv=tbv: e.tensor_tensor(out=tmpB[:, 0:n], in0=PS[1][:, 0:n], in1=tbv[:, 1, 0:n], op=ALU.mult),
                          reads=["ps1", ktb], writes=["tmpB"])
                    kb.op("pool", lambda e, n=n: e.tensor_tensor(out=tmpA[:, 0:n], in0=tmpA[:, 0:n], in1=tmpB[:, 0:n], op=ALU.add),
                          reads=["tmpA", "tmpB"], writes=["tmpA"])
                    kb.op("pool", lambda e, n=n, h=h, qoff=qoff: e.tensor_tensor(
                        out=qdv[:, h, 0:n], in0=tmpA[:, 0:n], in1=qdecv[:, h, qoff:qoff + n], op=ALU.mult),
                        reads=["tmpA", "qdec"], writes=[("qd", h)])
                    kb.op("dve", lambda e, n=n, tbv=tbv: e.tensor_tensor(out=tmpC[:, 0:n], in0=PS[2][:, 0:n], in1=tbv[:, 2, 0:n], op=ALU.mult),
                          reads=["ps2", ktb], writes=["tmpC"])
                    kb.op("dve", lambda e, n=n, tbv=tbv: e.tensor_tensor(out=tmpD[:, 0:n], in0=PS[3][:, 0:n], in1=tbv[:, 3, 0:n], op=ALU.mult),
                          reads=["ps3", ktb], writes=["tmpD"])
                    kb.op("pool", lambda e, n=n, h=h: e.tensor_tensor(out=kTv[:, h, 0:n], in0=tmpC[:, 0:n], in1=tmpD[:, 0:n], op=ALU.add),
                          reads=["tmpC", "tmpD"], writes=[("kT", h)])
                QD = [("qd", h) for h in range(4)]
                KT = [("kT", h) for h in range(4)]
                for ci in range(n // 128):
                    c = t0 // 128 + ci
                    cs = slice(ci * 128, (ci + 1) * 128)
                    ts = slice(t0 + ci * 128, t0 + (ci + 1) * 128)
                    for half in range(2):
                        for kc in range(KC):
                            kb.op("pe", lambda e, kc=kc, half=half, ts=ts: e.matmul(
                                PS[half][:, :], lhsT=hTv[:, kc, ts], rhs=Wvv[:, kc, half * 512:(half + 1) * 512],
                                start=(kc == 0), stop=(kc == KC - 1)),
                                reads=["Wv", ("h", c)], writes=["ps%d" % half], inc=(kc == KC - 1))
                        kb.op("act", lambda e, half=half: e.copy(v_tm[:, half * 512:(half + 1) * 512], PS[half][:, :]),
                              reads=["ps%d" % half], writes=[("v_tm", half)])
                    for half in range(2):
                        for kc in range(KC):
                            kb.op("pe", lambda e, kc=kc, half=half, ts=ts: e.matmul(
                                PS[2 + half][:, :], lhsT=hTv[:, kc, ts], rhs=Wgv[:, kc, half * 512:(half + 1) * 512],
                                start=(kc == 0), stop=(kc == KC - 1)),
                                reads=["Wg", ("h", c)], writes=["ps%d" % (2 + half)], inc=(kc == KC - 1))
                        kb.op("act", lambda e, half=half: e.activation(out=sg[:, half * 512:(half + 1) * 512], in_=PS[2 + half][:, :], func=AF.Silu),
                              reads=["ps%d" % (2 + half)], writes=[("sg", half)])
                    VT = [("v_tm", 0), ("v_tm", 1)]
                    p4 = PS[4][:, :].bitcast(BF16)
                    for h in range(4):
                        kb.op("pe", lambda e, h=h, cs=cs: e.transpose(p4[:, h * 128:(h + 1) * 128], kTv[:, h, cs], identb[:, :]),
                              reads=[("kT", h), "identb"], writes=["ps4"], inc=(h == 3))
                    for h in range(4):
                        kb.op("dve", lambda e, h=h, sel=sel: e.tensor_scalar(
                            out=kd_tm[:, h * 128:(h + 1) * 128], in0=p4[:, h * 128:(h + 1) * 128],
                            scalar1=kdec[:, sel * 4 + h:sel * 4 + h + 1], scalar2=None, op0=ALU.mult),
                            reads=["ps4", "kdec"], writes=["kd_tm"])
                    for h in range(4):
                        kb.op("pe", lambda e, h=h, cs=cs: e.matmul(PS[5][:, h * 128:(h + 1) * 128], lhsT=kTv[:, h, cs], rhs=qdv[:, h, cs],
                                                                 start=True, stop=True),
                              reads=[("kT", h), ("qd", h)], writes=["ps5"], inc=(h == 3))
                    kb.op("dve", lambda e, sel=sel: e.tensor_tensor(out=sc[:, :], in0=PS[5][:, :], in1=maskTv[:, sel, :], op=ALU.mult),
                          reads=["ps5", "maskT"], writes=["sc"])
                    if not samp:
                        for h in range(4):
                            ob = PS[6 + h // 2][:, (h % 2) * 256:(h % 2) * 256 + 256]
                            kb.op("pe", lambda e, h=h, ob=ob: e.matmul(ob, lhsT=sc[:, h * 128:(h + 1) * 128], rhs=v_tm[:, h * 256:(h + 1) * 256],
                                                                     start=True, stop=False),
                                  reads=["sc"] + VT, writes=["ps%d" % (6 + h // 2)], inc=False)
                            kb.op("pe", lambda e, h=h, ob=ob, cs=cs: e.matmul(ob, lhsT=qdv[:, h, cs], rhs=S_b[:, h * 256:(h + 1) * 256],
                                                                            start=False, stop=True),
                                  reads=[("qd", h), "S_b"], writes=["ps%d" % (6 + h // 2)], inc=True)
                        for h in range(4):
                            sbk = PS[h // 2][:, (h % 2) * 256:(h % 2) * 256 + 256]
                            kb.op("pe", lambda e, h=h, sbk=sbk: e.matmul(sbk, lhsT=kd_tm[:, h * 128:(h + 1) * 128], rhs=v_tm[:, h * 256:(h + 1) * 256],
                                                                       start=True, stop=True),
                                  reads=["kd_tm"] + VT, writes=["ps%d" % (h // 2)], inc=True)
                            kb.op("dve", lambda e, h=h, sbk=sbk: e.scalar_tensor_tensor(
                                out=S_f[:, h * 256:(h + 1) * 256], in0=S_f[:, h * 256:(h + 1) * 256], scalar=float(GAM[h] ** 128),
                                in1=sbk, op0=ALU.mult, op1=ALU.add),
                                reads=["ps%d" % (h // 2), "S_f"], writes=["S_f"])
                        kb.op("act", lambda e: e.copy(S_b[:, :], S_f[:, :]), reads=["S_f"], writes=["S_b"])
                        if c == SEQ // 128 - 1:
                            kb.dma(ret_p[l].rearrange("h d e -> d h e"), S_f[:, :].rearrange("p (h e) -> p h e", h=4),
                                   reads=["S_f"], writes=[("ret_p", l)])
                    else:
                        for h in range(4):
                            ob = PS[6 + h // 2][:, (h % 2) * 256:(h % 2) * 256 + 256]
                            for s in range(NSS):
                                kb.op("pool", lambda e, h=h, s=s: e.tensor_copy(qblkv[:, h, s, s * 8:(s + 1) * 8], qdv[:, h, s * 8:(s + 1) * 8]),
                                      reads=[("qd", h)], writes=["qblk"])
                            kb.op("pe", lambda e, h=h, ob=ob: e.matmul(ob, lhsT=sc[:, h * 128:(h + 1) * 128], rhs=v_tm[:, h * 256:(h + 1) * 256],
                                                                     start=True, stop=False),
                                  reads=["sc"] + VT, writes=["ps%d" % (6 + h // 2)], inc=False)
                            for s in range(NSS):
                                i2 = (h * NSS + s) % 2
                                s0f = S0f[i2]
                                s0b = S0b[i2]
                                sn = Snew[i2]
                                kb.dma(s0f[:, 0:256], W["state_ret"][l, s, h], writes=[("S0f", i2)])
                                kb.op("act", lambda e, s0f=s0f, s0b=s0b: e.copy(s0b[:, 0:256], s0f[:, 0:256]), reads=[("S0f", i2)], writes=[("S0b", i2)])
                                kb.op("pe", lambda e, h=h, ob=ob, s=s, s0b=s0b: e.matmul(
                                    ob, lhsT=qblkv[:, h, s, :], rhs=s0b[:, 0:256], start=False, stop=(s == NSS - 1)),
                                    reads=["qblk", ("S0b", i2)], writes=["ps%d" % (6 + h // 2)], inc=True)
                                kb.op("dve", lambda e, s=s, h=h: e.tensor_scalar(out=kdm[:, 0:128], in0=kd_tm[:, h * 128:(h + 1) * 128],
                                                                               scalar1=rowmask[:, s:s + 1], scalar2=None, op0=ALU.mult),
                                      reads=["kd_tm", "rowmask"], writes=["kdm"])
                                sbk = PS[i2][:, 0:256]
                                kb.op("pe", lambda e, h=h, sbk=sbk: e.matmul(sbk, lhsT=kdm[:, 0:128], rhs=v_tm[:, h * 256:(h + 1) * 256],
                                                                           start=True, stop=True),
                                      reads=["kdm"] + VT, writes=["ps%d" % i2], inc=True)
                                kb.op("dve", lambda e, h=h, sbk=sbk, s0f=s0f, sn=sn: e.scalar_tensor_tensor(
                                    out=sn[:, 0:256], in0=s0f[:, 0:256], scalar=float(GAM[h] ** 8),
                                    in1=sbk, op0=ALU.mult, op1=ALU.add),
                                    reads=["ps%d" % i2, ("S0f", i2)], writes=[("Snew", i2)])
                                kb.dma(ret_s[l, s, h], sn[:, 0:256], reads=[("Snew", i2)], writes=[("ret_s", l, s, h)])
                    for h in range(4):
                        ob = PS[6 + h // 2][:, (h % 2) * 256:(h % 2) * 256 + 256]
                        kb.op("dve", lambda e, h=h, ob=ob: e.bn_stats(bst[:, h * 6:(h + 1) * 6], ob), reads=["ps%d" % (6 + h // 2)], writes=[("bst", h)])
                        kb.op("dve", lambda e, h=h: e.bn_aggr(mv[:, h * 2:(h + 1) * 2], bst[:, h * 6:(h + 1) * 6]), reads=[("bst", h)], writes=[("mv", h)])
                    MV = [("mv", h) for h in range(4)]
                    kb.op("act", lambda e: e.activation(out=sdv[:, :], in_=mvv[:, :, 1], func=AF.Sqrt, bias=EPS, scale=1.0), reads=MV, writes=["sdv"])
                    kb.op("dve", lambda e: e.reciprocal(rsv[:, :], sdv[:, :]), reads=["sdv"], writes=["rsv"])
                    for h in range(4):
                        ob = PS[6 + h // 2][:, (h % 2) * 256:(h % 2) * 256 + 256]
                        kb.op("dve", lambda e, h=h, ob=ob: e.tensor_scalar(
                            out=on[:, h * 256:(h + 1) * 256], in0=ob, scalar1=mvv[:, h, 0:1], scalar2=rsv[:, h:h + 1],
                            op0=ALU.subtract, op1=ALU.mult),
                            reads=["ps%d" % (6 + h // 2), "rsv"] + MV, writes=["on"])
                    kb.op("pool", lambda e: e.tensor_tensor(out=og[:, :], in0=on[:, :], in1=sg[:, :], op=ALU.mult),
                          reads=["on", ("sg", 0), ("sg", 1)], writes=["og"])
                    ogt = ogT[c % 2]
                    for kc in range(KC):
                        kb.op("pe", lambda e, kc=kc: e.transpose(p4[:, kc * 128:(kc + 1) * 128], og[:, kc * 128:(kc + 1) * 128], identb[:, :]),
                              reads=["og", "identb"], writes=["ps4"], inc=(kc == KC - 1))
                    kb.op("act", lambda e, ogt=ogt: e.copy(ogt[:, :], p4[:, :]), reads=["ps4"], writes=[("ogT", c % 2)])
                    kb.dma(brscr[:, 0:KC, ts], ogt[:, :].rearrange("p (k t) -> p k t", k=KC), reads=[("ogT", c % 2)], writes=[("brscr", c)])
            kb.barrier()

    def stage_c(l, branch, wo_ap, kcb, gate_col0, rowscale_name=None, rowscale_ap=None, glu=False):
        win = W["w_in"][l]
        with contextlib.ExitStack() as pes:
            nco = 2048 if glu else 1024
            Wo = sbp(pes, "Wo", [128, kcb * nco], BF16)
            Wov = Wo[:, :].rearrange("p (k n) -> p k n", k=kcb)
            tglu = [sbp(pes, "tglu%d" % i, [128, 512], F32) for i in range(2)] if glu else None
            Wgt = sbp(pes, "Wgt", [128, KC * 1024], BF16)
            Wgtv = Wgt[:, :].rearrange("p (k n) -> p k n", k=KC)
            Wout = sbp(pes, "Wout", [128, KC * 1024], BF16)
            Woutv = Wout[:, :].rearrange("p (k n) -> p k n", k=KC)
            nbr = 2 if kcb == 8 else 1
            brt = [sbp(pes, "brt%d" % i, [128, kcb * 512], BF16) for i in range(nbr)]
            xt = [sbp(pes, "xt%d" % i, [128, KC * 512], F32) for i in range(2)]
            mt = sbp(pes, "mt", [128, KC * 512], BF16)
            mtv = mt[:, :].rearrange("p (k t) -> p k t", k=KC)
            sig = [sbp(pes, "sig%d" % i, [128, 512], F32) for i in range(2)]
            rs = None
            if rowscale_ap is not None:
                for k0 in range(0, kcb, KC):
                    pass
                rs = load_gain(rowscale_name, rowscale_ap, kcb)
            for k0 in range(0, kcb, 2):
                for j in range(nco // 256):
                    if rs is None:
                        load_w([(wo_ap[k0 * 128:(k0 + 2) * 128, j * 256:(j + 1) * 256], 0)], 2, 256,
                               dst=Wov[:, k0:k0 + 2, j * 256:(j + 1) * 256], dstkey="Wo")
                    else:
                        s = wslot[0] % len(wst)
                        wslot[0] = (s + 1) % len(wst)
                        st = wst[s][:, 0:512].rearrange("p (k n) -> p k n", k=2)
                        kb.dma(st, wo_ap[k0 * 128:(k0 + 2) * 128, j * 256:(j + 1) * 256].rearrange("(k p) n -> p k n", p=128), writes=[("wst", s)])
                        for kk in range(2):
                            kb.op("pool", lambda e, kk=kk, st=st, k0=k0, j=j: e.tensor_scalar(
                                out=Wov[:, k0 + kk, j * 256:(j + 1) * 256], in0=st[:, kk, :], scalar1=gcv[:, rs, k0 + kk:k0 + kk + 1],
                                scalar2=None, op0=ALU.mult),
                                reads=[("wst", s), ("g", rs)], writes=["Wo"])
            for j in range(4):
                load_w([(win[:, gate_col0 + j * 256:gate_col0 + (j + 1) * 256], 0)], KC, 256, dst=Wgtv[:, :, j * 256:(j + 1) * 256], dstkey="Wgt")
            for j in range(4):
                load_w([(W["w_out"][l][:, j * 256:(j + 1) * 256], 0)], KC, 256, dst=Woutv[:, :, j * 256:(j + 1) * 256], dstkey="Wout")
            bank = 0
            for ti, (t0, n) in enumerate(TILES):
                b_t = brt[ti % nbr]
                b_v = b_t[:, :].rearrange("p (k t) -> p k t", k=kcb)
                x_t = xt[ti % 2]
                x_v = x_t[:, :].rearrange("p (k t) -> p k t", k=KC)
                kb.dma(b_v[:, :, 0:n], brscr[:, 0:kcb, t0:t0 + n], reads=ck("brscr", t0, n), writes=[("brt", ti % nbr)])
                kb.dma(x_v[:, :, 0:n], xscr[:, :, t0:t0 + n], reads=[("xscr", t0)], writes=[("xt", ti % 2)])
                for oc in range(KC):
                    by = bank % 4
                    bg = (bank + 1) % 4
                    bank += 2
                    for kc in range(kcb):
                        kb.op("pe", lambda e, kc=kc, oc=oc, by=by, b_v=b_v, n=n: e.matmul(
                            PS[by][:, 0:n], lhsT=Wov[:, kc, oc * 128:(oc + 1) * 128], rhs=b_v[:, kc, 0:n], start=(kc == 0), stop=(kc == kcb - 1)),
                            reads=["Wo", ("brt", ti % nbr)], writes=["ps%d" % by], inc=(kc == kcb - 1))
                    for kc in range(KC):
                        kb.op("pe", lambda e, kc=kc, oc=oc, bg=bg, t0=t0, n=n: e.matmul(
                            PS[bg][:, 0:n], lhsT=Wgtv[:, kc, oc * 128:(oc + 1) * 128], rhs=hTv[:, kc, t0:t0 + n], start=(kc == 0), stop=(kc == KC - 1)),
                            reads=["Wgt"] + ck("h", t0, n), writes=["ps%d" % bg], inc=(kc == KC - 1))
                    sgm = sig[oc % 2]
                    if glu:
                        b2 = 6 + (oc % 2)
                        for kc in range(kcb):
                            kb.op("pe", lambda e, kc=kc, oc=oc, b2=b2, b_v=b_v, n=n: e.matmul(
                                PS[b2][:, 0:n], lhsT=Wov[:, kc, 1024 + oc * 128:1024 + (oc + 1) * 128], rhs=b_v[:, kc, 0:n], start=(kc == 0), stop=(kc == kcb - 1)),
                                reads=["Wo", ("brt", ti % nbr)], writes=["ps%d" % b2], inc=(kc == kcb - 1))
                        tg = tglu[oc % 2]
                        kb.op("act", lambda e, b2=b2, n=n, tg=tg: e.activation(out=tg[:, 0:n], in_=PS[b2][:, 0:n], func=AF.Sigmoid),
                              reads=["ps%d" % b2], writes=[("tglu", oc % 2)])
                        kb.op("dve", lambda e, by=by, n=n, tg=tg: e.tensor_tensor(out=tg[:, 0:n], in0=PS[by][:, 0:n], in1=tg[:, 0:n], op=ALU.mult),
                              reads=["ps%d" % by, ("tglu", oc % 2)], writes=[("tglu", oc % 2)])
                        kb.op("act", lambda e, bg=bg, n=n, sgm=sgm: e.activation(out=sgm[:, 0:n], in_=PS[bg][:, 0:n], func=AF.Sigmoid),
                              reads=["ps%d" % bg], writes=[("sig", oc % 2)])
                        kb.op("dve", lambda e, n=n, sgm=sgm, oc=oc, tg=tg: e.tensor_tensor(out=mtv[:, oc, 0:n], in0=tg[:, 0:n], in1=sgm[:, 0:n], op=ALU.mult),
                              reads=[("tglu", oc % 2), ("sig", oc % 2)], writes=[("mt", oc)])
                    else:
                        kb.op("act", lambda e, bg=bg, n=n, sgm=sgm: e.activation(out=sgm[:, 0:n], in_=PS[bg][:, 0:n], func=AF.Sigmoid),
                              reads=["ps%d" % bg], writes=[("sig", oc % 2)])
                        kb.op("dve", lambda e, by=by, n=n, sgm=sgm, oc=oc: e.tensor_tensor(out=mtv[:, oc, 0:n], in0=PS[by][:, 0:n], in1=sgm[:, 0:n], op=ALU.mult),
                              reads=["ps%d" % by, ("sig", oc % 2)], writes=[("mt", oc)])
                MT = [("mt", oc) for oc in range(KC)]
                for oc in range(KC):
                    bo = 4 + (oc % 2)
                    for kc in range(KC):
                        kb.op("pe", lambda e, kc=kc, oc=oc, bo=bo, n=n: e.matmul(
                            PS[bo][:, 0:n], lhsT=Woutv[:, kc, oc * 128:(oc + 1) * 128], rhs=mtv[:, kc, 0:n], start=(kc == 0), stop=(kc == KC - 1)),
                            reads=["Wout"] + MT, writes=["ps%d" % bo], inc=(kc == KC - 1))
                    kb.op("dve", lambda e, oc=oc, bo=bo, n=n, x_v=x_v: e.tensor_tensor(out=x_v[:, oc, 0:n], in0=PS[bo][:, 0:n], in1=x_v[:, oc, 0:n], op=ALU.add),
                          reads=["ps%d" % bo, ("xt", ti % 2)], writes=[("xt", ti % 2)])
                kb.dma(xscr[:, :, t0:t0 + n], x_v[:, :, 0:n], reads=[("xt", ti % 2)], writes=[("xscr", t0)])
            kb.barrier()

    def ssd(l):
        win = W["w_in"][l]
        XB0, Z0, DT0 = 6144, 4096, 9216
        Ident = AF.Identity
        with contextlib.ExitStack() as pes:
            cw = sbp(pes, "cw", [128, 24 * 5], F32)
            cwv = cw[:, :].rearrange("p (f k) -> p f k", k=5)
            cb = None
            with contextlib.ExitStack() as p0:
                cwt = sbp(p0, "cwt", [5, 3072], F32)
                kb.dma(cwt[0:4, :], W["ssd_conv_w"][l], writes=["cwt"])
                kb.dma(cwt[4:5, :], W["ssd_conv_b"][l:l + 1, :], writes=["cwt"])
                for fc in range(24):
                    kb.op("pe", lambda e, fc=fc: e.transpose(PS[7][:, fc * 5:(fc + 1) * 5], cwt[0:5, fc * 128:(fc + 1) * 128], identf[0:5, 0:5]),
                          reads=["cwt", "identf"], writes=["ps7"], inc=(fc == 23))
                kb.op("act", lambda e: e.copy(cw[:, :], PS[7][:, 0:120]), reads=["ps7"], writes=["cw"])
                kb.barrier()
            with contextlib.ExitStack() as p1:
                cin = sbp(p1, "cin", [48, 3072], F32)
                histT = sbp(p1, "histT", [128, 24 * 48], F32)
                hv = histT[:, :].rearrange("p (f s r) -> p f s r", f=24, s=16)
                tailT = sbp(p1, "tailT", [128, 24 * 48], F32)
                tv = tailT[:, :].rearrange("p (f s r) -> p f s r", f=24, s=16)
                tailP = sbp(p1, "tailP", [128, 72], F32)
                cout = sbp(p1, "cout", [48, 3072], F32)
                raw = [sbp(p1, "raw%d" % i, [128, 515], F32) for i in range(6)]
                accs = [sbp(p1, "acc%d" % i, [128, 512], F32) for i in range(4)]
                xc = [sbp(p1, "xc%d" % i, [128, 512], BF16) for i in range(3)]
                xst = [sbp(p1, "xst%d" % i, [128, 512], BF16) for i in range(3)]
                kb.dma(cin[:, :], W["state_conv"][l].rearrange("s r f -> (s r) f"), writes=["cin"])
                for fc in range(24):
                    bi = 6 + (fc // 4) % 2
                    j = fc % 4
                    kb.op("pe", lambda e, bi=bi, j=j, fc=fc: e.transpose(PS[bi][:, j * 48:(j + 1) * 48], cin[0:48, fc * 128:(fc + 1) * 128], identf[0:48, 0:48]),
                          reads=["cin", "identf"], writes=["ps%d" % bi], inc=(j == 3))
                    if j == 3:
                        kb.op("act", lambda e, bi=bi, fc=fc: e.copy(histT[:, (fc - 3) * 48:(fc + 1) * 48], PS[bi][:, 0:192]),
                              reads=["ps%d" % bi], writes=["histT"])
                NR = 6
                items = [(fcp, sub, ti) for fcp in range(12) for sub in range(2) for ti in range(len(TILES))]
                wcache = {}
                tbanks = [PS[5][:, :].bitcast(BF16), PS[6][:, :].bitcast(BF16)]

                def c1(k, item):
                    fcp, sub, ti = item
                    t0, n = TILES[ti]
                    fc = fcp * 2 + sub
                    d = {"fc": fc, "sub": sub, "t0": t0, "n": n, "samp": t0 >= SEQ, "fcp": fcp,
                         "bank": PS[k % 4], "kbank": "ps%d" % (k % 4),
                         "R": raw[k % NR], "kR": ("raw", k % NR), "prevR": raw[(k - 1) % NR], "kprev": ("raw", (k - 1) % NR),
                         "acc": accs[k % 4], "kacc": ("acc", k % 4), "xc": xc[k % 3], "kxc": ("xc", k % 3),
                         "xst": xst[k % 3], "kxs": ("xst", k % 3), "tb": tbanks[k % 2], "ktb": "ps%d" % (5 + k % 2)}
                    if d["samp"]:
                        R3 = d["R"][:, 0:176].rearrange("p (s c) -> p s c", c=11)
                        d["R3"] = R3
                        d["X"] = [R3[:, :, kk:kk + 8] for kk in range(4)]
                        d["A"] = d["acc"][:, 0:128].rearrange("p (s c) -> p s c", c=8)
                    else:
                        d["X"] = [d["R"][:, kk:kk + n] for kk in range(4)]
                        d["A"] = d["acc"][:, 0:n]
                    return d

                def p0(k, item):
                    d = c1(k, item)
                    if d["sub"] == 0 and d["t0"] == 0:
                        wcache[d["fcp"]] = load_w([(win[:, XB0 + d["fcp"] * 256:XB0 + (d["fcp"] + 1) * 256], 0)], KC, 256)
                    wx, kwx = wcache[d["fcp"]]
                    for kc in range(KC):
                        kb.op("pe", lambda e, kc=kc: e.matmul(
                            d["bank"][:, 0:d["n"]], lhsT=wx[:, kc, d["sub"] * 128:(d["sub"] + 1) * 128], rhs=hTv[:, kc, d["t0"]:d["t0"] + d["n"]],
                            start=(kc == 0), stop=(kc == KC - 1)),
                            reads=[kwx] + ck("h", d["t0"], d["n"]), writes=[d["kbank"]], inc=(kc == KC - 1))

                def p1(k, item):
                    d = c1(k, item)
                    R, n = d["R"], d["n"]
                    if not d["samp"]:
                        if d["t0"] == 0:
                            kb.op("pool", lambda e: e.memset(R[:, 0:3], 0.0), writes=[d["kR"]])
                        else:
                            kb.op("pool", lambda e: e.tensor_copy(R[:, 0:3], d["prevR"][:, 512:515]), reads=[d["kprev"]], writes=[d["kR"]])
                        kb.op("act", lambda e: e.copy(R[:, 3:3 + n], d["bank"][:, 0:n]), reads=[d["kbank"]], writes=[d["kR"]])
                    else:
                        kb.op("pool", lambda e: e.tensor_copy(d["R3"][:, :, 0:3], hv[:, d["fc"]]), reads=["histT"], writes=[d["kR"]])
                        kb.op("act", lambda e: e.copy(d["R3"][:, :, 3:11], d["bank"][:, 0:128].rearrange("p (s c) -> p s c", c=8)),
                              reads=[d["kbank"]], writes=[d["kR"]])

                def p2(k, item):
                    d = c1(k, item)
                    fc = d["fc"]
                    kb.op("act", lambda e: e.activation(out=d["A"], in_=d["X"][3], func=Ident, scale=cwv[:, fc, 3:4], bias=cwv[:, fc, 4:5]),
                          reads=[d["kR"], "cw"], writes=[d["kacc"]])

                def p3(k, item):
                    d = c1(k, item)
                    fc = d["fc"]
                    for kk in (2, 1, 0):
                        kb.op("dve", lambda e, kk=kk: e.scalar_tensor_tensor(
                            out=d["A"], in0=d["X"][kk], scalar=cwv[:, fc, kk:kk + 1], in1=d["A"], op0=ALU.mult, op1=ALU.add),
                            reads=[d["kR"], "cw", d["kacc"]], writes=[d["kacc"]])

                def p4(k, item):
                    d = c1(k, item)
                    fc, n = d["fc"], d["n"]
                    kb.op("act", lambda e: e.activation(out=d["xc"][:, 0:n], in_=d["acc"][:, 0:n], func=AF.Silu),
                          reads=[d["kacc"]], writes=[d["kxc"]])
                    if d["t0"] == 1536:
                        kb.op("pool", lambda e: e.tensor_copy(tailP[:, fc * 3:(fc + 1) * 3], d["R"][:, 512:515]), reads=[d["kR"]], writes=["tailP"])
                    if d["samp"]:
                        kb.op("pool", lambda e: e.tensor_copy(tv[:, fc], d["R3"][:, :, 8:11]), reads=[d["kR"]], writes=["tailT"])

                def p5_(k, item):
                    d = c1(k, item)
                    fc, n, t0 = d["fc"], d["n"], d["t0"]
                    if fc >= 16:
                        kb.dma(bc_scr[:, fc - 16, t0:t0 + n], d["xc"][:, 0:n], reads=[d["kxc"]], writes=[("bc_scr", fc, t0)])
                    if fc < 20:
                        nci = n // 128
                        for ci in range(nci):
                            kb.op("pe", lambda e, ci=ci: e.transpose(d["tb"][:, ci * 128:(ci + 1) * 128], d["xc"][:, ci * 128:(ci + 1) * 128], identb[:, :]),
                                  reads=[d["kxc"], "identb"], writes=[d["ktb"]], inc=(ci == nci - 1))

                def p6(k, item):
                    d = c1(k, item)
                    fc, n, t0 = d["fc"], d["n"], d["t0"]
                    if fc < 20:
                        kb.op("act", lambda e: e.copy(d["xst"][:, 0:n], d["tb"][:, 0:n]), reads=[d["ktb"]], writes=[d["kxs"]])
                        kb.dma(xsb_scr[t0:t0 + n, fc * 128:(fc + 1) * 128].rearrange("(c p) f -> p c f", p=128),
                               d["xst"][:, 0:n].rearrange("p (c f) -> p c f", f=128), reads=[d["kxs"]], writes=[("xsb_scr", fc, t0)])

                run_pipeline(items, [p0, p1, p2, p3, p4, p5_, p6])
                for fc in range(24):
                    bi = 6 + (fc // 4) % 2
                    j = fc % 4
                    kb.op("pe", lambda e, bi=bi, j=j, fc=fc: e.transpose(PS[bi][0:3, j * 128:(j + 1) * 128], tailP[:, fc * 3:(fc + 1) * 3], identf[:, :]),
                          reads=["tailP", "identf"], writes=["ps%d" % bi], inc=(j == 3))
                    if j == 3:
                        kb.op("act", lambda e, bi=bi, fc=fc: e.copy(cin[0:3, (fc - 3) * 128:(fc + 1) * 128], PS[bi][0:3, 0:512]),
                              reads=["ps%d" % bi, "histT"], writes=["cin"])
                kb.dma(conv_p[l], cin[0:3, :], reads=["cin"], writes=[("conv_p", l)])
                for fc in range(24):
                    bi = 6 + (fc // 4) % 2
                    j = fc % 4
                    kb.op("pe", lambda e, bi=bi, j=j, fc=fc: e.transpose(PS[bi][0:48, j * 128:(j + 1) * 128], tailT[:, fc * 48:(fc + 1) * 48], identf[:, :]),
                          reads=["tailT", "identf"], writes=["ps%d" % bi], inc=(j == 3))
                    if j == 3:
                        kb.op("act", lambda e, bi=bi, fc=fc: e.copy(cout[0:48, (fc - 3) * 128:(fc + 1) * 128], PS[bi][0:48, 0:512]),
                              reads=["ps%d" % bi], writes=["cout"])
                kb.dma(conv_s[l].rearrange("s r f -> (s r) f"), cout[:, :], reads=["cout"], writes=[("conv_s", l)])
                kb.barrier()

            if not stages.get("ssd_s2", True):
                return
            with contextlib.ExitStack() as p2:
                Wz = sbp(p2, "Wz", [128, KC * 2048], BF16)
                Wzv = Wz[:, :].rearrange("p (k n) -> p k n", k=KC)
                Wdt = sbp(p2, "Wdt", [128, KC * 32], BF16)
                Wdtv = Wdt[:, :].rearrange("p (k n) -> p k n", k=KC)
                triu = sbp(p2, "triu", [128, 256], F32)
                triuv = triu[:, :].rearrange("p (s n) -> p s n", s=2)
                negm = sbp(p2, "negm", [128, 256], F32)
                negmv = negm[:, :].rearrange("p (s n) -> p s n", s=2)
                ssm = sbp(p2, "ssm", [128, 256], F32)
                ssv = ssm[:, :].rearrange("p (s n) -> p s n", s=2)
                rm = sbp(p2, "rm", [128, 2048], F32)
                rmv = rm[:, :].rearrange("p (s n) -> p s n", s=16)
                rowmask = sbp(p2, "rowmask2", [128, 16], F32)
                dtb = sbp(p2, "dtb", [128, 32], F32)
                a_t = sbp(p2, "a_t", [128, 32], F32)
                dsk = sbp(p2, "dsk", [128, 32], F32)
                sT = sbp(p2, "sT", [128, 2048], F32)
                sTb = sbp(p2, "sTb", [128, 2048], BF16)
                xsb = sbp(p2, "xsb", [128, 2560], BF16)
                bct = sbp(p2, "bct", [128, 1024], BF16)
                bcv = bct[:, :].rearrange("p (g t) -> p g t", g=8)
                dtt = sbp(p2, "dtt", [128, 32], F32)
                ex1 = sbp(p2, "ex1", [128, 32], F32)
                dt_ = sbp(p2, "dt_", [128, 32], F32)
                dA = sbp(p2, "dA", [128, 32], F32)
                cum = sbp(p2, "cum", [128, 32], F32)
                wtmp = sbp(p2, "wtmp", [128, 32], F32)
                wend = sbp(p2, "wend", [128, 32], F32)
                dec = sbp(p2, "dec", [128, 32], F32)
                decs = sbp(p2, "decs", [128, 512], F32)
                decsv = decs[:, :].rearrange("p (s h) -> p s h", s=16)
                ecum = sbp(p2, "ecum", [128, 32], F32)
                xdt = sbp(p2, "xdt", [128, 2048], BF16)
                xsD = sbp(p2, "xsD", [128, 2048], BF16)
                xdtw = sbp(p2, "xdtw", [128, 2048], BF16)
                scT = sbp(p2, "scT", [128, 512], BF16)
                yoff = sbp(p2, "yoff", [128, 2048], F32)
                yy = sbp(p2, "yy", [128, 2048], F32)
                zs = sbp(p2, "zs", [128, 512], F32)
                seg = [sbp(p2, "seg%d" % i, [128, 512], F32) for i in range(2)]
                Lm = [sbp(p2, "Lm%d" % i, [128, 512], BF16) for i in range(2)]
                Mm = [sbp(p2, "Mm%d" % i, [128, 512], BF16) for i in range(2)]
                bst = sbp(p2, "bst2", [128, 24], F32)
                mv = sbp(p2, "mv2", [128, 8], F32)
                mvv = mv[:, :].rearrange("p (g t) -> p g t", g=4)
                ms = sbp(p2, "ms", [128, 4], F32)
                sdv = sbp(p2, "sdv2", [128, 4], F32)
                rsv = sbp(p2, "rsv2", [128, 4], F32)
                yn = sbp(p2, "yn", [128, 2048], BF16)
                ynT = sbp(p2, "ynT", [128, 2048], BF16)
                Cblk = sbp(p2, "Cblk", [128, 2048], BF16)
                Cbv = Cblk[:, :].rearrange("p (s t) -> p s t", s=16)
                s0 = [sbp(p2, "s0_%d" % i, [128, 512], F32) for i in range(2)]
                s0T = [sbp(p2, "s0T%d" % i, [128, 512], F32) for i in range(2)]
                s0Tb = [sbp(p2, "s0Tb%d" % i, [128, 512], BF16) for i in range(2)]
                Bm = [sbp(p2, "Bm%d" % i, [128, 128], BF16) for i in range(2)]
                snat = [sbp(p2, "snat%d" % i, [128, 512], F32) for i in range(2)]
                print("S2 sbuf remaining", nc.sbuf_bytes_remaining)

                kb.dma(triuv, CST["triu"], writes=["triu"])
                kb.dma(negmv, CST["negm"], writes=["negm"])
                kb.dma(ssv, CST["ss"], writes=["ss"])
                kb.dma(rmv, CST["rm"], writes=["rm"])
                kb.dma(rowmask[:, :], CST["rowmask"], writes=["rowmask"])
                kb.dma(dtb[:, :], W["ssd_dt_bias"][l:l + 1, :].to_broadcast([128, 32]), writes=["dtb"])
                kb.dma(a_t[:, :], W["ssd_a_log"][l:l + 1, :].to_broadcast([128, 32]), writes=["a_t"])
                kb.dma(dsk[:, :], W["ssd_d"][l:l + 1, :].to_broadcast([128, 32]), writes=["dsk"])
                kb.op("act", lambda e: e.activation(out=a_t[:, :], in_=a_t[:, :], func=AF.Exp), reads=["a_t"], writes=["a_t"])
                kb.op("dve", lambda e: e.tensor_scalar(out=a_t[:, :], in0=a_t[:, :], scalar1=-1.0, scalar2=None, op0=ALU.mult), reads=["a_t"], writes=["a_t"])
                kb.op("pool", lambda e: e.memset(sT[:, :], 0.0), writes=["sT"])
                kb.op("pool", lambda e: e.memset(sTb[:, :], 0.0), writes=["sTb"])
                kb.op("pool", lambda e: e.memset(Cblk[:, :], 0.0), writes=["Cblk"])
                for j in range(8):
                    load_w([(win[:, Z0 + j * 256:Z0 + (j + 1) * 256], 0)], KC, 256, dst=Wzv[:, :, j * 256:(j + 1) * 256], dstkey="Wz")
                load_w([(win[:, DT0:DT0 + 32], 0)], KC, 32, dst=Wdtv, dstkey="Wdt")
                p5 = PS[5][:, :].bitcast(BF16)
                hcount = 0
                for c in stages.get("ssd_chunks", list(range(NCH))):
                    ts = slice(c * 128, (c + 1) * 128)
                    samp = c * 128 >= SEQ
                    sel = 1 if samp else 0
                    kb.dma(xsb[:, :], xsb_scr[ts, :], reads=[("xsb_scr", fc, (c * 128 // 512) * 512) for fc in range(20)], writes=["xsb"])
                    kb.dma(bcv, bc_scr[:, :, ts], reads=[("bc_scr", fc, (c * 128 // 512) * 512) for fc in range(16, 24)], writes=["bct"])
                    for kc in range(KC):
                        kb.op("pe", lambda e, kc=kc, ts=ts: e.matmul(PS[0][:, 0:32], lhsT=hTv[:, kc, ts], rhs=Wdtv[:, kc, :], start=(kc == 0), stop=(kc == KC - 1)),
                              reads=["Wdt", ("h", c)], writes=["ps0"], inc=(kc == KC - 1))
                    kb.op("dve", lambda e: e.tensor_tensor(out=dtt[:, :], in0=PS[0][:, 0:32], in1=dtb[:, :], op=ALU.add), reads=["ps0", "dtb"], writes=["dtt"])
                    kb.op("act", lambda e: e.activation(out=ex1[:, :], in_=dtt[:, :], func=AF.Exp), reads=["dtt"], writes=["ex1"])
                    kb.op("act", lambda e: e.activation(out=dt_[:, :], in_=ex1[:, :], func=AF.Ln, bias=1.0, scale=1.0), reads=["ex1"], writes=["dt_"])
                    kb.op("dve", lambda e: e.tensor_tensor(out=dA[:, :], in0=dt_[:, :], in1=a_t[:, :], op=ALU.mult), reads=["dt_", "a_t"], writes=["dA"])
                    kb.op("pe", lambda e, sel=sel: e.matmul(PS[0][:, 32:64], lhsT=triuv[:, sel, :], rhs=dA[:, :], start=True, stop=True),
                          reads=["triu", "dA"], writes=["ps0"], inc=True)
                    kb.op("act", lambda e: e.copy(cum[:, :], PS[0][:, 32:64]), reads=["ps0"], writes=["cum"])
                    kb.op("pe", lambda e, sel=sel: e.matmul(PS[0][:, 64:96], lhsT=ssv[:, sel, :], rhs=dA[:, :], start=True, stop=True),
                          reads=["ss", "dA"], writes=["ps0"], inc=True)
                    kb.op("dve", lambda e: e.tensor_tensor(out=wtmp[:, :], in0=PS[0][:, 64:96], in1=cum[:, :], op=ALU.subtract), reads=["ps0", "cum"], writes=["wtmp"])
                    kb.op("act", lambda e: e.activation(out=wend[:, :], in_=wtmp[:, :], func=AF.Exp), reads=["wtmp"], writes=["wend"])
                    kb.op("act", lambda e: e.activation(out=dec[:, :], in_=PS[0][:, 64:96], func=AF.Exp), reads=["ps0"], writes=["dec"])
                    kb.op("act", lambda e: e.activation(out=ecum[:, :], in_=cum[:, :], func=AF.Exp), reads=["cum"], writes=["ecum"])
                    xs3 = xsb[:, 0:2048].rearrange("p (h q) -> p h q", h=32)
                    kb.op("dve", lambda e, xs3=xs3: e.tensor_tensor(out=xdt[:, :].rearrange("p (h q) -> p h q", h=32), in0=xs3,
                                                                  in1=dt_[:, :].unsqueeze(2).to_broadcast([128, 32, 64]), op=ALU.mult),
                          reads=["xsb", "dt_"], writes=["xdt"])
                    kb.op("pool", lambda e, xs3=xs3: e.tensor_tensor(out=xsD[:, :].rearrange("p (h q) -> p h q", h=32), in0=xs3,
                                                                   in1=dsk[:, :].unsqueeze(2).to_broadcast([128, 32, 64]), op=ALU.mult),
                          reads=["xsb", "dsk"], writes=["xsD"])
                    kb.op("pool", lambda e: e.tensor_tensor(out=xdtw[:, :].rearrange("p (h q) -> p h q", h=32), in0=xdt[:, :].rearrange("p (h q) -> p h q", h=32),
                                                          in1=wend[:, :].unsqueeze(2).to_broadcast([128, 32, 64]), op=ALU.mult),
                          reads=["xdt", "wend"], writes=["xdtw"])
                    for g in range(4):
                        kb.op("pe", lambda e, g=g: e.matmul(PS[1][:, g * 128:(g + 1) * 128], lhsT=bcv[:, g, :], rhs=bcv[:, 4 + g, :], start=True, stop=True),
                              reads=["bct"], writes=["ps1"], inc=(g == 3))
                    kb.op("act", lambda e: e.copy(scT[:, :], PS[1][:, :]), reads=["ps1"], writes=["scT"])
                    if samp:
                        for s in range(NSS):
                            kb.op("pe", lambda e, s=s: e.matmul(PS[1][:, s * 32:(s + 1) * 32], lhsT=rmv[:, s, :], rhs=dA[:, :], start=True, stop=True),
                                  reads=["rm", "dA", "scT"], writes=["ps1"], inc=(s == NSS - 1))
                        kb.op("act", lambda e: e.activation(out=decs[:, :], in_=PS[1][:, :], func=AF.Exp), reads=["ps1"], writes=["decs"])
                    for g in range(4):
                        gs_ = slice(g * 512, (g + 1) * 512)
                        if not samp:
                            kb.op("pe", lambda e, g=g, gs_=gs_: e.matmul(PS[2][:, :], lhsT=bcv[:, 4 + g, :], rhs=sTb[:, gs_], start=True, stop=True),
                                  reads=["bct", "sTb"], writes=["ps2"], inc=True)
                        else:
                            for s in range(NSS):
                                kb.op("pool", lambda e, s=s, g=g: e.tensor_copy(Cbv[:, s, s * 8:(s + 1) * 8], bcv[:, 4 + g, s * 8:(s + 1) * 8]),
                                      reads=["bct"], writes=["Cblk"])
                            dbg = stages.get("dbg", 9)
                            for s in (range(stages.get("smp_ns", NSS)) if dbg >= 2 else []):
                                i2 = s % 2
                                for b4 in range(4):
                                    kb.dma(s0[i2][:, b4 * 128:(b4 + 1) * 128],
                                           W["state_ssm"][l, s, g * 8 + 2 * b4:g * 8 + 2 * b4 + 2].rearrange("h q n -> (h q) n"), writes=[("s0", i2)])
                                if dbg < 2.2:
                                    continue
                                for b4 in range(4):
                                    kb.op("pe", lambda e, b4=b4, i2=i2: e.transpose(PS[3][:, b4 * 128:(b4 + 1) * 128], s0[i2][:, b4 * 128:(b4 + 1) * 128], identf[:, :]),
                                          reads=[("s0", i2), "identf"], writes=["ps3"], inc=(b4 == 3))
                                if dbg < 2.4:
                                    continue
                                kb.op("dve", lambda e, i2=i2: e.tensor_copy(s0T[i2][:, :], PS[3][:, :]), reads=["ps3"], writes=[("s0T", i2)])
                                if dbg < 2.6:
                                    continue
                                kb.op("act", lambda e, i2=i2: e.copy(s0Tb[i2][:, :], s0T[i2][:, :]), reads=[("s0T", i2)], writes=[("s0Tb", i2)])
                                if dbg < 3:
                                    continue
                                kb.op("pe", lambda e, s=s, i2=i2: e.matmul(PS[2][:, :], lhsT=Cbv[:, s, :], rhs=s0Tb[i2][:, :], start=(s == 0), stop=(s == NSS - 1)),
                                      reads=["Cblk", ("s0Tb", i2)], writes=["ps2"], inc=True)
                                if dbg < 4:
                                    continue
                                kb.op("dve", lambda e, s=s, g=g, i2=i2: e.tensor_scalar(out=Bm[i2][:, :], in0=xsb[:, 2048 + g * 128:2048 + (g + 1) * 128],
                                                                                   scalar1=rowmask[:, s:s + 1], scalar2=None, op0=ALU.mult),
                                      reads=["xsb", "rowmask"], writes=[("Bm", i2)])
                                kb.op("pe", lambda e, i2=i2, gs_=gs_: e.matmul(PS[4][:, :], lhsT=Bm[i2][:, :], rhs=xdtw[:, gs_], start=True, stop=True),
                                      reads=[("Bm", i2), "xdtw"], writes=["ps4"], inc=True)
                                kb.op("dve", lambda e, i2=i2, s=s, g=g: e.tensor_tensor(
                                    out=s0T[i2][:, :].rearrange("p (h q) -> p h q", h=8), in0=s0T[i2][:, :].rearrange("p (h q) -> p h q", h=8),
                                    in1=decsv[:, s, g * 8:(g + 1) * 8].unsqueeze(2).to_broadcast([128, 8, 64]), op=ALU.mult),
                                    reads=[("s0T", i2), "decs"], writes=[("s0T", i2)])
                                kb.op("dve", lambda e, i2=i2: e.tensor_tensor(out=s0T[i2][:, :], in0=PS[4][:, :], in1=s0T[i2][:, :], op=ALU.add),
                                      reads=["ps4", ("s0T", i2)], writes=[("s0T", i2)])
                                if dbg < 5:
                                    continue
                                for b4 in range(4):
                                    kb.op("pe", lambda e, b4=b4, i2=i2: e.transpose(PS[3][:, b4 * 128:(b4 + 1) * 128], s0T[i2][:, b4 * 128:(b4 + 1) * 128], identf[:, :]),
                                          reads=[("s0T", i2), "identf"], writes=["ps3"], inc=(b4 == 3))
                                kb.op("act", lambda e, i2=i2: e.copy(snat[i2][:, :], PS[3][:, :]), reads=["ps3"], writes=[("snat", i2)])
                                for b4 in range(4):
                                    kb.dma(ssm_s[l, s, g * 8 + 2 * b4:g * 8 + 2 * b4 + 2].rearrange("h q n -> (h q) n"),
                                           snat[i2][:, b4 * 128:(b4 + 1) * 128], reads=[("snat", i2)], writes=[("ssm_s", l, s, g, b4)])
                        if samp and stages.get("dbg", 9) < 3:
                            kb.op("pe", lambda e, g=g, gs_=gs_: e.matmul(PS[2][:, :], lhsT=bcv[:, 4 + g, :], rhs=sTb[:, gs_], start=True, stop=True),
                                  reads=["bct", "sTb"], writes=["ps2"], inc=True)
                        kb.op("dve", lambda e, g=g, gs_=gs_: e.tensor_tensor(
                            out=yoff[:, gs_].rearrange("p (h q) -> p h q", h=8), in0=PS[2][:, :].rearrange("p (h q) -> p h q", h=8),
                            in1=ecum[:, g * 8:(g + 1) * 8].unsqueeze(2).to_broadcast([128, 8, 64]), op=ALU.mult),
                            reads=["ps2", "ecum"], writes=[("yoff", g)])
                    for hq in range(8):
                        h0 = hq * 4
                        g = h0 // 8
                        i2 = hcount % 2
                        hcount += 1
                        cbk = 6 + i2
                        v4 = lambda ap: ap.rearrange("p (j t) -> p j t", j=4)
                        for j in range(4):
                            h = h0 + j
                            kb.op("pe", lambda e, h=h, j=j, cbk=cbk, sel=sel: e.matmul(PS[cbk][:, j * 128:(j + 1) * 128], lhsT=dA[:, h:h + 1].to_broadcast([128, 128]),
                                                                                    rhs=triuv[:, sel, :], start=True, stop=True),
                                  reads=["dA", "triu"], writes=["ps%d" % cbk], inc=(j == 3))
                        kb.op("dve", lambda e, h0=h0, cbk=cbk, i2=i2: e.tensor_tensor(
                            out=v4(seg[i2][:, :]), in0=v4(PS[cbk][:, :]), in1=cum[:, h0:h0 + 4].unsqueeze(2).to_broadcast([128, 4, 128]), op=ALU.subtract),
                            reads=["ps%d" % cbk, "cum"], writes=[("seg", i2)])
                        kb.op("pool", lambda e, i2=i2, sel=sel: e.tensor_tensor(
                            out=v4(seg[i2][:, :]), in0=v4(seg[i2][:, :]), in1=negmv[:, sel:sel + 1, :].to_broadcast([128, 4, 128]), op=ALU.add),
                            reads=[("seg", i2), "negm"], writes=[("seg", i2)])
                        kb.op("act", lambda e, i2=i2: e.activation(out=Lm[i2][:, :], in_=seg[i2][:, :], func=AF.Exp), reads=[("seg", i2)], writes=[("Lm", i2)])
                        kb.op("pool", lambda e, i2=i2, g=g: e.tensor_tensor(
                            out=v4(Mm[i2][:, :]), in0=v4(Lm[i2][:, :]), in1=scT[:, g * 128:(g + 1) * 128].unsqueeze(1).to_broadcast([128, 4, 128]), op=ALU.mult),
                            reads=[("Lm", i2), "scT"], writes=[("Mm", i2)])
                        ybk = 2 + (g % 2)
                        for j in range(4):
                            h = h0 + j
                            yo_ = PS[ybk][:, (h % 8) * 64:(h % 8 + 1) * 64]
                            kb.op("pe", lambda e, yo_=yo_, i2=i2, h=h, j=j: e.matmul(yo_, lhsT=Mm[i2][:, j * 128:(j + 1) * 128], rhs=xdt[:, h * 64:(h + 1) * 64], start=True, stop=False),
                                  reads=[("Mm", i2), "xdt"] + [("yoff", gg) for gg in range(4)], writes=["ps%d" % ybk], inc=False)
                            kb.op("pe", lambda e, yo_=yo_, h=h: e.matmul(yo_, lhsT=identb[:, :], rhs=xsD[:, h * 64:(h + 1) * 64], start=False, stop=True),
                                  reads=["identb", "xsD"], writes=["ps%d" % ybk], inc=True)
                        if h0 % 8 == 4:
                            kb.op("dve", lambda e, g=g, ybk=ybk: e.tensor_tensor(out=yy[:, g * 512:(g + 1) * 512], in0=PS[ybk][:, :], in1=yoff[:, g * 512:(g + 1) * 512], op=ALU.add),
                                  reads=["ps%d" % ybk, ("yoff", g)], writes=[("yy", g)])
                    for g in range(4):
                        zb = 0 + (g % 2)
                        for kc in range(KC):
                            kb.op("pe", lambda e, kc=kc, g=g, zb=zb, ts=ts: e.matmul(PS[zb][:, :], lhsT=hTv[:, kc, ts], rhs=Wzv[:, kc, g * 512:(g + 1) * 512],
                                                                                  start=(kc == 0), stop=(kc == KC - 1)),
                                  reads=["Wz", ("h", c), "cum", "wtmp", "dec"], writes=["ps%d" % zb], inc=(kc == KC - 1))
                        kb.op("act", lambda e, zb=zb: e.activation(out=zs[:, :], in_=PS[zb][:, :], func=AF.Silu), reads=["ps%d" % zb], writes=["zs"])
                        kb.op("dve", lambda e, g=g: e.tensor_tensor(out=yy[:, g * 512:(g + 1) * 512], in0=yy[:, g * 512:(g + 1) * 512], in1=zs[:, :], op=ALU.mult),
                              reads=[("yy", g), "zs"], writes=[("yy", g)])
                        kb.op("dve", lambda e, g=g: e.bn_stats(bst[:, g * 6:(g + 1) * 6], yy[:, g * 512:(g + 1) * 512]), reads=[("yy", g)], writes=[("bst", g)])
                        kb.op("dve", lambda e, g=g: e.bn_aggr(mv[:, g * 2:(g + 1) * 2], bst[:, g * 6:(g + 1) * 6]), reads=[("bst", g)], writes=[("mv", g)])
                    MV = [("mv", g) for g in range(4)]
                    kb.op("dve", lambda e: e.scalar_tensor_tensor(out=ms[:, :], in0=mvv[:, :, 0], scalar=1.0, in1=mvv[:, :, 0], op0=ALU.mult, op1=ALU.mult),
                          reads=MV, writes=["ms"])
                    kb.op("dve", lambda e: e.tensor_tensor(out=ms[:, :], in0=ms[:, :], in1=mvv[:, :, 1], op=ALU.add), reads=MV + ["ms"], writes=["ms"])
                    kb.op("act", lambda e: e.activation(out=sdv[:, :], in_=ms[:, :], func=AF.Sqrt, bias=EPS, scale=1.0), reads=["ms"], writes=["sdv"])
                    kb.op("dve", lambda e: e.reciprocal(rsv[:, :], sdv[:, :]), reads=["sdv"], writes=["rsv"])
                    for g in range(4):
                        kb.op("dve", lambda e, g=g: e.tensor_scalar(out=yn[:, g * 512:(g + 1) * 512], in0=yy[:, g * 512:(g + 1) * 512], scalar1=rsv[:, g:g + 1],
                                                                  scalar2=None, op0=ALU.mult),
                              reads=[("yy", g), "rsv"], writes=["yn"])
                    for half in range(2):
                        for j in range(8):
                            kc = half * 8 + j
                            kb.op("pe", lambda e, kc=kc, j=j: e.transpose(p5[:, j * 128:(j + 1) * 128], yn[:, kc * 128:(kc + 1) * 128], identb[:, :]),
                                  reads=["yn", "identb"], writes=["ps5"], inc=(j == 7))
                        kb.op("act", lambda e, half=half: e.copy(ynT[:, half * 1024:(half + 1) * 1024], p5[:, :]), reads=["ps5"], writes=[("ynT", half)])
                    kb.dma(brscr[:, 0:16, ts], ynT[:, :].rearrange("p (k t) -> p k t", k=16), reads=[("ynT", 0), ("ynT", 1)], writes=[("brscr", c)])
                    if not samp:
                        for g in range(4):
                            gs_ = slice(g * 512, (g + 1) * 512)
                            kb.op("pe", lambda e, g=g, gs_=gs_: e.matmul(PS[4][:, :], lhsT=xsb[:, 2048 + g * 128:2048 + (g + 1) * 128], rhs=xdtw[:, gs_], start=True, stop=True),
                                  reads=["xsb", "xdtw"], writes=["ps4"], inc=True)
                            kb.op("dve", lambda e, g=g, gs_=gs_: e.tensor_tensor(
                                out=sT[:, gs_].rearrange("p (h q) -> p h q", h=8), in0=sT[:, gs_].rearrange("p (h q) -> p h q", h=8),
                                in1=dec[:, g * 8:(g + 1) * 8].unsqueeze(2).to_broadcast([128, 8, 64]), op=ALU.mult),
                                reads=["sT", "dec"], writes=["sT"])
                            kb.op("dve", lambda e, gs_=gs_: e.tensor_tensor(out=sT[:, gs_], in0=PS[4][:, :], in1=sT[:, gs_], op=ALU.add),
                                  reads=["ps4", "sT"], writes=["sT"])
                        kb.op("act", lambda e: e.copy(sTb[:, :], sT[:, :]), reads=["sT"], writes=["sTb"])
                        if c == SEQ // 128 - 1:
                            for q4 in range(4):
                                for b4 in range(4):
                                    blk = q4 * 4 + b4
                                    kb.op("pe", lambda e, b4=b4, blk=blk: e.transpose(PS[3][:, b4 * 128:(b4 + 1) * 128], sT[:, blk * 128:(blk + 1) * 128], identf[:, :]),
                                          reads=["sT", "identf"], writes=["ps3"], inc=(b4 == 3))
                                kb.op("act", lambda e, q4=q4: e.copy(snat[q4 % 2][:, :], PS[3][:, :]), reads=["ps3"], writes=[("snat", q4 % 2)])
                                for b4 in range(4):
                                    kb.dma(ssm_p[l, q4 * 8 + 2 * b4:q4 * 8 + 2 * b4 + 2].rearrange("h q n -> (h q) n"),
                                           snat[q4 % 2][:, b4 * 128:(b4 + 1) * 128], reads=[("snat", q4 % 2)], writes=[("ssm_p", l, q4, b4)])
                kb.barrier()

    def s5(l):
        win = W["w_in"][l]
        U0 = 3072
        TWO_PI = 2.0 * np.pi
        MAGIC = 12582912.0
        with contextlib.ExitStack() as pes:
            uT = sbp(pes, "uT", [128, KC * T], BF16)
            uTv = uT[:, :].rearrange("p (k t) -> p k t", k=KC)
            Bpad = sbp(pes, "Bpad", [128, 64 * 128], BF16)
            Bspad = sbp(pes, "Bspad", [128, 64 * 128], BF16)
            Cpad = sbp(pes, "Cpad", [128, 64 * 128], BF16)
            Bpv = Bpad[:, :].rearrange("p (g n) -> p g n", g=64)
            Bsv = Bspad[:, :].rearrange("p (g n) -> p g n", g=64)
            Cpv = Cpad[:, :].rearrange("p (g n) -> p g n", g=64)
            mag = sbp(pes, "mag", [128, 64], F32)
            frc = sbp(pes, "frc", [128, 64], F32)
            dcol = sbp(pes, "dcol", [128, 8], F32)
            tpos = sbp(pes, "tpos", [128, 256], F32)
            tposv = tpos[:, :].rearrange("p (s t) -> p s t", s=2)
            smask = sbp(pes, "smask", [128, 128], F32)
            Pm = sbp(pes, "Pm", [128, 128], F32)
            xst = sbp(pes, "xstate", [128, 64], F32)
            xfs = sbp(pes, "xfs", [128, 64 * 16], F32)
            xfsv = xfs[:, :].rearrange("p (g s) -> p g s", g=64)
            x0T = sbp(pes, "x0T", [128, 16 * 64], F32)
            x0Tv = x0T[:, :].rearrange("p (s g) -> p s g", s=16)
            kb.dma(tposv, CST["tpos"], writes=["tpos"])
            kb.dma(smask[:, :], CST["smask"], writes=["smask"])
            kb.dma(Pm[:, :], CST["Pm"], writes=["Pm"])
            kb.dma(dcol[:, :], W["s5_d"][l].rearrange("(k p) -> p k", p=128), writes=["dcol"])
            kb.op("pool", lambda e: e.memset(xst[:, :], 0.0), writes=[("xst", g) for g in range(64)])
            for j in range(4):
                wu, kwu = load_w([(win[:, U0 + j * 256:U0 + (j + 1) * 256], 0)], KC, 256)
                for (t0, n) in TILES:
                    for sub in range(2):
                        oc = j * 2 + sub
                        bk = (oc % 2)
                        for kc in range(KC):
                            kb.op("pe", lambda e, kc=kc, bk=bk, sub=sub, t0=t0, n=n, wu=wu: e.matmul(
                                PS[bk][:, 0:n], lhsT=wu[:, kc, sub * 128:(sub + 1) * 128], rhs=hTv[:, kc, t0:t0 + n], start=(kc == 0), stop=(kc == KC - 1)),
                                reads=[kwu] + ck("h", t0, n), writes=["ps%d" % bk], inc=(kc == KC - 1))
                        kb.op("act", lambda e, bk=bk, oc=oc, t0=t0, n=n: e.copy(uTv[:, oc, t0:t0 + n], PS[bk][:, 0:n]), reads=["ps%d" % bk], writes=[("uT", oc)])
            with contextlib.ExitStack() as pp:
                def t64(name):
                    return sbp(pp, name, [128, 64], F32)
                aa = sbp(pp, "aa", [64, 256], F32)
                dt = t64("dt"); ar = t64("ar"); ai = t64("ai"); th = t64("th"); t1 = t64("t1"); t2 = t64("t2")
                sn = t64("sn"); cs = t64("cs"); abr = t64("abr"); abi = t64("abi"); den = t64("den")
                fre = t64("fre"); fim = t64("fim"); FP = t64("FP"); FQ = t64("FQ")
                bre2 = sbp(pp, "bre2", [128, 1024], F32)
                bim2 = sbp(pp, "bim2", [128, 1024], F32)
                Bn2 = sbp(pp, "Bn2", [128, 1024], F32)
                Bsn2 = sbp(pp, "Bsn2", [128, 1024], F32)
                tq = sbp(pp, "tq", [128, 1024], F32)
                Ball = sbp(pp, "Ball", [128, 256], F32)
                cT = sbp(pp, "cT", [128, 8 * 128], F32)
                cTv = cT[:, :].rearrange("p (k n) -> p k n", k=8)
                gmask = sbp(pp, "gmask", [128, 8], F32)
                cmask = sbp(pp, "cmask", [128, 1024], F32)
                cmv = cmask[:, :].rearrange("p (g n) -> p g n", g=8)
                kb.dma(gmask[:, :], CST["gmask"], writes=["gmask"])
                kb.dma(cmv, CST["cmask"], writes=["cmask"])
                kb.dma(dt[:, :], W["s5_log_dt"][l:l + 1, :].to_broadcast([128, 64]), writes=["dt"])
                kb.op("act", lambda e: e.activation(out=dt[:, :], in_=dt[:, :], func=AF.Exp), reads=["dt"], writes=["dt"])
                kb.dma(aa[:, 0:64], W["s5_a_re"][l], writes=["aa"])
                kb.dma(aa[:, 64:128], W["s5_a_re"][l], writes=["aa"])
                kb.dma(aa[:, 128:192], W["s5_a_im"][l], writes=["aa"])
                kb.dma(aa[:, 192:256], W["s5_a_im"][l], writes=["aa"])
                for i2 in range(2):
                    kb.op("pe", lambda e, i2=i2: e.transpose(PS[7][:, i2 * 64:(i2 + 1) * 64], aa[0:64, i2 * 128:(i2 + 1) * 128], identf[0:64, 0:64]),
                          reads=["aa", "identf"], writes=["ps7"], inc=(i2 == 1))
                kb.op("act", lambda e: e.copy(ar[:, :], PS[7][:, 0:64]), reads=["ps7"], writes=["ar"])
                kb.op("act", lambda e: e.copy(ai[:, :], PS[7][:, 64:128]), reads=["ps7"], writes=["ai"])
                TT = lambda o, a, b, op, eng="dve": kb.op(eng, lambda e: e.tensor_tensor(out=o[:, :], in0=a[:, :], in1=b[:, :], op=op),
                                                          reads=[id(a), id(b)], writes=[id(o)])
                TS = lambda o, a, s1, op0, s2=None, op1=None: kb.op("dve", (lambda e: e.tensor_scalar(out=o[:, :], in0=a[:, :], scalar1=s1, scalar2=s2, op0=op0, op1=op1)) if op1 is not None
                                                                      else (lambda e: e.tensor_scalar(out=o[:, :], in0=a[:, :], scalar1=s1, scalar2=None, op0=op0)),
                                                                      reads=[id(a)], writes=[id(o)])
                for tns, key in [(dt, "dt"), (ar, "ar"), (ai, "ai")]:
                    kb.writer[id(tns)] = kb.writer.get(key)
                TT(t1, dt, ar, ALU.mult)
                kb.op("act", lambda e: e.activation(out=mag[:, :], in_=t1[:, :], func=AF.Exp), reads=[id(t1)], writes=["mag"])
                TT(th, dt, ai, ALU.mult)
                TS(th, th, 1.0 / TWO_PI, ALU.mult)
                TS(t1, th, MAGIC, ALU.add)
                TS(t1, t1, MAGIC, ALU.subtract)
                kb.op("dve", lambda e: e.tensor_tensor(out=frc[:, :], in0=th[:, :], in1=t1[:, :], op=ALU.subtract), reads=[id(th), id(t1)], writes=["frc"])
                kb.op("act", lambda e: e.activation(out=sn[:, :], in_=frc[:, :], func=AF.Sin, scale=TWO_PI), reads=["frc"], writes=[id(sn)])
                kb.op("dve", lambda e: e.tensor_scalar(out=t2[:, :], in0=frc[:, :], scalar1=0.25, scalar2=None, op0=ALU.add), reads=["frc"], writes=[id(t2)])
                TS(t1, t2, 0.5, ALU.is_gt)
                TT(t2, t2, t1, ALU.subtract)
                kb.op("act", lambda e: e.activation(out=cs[:, :], in_=t2[:, :], func=AF.Sin, scale=TWO_PI), reads=[id(t2)], writes=[id(cs)])
                kb.op("dve", lambda e: e.tensor_tensor(out=abr[:, :], in0=mag[:, :], in1=cs[:, :], op=ALU.mult), reads=["mag", id(cs)], writes=[id(abr)])
                kb.op("dve", lambda e: e.tensor_tensor(out=abi[:, :], in0=mag[:, :], in1=sn[:, :], op=ALU.mult), reads=["mag", id(sn)], writes=[id(abi)])
                TS(abr, abr, -1.0, ALU.add)
                TT(t1, ar, ar, ALU.mult)
                TT(t2, ai, ai, ALU.mult)
                TT(den, t1, t2, ALU.add)
                kb.op("dve", lambda e: e.reciprocal(den[:, :], den[:, :]), reads=[id(den)], writes=[id(den)])
                TT(t1, abr, ar, ALU.mult)
                TT(t2, abi, ai, ALU.mult)
                TT(fre, t1, t2, ALU.add)
                TT(fre, fre, den, ALU.mult)
                TT(t1, abi, ar, ALU.mult)
                TT(t2, abr, ai, ALU.mult)
                TT(fim, t1, t2, ALU.subtract)
                TT(fim, fim, den, ALU.mult)
                kb.op("dve", lambda e: e.tensor_copy(FP[0:64, :], fre[0:64, :]), reads=[id(fre)], writes=[id(FP)])
                kb.op("dve", lambda e: e.tensor_copy(FP[64:128, :], fim[64:128, :]), reads=[id(fim)], writes=[id(FP)])
                kb.op("dve", lambda e: e.tensor_scalar(out=FQ[0:64, :], in0=fim[0:64, :], scalar1=-1.0, scalar2=None, op0=ALU.mult), reads=[id(fim)], writes=[id(FQ)])
                kb.op("dve", lambda e: e.tensor_copy(FQ[64:128, :], fre[64:128, :]), reads=[id(fre)], writes=[id(FQ)])
                for half in range(2):
                    kb.dma(bre2[half * 64:(half + 1) * 64, :].rearrange("p (g c) -> p g c", g=64), W["s5_b_re"][l].rearrange("g n c -> n g c"), writes=["bre2"])
                    kb.dma(bim2[half * 64:(half + 1) * 64, :].rearrange("p (g c) -> p g c", g=64), W["s5_b_im"][l].rearrange("g n c -> n g c"), writes=["bim2"])
                v3 = lambda t: t[:, :].rearrange("p (g c) -> p g c", g=64)
                bc = lambda t: t[:, :].unsqueeze(2).to_broadcast([128, 64, 16])
                kb.op("dve", lambda e: e.tensor_tensor(out=v3(Bn2), in0=v3(bre2), in1=bc(FP), op=ALU.mult), reads=["bre2", id(FP)], writes=["Bn2"])
                kb.op("dve", lambda e: e.tensor_tensor(out=v3(tq), in0=v3(bim2), in1=bc(FQ), op=ALU.mult), reads=["bim2", id(FQ)], writes=["tq"])
                kb.op("dve", lambda e: e.tensor_tensor(out=Bn2[:, :], in0=Bn2[:, :], in1=tq[:, :], op=ALU.add), reads=["Bn2", "tq"], writes=["Bn2"])
                kb.op("dve", lambda e: e.tensor_tensor(out=v3(Bsn2), in0=v3(bim2), in1=bc(FP), op=ALU.mult), reads=["bim2", id(FP)], writes=["Bsn2"])
                kb.op("dve", lambda e: e.tensor_tensor(out=v3(tq), in0=v3(bre2), in1=bc(FQ), op=ALU.mult), reads=["bre2", id(FQ), "Bn2"], writes=["tq"])
                kb.op("dve", lambda e: e.tensor_tensor(out=Bsn2[:, :], in0=Bsn2[:, :], in1=tq[:, :], op=ALU.subtract), reads=["Bsn2", "tq"], writes=["Bsn2"])
                kb.dma(cTv[:, :, 0:64], W["s5_c_re"][l].rearrange("(k g) c n -> (g c) k n", k=8), writes=["cT"])
                kb.dma(cTv[:, :, 64:128], W["s5_c_im"][l].rearrange("(k g) c n -> (g c) k n", k=8), writes=["cT"])
                kb.op("dve", lambda e: e.tensor_scalar(out=cTv[:, :, 64:128], in0=cTv[:, :, 64:128], scalar1=-1.0, scalar2=None, op0=ALU.mult), reads=["cT"], writes=["cT"])
                for gc in range(8):
                    kb.op("pe", lambda e, gc=gc: e.transpose(PS[6][:, 0:128], Bn2[:, gc * 128:(gc + 1) * 128], identf[:, :]), reads=["Bn2", "identf"], writes=["ps6"], inc=False)
                    kb.op("pe", lambda e, gc=gc: e.transpose(PS[6][:, 128:256], Bsn2[:, gc * 128:(gc + 1) * 128], identf[:, :]), reads=["Bsn2", "identf"], writes=["ps6"], inc=True)
                    kb.op("act", lambda e: e.copy(Ball[:, :], PS[6][:, 0:256]), reads=["ps6"], writes=["Ball"])
                    kb.op("pe", lambda e, gc=gc: e.transpose(PS[7][:, 0:128], cTv[:, gc, :], identf[:, :]), reads=["cT", "identf"], writes=["ps7"], inc=True)
                    for g8 in range(8):
                        g = gc * 8 + g8
                        kb.op("dve", lambda e, g=g, g8=g8: e.tensor_scalar(out=Bpv[:, g, :], in0=Ball[:, 0:128], scalar1=gmask[:, g8:g8 + 1], scalar2=None, op0=ALU.mult),
                              reads=["Ball", "gmask"], writes=["Bpad"])
                        kb.op("pool", lambda e, g=g, g8=g8: e.tensor_scalar(out=Bsv[:, g, :], in0=Ball[:, 128:256], scalar1=gmask[:, g8:g8 + 1], scalar2=None, op0=ALU.mult),
                              reads=["Ball", "gmask"], writes=["Bspad"])
                        kb.op("dve", lambda e, g=g, g8=g8: e.tensor_tensor(out=Cpv[:, g, :], in0=PS[7][:, 0:128], in1=cmv[:, g8, :], op=ALU.mult),
                              reads=["ps7", "cmask"], writes=["Cpad"])
                kb.barrier()
            with contextlib.ExitStack() as px:
                s0in = sbp(px, "s0in", [64, 16 * 128], F32)
                s0r = sbp(px, "s0r", [64, 16 * 128], F32)
                kb.dma(s0in[:, :].rearrange("p (s x) -> p s x", s=16), W["state_s5"][l].rearrange("s g n r -> g s (n r)"), writes=["s0in"])
                kb.op("dve", lambda e: e.tensor_copy(s0r[:, :].rearrange("p (s r n) -> p s r n", s=16, r=2), s0in[:, :].rearrange("p (s n r) -> p s r n", s=16, r=2)),
                      reads=["s0in"], writes=["s0r"])
                for s in range(NSS):
                    bk = 6 + (s // 8) % 2
                    kb.op("pe", lambda e, s=s, bk=bk: e.transpose(PS[bk][:, (s % 8) * 64:(s % 8 + 1) * 64], s0r[0:64, s * 128:(s + 1) * 128], identf[0:64, 0:64]),
                          reads=["s0r", "identf"], writes=["ps%d" % bk], inc=(s % 8 == 7))
                    if s % 8 == 7:
                        kb.op("act", lambda e, s=s, bk=bk: e.copy(x0T[:, (s - 7) * 64:(s + 1) * 64], PS[bk][:, :]), reads=["ps%d" % bk], writes=["x0T"])
                kb.barrier()
            with contextlib.ExitStack() as pm:
                cosT = sbp(pm, "cosT", [128, 2 * 1024], F32)
                sinT = sbp(pm, "sinT", [128, 2 * 1024], F32)
                cosv = cosT[:, :].rearrange("p (s g t) -> p s g t", s=2, g=8)
                sinv = sinT[:, :].rearrange("p (s g t) -> p s g t", s=2, g=8)
                ta = sbp(pm, "ta", [128, 1024], F32)
                tb = sbp(pm, "tb", [128, 1024], F32)
                rms_ = sbp(pm, "rms_", [128, 128], F32)
                w1 = [sbp(pm, "w1_%d" % i, [128, 128], F32) for i in range(4)]
                w2 = [sbp(pm, "w2_%d" % i, [128, 128], F32) for i in range(4)]
                zz = [sbp(pm, "zz%d" % i, [128, 128], F32) for i in range(4)]
                x1 = [sbp(pm, "x1_%d" % i, [128, 128], F32) for i in range(6)]
                x2 = [sbp(pm, "x2_%d" % i, [128, 128], F32) for i in range(4)]
                xb = [sbp(pm, "xb%d" % i, [128, 128], BF16) for i in range(4)]
                xb2 = [sbp(pm, "xb2_%d" % i, [128, 128], BF16) for i in range(4)]
                yvL = [sbp(pm, "yv%d" % i, [128, 128], F32) for i in range(2)]
                ytL = [sbp(pm, "yt%d" % i, [128, 128], F32) for i in range(2)]
                ysgL = [sbp(pm, "ysg%d" % i, [128, 128], F32) for i in range(2)]
                yo = [sbp(pm, "yo%d" % i, [128, 128], BF16) for i in range(2)]
                fin = sbp(pm, "s5fin", [64, 128], F32)
                fin2 = [sbp(pm, "s5fin2_%d" % i, [64, 128], F32) for i in range(2)]
                it = 0
                for gc in range(8):
                    for sel in range(2):
                        kb.op("dve", lambda e, sel=sel, gc=gc: e.tensor_tensor(
                            out=ta[:, :].rearrange("p (g t) -> p g t", g=8), in0=tposv[:, sel:sel + 1, :].to_broadcast([128, 8, 128]),
                            in1=frc[:, gc * 8:(gc + 1) * 8].unsqueeze(2).to_broadcast([128, 8, 128]), op=ALU.mult),
                            reads=["tpos", "frc"], writes=["ta"])
                        kb.op("dve", lambda e: e.tensor_scalar(out=tb[:, :], in0=ta[:, :], scalar1=MAGIC, scalar2=None, op0=ALU.add), reads=["ta"], writes=["tb"])
                        kb.op("dve", lambda e: e.tensor_scalar(out=tb[:, :], in0=tb[:, :], scalar1=MAGIC, scalar2=None, op0=ALU.subtract), reads=["tb"], writes=["tb"])
                        kb.op("dve", lambda e: e.tensor_tensor(out=ta[:, :], in0=ta[:, :], in1=tb[:, :], op=ALU.subtract), reads=["ta", "tb"], writes=["ta"])
                        kb.op("act", lambda e, sel=sel: e.activation(out=sinT[:, sel * 1024:(sel + 1) * 1024], in_=ta[:, :], func=AF.Sin, scale=TWO_PI),
                              reads=["ta"], writes=[("sinT", sel)])
                        kb.op("dve", lambda e: e.tensor_scalar(out=ta[:, :], in0=ta[:, :], scalar1=0.25, scalar2=None, op0=ALU.add), reads=["ta"], writes=["ta"])
                        kb.op("dve", lambda e: e.tensor_scalar(out=tb[:, :], in0=ta[:, :], scalar1=0.5, scalar2=None, op0=ALU.is_gt), reads=["ta"], writes=["tb"])
                        kb.op("dve", lambda e: e.tensor_tensor(out=ta[:, :], in0=ta[:, :], in1=tb[:, :], op=ALU.subtract), reads=["ta", "tb"], writes=["ta"])
                        kb.op("act", lambda e, sel=sel: e.activation(out=cosT[:, sel * 1024:(sel + 1) * 1024], in_=ta[:, :], func=AF.Sin, scale=TWO_PI),
                              reads=["ta"], writes=[("cosT", sel)])
                    items = [(c, g8) for c in range(NCH) for g8 in range(8)]

                    def ctx(k, item):
                        c, g8 = item
                        d = {"c": c, "g8": g8, "g": gc * 8 + g8, "ts": slice(c * 128, (c + 1) * 128), "samp": c * 128 >= SEQ}
                        d["sel"] = 1 if d["samp"] else 0
                        q = k % 4
                        d["pA"], d["kA"] = PS[q][:, 0:128], ("spbank", q)
                        d["pB"], d["kB"] = PS[q][:, 128:256], ("spbank", q)
                        d["pC"], d["kC"] = PS[q][:, 256:384], ("spbank", q)
                        d["w1"], d["kw1"] = w1[k % 4], ("w1", k % 4)
                        d["w2"], d["kw2"] = w2[k % 4], ("w2", k % 4)
                        d["zz"], d["kzz"] = zz[k % 4], ("zz", k % 4)
                        d["x1"], d["kx1"] = x1[k % 6], ("x1", k % 6)
                        d["x2"], d["kx2"] = x2[k % 4], ("x2", k % 4)
                        d["xb"], d["kxb"] = xb[k % 4], ("xb", k % 4)
                        return d

                    def sA(k, item):
                        d = ctx(k, item)
                        kb.op("pe", lambda e: e.matmul(d["pA"], lhsT=Bpv[:, d["g"], :], rhs=uTv[:, gc, d["ts"]], start=True, stop=True),
                              reads=["Bpad", ("uT", gc)], writes=[d["kA"]], inc=True)
                        kb.op("pe", lambda e: e.matmul(d["pB"], lhsT=Bsv[:, d["g"], :], rhs=uTv[:, gc, d["ts"]], start=True, stop=True),
                              reads=["Bspad", ("uT", gc)], writes=[d["kB"]], inc=True)

                    def sB(k, item):
                        d = ctx(k, item)
                        kb.op("dve", lambda e: e.tensor_tensor(out=d["w1"][:, :], in0=d["pA"], in1=cosv[:, d["sel"], d["g8"], :], op=ALU.mult),
                              reads=[d["kA"], ("cosT", d["sel"])], writes=[d["kw1"]])
                        kb.op("dve", lambda e: e.tensor_tensor(out=d["w2"][:, :], in0=d["pB"], in1=sinv[:, d["sel"], d["g8"], :], op=ALU.mult),
                              reads=[d["kB"], ("sinT", d["sel"])], writes=[d["kw2"]])

                    def sC(k, item):
                        d = ctx(k, item)
                        kb.op("pool", lambda e: e.tensor_tensor(out=d["w1"][:, :], in0=d["w1"][:, :], in1=d["w2"][:, :], op=ALU.add),
                              reads=[d["kw1"], d["kw2"]], writes=[d["kw1"]])

                    def sD(k, item):
                        d = ctx(k, item)
                        g = d["g"]
                        if not d["samp"]:
                            kb.op("dve", lambda e: e.tensor_tensor_scan(
                                out=d["zz"][:, :], data0=mag[:, g:g + 1].to_broadcast([128, 128]), data1=d["w1"][:, :], initial=xst[:, g:g + 1],
                                op0=ALU.mult, op1=ALU.add),
                                reads=[d["kw1"], "mag", ("xst", g)], writes=[d["kzz"]])
                        else:
                            kb.op("dve", lambda e: e.scalar_tensor_tensor(
                                out=d["w1"][:, 0:128:8], in0=x0Tv[:, :, g], scalar=mag[:, g:g + 1], in1=d["w1"][:, 0:128:8], op0=ALU.mult, op1=ALU.add),
                                reads=[d["kw1"], "mag", "x0T"], writes=[d["kw1"]])
                            kb.op("dve", lambda e: e.tensor_scalar(out=rms_[:, :], in0=smask[:, :], scalar1=mag[:, g:g + 1], scalar2=None, op0=ALU.mult),
                                  reads=["smask", "mag"], writes=["rms_"])
                            kb.op("dve", lambda e: e.tensor_tensor_scan(
                                out=d["zz"][:, :], data0=rms_[:, :], data1=d["w1"][:, :], initial=0.0, op0=ALU.mult, op1=ALU.add),
                                reads=[d["kw1"], "rms_"], writes=[d["kzz"]])

                    def sE(k, item):
                        d = ctx(k, item)
                        kb.op("pe", lambda e: e.matmul(d["pC"], lhsT=Pm[:, :], rhs=d["zz"][:, :], start=True, stop=True),
                              reads=["Pm", d["kzz"]], writes=[d["kC"]], inc=True)
                        kb.op("pool", lambda e: e.tensor_tensor(out=d["x1"][:, :], in0=d["zz"][:, :], in1=cosv[:, d["sel"], d["g8"], :], op=ALU.mult),
                              reads=[d["kzz"], ("cosT", d["sel"])], writes=[d["kx1"]])

                    def sF(k, item):
                        d = ctx(k, item)
                        kb.op("dve", lambda e: e.tensor_tensor(out=d["x2"][:, :], in0=d["pC"], in1=sinv[:, d["sel"], d["g8"], :], op=ALU.mult),
                              reads=[d["kC"], ("sinT", d["sel"])], writes=[d["kx2"]])

                    def sG(k, item):
                        d = ctx(k, item)
                        g = d["g"]
                        if d["samp"]:
                            kb.op("pool", lambda e: e.tensor_tensor(out=xfsv[:, g, :], in0=d["x1"][:, 7:128:8], in1=d["x2"][:, 7:128:8], op=ALU.add),
                                  reads=[d["kx1"], d["kx2"]], writes=[("xfs", g)])

                    def sH(k, item):
                        d = ctx(k, item)
                        g = d["g"]
                        kb.op("act", lambda e: e.copy(d["xb"][:, :], d["x1"][:, :]), reads=[d["kx1"]], writes=[d["kxb"]])
                        kb.op("act", lambda e: e.copy(xb2[k % 4][:, :], d["x2"][:, :]), reads=[d["kx2"]], writes=[("xb2", k % 4)])
                        if not d["samp"]:
                            kb.op("act", lambda e: e.activation(out=xst[:, g:g + 1], in_=d["x1"][:, 127:128], func=AF.Identity,
                                                                bias=d["x2"][:, 127:128], scale=1.0),
                                  reads=[d["kx1"], d["kx2"]], writes=[("xst", g)])

                    def sI(k, item):
                        d = ctx(k, item)
                        c, g8, g, ts = d["c"], d["g8"], d["g"], d["ts"]
                        yb = 6 + (c % 2)
                        kb.op("pe", lambda e: e.matmul(PS[yb][:, 0:128], lhsT=Cpv[:, g, :], rhs=d["xb"][:, :], start=(g8 == 0), stop=False),
                              reads=["Cpad", d["kxb"]], writes=["ps%d" % yb], inc=False)
                        kb.op("pe", lambda e: e.matmul(PS[yb][:, 0:128], lhsT=Cpv[:, g, :], rhs=xb2[k % 4][:, :], start=False, stop=(g8 == 7)),
                              reads=["Cpad", ("xb2", k % 4)], writes=["ps%d" % yb], inc=True)

                    def sJ(k, item):
                        d = ctx(k, item)
                        c, g8, ts = d["c"], d["g8"], d["ts"]
                        if g8 != 7:
                            return
                        yb = 6 + (c % 2)
                        yv, yt, ysg = yvL[c % 2], ytL[c % 2], ysgL[c % 2]
                        kyv, kyt, kys = ("yv", c % 2), ("yt", c % 2), ("ysg", c % 2)
                        kb.op("dve", lambda e: e.scalar_tensor_tensor(out=yv[:, :], in0=uTv[:, gc, ts], scalar=dcol[:, gc:gc + 1], in1=PS[yb][:, 0:128],
                                                                     op0=ALU.mult, op1=ALU.add),
                              reads=["ps%d" % yb, ("uT", gc), "dcol"], writes=[kyv])
                        kb.op("act", lambda e: e.activation(out=yt[:, :], in_=yv[:, :], func=AF.Square), reads=[kyv], writes=[kyt])
                        kb.op("pool", lambda e: e.tensor_scalar(out=yt[:, :], in0=yt[:, :], scalar1=0.044715, scalar2=1.0, op0=ALU.mult, op1=ALU.add), reads=[kyt], writes=[kyt])
                        kb.op("pool", lambda e: e.tensor_tensor(out=yt[:, :], in0=yt[:, :], in1=yv[:, :], op=ALU.mult), reads=[kyt, kyv], writes=[kyt])
                        kb.op("act", lambda e: e.activation(out=ysg[:, :], in_=yt[:, :], func=AF.Sigmoid, scale=float(2.0 * np.sqrt(2.0 / np.pi))), reads=[kyt], writes=[kys])
                        yob = yo[c % 2]
                        kb.op("pool", lambda e: e.tensor_tensor(out=yob[:, :], in0=ysg[:, :], in1=yv[:, :], op=ALU.mult), reads=[kys, kyv], writes=[("yo", c % 2)])
                        kb.dma(brscr[:, gc, ts], yob[:, :], reads=[("yo", c % 2)], writes=[("brscr5", c, gc)])

                    run_pipeline(items, [sA, sB, sC, sD, sE, sF, sG, sH, sI, sJ])
                kb.op("pe", lambda e: e.transpose(PS[7][0:64, 0:128], xst[:, :], identf[:, :]), reads=[("xst", g) for g in range(64)] + ["identf"], writes=["ps7"], inc=True)
                kb.op("dve", lambda e: e.tensor_copy(fin[:, :].rearrange("p (n r) -> p r n", r=2), PS[7][0:64, 0:128].rearrange("p (r n) -> p r n", r=2)),
                      reads=["ps7"], writes=["s5fin"])
                kb.dma(s5_p[l].rearrange("g n r -> g (n r)"), fin[:, :], reads=["s5fin"], writes=[("s5_p", l)])
                for s in range(NSS):
                    kb.op("pe", lambda e, s=s: e.transpose(PS[7][0:64, 0:128], xfsv[:, :, s], identf[:, :]), reads=[("xfs", g) for g in range(64)] + ["identf"], writes=["ps7"], inc=True)
                    f2 = fin2[s % 2]
                    kb.op("dve", lambda e, f2=f2: e.tensor_copy(f2[:, :].rearrange("p (n r) -> p r n", r=2), PS[7][0:64, 0:128].rearrange("p (r n) -> p r n", r=2)),
                          reads=["ps7"], writes=[("fin2", s % 2)])
                    kb.dma(s5_s[l, s].rearrange("g n r -> g (n r)"), f2[:, :], reads=[("fin2", s % 2)], writes=[("s5_s", l, s)])
                kb.barrier()

    for l in range(NL + 1):
        phase_x(l)
        if l < NL:
            if stages.get("ret", True):
                retention(l)
                stage_c(l, "ret", W["ret_w_o"][l], 8, 9248, "retln%d" % l, W["ret_ln_g"][l])
            if stages.get("s5", True):
                s5(l)
                stage_c(l, "s5", W["s5_w_glu"][l], 8, 9248 + 1024, glu=True)
            if stages.get("ssd", True):
                ssd(l)
            if stages.get("ssd", True) and stages.get("ssd_s2", True) and stages.get("ssd_c", True):
                stage_c(l, "ssd", W["ssd_w_o"][l], 16, 9248 + 2048, "ssdn%d" % l, W["ssd_norm"][l])

    kb.finish()
    print("instructions:", kb.ninst, {e: kb.ccnt[e] for e in kb.ccnt})
    return nc, es


_CONSTS = None
STAGES = {"layers": DEPTH, "ffn1": True, "ffn2": True, "ret": True, "ssd": True, "s5": True}
WNAMES = ["ffn1_norm", "ffn1_w_gu", "ffn1_w_down", "ffn2_norm", "ffn2_w_gu", "ffn2_w_down", "mix_norm", "w_in", "ret_ln_g", "ret_w_o", "w_out",
          "ssd_conv_w", "ssd_conv_b", "ssd_dt_bias", "ssd_a_log", "ssd_d", "ssd_norm", "ssd_w_o",
          "s5_a_re", "s5_a_im", "s5_log_dt", "s5_b_re", "s5_b_im", "s5_c_re", "s5_c_im", "s5_d", "s5_w_glu"]


def make_in_map(inp, c, consts):
    xp = np.asarray(inp["x_prompt"])
    xs = np.asarray(inp["x_sample"])
    x_core = np.concatenate([xp[c], xs[c * NSS:(c + 1) * NSS].reshape(NSS * DSEQ, D)], axis=0)
    m = {"x_in": np.ascontiguousarray(x_core)}
    m.update(consts)
    for k in WNAMES:
        m[k] = np.asarray(inp[k])
    m["final_norm"] = np.asarray(inp["final_norm"]).reshape(1, D)
    for k in ["state_ret", "state_ssm", "state_conv", "state_s5"]:
        m[k] = np.ascontiguousarray(np.asarray(inp[k])[:, c * NSS:(c + 1) * NSS])
    return m


def kernel(**inp):
    nc, es = build(STAGES)
    consts = host_consts()
    in_maps = [make_in_map(inp, c, consts) for c in range(NCORES)]
    res = run_bass_kernel_spmd(nc, in_maps, core_ids=list(range(NCORES)))
    es.close()
    R = res.results
    ys = [r["y_out"] for r in R]
    y_prompt = np.stack([y[:SEQ] for y in ys], axis=0)
    y_sample = np.concatenate([y[SEQ:].reshape(NSS, DSEQ, D) for y in ys], axis=0)
    def pstack(k):
        return np.stack([r[k] for r in R], axis=1)

    def scat(k):
        return np.concatenate([r[k] for r in R], axis=1)

    return (y_prompt, y_sample, pstack("ret_p"), scat("ret_s"), pstack("s5_p"), scat("s5_s"),
            pstack("ssm_p"), scat("ssm_s"), pstack("conv_p"), scat("conv_s"))
```
